# Optimizing a Trainium2 kernel written in Bass

```python
import jax, jax.numpy as jnp
from jax import lax
import numpy as np

D_MODEL = 2048
BATCH = 16
SEQ = 2048
DEPTH = 1

N_HEADS = 16
HEAD_DIM = 64
N_KV = 4
HEADS_PER_KV = N_HEADS // N_KV
ATTN_WIDTH = N_HEADS * HEAD_DIM
KV_WIDTH = N_KV * HEAD_DIM
N_NSA_BRANCHES = 3
CMP_BLOCK = 32
CMP_STRIDE = 16
CMP_HIDDEN = 4 * HEAD_DIM
SEL_BLOCK = 64
N_SEL = 16
N_FORCED_LOCAL = 2
WINDOW = 512
Q_CHUNK = 32
FORCE_BONUS = 1e4
NEG_INF = -1e30
ROPE_THETA = 500000.0
ROPE_DIM = HEAD_DIM // 4
HEAD_OUT_WIDTH = D_MODEL // N_HEADS
MAX_POS_OFFSET = 1024
POOL_WINDOWS = (2, 4, 8, 16)
POOL_GROUPS = 4
POOL_WIDTH = 1024
POOL_GROUP_WIDTH = POOL_WIDTH // POOL_GROUPS
POOL_OUT_WIDTH = D_MODEL // POOL_GROUPS
N_MERGE_BRANCHES = 2
IN_SPLITS = (ATTN_WIDTH, KV_WIDTH, KV_WIDTH, KV_WIDTH, KV_WIDTH, KV_WIDTH, KV_WIDTH,
             N_HEADS * N_NSA_BRANCHES, POOL_WIDTH, N_MERGE_BRANCHES * D_MODEL)
IN_WIDTH = ATTN_WIDTH + 6 * KV_WIDTH + N_HEADS * N_NSA_BRANCHES + POOL_WIDTH + N_MERGE_BRANCHES * D_MODEL
N_EXPERT_GROUPS = 4
EXPERTS_PER_GROUP = 4
N_EXPERTS = N_EXPERT_GROUPS * EXPERTS_PER_GROUP
TOP_K_EXPERTS = 2
D_EXPERT = 256
RMS_EPS = 1e-6

kernel_name = "nsa_pool_hier_moe_hybrid_block"


def _rms(x, g):
    xf = x.astype(jnp.float32)
    inv = lax.rsqrt(jnp.mean(xf * xf, axis=-1, keepdims=True) + RMS_EPS)
    return (xf * inv * g.astype(jnp.float32)).astype(x.dtype)


def _rope_tables(pos):
    inv_freq = ROPE_THETA ** (-jnp.arange(0, ROPE_DIM, 2, dtype=jnp.float32) / ROPE_DIM)
    ang = pos.astype(jnp.float32)[..., None] * inv_freq
    return jnp.cos(ang)[:, :, None, :], jnp.sin(ang)[:, :, None, :]


def _partial_rope(x, cos, sin):
    half = ROPE_DIM // 2
    x1, x2, rest = x[..., :half], x[..., half:ROPE_DIM], x[..., ROPE_DIM:]
    c, s = cos.astype(x.dtype), sin.astype(x.dtype)
    return jnp.concatenate([x1 * c - x2 * s, x2 * c + x1 * s, rest], axis=-1)


def _masked_softmax(s, mask):
    s = jnp.where(mask, s.astype(jnp.float32), NEG_INF)
    return jax.nn.softmax(s, axis=-1) * mask.astype(jnp.float32)


def _compress(kv, pos_emb, w1, w2):
    B, S = kv.shape[:2]
    n_cmp = (S - CMP_BLOCK) // CMP_STRIDE + 1
    idx = np.arange(n_cmp)[:, None] * CMP_STRIDE + np.arange(CMP_BLOCK)[None, :]
    blocks = kv[:, idx] + pos_emb[None, None, :, None, :]
    flat = blocks.transpose(0, 1, 3, 2, 4).reshape(B, n_cmp, N_KV, CMP_BLOCK * HEAD_DIM)
    return jax.nn.silu(flat @ w1) @ w2


def _nsa_attention(q, kc, vc, ks, vs, kw, vw, gates):
    B, S = q.shape[:2]
    n_cmp = kc.shape[1]
    n_blk = S // SEL_BLOCK
    k_top = min(N_SEL, n_blk)
    n_chunks = S // Q_CHUNK
    scale = HEAD_DIM ** -0.5
    cmp_end = np.arange(n_cmp) * CMP_STRIDE + CMP_BLOCK - 1
    a0 = np.arange(n_cmp)[:, None] * CMP_STRIDE
    b0 = np.arange(n_blk)[None, :] * SEL_BLOCK
    ov = np.clip(np.minimum(a0 + CMP_BLOCK, b0 + SEL_BLOCK) - np.maximum(a0, b0), 0, None)
    overlap = jnp.asarray(ov / CMP_BLOCK, dtype=jnp.float32)

    kc_t, vc_t = kc.transpose(0, 2, 1, 3), vc.transpose(0, 2, 1, 3)
    ks_b = ks.reshape(B, n_blk, SEL_BLOCK, N_KV, HEAD_DIM).transpose(0, 3, 1, 2, 4)
    vs_b = vs.reshape(B, n_blk, SEL_BLOCK, N_KV, HEAD_DIM).transpose(0, 3, 1, 2, 4)
    pad = ((0, 0), (0, 0), (WINDOW, 0), (0, 0))
    kw_p = jnp.pad(kw.transpose(0, 2, 1, 3), pad)
    vw_p = jnp.pad(vw.transpose(0, 2, 1, 3), pad)
    q_ch = q.reshape(B, n_chunks, Q_CHUNK, N_KV, HEADS_PER_KV, HEAD_DIM).transpose(1, 0, 3, 4, 2, 5)
    g_ch = gates.reshape(B, n_chunks, Q_CHUNK, N_KV, HEADS_PER_KV, N_NSA_BRANCHES).transpose(1, 0, 3, 4, 2, 5)
    b_ix = jnp.arange(B)[:, None, None, None]
    g_ix = jnp.arange(N_KV)[None, :, None, None]
    blk = jnp.arange(n_blk)

    def chunk(args):
        c, qc, gc = args
        q0 = c * Q_CHUNK
        t = q0 + jnp.arange(Q_CHUNK)
        s = jnp.einsum("bgrqd,bgnd->bgrqn", qc, kc_t) * scale
        p_cmp = _masked_softmax(s, cmp_end[None, :] <= t[:, None])
        o_cmp = jnp.einsum("bgrqn,bgnd->bgrqd", p_cmp.astype(vc_t.dtype), vc_t)
        imp = jnp.einsum("bgrqn,nj->bgqj", p_cmp, overlap)
        dist = (t // SEL_BLOCK)[:, None] - blk[None, :]
        valid_blk = dist >= 0
        forced = (blk[None, :] == 0) | (valid_blk & (dist < N_FORCED_LOCAL))
        score = jnp.where(valid_blk, imp + FORCE_BONUS * forced.astype(jnp.float32), NEG_INF)
        _, sel = lax.top_k(score, k_top)
        ks_g = ks_b[b_ix, g_ix, sel].reshape(B, N_KV, Q_CHUNK, k_top * SEL_BLOCK, HEAD_DIM)
        vs_g = vs_b[b_ix, g_ix, sel].reshape(B, N_KV, Q_CHUNK, k_top * SEL_BLOCK, HEAD_DIM)
        kpos = (sel[..., None] * SEL_BLOCK + jnp.arange(SEL_BLOCK)).reshape(B, N_KV, Q_CHUNK, k_top * SEL_BLOCK)
        mask_s = (kpos <= t[:, None])[:, :, None]
        s = jnp.einsum("bgrqd,bgqkd->bgrqk", qc, ks_g) * scale
        p = _masked_softmax(s, mask_s)
        o_slc = jnp.einsum("bgrqk,bgqkd->bgrqd", p.astype(vs_g.dtype), vs_g)
        kw_c = lax.dynamic_slice_in_dim(kw_p, q0, WINDOW + Q_CHUNK, axis=2)
        vw_c = lax.dynamic_slice_in_dim(vw_p, q0, WINDOW + Q_CHUNK, axis=2)
        wpos = q0 - WINDOW + jnp.arange(WINDOW + Q_CHUNK)
        rel = t[:, None] - wpos[None, :]
        mask_w = (wpos[None, :] >= 0) & (rel >= 0) & (rel < WINDOW)
        s = jnp.einsum("bgrqd,bgkd->bgrqk", qc, kw_c) * scale
        p = _masked_softmax(s, mask_w)
        o_swa = jnp.einsum("bgrqk,bgkd->bgrqd", p.astype(vw_c.dtype), vw_c)
        return gc[..., 0:1] * o_cmp + gc[..., 1:2] * o_slc + gc[..., 2:3] * o_swa

    out = lax.map(chunk, (jnp.arange(n_chunks), q_ch, g_ch))
    return out.transpose(1, 0, 4, 2, 3, 5).reshape(B, S, N_HEADS, HEAD_DIM)


def _multiscale_pool(u):
    B, S, _ = u.shape
    ug = u.reshape(B, S, POOL_GROUPS, POOL_GROUP_WIDTH).astype(jnp.float32)
    csum = jnp.pad(jnp.cumsum(ug, axis=1), ((0, 0), (1, 0), (0, 0), (0, 0)))
    t = np.arange(S)
    outs = []
    for gi, w in enumerate(POOL_WINDOWS):
        lo = np.maximum(t + 1 - w, 0)
        cnt = jnp.asarray((t + 1 - lo).astype(np.float32))
        mean = (csum[:, t + 1, gi] - csum[:, lo, gi]) / cnt[None, :, None]
        outs.append(mean - ug[:, :, gi])
    return jnp.stack(outs, axis=2).astype(u.dtype)


def _mixer_sublayer(x, positions, attn_norm_g, w_in, q_norm_g, k_norm_cmp_g, k_norm_slc_g,
                    k_norm_swa_g, cmp_pos_emb_k, cmp_w1_k, cmp_w2_k, cmp_pos_emb_v, cmp_w1_v,
                    cmp_w2_v, w_head_out, w_pool, pool_scale, w_out):
    B, S, _ = x.shape
    h = _rms(x, attn_norm_g)
    proj = h @ w_in
    splits = np.cumsum(IN_SPLITS)[:-1].tolist()
    (q, k_cmp, v_cmp, k_slc, v_slc, k_swa, v_swa, branch_logits, pool_in,
     merge_logits) = jnp.split(proj, splits, axis=-1)
    q = q.reshape(B, S, N_HEADS, HEAD_DIM)
    kv_shape = (B, S, N_KV, HEAD_DIM)
    k_cmp, v_cmp, k_slc, v_slc, k_swa, v_swa = [a.reshape(kv_shape) for a in (k_cmp, v_cmp, k_slc, v_slc, k_swa, v_swa)]
    cos_t, sin_t = _rope_tables(positions)
    q = _partial_rope(_rms(q, q_norm_g), cos_t, sin_t)
    k_slc = _partial_rope(_rms(k_slc, k_norm_slc_g), cos_t, sin_t)
    k_swa = _partial_rope(_rms(k_swa, k_norm_swa_g), cos_t, sin_t)
    n_cmp = (S - CMP_BLOCK) // CMP_STRIDE + 1
    cmp_end = np.arange(n_cmp) * CMP_STRIDE + CMP_BLOCK - 1
    cos_c, sin_c = _rope_tables(positions[:, cmp_end])
    kc = _partial_rope(_rms(_compress(k_cmp, cmp_pos_emb_k, cmp_w1_k, cmp_w2_k), k_norm_cmp_g), cos_c, sin_c)
    vc = _compress(v_cmp, cmp_pos_emb_v, cmp_w1_v, cmp_w2_v)
    gates = jax.nn.sigmoid(branch_logits.reshape(B, S, N_HEADS, N_NSA_BRANCHES))
    o = _nsa_attention(q, kc, vc, k_slc, v_slc, k_swa, v_swa, gates)
    attn_branch = jnp.einsum("bshd,hde->bshe", o, w_head_out).reshape(B, S, D_MODEL)
    pooled = _multiscale_pool(pool_in)
    pool_branch = jnp.einsum("bsgc,gce->bsge", pooled, w_pool).reshape(B, S, D_MODEL) * pool_scale
    merge = jax.nn.sigmoid(merge_logits.reshape(B, S, N_MERGE_BRANCHES, D_MODEL))
    y = merge[:, :, 0] * attn_branch + merge[:, :, 1] * pool_branch
    return x + y @ w_out


def _moe_sublayer(x, ffn_norm_g, w_router_group, b_router_group, w_router_expert,
                  b_router_expert, w_expert_gate, w_expert_up, w_expert_down):
    B, S, D = x.shape
    ht = _rms(x, ffn_norm_g).reshape(B * S, D)
    T = ht.shape[0]
    g_prob = jax.nn.softmax((ht @ w_router_group).astype(jnp.float32) + b_router_group, axis=-1)
    p_group, g_sel = lax.top_k(g_prob, 1)
    e_logit = ((ht @ w_router_expert).astype(jnp.float32) + b_router_expert).reshape(T, N_EXPERT_GROUPS, EXPERTS_PER_GROUP)
    e_logit = e_logit[jnp.arange(T), g_sel[:, 0]]
    top_val, top_idx = lax.top_k(e_logit, TOP_K_EXPERTS)
    w_top = jax.nn.softmax(top_val, axis=-1) * p_group
    expert_id = g_sel * EXPERTS_PER_GROUP + top_idx
    combine = jnp.sum(jax.nn.one_hot(expert_id, N_EXPERTS, dtype=jnp.float32) * w_top[..., None], axis=1)
    combine = combine.astype(ht.dtype)
    y = jnp.zeros_like(ht)
    for e in range(N_EXPERTS):
        hid = jax.nn.silu(ht @ w_expert_gate[e]) * (ht @ w_expert_up[e])
        y = y + combine[:, e:e + 1] * (hid @ w_expert_down[e])
    return x + y.reshape(B, S, D)


def setup_inputs(seed: int = 0) -> dict:
    key = jax.random.key(seed)
    ks = jax.random.split(key, 26)
    f32 = jnp.float32
    L = DEPTH

    def nrm(k, shape, fan_in):
        return jax.random.normal(k, shape, f32) * (fan_in ** -0.5)

    def gain(k, shape):
        return 1.0 + 0.05 * jax.random.normal(k, shape, f32)

    x = jax.random.normal(ks[0], (BATCH, SEQ, D_MODEL), f32)
    positions = (jax.random.randint(ks[1], (BATCH, 1), 0, MAX_POS_OFFSET, dtype=jnp.int32)
                 + jnp.arange(SEQ, dtype=jnp.int32)[None, :])
    return {
        "x": x,
        "positions": positions,
        "attn_norm_g": gain(ks[2], (L, D_MODEL)),
        "w_in": nrm(ks[3], (L, D_MODEL, IN_WIDTH), D_MODEL),
        "q_norm_g": gain(ks[4], (L, HEAD_DIM)),
        "k_norm_cmp_g": gain(ks[5], (L, HEAD_DIM)),
        "k_norm_slc_g": gain(ks[6], (L, HEAD_DIM)),
        "k_norm_swa_g": gain(ks[7], (L, HEAD_DIM)),
        "cmp_pos_emb_k": 0.02 * jax.random.normal(ks[8], (L, CMP_BLOCK, HEAD_DIM), f32),
        "cmp_w1_k": nrm(ks[9], (L, CMP_BLOCK * HEAD_DIM, CMP_HIDDEN), CMP_BLOCK * HEAD_DIM),
        "cmp_w2_k": nrm(ks[10], (L, CMP_HIDDEN, HEAD_DIM), CMP_HIDDEN),
        "cmp_pos_emb_v": 0.02 * jax.random.normal(ks[11], (L, CMP_BLOCK, HEAD_DIM), f32),
        "cmp_w1_v": nrm(ks[12], (L, CMP_BLOCK * HEAD_DIM, CMP_HIDDEN), CMP_BLOCK * HEAD_DIM),
        "cmp_w2_v": nrm(ks[13], (L, CMP_HIDDEN, HEAD_DIM), CMP_HIDDEN),
        "w_head_out": nrm(ks[14], (L, N_HEADS, HEAD_DIM, HEAD_OUT_WIDTH), HEAD_DIM),
        "w_pool": nrm(ks[15], (L, POOL_GROUPS, POOL_GROUP_WIDTH, POOL_OUT_WIDTH), POOL_GROUP_WIDTH),
        "pool_scale": gain(ks[16], (L, D_MODEL)),
        "w_out": nrm(ks[17], (L, D_MODEL, D_MODEL), D_MODEL),
        "ffn_norm_g": gain(ks[18], (L, D_MODEL)),
        "w_router_group": nrm(ks[19], (L, D_MODEL, N_EXPERT_GROUPS), D_MODEL),
        "b_router_group": 0.01 * jax.random.normal(ks[20], (L, N_EXPERT_GROUPS), f32),
        "w_router_expert": nrm(ks[21], (L, D_MODEL, N_EXPERTS), D_MODEL),
        "b_router_expert": 0.01 * jax.random.normal(ks[22], (L, N_EXPERTS), f32),
        "w_expert_gate": nrm(ks[23], (L, N_EXPERTS, D_MODEL, D_EXPERT), D_MODEL),
        "w_expert_up": nrm(ks[24], (L, N_EXPERTS, D_MODEL, D_EXPERT), D_MODEL),
        "w_expert_down": nrm(ks[25], (L, N_EXPERTS, D_EXPERT, D_MODEL), D_EXPERT),
    }


def reference(x, positions, attn_norm_g, w_in, q_norm_g, k_norm_cmp_g, k_norm_slc_g,
              k_norm_swa_g, cmp_pos_emb_k, cmp_w1_k, cmp_w2_k, cmp_pos_emb_v, cmp_w1_v,
              cmp_w2_v, w_head_out, w_pool, pool_scale, w_out, ffn_norm_g, w_router_group,
              b_router_group, w_router_expert, b_router_expert, w_expert_gate, w_expert_up,
              w_expert_down):
    for layer in range(DEPTH):
        x = _mixer_sublayer(x, positions, attn_norm_g[layer], w_in[layer], q_norm_g[layer],
                            k_norm_cmp_g[layer], k_norm_slc_g[layer], k_norm_swa_g[layer],
                            cmp_pos_emb_k[layer], cmp_w1_k[layer], cmp_w2_k[layer],
                            cmp_pos_emb_v[layer], cmp_w1_v[layer], cmp_w2_v[layer],
                            w_head_out[layer], w_pool[layer], pool_scale[layer], w_out[layer])
        x = _moe_sublayer(x, ffn_norm_g[layer], w_router_group[layer], b_router_group[layer],
                          w_router_expert[layer], b_router_expert[layer], w_expert_gate[layer],
                          w_expert_up[layer], w_expert_down[layer])
    return x
```

```python
import numpy as np
from contextlib import ExitStack
import concourse.bass as bass
import concourse.mybir as mybir
from concourse.bass_utils import run_bass_kernel_spmd

F32 = mybir.dt.float32
BF16 = mybir.dt.bfloat16
I32 = mybir.dt.int32
ALU = mybir.AluOpType
AF = mybir.ActivationFunctionType
AX = mybir.AxisListType

D = 2048
SEQ = 2048
NSEQ = 2
NCORES = 8
TB = 512
NQB = SEQ // TB
H = 16
G = 4
NCMP = 127
INW = 7728
OFF_Q, OFF_KC, OFF_VC, OFF_KS, OFF_VS, OFF_KW, OFF_VW, OFF_GATE, OFF_POOL, OFF_MERGE = (
    0, 1024, 1280, 1536, 1792, 2048, 2304, 2560, 2608, 3632)
NE = 16
TWO_PI = 6.283185307179586
PI_SAFE = 3.1415925


class _Rec:
    def __init__(self):
        self.call = None

    def __getattr__(self, name):
        def f(*a, **k):
            assert self.call is None
            self.call = (name, a, k)
            return self
        return f


def _replay(fn):
    rec = _Rec()
    fn(rec)
    name, a, k = rec.call
    return lambda e: getattr(e, name)(*a, **k)


class Sched:
    ENG = ('pe', 'act', 'dve', 'pool', 'sp')

    def __init__(self, nc, es):
        self.nc = nc
        self.es = es
        self.lists = {e: [] for e in self.ENG}
        self.sem = {e: es.enter_context(nc.semaphore('s_' + e)) for e in self.ENG}
        self.cnt = {e: 0 for e in self.ENG}
        self.seen = {e: {} for e in self.ENG}
        self.lastw = {}
        self.readers = {}
        self.dsem = {}

    def _deps(self, reads, writes):
        deps = []
        for k in reads:
            d = self.lastw.get(k)
            if d is not None:
                deps.append(d)
        for k in writes:
            d = self.lastw.get(k)
            if d is not None:
                deps.append(d)
            r = self.readers.get(k)
            if r:
                deps.extend(r.values())
        return deps

    def _emit_waits(self, eng, deps):
        need = {}
        seen = self.seen[eng]
        for (sname, sem, val) in deps:
            if eng == 'pe' and sname == 'Epe':
                continue
            if seen.get(sname, 0) < val:
                if sname not in need or need[sname][1] < val:
                    need[sname] = (sem, val)
        for sname, (sem, val) in need.items():
            seen[sname] = val
            self.lists[eng].append(lambda e, sem=sem, val=val: e.wait_ge(sem, val))

    def _reg(self, dep, reads, writes):
        for k in writes:
            self.lastw[k] = dep
            self.readers[k] = {}
        for k in reads:
            if k not in writes:
                r = self.readers.setdefault(k, {})
                o = r.get(dep[0])
                if o is None or o[2] < dep[2]:
                    r[dep[0]] = dep

    dead = False

    def op(self, eng, fn, reads=(), writes=()):
        if self.dead:
            return
        fn = _replay(fn)
        self._emit_waits(eng, self._deps(reads, writes))
        self.cnt[eng] += 1
        sem = self.sem[eng]
        self.lists[eng].append(lambda e, fn=fn, sem=sem: fn(e).then_inc(sem, 1))
        dep = ('E' + eng, sem, self.cnt[eng])
        self._reg(dep, reads, writes)

    def dma(self, eng, slot, fn, reads=(), writes=()):
        if self.dead:
            return
        fn = _replay(fn)
        self._emit_waits(eng, self._deps(reads, writes))
        if slot not in self.dsem:
            self.dsem[slot] = [self.es.enter_context(self.nc.semaphore('d_' + slot)), 0]
        ent = self.dsem[slot]
        ent[1] += 16
        sem = ent[0]
        self.lists[eng].append(lambda e, fn=fn, sem=sem: fn(e).then_inc(sem, 16))
        dep = ('D' + slot, sem, ent[1])
        self._reg(dep, reads, writes)

    def alias_in(self, new_keys, old_keys):
        acc = {}
        for k in old_keys:
            for d in [self.lastw.get(k)] + list(self.readers.get(k, {}).values()):
                if d is not None and (d[0] not in acc or acc[d[0]][2] < d[2]):
                    acc[d[0]] = d
        for k in new_keys:
            self.lastw.pop(k, None)
            self.readers[k] = dict(acc)

    def alias_out(self, new_keys, old_keys):
        acc = {}
        for k in new_keys:
            for d in [self.lastw.get(k)] + list(self.readers.get(k, {}).values()):
                if d is not None and (d[0] not in acc or acc[d[0]][2] < d[2]):
                    acc[d[0]] = d
        for k in old_keys:
            r = self.readers.setdefault(k, {})
            for n_, d in acc.items():
                if n_ not in r or r[n_][2] < d[2]:
                    r[n_] = d

    def barrier(self):
        deps = [('E' + e, self.sem[e], self.cnt[e]) for e in self.ENG if self.cnt[e] > 0]
        deps += [('D' + s, ent[0], ent[1]) for s, ent in self.dsem.items()]
        for e in self.ENG:
            self._emit_waits(e, deps)

    def emit(self):
        nc = self.nc
        L = self.lists
        with nc.Block() as block:
            @block.tensor
            def _(e):
                for f in L['pe']:
                    f(e)

            @block.scalar
            def _(e):
                for f in L['act']:
                    f(e)

            @block.vector
            def _(e):
                for f in L['dve']:
                    f(e)

            @block.gpsimd
            def _(e):
                for f in L['pool']:
                    f(e)

            @block.sync
            def _(e):
                for f in L['sp']:
                    f(e)


def make_consts():
    c = {}
    tri = np.zeros((2, 128, 128), np.float32)
    k = np.arange(128)[:, None]
    q = np.arange(128)[None, :]
    tri[0] = (k <= q)
    tri[1] = (k > q)
    c['c_tri'] = tri.transpose(1, 0, 2).copy()
    n = np.arange(NCMP)[:, None]
    t = np.arange(SEQ)[None, :]
    mk = np.zeros((128, SEQ), np.float32)
    mk[:NCMP] = (16 * n + 31 <= t)
    c['c_maskc'] = mk
    tt = np.arange(SEQ)
    tb = tt // 64
    j = np.arange(32)[None, :]
    dist = tb[:, None] - j
    valid = dist >= 0
    forced = (j == 0) | (valid & (dist < 2))
    bonus = np.where(valid, 1e4 * forced.astype(np.float32), -1e30).astype(np.float32)
    c['c_bonus'] = bonus.reshape(16, 128, 32).transpose(1, 0, 2).copy()
    em = np.zeros((32, 16, 128), np.float32)
    for kt in range(16):
        for kk in range(128):
            em[2 * kt + kk // 64, kt, kk] = 1.0
    emp = np.zeros((128, 16, 128), np.float32)
    emp[:32] = em
    c['c_emat'] = emp
    a0 = np.arange(NCMP)[:, None] * 16
    b0 = np.arange(32)[None, :] * 64
    ov = np.clip(np.minimum(a0 + 32, b0 + 64) - np.maximum(a0, b0), 0, None) / 32.0
    ovp = np.zeros((128, 33), np.float32)
    ovp[:NCMP, 0] = 1.0
    ovp[:NCMP, 1:] = ov
    c['c_ov'] = ovp
    inv_freq = (500000.0 ** (-np.arange(0, 16, 2, dtype=np.float32) / 16)).astype(np.float32)
    fr = np.zeros((128, 1), np.float32)
    for p in range(128):
        if p % 64 < 16:
            fr[p, 0] = inv_freq[p % 8]
    c['c_freq'] = fr
    rm = np.zeros((128, 128), np.float32)
    for b in (0, 64):
        for d in range(8):
            rm[b + d + 8, b + d] = -1.0
            rm[b + d, b + d + 8] = 1.0
    c['c_rm'] = rm
    ob = np.zeros((128, 128), np.float32)
    ob[:64, :64] = 1.0 / 64
    ob[64:, 64:] = 1.0 / 64
    c['c_onesblk'] = ob
    se = np.zeros((128, 16, 128), np.float32)
    for e in range(16):
        se[e, e, :] = 1.0
    c['c_sele'] = se
    ic = np.zeros((128, 4, 16), np.float32)
    for gi, w in enumerate((2, 4, 8, 16)):
        for xx in range(16):
            ic[:, gi, xx] = 1.0 / min(xx + 1, w)
    c['c_icnt'] = ic
    return c


CONST_SHAPES = {
    'c_tri': [128, 2, 128], 'c_maskc': [128, SEQ], 'c_bonus': [128, 16, 32], 'c_emat': [128, 16, 128],
    'c_ov': [128, 33], 'c_freq': [128, 1], 'c_rm': [128, 128], 'c_onesblk': [128, 128],
    'c_sele': [128, 16, 128], 'c_icnt': [128, 4, 16],
}

WEIGHT_SHAPES = {
    'attn_norm_g': [D], 'w_in': [D, INW], 'q_norm_g': [64], 'k_norm_cmp_g': [64], 'k_norm_slc_g': [64],
    'k_norm_swa_g': [64], 'cmp_pos_emb_k': [32, 64], 'cmp_w1_k': [2048, 256], 'cmp_w2_k': [256, 64],
    'cmp_pos_emb_v': [32, 64], 'cmp_w1_v': [2048, 256], 'cmp_w2_v': [256, 64],
    'w_head_out': [16, 64, 128], 'w_pool': [4, 256, 512], 'pool_scale': [D], 'w_out': [D, D],
    'ffn_norm_g': [D], 'w_router_group': [D, 4], 'b_router_group': [4], 'w_router_expert': [D, 16],
    'b_router_expert': [16], 'w_expert_gate': [16, D, 256], 'w_expert_up': [16, D, 256],
    'w_expert_down': [16, 256, D],
}


MARKS = []


class _Stop(Exception):
    pass


def build(dbg=False, nseq=NSEQ, nqb=NQB, stop_after=None, skip_prep=False):
    nc = bass.Bass("TRN2", target_bir_lowering=False)
    x = nc.dram_tensor("x", [NSEQ, SEQ, D], F32, kind="ExternalInput").ap()
    positions = nc.dram_tensor("positions", [NSEQ, SEQ], I32, kind="ExternalInput").ap()
    W = {k: nc.dram_tensor(k, s, F32, kind="ExternalInput").ap() for k, s in WEIGHT_SHAPES.items()}
    C = {k: nc.dram_tensor(k, s, F32, kind="ExternalInput").ap() for k, s in CONST_SHAPES.items()}
    out = nc.dram_tensor("out", [NSEQ, SEQ, D], F32, kind="ExternalOutput").ap()
    WinP = nc.dram_tensor("WinP", [56, 128, 2048], BF16).ap()
    WVS = nc.dram_tensor("WVS", [2, 128, 4096], BF16).ap()
    WguP = nc.dram_tensor("WguP", [64, 128, 2048], BF16).ap()
    WdS = nc.dram_tensor("WdS", [16, 128, 4096], BF16).ap()
    WoS = nc.dram_tensor("WoS", [8, 128, 4096], BF16).ap()
    W1S = nc.dram_tensor("W1S", [4, 128, 4096], BF16).ap()
    dbg_out = {}

    with ExitStack() as es:
        S = Sched(nc, es)

        def sb(name, shape, dt):
            return es.enter_context(nc.sbuf_tensor(name, shape, dt))

        def ps(name, shape, dt):
            return es.enter_context(nc.psum_tensor(name, shape, dt))

        X1 = sb("X1", [128, 4, 2048], F32)
        hT = sb("hT", [128, 16, 512], BF16)
        QO = sb("QO", [128, 16, 512], BF16)
        yT = sb("yT", [128, 16, 512], BF16)
        KT = sb("KT", [128, 2, 2, SEQ], BF16)
        VA = sb("VA", [128, 2, 16, 4 * 65], BF16)
        kcT = sb("kcT", [128, 512], BF16)
        vcA = sb("vcA", [128, 4, 97], BF16)
        CSt = sb("CSt", [128, 2, 512], F32)
        posi = sb("posi", [128, 512], I32)
        TF = sb("TF", [128, 4, 512], F32)
        BFW = sb("BFW", [128, 5, 512], BF16)
        PT = BFW[:, 0:3]
        TBb = BFW[:, 3:5]
        XNB = BFW[:, 0:4].rearrange("p a f -> p (a f)")
        XK = [('pt', 0), ('pt', 1), ('pt', 2), ('tb', 0)]
        Otot = sb("Otot", [128, 2, 4, 128], F32)
        Obf = sb("Obf", [128, 2, 4, 128], BF16)
        Cc = Otot
        hidT = QO[:, 0:4].rearrange("p (a b) f -> p a b f", a=2)
        CCK = ['Cc', ('otot', 0), ('otot', 1)]
        gates = sb("gates", [128, 4, 48], F32)
        sc2 = sb("sc2", [128, 32], F32)
        top8 = sb("top8", [128, 16], F32)
        selb = sb("selb", [128, 4, 32], BF16)
        selbT = sb("selbT", [128, 512], BF16)
        QP = sb("QP", [128, 4, 512], BF16)
        zeros128 = sb("zeros128", [128, 128], BF16)
        Rg = sb("Rg", [128, 4, 128], BF16)
        small = sb("small", [128, 64], F32)
        ubuf = sb("ubuf", [128, 1, 528], F32)
        carry = sb("carry", [128, 8, 16], F32)
        PLT = sb("PLT", [128, 2, 2, 512], BF16)
        rt = sb("rt", [128, 96], F32)
        comb = sb("comb", [128, 4, 16], F32)
        combb = sb("combb", [128, 4, 16], BF16)
        combT = sb("combT", [16, 512], BF16)
        PAN = sb("PAN", [128, 3, 16, 128], BF16)
        WB = sb("WB", [128, 2, 4096], BF16)
        WG = sb("WG", [128, 16, 48], BF16)
        Wpool = sb("Wpool", [128, 8, 512], BF16)
        Who = sb("Who", [128, 8, 128], BF16)
        W2k = sb("W2k", [128, 2, 128], BF16)
        W2v = sb("W2v", [128, 2, 64], BF16)
        Wr = sb("Wr", [128, 16, 20], BF16)
        peT = sb("peT", [64, 2, 32], BF16)
        bh = sb("bh", [128, 4], F32)
        gvec = sb("gvec", [128, 40], F32)
        pscale = sb("pscale", [128, 16], F32)
        brt = sb("brt", [128, 20], F32)
        ident = sb("ident", [128, 128], BF16)
        tri = sb("tri", [128, 2, 128], BF16)
        maskc = sb("maskc", [128, 512], BF16)
        bonus = sb("bonus", [128, 4, 32], F32)
        emat = sb("emat", [128, 16, 128], BF16)
        freq = sb("freq", [128, 1], F32)
        rmat = sb("rmat", [128, 128], BF16)
        onesblk = sb("onesblk", [128, 128], BF16)
        sele = sb("sele", [128, 16, 128], BF16)
        icnt = sb("icnt", [128, 4, 16], F32)
        ssq = sb("ssq", [128, 8], F32)

        tmpo = TF[:, 3, 0:256].rearrange("p (a b) -> p a b", a=4)
        tmpi = TF[:, 3, 256:384].rearrange("p (a b) -> p a b", a=4)
        imp = TF[:, 2, 0:128].rearrange("p (a b) -> p a b", a=4)
        scb = TF[:, 2, 128:256].rearrange("p (a b) -> p a b", a=4)
        PSA = [ps("psa%d" % i, [128, 512], F32) for i in range(3)]
        PSS = [ps("pss%d" % i, [128, 512], F32) for i in range(2)]
        PSO = [ps("pso%d" % i, [128, 4, 128], F32) for i in range(2)]
        PST = ps("pst", [128, 1, 512], BF16)

        rr = {'psa': 0, 'pss': 0, 'pso': 0, 'pst': 0, 'pan': 0, 'wb': 0, 'pt': 0, 'ew': 0}

        def nxt(name, n):
            i = rr[name]
            rr[name] = (i + 1) % n
            return i

        def psa():
            i = nxt('psa', 3)
            return PSA[i], ('psa', i)

        def chk(stage):
            if stage[0] != 'h':
                MARKS.append((stage, S.cnt['pe'], S.cnt['act']))
            if stop_after == stage:
                S.dead = True

        chk('start')
        ukey = [0]

        def uk():
            ukey[0] += 1
            return ('cst', ukey[0])

        def ld(dst, src, key, eng='sp', slot='cst'):
            S.dma(eng, slot, lambda e: e.dma_start(out=dst, in_=src), writes=[uk()])

        ld(tri[:], C['c_tri'], 'tri', 'pool', 'cstp')
        ld(emat[:], C['c_emat'], 'emat', 'pool', 'cstp')
        ld(rmat[:], C['c_rm'], 'rmat', 'pool', 'cstp')
        ld(onesblk[:], C['c_onesblk'], 'onesblk', 'pool', 'cstp')
        ld(freq[:], C['c_freq'], 'freq')
        ld(sele[:], C['c_sele'], 'sele', 'pool', 'cstp')
        ld(icnt[:], C['c_icnt'], 'icnt')
        chk('c0')
        for g in range(4):
            ld(vcA[:, g, 64:97], C['c_ov'], 'vcA', 'pool', 'cstp')
        chk('c1')
        nsc = lambda e, o, i: e.dma_start(out=o, in_=i, allow_slow_non_contiguous=True)
        S.dma('sp', 'cst', lambda e: nsc(e, gvec[:, 0:16], W['attn_norm_g'].rearrange("(c p) -> p c", p=128)), writes=[uk()])
        S.dma('sp', 'cst', lambda e: nsc(e, gvec[:, 16:32], W['ffn_norm_g'].rearrange("(c p) -> p c", p=128)), writes=[uk()])
        S.dma('sp', 'cst', lambda e: nsc(e, pscale[:], W['pool_scale'].rearrange("(c p) -> p c", p=128)), writes=[uk()])
        for i, nm in enumerate(('q_norm_g', 'k_norm_cmp_g', 'k_norm_slc_g', 'k_norm_swa_g')):
            for hb in (0, 64):
                S.dma('sp', 'cst', lambda e, i=i, nm=nm, hb=hb: nsc(e, gvec[hb:hb + 64, 32 + i:33 + i], W[nm].rearrange("(p o) -> p o", o=1)), writes=[uk()])
        chk('c2')
        S.dma('sp', 'cst', lambda e: e.dma_start(out=brt[:, 0:4], in_=W['b_router_group'].partition_broadcast(128)), writes=[uk()])
        S.dma('sp', 'cst', lambda e: e.dma_start(out=brt[:, 4:20], in_=W['b_router_expert'].partition_broadcast(128)), writes=[uk()])
        chk('c3')
        S.op('dve', lambda e: e.memset(gvec[:, 36:37], 1.0), writes=['gvec'])
        S.op('dve', lambda e: e.memset(VA[:].rearrange("p a b c -> p (a b c)"), 1.0), writes=['VA'])
        S.op('dve', lambda e: e.memset(ssq[:], 0.0), writes=['ssq'])
        S.op('dve', lambda e: e.memset(selbT[:], 0.0), writes=['selbT'])
        S.op('dve', lambda e: e.memset(zeros128[:], 0.0), writes=['zeros128'])
        S.op('pool', lambda e: e.memset(QP[:].rearrange("p a f -> p (a f)"), 0.0), writes=[('qp', i) for i in range(4)])
        S.op('pool', lambda e: e.memset(ident[:], 0.0), writes=['ident'])
        S.op('pool', lambda e: e.affine_select(out=ident[:], in_=ident[:], pattern=[[-1, 128]], compare_op=ALU.not_equal, fill=1.0, base=0, channel_multiplier=1), reads=['ident'], writes=['ident'])

        X1f = X1[:].rearrange("p a f -> p (a f)")
        chk('const')
        S.barrier()
        for v_ in range(4):
            S.op('dve', lambda e: e.tensor_scalar_mul(out=Rg[:, v_, :], in0=rmat[:], scalar1=gvec[:, 32 + v_:33 + v_]), reads=['gvec'], writes=['Rg'])
        if skip_prep:
            S.dead = True
        hTf = hT[:].rearrange("p a f -> p (a f)")
        st_in = [X1f[:, i * 4096:(i + 1) * 4096] for i in range(2)]
        st_out = [hTf[:, i * 4096:(i + 1) * 4096] for i in range(2)]
        pj = [0]
        EW3 = ('act', 'dve', 'act', 'act', 'pool', 'act', 'act', 'dve', 'act', 'act', 'act', 'dve', 'act', 'pool', 'act', 'act')

        def scale_op(eng, o, i, sc):
            if eng == 'act':
                S_fn = lambda e: e.mul(out=o, in_=i, mul=sc)
            else:
                S_fn = lambda e: e.tensor_scalar_mul(out=o, in0=i, scalar1=sc)
            return S_fn

        def prep(loads, mode, gofs, stores, resident=None):
            i = pj[0] % 2
            pj[0] += 1
            for (dfn, src) in loads:
                S.dma('sp', 'pin%d' % i, lambda e, dfn=dfn, src=src: e.dma_start(out=dfn(st_in[i]), in_=src), writes=[('pin', i)])
            sin = st_in[i].rearrange("p (k c) -> p k c", c=256)
            for kc in range(16):
                sc = gvec[:, gofs + kc:gofs + kc + 1] if gofs is not None else gvec[:, 36:37]
                if resident is not None:
                    o, ii = resident(sin, kc)
                    wk = [resident.key]
                elif mode == 'A':
                    o = st_out[i].rearrange("p (n k c) -> p n k c", n=2, k=16)[:, :, kc, :]
                    ii = sin[:, kc, :].rearrange("p (n c) -> p n c", n=2)
                    wk = [('pout', i, kc)]
                elif mode == 'Aq':
                    o = st_out[i].rearrange("p (n k hf d) -> p n k hf d", n=2, k=16, hf=2)[:, :, kc, :, :]
                    ii = sin[:, kc, :].rearrange("p (hf r d) -> p r hf d", hf=2, r=2)
                    wk = [('pout', i, kc)]
                else:
                    o = st_out[i][:, kc * 256:(kc + 1) * 256]
                    ii = sin[:, kc, :]
                    wk = [('pout', i, kc)]
                eng = EW3[kc % 16]
                if o.shape[0] != 128:
                    sc = sc[0:o.shape[0]]
                if mode == 'Aq':
                    for n_ in range(2):
                        S.op(eng, scale_op(eng, o[:, n_], ii[:, n_], sc), reads=[('pin', i), 'gvec'], writes=wk)
                    continue
                S.op(eng, scale_op(eng, o, ii, sc), reads=[('pin', i), 'gvec'], writes=wk)
            prev = pend[:]
            del pend[:]
            for (dst, sfn) in stores:
                pend.append((i, dst, sfn))
            flush(prev)

        pend = []

        def flush(lst):
            for (i, dst, sfn) in lst:
                S.dma('sp', 'pout%d' % i, lambda e, dst=dst, sfn=sfn, i=i: e.dma_start(out=dst, in_=sfn(st_out[i])),
                      reads=[('pout', i, kc) for kc in range(16)], writes=['scratch'])

        def win_src(c0, n):
            return W['w_in'][:, c0:c0 + n].rearrange("(k p) c -> p k c", p=128)

        def st_cols(a, n):
            return lambda st: st.rearrange("p (k c) -> p k c", c=256)[:, :, a:a + n]

        def store_panels(p0):
            return [(WinP[p0:p0 + 2].rearrange("n p f -> p n f"), lambda st: st.rearrange("p (n f) -> p n f", n=2))]

        for m in range(2):
            for r2 in range(2):
                c_a = (8 * m + 2 * r2) * 64
                c_b = (8 * m + 4 + 2 * r2) * 64
                prep([(st_cols(0, 128), win_src(c_a, 128)), (st_cols(128, 128), win_src(c_b, 128))], 'Aq', 0,
                     store_panels(4 * m + 2 * r2))
        for pi, c0 in ((8, OFF_KC), (10, OFF_VC), (12, OFF_KS), (14, OFF_KW)):
            prep([(st_cols(0, 256), win_src(c0, 256))], 'A', 0, store_panels(pi))
        for i4 in range(4):
            prep([(st_cols(0, 256), win_src(OFF_POOL + i4 * 256, 256))], 'A', 0, store_panels(16 + 2 * i4))
        for i16 in range(16):
            prep([(st_cols(0, 256), win_src(OFF_MERGE + i16 * 256, 256))], 'A', 0, store_panels(24 + 2 * i16))
        for vi, c0 in ((0, OFF_VS), (1, OFF_VW)):
            prep([(st_cols(0, 256), win_src(c0, 256))], 'B', 0, [(WVS[vi], lambda st: st)])

        class Res:
            def __init__(self, fn, key):
                self.fn, self.key = fn, key

            def __call__(self, sin, kc):
                return self.fn(sin, kc)

        prep([(st_cols(0, 48), win_src(OFF_GATE, 48))], 'R', 0, [],
             resident=Res(lambda sin, kc: (WG[:, kc, :], sin[:, kc, 0:48]), 'WG'))
        prep([(st_cols(0, 4), W['w_router_group'].rearrange("(k p) c -> p k c", p=128)),
              (st_cols(4, 16), W['w_router_expert'].rearrange("(k p) c -> p k c", p=128))], 'R', 16, [],
             resident=Res(lambda sin, kc: (Wr[:, kc, :], sin[:, kc, 0:20]), 'Wr'))
        for e_ in range(NE):
            for gi_, nm in enumerate(('w_expert_gate', 'w_expert_up')):
                prep([(st_cols(0, 256), W[nm][e_].rearrange("(k p) c -> p k c", p=128))], 'A', 16,
                     [(WguP[e_ * 4 + gi_ * 2:e_ * 4 + gi_ * 2 + 2].rearrange("n p f -> p n f"), lambda st: st.rearrange("p (n f) -> p n f", n=2))])
        for eg in range(4):
            for cc in range(4):
                src = W['w_expert_down'][eg * 4:eg * 4 + 4, :, cc * 512:(cc + 1) * 512].rearrange("e (h p) c -> p e h c", p=128)
                prep([(lambda st: st.rearrange("p (e h c) -> p e h c", e=4, h=2), src)], 'B', None, [(WdS[eg * 4 + cc], lambda st: st)])
        for c8 in range(8):
            prep([(st_cols(0, 256), W['w_out'][:, c8 * 256:(c8 + 1) * 256].rearrange("(k p) c -> p k c", p=128))], 'B', None,
                 [(WoS[c8], lambda st: st)])
        for kv, nm in enumerate(('cmp_w1_k', 'cmp_w1_v')):
            for lh in range(2):
                src = W[nm][lh * 1024:(lh + 1) * 1024, :].rearrange("(l d) h -> d l h", d=64)
                prep([(lambda st: st.rearrange("p (l h) -> p l h", h=256)[0:64], src),
                      (lambda st: st.rearrange("p (l h) -> p l h", h=256)[64:128], src)], 'B', None,
                     [(W1S[kv * 2 + lh], lambda st: st)])
        prep([(lambda st: st.rearrange("p (g j e) -> p g j e", g=4, j=2),
               W['w_pool'].rearrange("g (j p) e -> p g j e", p=128))], 'R', None, [],
             resident=Res(lambda sin, kc: (Wpool[:, kc // 2, (kc % 2) * 256:(kc % 2) * 256 + 256], sin[:, kc, :]), 'Wpool'))

        def res_small(sin, kc):
            flat = sin.rearrange("p k c -> p (k c)")
            if kc < 4:
                return Who[:, 2 * kc:2 * kc + 2, :], flat[:, kc * 256:(kc + 1) * 256].rearrange("p (a b) -> p a b", a=2)
            if kc == 4:
                return W2k[:, :, 0:64], flat[:, 1024:1280].rearrange("p (a b) -> p a b", a=2)[:, :, 0:64]
            if kc == 5:
                return W2k[:, :, 64:128], flat[:, 1024:1280].rearrange("p (a b) -> p a b", a=2)[:, :, 0:64]
            if kc == 6:
                return W2v[:, :, :], flat[:, 1280:1536].rearrange("p (a b) -> p a b", a=2)[:, :, 0:64]
            if kc == 7:
                return peT[:, :, :], flat[0:64, 1536:1664].rearrange("p (a b) -> p a b", a=2)[:, :, 0:32]
            return small[:, 32 + kc:33 + kc], flat[:, 4000 + kc:4001 + kc]

        ifl = lambda st: st
        who_src = W['w_head_out'].rearrange("(c two) d e -> (two d) c e", two=2)
        loads = [(lambda st: st[:, 0:1024].rearrange("p (c e) -> p c e", c=8), who_src)]
        for hc in range(2):
            loads.append((lambda st, hc=hc: st[:, 1024 + hc * 128:1024 + hc * 128 + 64], W['cmp_w2_k'][hc * 128:(hc + 1) * 128, :]))
            loads.append((lambda st, hc=hc: st[:, 1280 + hc * 128:1280 + hc * 128 + 64], W['cmp_w2_v'][hc * 128:(hc + 1) * 128, :]))
        i_sm = pj[0] % 2
        S.op('dve', lambda e: e.memset(st_in[i_sm], 0.0), writes=[('pin', i_sm)])
        for kv, nm in enumerate(('cmp_pos_emb_k', 'cmp_pos_emb_v')):
            S.dma('sp', 'pin%d' % i_sm, lambda e, kv=kv, nm=nm: nsc(e, st_in[i_sm][0:64, 1536 + kv * 64:1536 + kv * 64 + 32], W[nm].rearrange("l d -> d l")), writes=[('pin', i_sm)])
        prep(loads, 'R', None, [], resident=Res(res_small, 'smallres'))
        flush(pend)
        if skip_prep:
            S.dead = False
        chk('prep')
        S.barrier()

        base_pan = [(PAN[:, i], ('pan', i), 'pan%d' % i) for i in range(3)]
        pan_slots = list(base_pan)

        def set_pan_slots(extra):
            del pan_slots[:]
            pan_slots.extend(base_pan + extra)
            rr['pan'] = 0

        def load_panel(src):
            i = nxt('pan', len(pan_slots))
            ap_, key_, sname = pan_slots[i]
            S.dma('sp', sname, lambda e: e.dma_start(out=ap_.rearrange("p k c -> p (k c)"), in_=src), reads=['scratch'], writes=[key_])
            return ap_, key_

        base_wb = [(WB[:, i, :], ('wb', i), 'wb%d' % i) for i in range(2)]
        wb_slots = list(base_wb)

        def set_wb_slots(extra):
            del wb_slots[:]
            wb_slots.extend(base_wb + extra)
            rr['wb'] = 0

        def load_wb(src):
            i = nxt('wb', len(wb_slots))
            ap_, key_, sname = wb_slots[i]
            S.dma('sp', sname, lambda e: e.dma_start(out=ap_, in_=src), reads=['scratch'], writes=[key_])
            return ap_, key_

        def mmA(src, actT, akeys, n=512):
            pan_ap, pan_key = load_panel(src)
            pb, pk = psa()
            for kc in range(16):
                S.op('pe', lambda e, kc=kc: e.matmul(pb[:, 0:n], lhsT=pan_ap[:, kc, :], rhs=actT[:, kc, 0:n], start=(kc == 0), stop=(kc == 15)),
                     reads=[pan_key] + akeys, writes=[pk])
            return pb, pk

        def ew():
            i = nxt('ew', 2)
            return ('dve', 'pool')[i]

        def rope_tables(s, t0):
            S.dma('sp', 'posi', lambda e: e.dma_start(out=posi[:], in_=positions[s, t0:t0 + 512].partition_broadcast(128)), writes=['posi'])
            ang, kf, ki = TF[:, 0, :], TF[:, 1, :], posi[:]
            S.op('dve', lambda e: e.tensor_copy(out=ang, in_=posi[:]), reads=['posi'], writes=[('tf', 0)])
            S.op('dve', lambda e: e.tensor_scalar_mul(out=ang, in0=ang, scalar1=freq[:, 0:1]), reads=[('tf', 0), 'freq'], writes=[('tf', 0)])
            for ci, ph in ((0, 1.5707963267948966), (1, 0.0)):
                S.op('dve', lambda e, ph=ph: e.tensor_scalar(out=kf, in0=ang, scalar1=ph, scalar2=1.0 / TWO_PI, op0=ALU.add, op1=ALU.mult),
                     reads=[('tf', 0)], writes=[('tf', 1)])
                S.op('dve', lambda e: e.tensor_copy(out=ki, in_=kf), reads=[('tf', 1)], writes=['posi'])
                S.op('dve', lambda e: e.tensor_copy(out=kf, in_=ki), reads=['posi'], writes=[('tf', 1)])
                r = TF[:, 2, :]
                S.op('dve', lambda e: e.scalar_tensor_tensor(out=r, in0=kf, scalar=-6.28125, in1=ang, op0=ALU.mult, op1=ALU.add),
                     reads=[('tf', 0), ('tf', 1)], writes=[('tf', 2)])
                S.op('dve', lambda e: e.scalar_tensor_tensor(out=r, in0=kf, scalar=-(TWO_PI - 6.28125), in1=r, op0=ALU.mult, op1=ALU.add),
                     reads=[('tf', 1), ('tf', 2)], writes=[('tf', 2)])
                S.op('dve', lambda e, ph=ph: e.tensor_scalar(out=r, in0=r, scalar1=ph, scalar2=PI_SAFE, op0=ALU.add, op1=ALU.min),
                     reads=[('tf', 2)], writes=[('tf', 2)])
                S.op('dve', lambda e: e.tensor_scalar_max(out=r, in0=r, scalar1=-PI_SAFE), reads=[('tf', 2)], writes=[('tf', 2)])
                S.op('act', lambda e, ci=ci: e.activation(out=CSt[:, ci, :], in_=r, func=AF.Sin), reads=[('tf', 2)], writes=['CSt'])

        def normrope(pb, pk, gcol, Ct, St, cskeys, out_ap, okeys, n=512, view=None):
            vw = view if view is not None else (lambda a: a)
            v = gcol - 32
            sq, psb = TBb[:, 0, 0:n], TBb[:, 1, 0:n]
            rstd, t1, t2 = TF[:, 0, 0:n], TF[:, 1, 0:n], TF[:, 2, 0:n]
            S.op('act', lambda e: e.activation(out=sq, in_=pb[:, 0:n], func=AF.Square), reads=[pk], writes=[('tb', 0)])
            S.op('act', lambda e: e.copy(out=psb, in_=pb[:, 0:n]), reads=[pk], writes=[('tb', 1)])
            mb, mk = psa()
            S.op('pe', lambda e: e.matmul(mb[:, 0:n], lhsT=onesblk[:], rhs=sq, start=True, stop=True), reads=[('tb', 0), 'onesblk'], writes=[mk])
            rb, rk = psa()
            S.op('pe', lambda e: e.matmul(rb[:, 0:n], lhsT=Rg[:, v, :], rhs=psb, start=True, stop=True), reads=[('tb', 1), 'Rg'], writes=[rk])
            S.op('act', lambda e: e.activation(out=rstd, in_=mb[:, 0:n], func=AF.Sqrt, bias=small[:, 63:64], scale=1.0), reads=[mk, 'eps'], writes=[('tf', 0)])
            S.op('dve', lambda e: e.reciprocal(out=rstd, in_=rstd), reads=[('tf', 0)], writes=[('tf', 0)])
            S.op('dve', lambda e: e.scalar_tensor_tensor(out=vw(t1), in0=vw(pb[:, 0:n]), scalar=gvec[:, gcol:gcol + 1], in1=Ct, op0=ALU.mult, op1=ALU.mult),
                 reads=[pk, 'gvec'] + cskeys, writes=[('tf', 1)])
            S.op('dve', lambda e: e.tensor_tensor(out=vw(t2), in0=vw(rb[:, 0:n]), in1=St, op=ALU.mult), reads=[rk] + cskeys, writes=[('tf', 2)])
            S.op('pool', lambda e: e.tensor_tensor(out=t1, in0=t1, in1=t2, op=ALU.add), reads=[('tf', 1), ('tf', 2)], writes=[('tf', 1)])
            S.op('pool', lambda e: e.tensor_tensor(out=out_ap, in0=t1, in1=rstd, op=ALU.mult), reads=[('tf', 1), ('tf', 0)], writes=okeys)

        S.op('dve', lambda e: e.memset(small[:, 63:64], 1e-6), writes=['eps'])

        def make_hT():
            for qi in range(4):
                S.op('act', lambda e, qi=qi: e.activation(out=QO[:, 8:12].rearrange("p a f -> p (a f)"), in_=X1[:, qi, :], func=AF.Square, accum_out=ssq[:, qi:qi + 1]),
                     reads=[('X1', qi)], writes=[('QO', 1), ('ssq', qi)])
                chk('h1')
                S.op('dve', lambda e, qi=qi: e.tensor_scalar(out=ssq[:, 4 + qi:5 + qi], in0=ssq[:, qi:qi + 1], scalar1=1.0 / D, scalar2=1e-6, op0=ALU.mult, op1=ALU.add),
                     reads=[('ssq', qi)], writes=[('ssq2', qi)])
                S.op('dve', lambda e, qi=qi: e.memset(ssq[:, qi:qi + 1], 0.0), reads=[('ssq2', qi)], writes=[('ssq', qi)])
                S.op('act', lambda e, qi=qi: e.activation(out=ssq[:, 4 + qi:5 + qi], in_=ssq[:, 4 + qi:5 + qi], func=AF.Sqrt), reads=[('ssq2', qi)], writes=[('ssq2', qi)])
                S.op('dve', lambda e, qi=qi: e.reciprocal(out=ssq[:, 4 + qi:5 + qi], in_=ssq[:, 4 + qi:5 + qi]), reads=[('ssq2', qi)], writes=[('ssq2', qi)])
                S.op('dve', lambda e, qi=qi: e.tensor_scalar_mul(out=XNB, in0=X1[:, qi, :], scalar1=ssq[:, 4 + qi:5 + qi]),
                     reads=[('X1', qi), ('ssq2', qi)], writes=XK)
                chk('h3')
                for k4 in range(4):
                    ti = nxt('pst', 1)
                    for kk in range(4):
                        kc = k4 * 4 + kk
                        S.op('pe', lambda e, kc=kc, kk=kk, ti=ti: e.transpose(out=PST[:, ti, kk * 128:(kk + 1) * 128], in_=XNB[:, kc * 128:(kc + 1) * 128], identity=ident[:]),
                             reads=XK + ['ident'], writes=[('pst', ti)])
                    chk('h4')
                    eng = 'act'
                    o = hT[:, k4 * 4:k4 * 4 + 4, qi * 128:(qi + 1) * 128]
                    ii = PST[:, ti, :].rearrange("p (a b) -> p a b", a=4)
                    if eng == 'act':
                        S.op('act', lambda e, o=o, ii=ii: e.copy(out=o, in_=ii), reads=[('pst', ti)], writes=[('hT', qi)])
                    else:
                        S.op('dve', lambda e, o=o, ii=ii: e.tensor_copy(out=o, in_=ii), reads=[('pst', ti)], writes=[('hT', qi)])
                    chk('h5' if k4 == 0 else ('h6' if k4 == 1 else 'h7'))

        HK = [('hT', qi) for qi in range(4)]
        YK = [('yT', qi) for qi in range(4)]

        def load_x(s, t0):
            for qi in range(4):
                S.dma('sp', 'x%d' % qi, lambda e, qi=qi: e.dma_start(out=X1[:, qi, :], in_=x[s, t0 + qi * 128:t0 + (qi + 1) * 128, :]), writes=[('X1', qi)])

        def dump(name, ap_sb, shape, keys, dt=F32):
            if not dbg:
                return
            if name not in dbg_out:
                dbg_out[name] = nc.dram_tensor("dbg_" + name, shape, dt, kind="ExternalOutput").ap()
            S.dma('sp', 'dbg', lambda e: e.dma_start(out=dbg_out[name], in_=ap_sb), reads=keys, writes=['dbg_' + name])

        for s in range(nseq):
            S.op('pool', lambda e: e.memset(carry[:].rearrange("p a b -> p (a b)"), 0.0), writes=['carry'])
            for tb in range(NQB):
                t0 = tb * TB
                load_x(s, t0)
                chk('p1x')
                make_hT()
                chk('p1a')
                rope_tables(s, t0)
                chk('p1b')
                for ci in range(2):
                    m0 = 1 if tb == 0 else 0
                    S.op('pool', lambda e, ci=ci, m0=m0, tb=tb: e.tensor_copy(out=Cc[:, ci, 0, 32 * tb - 1 + m0:32 * tb + 31], in_=CSt[:, ci, 15 + 16 * m0:512:16]),
                         reads=['CSt'], writes=CCK)
                if dbg and s == 0 and tb == 0:
                    dump('hT', hT[:], [128, 16, 512], HK, BF16)
                    dump('CS', CSt[:], [128, 2, 512], ['CSt'])
                chk('p1c')
                rawv = yT[:].rearrange("p a f -> p (a f)").rearrange("p (c t) -> p c t", c=4)
                for pi in range(8, 12):
                    pb, pk = mmA(WinP[pi], hT, HK)
                    o = rawv[:, pi - 8, t0:t0 + 512]
                    S.op('act', lambda e, o=o, pb=pb: e.copy(out=o, in_=pb[:]), reads=[pk], writes=YK + ['raw'])
                chk('p1d')
                for br in range(2):
                    for j in range(2):
                        pb, pk = mmA(WinP[12 + br * 2 + j], hT, HK)
                        normrope(pb, pk, 34 + br, CSt[:, 0, :], CSt[:, 1, :], ['CSt'], KT[:, br, j, t0:t0 + 512], ['KT'])
                chk('p1e')
                for br in range(2):
                    wb_ap, wb_key = load_wb(WVS[br])
                    wv = wb_ap.rearrange("p (k c) -> p k c", c=256)
                    for qi in range(4):
                        pb, pk = psa()
                        for kc in range(16):
                            S.op('pe', lambda e, kc=kc, qi=qi, pb=pb, wv=wv: e.matmul(pb[:, 0:256], lhsT=hT[:, kc, qi * 128:(qi + 1) * 128], rhs=wv[:, kc, :], start=(kc == 0), stop=(kc == 15)),
                                 reads=[wb_key, ('hT', qi)], writes=[pk])
                        o = VA[:, br, tb * 4 + qi, :].rearrange("p (g c) -> p g c", c=65)[:, :, 0:64]
                        ii = pb[:, 0:256].rearrange("p (g c) -> p g c", c=64)
                        S.op('dve', lambda e, o=o, ii=ii: e.tensor_copy(out=o, in_=ii), reads=[pk], writes=['VA'])
            chk('pass1')
            for ci in range(2):
                for g in range(1, 4):
                    S.op('pool', lambda e, ci=ci, g=g: e.tensor_copy(out=Cc[:, ci, g, 0:127], in_=Cc[:, ci, 0, 0:127]), reads=['Cc'], writes=CCK)
            rawv = yT[:].rearrange("p a f -> p (a f)").rearrange("p (c t) -> p c t", c=4)
            for kv in range(2):
                wis = [load_wb(W1S[kv * 2 + lh]) for lh in range(2)]
                w1v = [wa.rearrange("p (l h) -> p l h", h=256) for (wa, _) in wis]
                wkeys = [wk_ for (_, wk_) in wis]
                pb, pk = psa()
                for hc in range(2):
                    for l in range(32):
                        S.op('pe', lambda e, hc=hc, l=l, pb=pb: e.matmul(pb[:, hc:hc + 1], lhsT=w1v[l // 16][0:64, l % 16, hc * 128:(hc + 1) * 128], rhs=peT[0:64, kv, l:l + 1], start=(l == 0), stop=(l == 31)),
                             reads=wkeys + ['smallres'], writes=[pk])
                S.op('dve', lambda e, pb=pb, kv=kv: e.tensor_copy(out=bh[:, kv * 2:kv * 2 + 2], in_=pb[:, 0:2]), reads=[pk], writes=['bh'])
                for g in range(4):
                    base = (g % 2) * 64
                    for hc in range(2):
                        pb, pk = psa()
                        for l in range(32):
                            S.op('pe', lambda e, hc=hc, l=l, pb=pb, base=base, g=g: e.matmul(
                                pb[:, 0:127], lhsT=w1v[l // 16][base:base + 64, l % 16, hc * 128:(hc + 1) * 128],
                                rhs=rawv[base:base + 64, kv * 2 + g // 2, l:l + 16 * 126 + 1:16], start=(l == 0), stop=(l == 31)),
                                reads=wkeys + ['raw'], writes=[pk])
                        S.op('act', lambda e, pb=pb, hc=hc, g=g, kv=kv: e.activation(out=hidT[:, kv, hc, g * 127:(g + 1) * 127], in_=pb[:, 0:127], func=AF.Silu, bias=bh[:, kv * 2 + hc:kv * 2 + hc + 1], scale=1.0),
                             reads=[pk, 'bh'], writes=[('QO', 0)])
            pb, pk = psa()
            for hc in range(2):
                S.op('pe', lambda e, hc=hc, pb=pb: e.matmul(pb[:, 0:508], lhsT=W2k[:, hc, :], rhs=hidT[:, 0, hc, 0:508], start=(hc == 0), stop=(hc == 1)),
                     reads=[('QO', 0), 'smallres'], writes=[pk])
            normrope(pb, pk, 33, Cc[:, 0, :, 0:127], Cc[:, 1, :, 0:127], ['Cc'], kcT[:, 0:508], ['kcT'], n=508,
                     view=lambda a_: a_.rearrange("p (g n) -> p g n", g=4))
            pb, pk = psa()
            for g in range(4):
                for hc in range(2):
                    S.op('pe', lambda e, hc=hc, g=g, pb=pb: e.matmul(pb[0:127, g * 64:(g + 1) * 64], lhsT=hidT[:, 1, hc, g * 127:(g + 1) * 127], rhs=W2v[:, hc, :], start=(hc == 0), stop=(hc == 1)),
                         reads=[('QO', 0), 'smallres'], writes=[pk])
            S.op('dve', lambda e, pb=pb: e.tensor_copy(out=vcA[0:127, :, 0:64], in_=pb[0:127, 0:256].rearrange("p (g c) -> p g c", g=4)), reads=[pk], writes=['vcA'])
            if dbg and s == 0:
                dump('KT', KT[:], [128, 2, 2, SEQ], ['KT'], BF16)
                dump('VA', VA[:], [128, 2, 16, 260], ['VA'], BF16)
                dump('kcT', kcT[:], [128, 512], ['kcT'], BF16)
                dump('vcA', vcA[:], [128, 4, 97], ['vcA'], BF16)
                dump('hidT', hidT, [128, 2, 2, 512], [('QO', 0)], BF16)

            chk('compress')
            qT = QO[:, 0:8]
            OT = QO[:, 8:16]
            for qb in range(nqb):
                t0 = qb * TB
                first = (s == 0 and qb == 0)
                load_x(s, t0)
                S.dma('pool', 'mk', lambda e, t0=t0: e.dma_start(out=maskc[:], in_=C['c_maskc'][:, t0:t0 + 512]), writes=['maskc'])
                S.dma('sp', 'bon', lambda e, qb=qb: e.dma_start(out=bonus[:], in_=C['c_bonus'][:, qb * 4:qb * 4 + 4, :]), writes=['bonus'])
                chk('blk')
                make_hT()
                chk('mkh')
                rope_tables(s, t0)
                for j in range(8):
                    pb, pk = mmA(WinP[j], hT, HK)
                    normrope(pb, pk, 32, CSt[:, 0, :], CSt[:, 1, :], ['CSt'], qT[:, j, :], [('QO', 0)])
                for qi in range(4):
                    pb, pk = psa()
                    for kc in range(16):
                        S.op('pe', lambda e, kc=kc, qi=qi, pb=pb: e.matmul(pb[:, 0:48], lhsT=hT[:, kc, qi * 128:(qi + 1) * 128], rhs=WG[:, kc, :], start=(kc == 0), stop=(kc == 15)),
                             reads=['WG', ('hT', qi)], writes=[pk])
                    S.op('act', lambda e, qi=qi, pb=pb: e.activation(out=gates[:, qi, :], in_=pb[:, 0:48], func=AF.Sigmoid), reads=[pk], writes=['gates'])
                if dbg and first:
                    dump('qT', QO[:, 0:8], [128, 8, 512], [('QO', 0)], BF16)
                    dump('gates', gates[:], [128, 4, 48], ['gates'])

                chk('qproj')
                def finalize(pso, pok, h, br, pi, hp, first_write, with_imp):
                    rs4 = small[:, 0:4]
                    fac = small[:, 4:8]
                    S.op('dve', lambda e: e.tensor_scalar_max(out=rs4, in0=pso[:, :, 64], scalar1=1e-30), reads=[pok], writes=['rs4'])
                    S.op('dve', lambda e: e.reciprocal(out=rs4, in_=rs4), reads=['rs4'], writes=['rs4'])
                    S.op('dve', lambda e: e.tensor_tensor(out=fac, in0=rs4, in1=gates[:, :, 3 * h + br], op=ALU.mult), reads=['rs4', 'gates'], writes=['fac'])
                    fb = fac.unsqueeze(2).to_broadcast([128, 4, 64])
                    od = Otot[:, pi, :, hp * 64:(hp + 1) * 64]
                    if first_write:
                        S.op('dve', lambda e: e.tensor_tensor(out=od, in0=pso[:, :, 0:64], in1=fb, op=ALU.mult), reads=[pok, 'fac'], writes=[('otot', pi), 'Cc'])
                    else:
                        S.op('dve', lambda e: e.tensor_tensor(out=tmpo, in0=pso[:, :, 0:64], in1=fb, op=ALU.mult), reads=[pok, 'fac'], writes=[('tf', 3)])
                        S.op('pool', lambda e: e.tensor_tensor(out=od, in0=od, in1=tmpo, op=ALU.add), reads=[('tf', 3), ('otot', pi)], writes=[('otot', pi)])
                    if with_imp is not None:
                        rb_ = rs4.unsqueeze(2).to_broadcast([128, 4, 32])
                        if with_imp == 0:
                            S.op('dve', lambda e: e.tensor_tensor(out=imp, in0=pso[:, :, 65:97], in1=rb_, op=ALU.mult), reads=[pok, 'rs4'], writes=[('tf', 2)])
                        else:
                            S.op('dve', lambda e: e.tensor_tensor(out=tmpi, in0=pso[:, :, 65:97], in1=rb_, op=ALU.mult), reads=[pok, 'rs4'], writes=[('tf', 3)])
                            S.op('pool', lambda e: e.tensor_tensor(out=imp, in0=imp, in1=tmpi, op=ALU.add), reads=[('tf', 3), ('tf', 2)], writes=[('tf', 2)])

                for g in range(G):
                    base = (g % 2) * 64
                    heads = [4 * g + r for r in range(4)]

                    def qsl(h):
                        m_, rem = h // 8, h % 8
                        return 4 * m_ + rem % 4
                    SCB = [(PSS[0], ('pss', 0)), (PSS[1], ('pss', 1)), (PSA[0], ('psa', 0)), (PSA[1], ('psa', 1)), (PSA[2], ('psa', 2))]
                    PTB = [(BFW[:, i, :], k_) for i, k_ in enumerate([('pt', 0), ('pt', 1), ('pt', 2), ('tb', 0), ('tb', 1)])]

                    def sc_next():
                        i = nxt('pss', 5)
                        return SCB[i]

                    def pt_next():
                        i = nxt('pt', 5)
                        return PTB[i]
                    cs = []
                    for r, h in enumerate(heads):
                        jq = qsl(h)
                        sb_, sk_ = sc_next()
                        S.op('pe', lambda e: e.matmul(sb_[0:127, :], lhsT=kcT[base:base + 64, g * 127:(g + 1) * 127], rhs=qT[base:base + 64, jq, :], start=True, stop=True),
                             reads=['kcT', ('QO', 0)], writes=[sk_])
                        cs.append((sb_, sk_))
                    for r, h in enumerate(heads):
                        sb_, sk_ = cs[r]
                        pb_, pk_ = pt_next()
                        S.op('act', lambda e: e.activation(out=pb_[0:127, :], in_=sb_[0:127, :], func=AF.Exp, scale=0.125), reads=[sk_], writes=[pk_])
                        S.op('pool', lambda e: e.tensor_tensor(out=pb_[0:127, :], in0=pb_[0:127, :], in1=maskc[0:127, :], op=ALU.mult),
                             reads=[pk_, 'maskc'], writes=[pk_])
                        oi = nxt('pso', 2)
                        for qi in range(4):
                            S.op('pe', lambda e: e.matmul(PSO[oi][:, qi, 0:97], lhsT=pb_[0:127, qi * 128:(qi + 1) * 128], rhs=vcA[0:127, g, :], start=True, stop=True),
                                 reads=[pk_, 'vcA'], writes=[('pso', oi)])
                        finalize(PSO[oi], ('pso', oi), h, 0, r // 2, r % 2, True, (r if qb >= 2 else None))
                    def emit_selection_dve(g=g):
                        S.op('dve', lambda e: e.tensor_tensor(out=scb, in0=imp, in1=bonus[:, :, :], op=ALU.add), reads=[('tf', 2), 'bonus'], writes=[('tf', 2)])
                        for qi in range(4):
                            S.op('dve', lambda e, qi=qi: e.max(out=top8[:, 0:8], in_=scb[:, qi, :]), reads=[('tf', 2)], writes=['top8'])
                            S.op('dve', lambda e, qi=qi: e.match_replace(out=sc2[:], in_to_replace=top8[:, 0:8], in_values=scb[:, qi, :], imm_value=-1e30), reads=[('tf', 2), 'top8'], writes=['sc2'])
                            S.op('dve', lambda e: e.max(out=top8[:, 8:16], in_=sc2[:]), reads=['sc2'], writes=['top8'])
                            S.op('dve', lambda e, qi=qi: e.tensor_scalar(out=sc2[:], in0=scb[:, qi, :], scalar1=top8[:, 15:16], scalar2=None, op0=ALU.is_ge), reads=[('tf', 2), 'top8'], writes=['sc2'])
                            S.op('dve', lambda e, qi=qi: e.tensor_scalar(out=selb[:, qi, :], in0=sc2[:], scalar1=-1.0, scalar2=30000.0, op0=ALU.add, op1=ALU.mult), reads=['sc2'], writes=['selb'])

                    def emit_selection_pe(g=g):
                        ti = nxt('pst', 1)
                        for qi in range(4):
                            S.op('pe', lambda e, qi=qi, ti=ti: e.transpose(out=PST[0:32, ti, qi * 128:(qi + 1) * 128], in_=selb[:, qi, :], identity=ident[:]),
                                 reads=['selb', 'ident'], writes=[('pst', ti)])
                        S.op('dve', lambda e, ti=ti: e.tensor_copy(out=selbT[0:32, :], in_=PST[0:32, ti, :]), reads=[('pst', ti)], writes=['selbT'])
                        if dbg and s == 0 and qb == 2 and g == 0:
                            dump('selb', selb[:], [128, 4, 32], ['selb'], BF16)
                            dump('imp', imp, [128, 4, 32], ['imp'])
                    if qb >= 2:
                        emit_selection_dve()
                    steps = []
                    qp_rr = [0]
                    cb_after = {}
                    order = [(0, 2), (1, 2), (0, 1), (1, 1), (2, 2), (3, 2), (2, 1), (3, 1)]
                    for ui, (r, br) in enumerate(order):
                        h = heads[r]
                        kts = list(range(0, 4 * qb + 4)) if br == 1 else list(range(max(4 * qb - 4, 0), 4 * qb + 4))
                        unit = {'h': h, 'r': r, 'br': br, 'oi': None, 'qp': None, 'newhead': br == 2}
                        for kt in kts:
                            steps.append({'u': unit, 'kt': kt, 'first': kt == kts[0], 'last': kt == kts[-1]})
                        if ui == 2 and qb >= 2:
                            steps[len(steps) - len(kts)]['pre'] = emit_selection_pe

                    hq = {}

                    def emit_score(st):
                        u, kt = st['u'], st['kt']
                        br, jq = u['br'], qsl(u['h'])
                        dloc = kt - 4 * qb
                        lo = max(dloc, 0)
                        hi = 3 if br == 1 else min(dloc + 4, 3)
                        c0 = lo * 128
                        n = (hi - lo + 1) * 128
                        sb_, sk_ = sc_next()
                        use_mask = (br == 1 and qb >= 2)
                        if st['first'] and u['newhead']:
                            qs = (g % 2) * 2 + (qp_rr[0] % 2)
                            qp_rr[0] += 1
                            hq[u['h']] = qs
                            S.op('dve', lambda e: e.tensor_copy(out=QP[base:base + 64, qs, :], in_=qT[base:base + 64, jq, :]), reads=[('QO', 0)], writes=[('qp', qs)])
                        qs = hq[u['h']]
                        S.op('pe', lambda e: e.matmul(
                            sb_[:, 0:n], lhsT=KT[:, br - 1, g // 2, kt * 128:(kt + 1) * 128], rhs=QP[:, qs, c0:c0 + n], start=True, stop=(not use_mask)),
                            reads=['KT', ('qp', qs)], writes=[sk_])
                        if use_mask:
                            S.op('pe', lambda e: e.matmul(sb_[:, 0:n], lhsT=emat[:, kt, :], rhs=selbT[:, c0:c0 + n], start=False, stop=True),
                                 reads=['emat', 'selbT'], writes=[sk_])
                        st['ctx'] = (dloc, lo, hi, c0, n, sb_, sk_)

                    def emit_rest(st):
                        u, kt = st['u'], st['kt']
                        br = u['br']
                        dloc, lo, hi, c0, n, sb_, sk_ = st['ctx']
                        if st['first']:
                            u['oi'] = nxt('pso', 2)
                            oi0 = u['oi']
                            S.op('pe', lambda e: e.matmul(PSO[oi0][:, :, :], lhsT=zeros128[:], rhs=emat[:, 0:4, :], start=True, stop=False),
                                 reads=['emat', 'zeros128'], writes=[('pso', oi0)])
                        oi = u['oi']
                        pb_, pk_ = pt_next()
                        S.op('act', lambda e: e.activation(out=pb_[:, 0:n], in_=sb_[:, 0:n], func=AF.Exp, scale=0.125), reads=[sk_], writes=[pk_])
                        if dloc >= 0:
                            S.op('pool', lambda e: e.tensor_tensor(out=pb_[:, 0:128], in0=pb_[:, 0:128], in1=tri[:, 0, :], op=ALU.mult),
                                 reads=[pk_, 'tri'], writes=[pk_])
                        if br == 2 and 0 <= dloc + 4 <= 3:
                            cf = (hi - lo) * 128
                            S.op('pool', lambda e: e.tensor_tensor(out=pb_[:, cf:cf + 128], in0=pb_[:, cf:cf + 128], in1=tri[:, 1, :], op=ALU.mult),
                                 reads=[pk_, 'tri'], writes=[pk_])
                        for qi in range(lo, hi + 1):
                            sp = bool(st['last'] and qi == hi)
                            S.op('pe', lambda e: e.matmul(
                                PSO[oi][:, qi, 0:65], lhsT=pb_[:, (qi - lo) * 128:(qi - lo + 1) * 128], rhs=VA[:, br - 1, kt, g * 65:(g + 1) * 65], start=False, stop=sp),
                                reads=[pk_, 'VA'], writes=[('pso', oi)])
                        if st['last']:
                            finalize(PSO[oi], ('pso', oi), u['h'], br, u['r'] // 2, u['r'] % 2, False, None)

                    LA = 3
                    n_sc = 0
                    for i_st in range(len(steps)):
                        while n_sc < min(len(steps), i_st + 1 + LA):
                            if 'pre' in steps[n_sc]:
                                steps[n_sc]['pre']()
                            emit_score(steps[n_sc])
                            n_sc += 1
                        emit_rest(steps[i_st])
                        if i_st in cb_after:
                            cb_after[i_st]()
                    for pi in range(2):
                        S.op('act', lambda e, pi=pi: e.copy(out=Obf[:, pi], in_=Otot[:, pi]), reads=[('otot', pi)], writes=[('obf', pi)])
                        ti = nxt('pst', 1)
                        for qi in range(4):
                            S.op('pe', lambda e, pi=pi, qi=qi, ti=ti: e.transpose(out=PST[:, ti, qi * 128:(qi + 1) * 128], in_=Obf[:, pi, qi, :], identity=ident[:]),
                                 reads=[('obf', pi), 'ident'], writes=[('pst', ti)])
                        S.op('act', lambda e, pi=pi, ti=ti, g=g: e.copy(out=OT[:, 2 * g + pi, :], in_=PST[:, ti, :]), reads=[('pst', ti)], writes=[('QO', 1)])
                if dbg and first:
                    dump('OT', QO[:, 8:16], [128, 8, 512], [('QO', 1)], BF16)

                chk('attn')
                QOf = QO[:].rearrange("p a f -> p (a f)")
                BFf = BFW[:].rearrange("p a f -> p (a f)")
                ex_y = [(QOf[:, i * 2048:(i + 1) * 2048].rearrange("p (k c) -> p k c", c=128), ('QOs', i), 'pxq%d' % i) for i in range(2)]
                ex_y.append((BFf[:, 0:2048].rearrange("p (k c) -> p k c", c=128), ('BFs', 0), 'pxb0'))
                S.alias_in([('QOs', 0), ('QOs', 1)], [('QO', 0)])
                S.alias_in([('BFs', 0)], XK + [('tb', 1)])
                set_pan_slots(ex_y)
                def pool_group(gi):
                    w = (2, 4, 8, 16)[gi]
                    for j in range(2):
                        pb, pk = mmA(WinP[16 + gi * 2 + j], hT, HK)
                        U = ubuf[:, 0, :]
                        S.op('pool', lambda e, gi=gi, j=j: e.tensor_copy(out=ubuf[:, 0, 0:16], in_=carry[:, gi * 2 + j, :]), reads=['carry'], writes=['ub0'])
                        S.op('act', lambda e, pb=pb: e.copy(out=ubuf[:, 0, 16:528], in_=pb[:]), reads=[pk], writes=['ub0'])
                        S.op('pool', lambda e, gi=gi, j=j: e.tensor_copy(out=carry[:, gi * 2 + j, :], in_=ubuf[:, 0, 512:528]), reads=['ub0'], writes=['carry'])
                        TFf = TF[:].rearrange("p a f -> p (a f)")
                        ubs = [ubuf[:, 0, :], TFf[:, 0:528], TFf[:, 528:1056]]
                        ubk = [['ub0'], [('tf', 0), ('tf', 1)], [('tf', 1), ('tf', 2)]]
                        cur = 0
                        for kstep in range(gi + 1):
                            sh = 1 << kstep
                            nx_ = 1 if cur != 1 else 2
                            S.op('pool', lambda e, cur=cur, nx_=nx_, sh=sh, ubs=ubs: e.tensor_tensor(out=ubs[nx_][:, sh:528], in0=ubs[cur][:, sh:528], in1=ubs[cur][:, 0:528 - sh], op=ALU.add),
                                 reads=ubk[cur], writes=ubk[nx_])
                            cur = nx_
                        S.op('dve', lambda e, cur=cur, j=j, w=w, ubs=ubs: e.scalar_tensor_tensor(out=PLT[:, gi % 2, j, :], in0=ubs[cur][:, 16:528], scalar=1.0 / w, in1=ubuf[:, 0, 16:528], op0=ALU.mult, op1=ALU.subtract),
                             reads=ubk[cur] + ['ub0'], writes=[('PLT', gi % 2)])
                        if qb == 0:
                            S.op('dve', lambda e, cur=cur, gi=gi, ubs=ubs: e.tensor_tensor(out=small[:, 8:24], in0=ubs[cur][:, 16:32], in1=icnt[:, gi, :], op=ALU.mult), reads=ubk[cur] + ['icnt'], writes=['t16'])
                            S.op('dve', lambda e, j=j: e.tensor_tensor(out=PLT[:, gi % 2, j, 0:16], in0=small[:, 8:24], in1=ubuf[:, 0, 16:32], op=ALU.subtract), reads=['t16', 'ub0', ('PLT', gi % 2)], writes=[('PLT', gi % 2)])
                pool_group(0)
                for c in range(16):
                    gi = c // 4
                    pa, pak = psa()
                    cb = (c % 2) * 64
                    S.op('pe', lambda e, pa=pa, c=c, cb=cb: e.matmul(pa[:], lhsT=Who[cb:cb + 64, c // 2, :], rhs=OT[cb:cb + 64, c // 2, :], start=True, stop=True),
                         reads=['smallres', ('QO', 1)], writes=[pak])
                    pm, pmk = mmA(WinP[24 + c], hT, HK)
                    S.op('act', lambda e, pm=pm: e.activation(out=TF[:, 0, :], in_=pm[:], func=AF.Sigmoid), reads=[pmk], writes=[('tf', 0)])
                    S.op('dve', lambda e, pa=pa: e.tensor_tensor(out=TF[:, 1, :], in0=pa[:], in1=TF[:, 0, :], op=ALU.mult), reads=[pak, ('tf', 0)], writes=[('tf', 1)])
                    pp, ppk = psa()
                    for j in range(2):
                        S.op('pe', lambda e, pp=pp, j=j, c=c, gi=gi: e.matmul(pp[:], lhsT=Wpool[:, gi * 2 + j, (c % 4) * 128:(c % 4 + 1) * 128], rhs=PLT[:, gi % 2, j, :], start=(j == 0), stop=(j == 1)),
                             reads=['Wpool', ('PLT', gi % 2)], writes=[ppk])
                    pm1, pm1k = mmA(WinP[40 + c], hT, HK)
                    S.op('act', lambda e, pm1=pm1: e.activation(out=TF[:, 2, :], in_=pm1[:], func=AF.Sigmoid), reads=[pm1k], writes=[('tf', 2)])
                    S.op('dve', lambda e, pp=pp, c=c: e.scalar_tensor_tensor(out=TF[:, 3, :], in0=pp[:], scalar=pscale[:, c:c + 1], in1=TF[:, 2, :], op0=ALU.mult, op1=ALU.mult),
                         reads=[ppk, ('tf', 2), 'pscale'], writes=[('tf', 3)])
                    S.op('pool', lambda e, c=c: e.tensor_tensor(out=yT[:, c, :], in0=TF[:, 1, :], in1=TF[:, 3, :], op=ALU.add), reads=[('tf', 1), ('tf', 3)], writes=YK + ['raw'])
                    if c % 4 == 0 and gi < 3:
                        pool_group(gi + 1)
                if dbg and first:
                    dump('yT', yT[:], [128, 16, 512], YK, BF16)

                chk('ymix')
                S.alias_out([('QOs', 0), ('QOs', 1)], [('QO', 0)])
                S.alias_out([('BFs', 0)], XK + [('tb', 1)])
                set_pan_slots([])
                S.alias_in([('QOw', 0)], [('QO', 0)])
                set_wb_slots([(QO[:, 0:8].rearrange("p a f -> p (a f)"), ('QOw', 0), 'wbx0')])
                for c8 in range(8):
                    wb_ap, wb_key = load_wb(WoS[c8])
                    wv = wb_ap.rearrange("p (k c) -> p k c", c=256)
                    for qi in range(4):
                        pb, pk = psa()
                        for kc in range(16):
                            S.op('pe', lambda e, kc=kc, qi=qi, pb=pb, wv=wv: e.matmul(pb[:, 0:256], lhsT=yT[:, kc, qi * 128:(qi + 1) * 128], rhs=wv[:, kc, :], start=(kc == 0), stop=(kc == 15)),
                                 reads=[wb_key, ('yT', qi)], writes=[pk])
                        xs = X1[:, qi, c8 * 256:(c8 + 1) * 256]
                        S.op('dve', lambda e, xs=xs, pb=pb: e.tensor_tensor(out=xs, in0=pb[:, 0:256], in1=xs, op=ALU.add), reads=[pk, ('X1', qi)], writes=[('X1', qi)])
                S.alias_out([('QOw', 0)], [('QO', 0)])
                set_wb_slots([])
                if dbg and first:
                    dump('x1', X1[:], [128, 4, 2048], [('X1', qi) for qi in range(4)])

                chk('wout')
                make_hT()
                chk('mkh2')
                for qi in range(4):
                    pb, pk = psa()
                    for kc in range(16):
                        S.op('pe', lambda e, kc=kc, qi=qi, pb=pb: e.matmul(pb[:, 0:20], lhsT=hT[:, kc, qi * 128:(qi + 1) * 128], rhs=Wr[:, kc, :], start=(kc == 0), stop=(kc == 15)),
                             reads=['Wr', ('hT', qi)], writes=[pk])
                    lg = rt[:, 0:20]
                    mx, nmx, se, pg, gm, pen = rt[:, 20:21], rt[:, 21:22], rt[:, 22:23], rt[:, 23:24], rt[:, 24:28], rt[:, 28:32]
                    lm = rt[:, 32:48]
                    t8 = rt[:, 48:56]
                    dv, e2, den, w1, w2 = rt[:, 56:57], rt[:, 57:58], rt[:, 58:59], rt[:, 59:60], rt[:, 60:61]
                    ex = rt[:, 64:68]
                    m1 = rt[:, 68:84]
                    RK = ['rt']
                    S.op('dve', lambda e, pb=pb: e.tensor_tensor(out=lg, in0=pb[:, 0:20], in1=brt[:], op=ALU.add), reads=[pk, 'brt'], writes=RK)
                    S.op('dve', lambda e: e.tensor_reduce(out=mx, in_=lg[:, 0:4], axis=AX.X, op=ALU.max), reads=RK, writes=RK)
                    S.op('dve', lambda e: e.tensor_scalar_mul(out=nmx, in0=mx, scalar1=-1.0), reads=RK, writes=RK)
                    S.op('dve', lambda e: e.memset(se, 0.0), reads=RK, writes=RK)
                    S.op('act', lambda e: e.activation(out=ex, in_=lg[:, 0:4], func=AF.Exp, bias=nmx, scale=1.0, accum_out=se), reads=RK, writes=RK)
                    S.op('dve', lambda e: e.reciprocal(out=pg, in_=se), reads=RK, writes=RK)
                    S.op('dve', lambda e: e.tensor_scalar(out=gm, in0=lg[:, 0:4], scalar1=mx, scalar2=None, op0=ALU.is_ge), reads=RK, writes=RK)
                    S.op('dve', lambda e: e.tensor_scalar(out=pen, in0=gm, scalar1=-1.0, scalar2=1e30, op0=ALU.add, op1=ALU.mult), reads=RK, writes=RK)
                    S.op('dve', lambda e: e.tensor_tensor(out=lm.rearrange("p (g j) -> p g j", g=4), in0=lg[:, 4:20].rearrange("p (g j) -> p g j", g=4),
                                                         in1=pen.unsqueeze(2).to_broadcast([128, 4, 4]), op=ALU.add), reads=RK, writes=RK)
                    S.op('dve', lambda e: e.max(out=t8, in_=lm), reads=RK, writes=RK)
                    S.op('dve', lambda e: e.tensor_tensor(out=dv, in0=t8[:, 1:2], in1=t8[:, 0:1], op=ALU.subtract), reads=RK, writes=RK)
                    S.op('act', lambda e: e.activation(out=e2, in_=dv, func=AF.Exp), reads=RK, writes=RK)
                    S.op('dve', lambda e: e.tensor_scalar_add(out=den, in0=e2, scalar1=1.0), reads=RK, writes=RK)
                    S.op('dve', lambda e: e.reciprocal(out=den, in_=den), reads=RK, writes=RK)
                    S.op('dve', lambda e: e.tensor_tensor(out=w1, in0=pg, in1=den, op=ALU.mult), reads=RK, writes=RK)
                    S.op('dve', lambda e: e.tensor_tensor(out=w2, in0=w1, in1=e2, op=ALU.mult), reads=RK, writes=RK)
                    S.op('dve', lambda e: e.tensor_scalar(out=m1, in0=lm, scalar1=t8[:, 0:1], scalar2=w1, op0=ALU.is_equal, op1=ALU.mult), reads=RK, writes=RK)
                    S.op('dve', lambda e: e.tensor_scalar(out=lm, in0=lm, scalar1=t8[:, 1:2], scalar2=w2, op0=ALU.is_equal, op1=ALU.mult), reads=RK, writes=RK)
                    S.op('dve', lambda e, qi=qi: e.tensor_tensor(out=comb[:, qi, :], in0=m1, in1=lm, op=ALU.add), reads=RK, writes=['comb'])
                S.op('dve', lambda e: e.tensor_copy(out=combb[:], in_=comb[:]), reads=['comb'], writes=['combb'])
                ti = nxt('pst', 1)
                for qi in range(4):
                    S.op('pe', lambda e, qi=qi, ti=ti: e.transpose(out=PST[0:16, ti, qi * 128:(qi + 1) * 128], in_=combb[:, qi, :], identity=ident[:]), reads=['combb', 'ident'], writes=[('pst', ti)])
                S.op('act', lambda e, ti=ti: e.copy(out=combT[:, :], in_=PST[0:16, ti, :]), reads=[('pst', ti)], writes=['combT'])
                if dbg and first:
                    dump('comb', comb[:], [128, 4, 16], ['comb'])
                    dump('h2T', hT[:], [128, 16, 512], HK, BF16)
                yTf = yT[:].rearrange("p a f -> p (a f)")
                ex_m = [(yTf[:, i * 2048:(i + 1) * 2048].rearrange("p (k c) -> p k c", c=128), ('yTs', i), 'pxy%d' % i) for i in range(4)]
                S.alias_in([('yTs', i) for i in range(4)], YK + ['raw'])
                set_pan_slots(ex_m)
                for eg in range(4):
                    par = eg % 2
                    hw = QO[:, par * 8:(par + 1) * 8]
                    for el in range(4):
                        e_ = eg * 4 + el
                        pc, pck = psa()
                        S.op('pe', lambda e, pc=pc, e_=e_: e.matmul(pc[:], lhsT=sele[0:16, e_, :], rhs=combT[:, :], start=True, stop=True), reads=['sele', 'combT'], writes=[pck])
                        S.op('act', lambda e, pc=pc: e.copy(out=TF[:, 2, :], in_=pc[:]), reads=[pck], writes=[('tf', 2)])
                        for hc in range(2):
                            pg_, pgk = mmA(WguP[e_ * 4 + hc], hT, HK)
                            S.op('act', lambda e, pg_=pg_: e.activation(out=TF[:, 0, :], in_=pg_[:], func=AF.Silu), reads=[pgk], writes=[('tf', 0)])
                            pu, puk = mmA(WguP[e_ * 4 + 2 + hc], hT, HK)
                            S.op('dve', lambda e, pu=pu: e.tensor_tensor(out=TF[:, 1, :], in0=pu[:], in1=TF[:, 0, :], op=ALU.mult), reads=[puk, ('tf', 0)], writes=[('tf', 1)])
                            S.op('pool', lambda e, hw=hw, el=el, hc=hc: e.tensor_tensor(out=hw[:, el * 2 + hc, :], in0=TF[:, 1, :], in1=TF[:, 2, :], op=ALU.mult),
                                 reads=[('tf', 1), ('tf', 2)], writes=[('QO', par)])
                    for cc in range(4):
                        wb_ap, wb_key = load_wb(WdS[eg * 4 + cc])
                        wv = wb_ap.rearrange("p (k c) -> p k c", c=512)
                        for qi in range(4):
                            pb, pk = psa()
                            for k8 in range(8):
                                S.op('pe', lambda e, k8=k8, qi=qi, pb=pb, wv=wv, hw=hw: e.matmul(pb[:], lhsT=hw[:, k8, qi * 128:(qi + 1) * 128], rhs=wv[:, k8, :], start=(k8 == 0), stop=(k8 == 7)),
                                     reads=[wb_key, ('QO', par)], writes=[pk])
                            xs = X1[:, qi, cc * 512:(cc + 1) * 512]
                            S.op('dve', lambda e, xs=xs, pb=pb: e.tensor_tensor(out=xs, in0=pb[:], in1=xs, op=ALU.add), reads=[pk, ('X1', qi)], writes=[('X1', qi)])
                S.alias_out([('yTs', i) for i in range(4)], YK + ['raw'])
                set_pan_slots([])
                chk('moe')
                for qi in range(4):
                    S.dma('sp', 'out%d' % qi, lambda e, qi=qi: e.dma_start(out=out[s, t0 + qi * 128:t0 + (qi + 1) * 128, :], in_=X1[:, qi, :]), reads=[('X1', qi)], writes=['out'])
        S.barrier()
        S.emit()
    return nc, dbg_out


_CACHE = {}


def _layout_inputs(inputs):
    consts = make_consts()
    wts = {}
    for k in WEIGHT_SHAPES:
        a = np.asarray(inputs[k])
        wts[k] = np.ascontiguousarray(a.reshape(a.shape[1:]), dtype=np.float32)
    xs = np.asarray(inputs['x'], dtype=np.float32)
    pos = np.asarray(inputs['positions'], dtype=np.int32)
    in_maps = []
    for c in range(NCORES):
        m = {'x': np.ascontiguousarray(xs[c * NSEQ:(c + 1) * NSEQ]), 'positions': np.ascontiguousarray(pos[c * NSEQ:(c + 1) * NSEQ])}
        m.update(wts)
        m.update(consts)
        in_maps.append(m)
    return in_maps


def kernel(**inputs):
    if 'nc' not in _CACHE:
        _CACHE['nc'] = build()[0]
    nc = _CACHE['nc']
    in_maps = _layout_inputs(inputs)
    res = run_bass_kernel_spmd(nc, in_maps, core_ids=list(range(NCORES)))
    outs = [np.asarray(r['out']) for r in res.results]
    return np.concatenate(outs, axis=0).astype(np.float32)
```

```python
import numpy as np
from contextlib import ExitStack
import concourse.bass as bass
import concourse.mybir as mybir
from concourse.bass_utils import run_bass_kernel_spmd

F32 = mybir.dt.float32
BF16 = mybir.dt.bfloat16
I32 = mybir.dt.int32
ALU = mybir.AluOpType
AF = mybir.ActivationFunctionType
AX = mybir.AxisListType

D = 2048
SEQ = 2048
NSEQ = 2
NCORES = 8
TB = 512
NQB = SEQ // TB
H = 16
G = 4
NCMP = 127
INW = 7728
OFF_Q, OFF_KC, OFF_VC, OFF_KS, OFF_VS, OFF_KW, OFF_VW, OFF_GATE, OFF_POOL, OFF_MERGE = (
    0, 1024, 1280, 1536, 1792, 2048, 2304, 2560, 2608, 3632)
NE = 16
TWO_PI = 6.283185307179586
PI_SAFE = 3.1415925


class _Rec:
    def __init__(self):
        self.call = None

    def __getattr__(self, name):
        def f(*a, **k):
            assert self.call is None
            self.call = (name, a, k)
            return self
        return f


def _replay(fn):
    rec = _Rec()
    fn(rec)
    name, a, k = rec.call
    return lambda e: getattr(e, name)(*a, **k)


class Sched:
    ENG = ('pe', 'act', 'dve', 'pool', 'sp')

    def __init__(self, nc, es):
        self.nc = nc
        self.es = es
        self.lists = {e: [] for e in self.ENG}
        self.sem = {e: es.enter_context(nc.semaphore('s_' + e)) for e in self.ENG}
        self.cnt = {e: 0 for e in self.ENG}
        self.seen = {e: {} for e in self.ENG}
        self.lastw = {}
        self.readers = {}
        self.dsem = {}

    def _deps(self, reads, writes):
        deps = []
        for k in reads:
            d = self.lastw.get(k)
            if d is not None:
                deps.append(d)
        for k in writes:
            d = self.lastw.get(k)
            if d is not None:
                deps.append(d)
            r = self.readers.get(k)
            if r:
                deps.extend(r.values())
        return deps

    def _emit_waits(self, eng, deps):
        need = {}
        seen = self.seen[eng]
        for (sname, sem, val) in deps:
            if eng == 'pe' and sname == 'Epe':
                continue
            if seen.get(sname, 0) < val:
                if sname not in need or need[sname][1] < val:
                    need[sname] = (sem, val)
        for sname, (sem, val) in need.items():
            seen[sname] = val
            self.lists[eng].append(lambda e, sem=sem, val=val: e.wait_ge(sem, val))

    def _reg(self, dep, reads, writes):
        for k in writes:
            self.lastw[k] = dep
            self.readers[k] = {}
        for k in reads:
            if k not in writes:
                r = self.readers.setdefault(k, {})
                o = r.get(dep[0])
                if o is None or o[2] < dep[2]:
                    r[dep[0]] = dep

    dead = False

    def op(self, eng, fn, reads=(), writes=()):
        if self.dead:
            return
        fn = _replay(fn)
        self._emit_waits(eng, self._deps(reads, writes))
        self.cnt[eng] += 1
        sem = self.sem[eng]
        self.lists[eng].append(lambda e, fn=fn, sem=sem: fn(e).then_inc(sem, 1))
        dep = ('E' + eng, sem, self.cnt[eng])
        self._reg(dep, reads, writes)

    def dma(self, eng, slot, fn, reads=(), writes=()):
        if self.dead:
            return
        fn = _replay(fn)
        self._emit_waits(eng, self._deps(reads, writes))
        if slot not in self.dsem:
            self.dsem[slot] = [self.es.enter_context(self.nc.semaphore('d_' + slot)), 0]
        ent = self.dsem[slot]
        ent[1] += 16
        sem = ent[0]
        self.lists[eng].append(lambda e, fn=fn, sem=sem: fn(e).then_inc(sem, 16))
        dep = ('D' + slot, sem, ent[1])
        self._reg(dep, reads, writes)

    def alias_in(self, new_keys, old_keys):
        acc = {}
        for k in old_keys:
            for d in [self.lastw.get(k)] + list(self.readers.get(k, {}).values()):
                if d is not None and (d[0] not in acc or acc[d[0]][2] < d[2]):
                    acc[d[0]] = d
        for k in new_keys:
            self.lastw.pop(k, None)
            self.readers[k] = dict(acc)

    def alias_out(self, new_keys, old_keys):
        acc = {}
        for k in new_keys:
            for d in [self.lastw.get(k)] + list(self.readers.get(k, {}).values()):
                if d is not None and (d[0] not in acc or acc[d[0]][2] < d[2]):
                    acc[d[0]] = d
        for k in old_keys:
            r = self.readers.setdefault(k, {})
            for n_, d in acc.items():
                if n_ not in r or r[n_][2] < d[2]:
                    r[n_] = d

    def barrier(self):
        deps = [('E' + e, self.sem[e], self.cnt[e]) for e in self.ENG if self.cnt[e] > 0]
        deps += [('D' + s, ent[0], ent[1]) for s, ent in self.dsem.items()]
        for e in self.ENG:
            self._emit_waits(e, deps)

    def emit(self):
        nc = self.nc
        L = self.lists
        with nc.Block() as block:
            @block.tensor
            def _(e):
                for f in L['pe']:
                    f(e)

            @block.scalar
            def _(e):
                for f in L['act']:
                    f(e)

            @block.vector
            def _(e):
                for f in L['dve']:
                    f(e)

            @block.gpsimd
            def _(e):
                for f in L['pool']:
                    f(e)

            @block.sync
            def _(e):
                for f in L['sp']:
                    f(e)


def make_consts():
    c = {}
    tri = np.zeros((2, 128, 128), np.float32)
    k = np.arange(128)[:, None]
    q = np.arange(128)[None, :]
    tri[0] = (k <= q)
    tri[1] = (k > q)
    c['c_tri'] = tri.transpose(1, 0, 2).copy()
    n = np.arange(NCMP)[:, None]
    t = np.arange(SEQ)[None, :]
    mk = np.zeros((128, SEQ), np.float32)
    mk[:NCMP] = (16 * n + 31 <= t)
    c['c_maskc'] = mk
    tt = np.arange(SEQ)
    tb = tt // 64
    j = np.arange(32)[None, :]
    dist = tb[:, None] - j
    valid = dist >= 0
    forced = (j == 0) | (valid & (dist < 2))
    bonus = np.where(valid, 1e4 * forced.astype(np.float32), -1e30).astype(np.float32)
    c['c_bonus'] = bonus.reshape(16, 128, 32).transpose(1, 0, 2).copy()
    em = np.zeros((32, 16, 128), np.float32)
    for kt in range(16):
        for kk in range(128):
            em[2 * kt + kk // 64, kt, kk] = 1.0
    emp = np.zeros((128, 16, 128), np.float32)
    emp[:32] = em
    c['c_emat'] = emp
    a0 = np.arange(NCMP)[:, None] * 16
    b0 = np.arange(32)[None, :] * 64
    ov = np.clip(np.minimum(a0 + 32, b0 + 64) - np.maximum(a0, b0), 0, None) / 32.0
    ovp = np.zeros((128, 33), np.float32)
    ovp[:NCMP, 0] = 1.0
    ovp[:NCMP, 1:] = ov
    c['c_ov'] = ovp
    inv_freq = (500000.0 ** (-np.arange(0, 16, 2, dtype=np.float32) / 16)).astype(np.float32)
    fr = np.zeros((128, 1), np.float32)
    for p in range(128):
        if p % 64 < 16:
            fr[p, 0] = inv_freq[p % 8]
    c['c_freq'] = fr
    rm = np.zeros((128, 128), np.float32)
    for b in (0, 64):
        for d in range(8):
            rm[b + d + 8, b + d] = -1.0
            rm[b + d, b + d + 8] = 1.0
    c['c_rm'] = rm
    ob = np.zeros((128, 128), np.float32)
    ob[:64, :64] = 1.0 / 64
    ob[64:, 64:] = 1.0 / 64
    c['c_onesblk'] = ob
    se = np.zeros((128, 16, 128), np.float32)
    for e in range(16):
        se[e, e, :] = 1.0
    c['c_sele'] = se
    ic = np.zeros((128, 4, 16), np.float32)
    for gi, w in enumerate((2, 4, 8, 16)):
        for xx in range(16):
            ic[:, gi, xx] = 1.0 / min(xx + 1, w)
    c['c_icnt'] = ic
    return c


CONST_SHAPES = {
    'c_tri': [128, 2, 128], 'c_maskc': [128, SEQ], 'c_bonus': [128, 16, 32], 'c_emat': [128, 16, 128],
    'c_ov': [128, 33], 'c_freq': [128, 1], 'c_rm': [128, 128], 'c_onesblk': [128, 128],
    'c_sele': [128, 16, 128], 'c_icnt': [128, 4, 16],
}

WEIGHT_SHAPES = {
    'attn_norm_g': [D], 'w_in': [D, INW], 'q_norm_g': [64], 'k_norm_cmp_g': [64], 'k_norm_slc_g': [64],
    'k_norm_swa_g': [64], 'cmp_pos_emb_k': [32, 64], 'cmp_w1_k': [2048, 256], 'cmp_w2_k': [256, 64],
    'cmp_pos_emb_v': [32, 64], 'cmp_w1_v': [2048, 256], 'cmp_w2_v': [256, 64],
    'w_head_out': [16, 64, 128], 'w_pool': [4, 256, 512], 'pool_scale': [D], 'w_out': [D, D],
    'ffn_norm_g': [D], 'w_router_group': [D, 4], 'b_router_group': [4], 'w_router_expert': [D, 16],
    'b_router_expert': [16], 'w_expert_gate': [16, D, 256], 'w_expert_up': [16, D, 256],
    'w_expert_down': [16, 256, D],
}


MARKS = []


class _Stop(Exception):
    pass


def build(dbg=False, nseq=NSEQ, nqb=NQB, stop_after=None, skip_prep=False):
    nc = bass.Bass("TRN2", target_bir_lowering=False)
    x = nc.dram_tensor("x", [NSEQ, SEQ, D], F32, kind="ExternalInput").ap()
    positions = nc.dram_tensor("positions", [NSEQ, SEQ], I32, kind="ExternalInput").ap()
    W = {k: nc.dram_tensor(k, s, F32, kind="ExternalInput").ap() for k, s in WEIGHT_SHAPES.items()}
    C = {k: nc.dram_tensor(k, s, F32, kind="ExternalInput").ap() for k, s in CONST_SHAPES.items()}
    out = nc.dram_tensor("out", [NSEQ, SEQ, D], F32, kind="ExternalOutput").ap()
    WinP = nc.dram_tensor("WinP", [56, 128, 2048], BF16).ap()
    WVS = nc.dram_tensor("WVS", [2, 128, 4096], BF16).ap()
    WguP = nc.dram_tensor("WguP", [64, 128, 2048], BF16).ap()
    WdS = nc.dram_tensor("WdS", [16, 128, 4096], BF16).ap()
    WoS = nc.dram_tensor("WoS", [8, 128, 4096], BF16).ap()
    W1S = nc.dram_tensor("W1S", [4, 128, 4096], BF16).ap()
    dbg_out = {}

    with ExitStack() as es:
        S = Sched(nc, es)

        def sb(name, shape, dt):
            return es.enter_context(nc.sbuf_tensor(name, shape, dt))

        def ps(name, shape, dt):
            return es.enter_context(nc.psum_tensor(name, shape, dt))

        X1 = sb("X1", [128, 4, 2048], F32)
        hT = sb("hT", [128, 16, 512], BF16)
        QO = sb("QO", [128, 16, 512], BF16)
        yT = sb("yT", [128, 16, 512], BF16)
        KT = sb("KT", [128, 2, 2, SEQ], BF16)
        VA = sb("VA", [128, 2, 16, 4 * 65], BF16)
        kcT = sb("kcT", [128, 512], BF16)
        vcA = sb("vcA", [128, 4, 97], BF16)
        CSt = sb("CSt", [128, 2, 512], F32)
        posi = sb("posi", [128, 512], I32)
        TF = sb("TF", [128, 4, 512], F32)
        BFW = sb("BFW", [128, 5, 512], BF16)
        PT = BFW[:, 0:3]
        TBb = BFW[:, 3:5]
        XNB = BFW[:, 0:4].rearrange("p a f -> p (a f)")
        XK = [('pt', 0), ('pt', 1), ('pt', 2), ('tb', 0)]
        Otot = sb("Otot", [128, 2, 4, 128], F32)
        Obf = sb("Obf", [128, 2, 4, 128], BF16)
        Cc = Otot
        hidT = QO[:, 0:4].rearrange("p (a b) f -> p a b f", a=2)
        CCK = ['Cc', ('otot', 0), ('otot', 1)]
        gates = sb("gates", [128, 4, 48], F32)
        sc2 = sb("sc2", [128, 32], F32)
        top8 = sb("top8", [128, 16], F32)
        selb = sb("selb", [128, 4, 32], BF16)
        selbT = sb("selbT", [128, 512], BF16)
        QP = sb("QP", [128, 4, 512], BF16)
        zeros128 = sb("zeros128", [128, 128], BF16)
        Rg = sb("Rg", [128, 4, 128], BF16)
        small = sb("small", [128, 64], F32)
        ubuf = sb("ubuf", [128, 1, 528], F32)
        carry = sb("carry", [128, 8, 16], F32)
        PLT = sb("PLT", [128, 2, 2, 512], BF16)
        rt = sb("rt", [128, 96], F32)
        comb = sb("comb", [128, 4, 16], F32)
        combb = sb("combb", [128, 4, 16], BF16)
        combT = sb("combT", [16, 512], BF16)
        PAN = sb("PAN", [128, 3, 16, 128], BF16)
        WB = sb("WB", [128, 2, 4096], BF16)
        WG = sb("WG", [128, 16, 48], BF16)
        Wpool = sb("Wpool", [128, 8, 512], BF16)
        Who = sb("Who", [128, 8, 128], BF16)
        W2k = sb("W2k", [128, 2, 128], BF16)
        W2v = sb("W2v", [128, 2, 64], BF16)
        Wr = sb("Wr", [128, 16, 20], BF16)
        peT = sb("peT", [64, 2, 32], BF16)
        bh = sb("bh", [128, 4], F32)
        gvec = sb("gvec", [128, 40], F32)
        pscale = sb("pscale", [128, 16], F32)
        brt = sb("brt", [128, 20], F32)
        ident = sb("ident", [128, 128], BF16)
        tri = sb("tri", [128, 2, 128], BF16)
        maskc = sb("maskc", [128, 512], BF16)
        bonus = sb("bonus", [128, 4, 32], F32)
        emat = sb("emat", [128, 16, 128], BF16)
        freq = sb("freq", [128, 1], F32)
        rmat = sb("rmat", [128, 128], BF16)
        onesblk = sb("onesblk", [128, 128], BF16)
        sele = sb("sele", [128, 16, 128], BF16)
        icnt = sb("icnt", [128, 4, 16], F32)
        ssq = sb("ssq", [128, 8], F32)

        tmpo = TF[:, 3, 0:256].rearrange("p (a b) -> p a b", a=4)
        tmpi = TF[:, 3, 256:384].rearrange("p (a b) -> p a b", a=4)
        imp = TF[:, 2, 0:128].rearrange("p (a b) -> p a b", a=4)
        scb = TF[:, 2, 128:256].rearrange("p (a b) -> p a b", a=4)
        PSA = [ps("psa%d" % i, [128, 512], F32) for i in range(3)]
        PSS = [ps("pss%d" % i, [128, 512], F32) for i in range(2)]
        PSO = [ps("pso%d" % i, [128, 4, 128], F32) for i in range(2)]
        PST = ps("pst", [128, 1, 512], BF16)

        rr = {'psa': 0, 'pss': 0, 'pso': 0, 'pst': 0, 'pan': 0, 'wb': 0, 'pt': 0, 'ew': 0}

        def nxt(name, n):
            i = rr[name]
            rr[name] = (i + 1) % n
            return i

        def psa():
            i = nxt('psa', 3)
            return PSA[i], ('psa', i)

        def chk(stage):
            if stage[0] != 'h':
                MARKS.append((stage, S.cnt['pe'], S.cnt['act']))
            if stop_after == stage:
                S.dead = True

        chk('start')
        ukey = [0]

        def uk():
            ukey[0] += 1
            return ('cst', ukey[0])

        def ld(dst, src, key, eng='sp', slot='cst'):
            S.dma(eng, slot, lambda e: e.dma_start(out=dst, in_=src), writes=[uk()])

        ld(tri[:], C['c_tri'], 'tri', 'pool', 'cstp')
        ld(emat[:], C['c_emat'], 'emat', 'pool', 'cstp')
        ld(rmat[:], C['c_rm'], 'rmat', 'pool', 'cstp')
        ld(onesblk[:], C['c_onesblk'], 'onesblk', 'pool', 'cstp')
        ld(freq[:], C['c_freq'], 'freq')
        ld(sele[:], C['c_sele'], 'sele', 'pool', 'cstp')
        ld(icnt[:], C['c_icnt'], 'icnt')
        chk('c0')
        for g in range(4):
            ld(vcA[:, g, 64:97], C['c_ov'], 'vcA', 'pool', 'cstp')
        chk('c1')
        nsc = lambda e, o, i: e.dma_start(out=o, in_=i, allow_slow_non_contiguous=True)
        for q4 in range(4):
            cs_ = slice(q4 * 4, q4 * 4 + 4)
            S.dma('sp', 'cst', lambda e: nsc(e, gvec[:, q4 * 4:q4 * 4 + 4], W['attn_norm_g'].rearrange("(c p) -> p c", p=128)[:, cs_]), writes=[uk()])
            S.dma('sp', 'cst', lambda e: nsc(e, gvec[:, 16 + q4 * 4:16 + q4 * 4 + 4], W['ffn_norm_g'].rearrange("(c p) -> p c", p=128)[:, cs_]), writes=[uk()])
            S.dma('sp', 'cst', lambda e: nsc(e, pscale[:, cs_], W['pool_scale'].rearrange("(c p) -> p c", p=128)[:, cs_]), writes=[uk()])
        for i, nm in enumerate(('q_norm_g', 'k_norm_cmp_g', 'k_norm_slc_g', 'k_norm_swa_g')):
            for hb in (0, 64):
                S.dma('sp', 'cst', lambda e, i=i, nm=nm, hb=hb: nsc(e, gvec[hb:hb + 64, 32 + i:33 + i], W[nm].rearrange("(p o) -> p o", o=1)), writes=[uk()])
        chk('c2')
        S.dma('sp', 'cst', lambda e: e.dma_start(out=brt[:, 0:4], in_=W['b_router_group'].partition_broadcast(128)), writes=[uk()])
        S.dma('sp', 'cst', lambda e: e.dma_start(out=brt[:, 4:20], in_=W['b_router_expert'].partition_broadcast(128)), writes=[uk()])
        chk('c3')
        S.op('dve', lambda e: e.memset(gvec[:, 36:37], 1.0), writes=['gvec'])
        S.op('dve', lambda e: e.memset(VA[:].rearrange("p a b c -> p (a b c)"), 1.0), writes=['VA'])
        S.op('dve', lambda e: e.memset(ssq[:], 0.0), writes=['ssq'])
        S.op('dve', lambda e: e.memset(selbT[:], 0.0), writes=['selbT'])
        S.op('dve', lambda e: e.memset(zeros128[:], 0.0), writes=['zeros128'])
        S.op('pool', lambda e: e.memset(QP[:].rearrange("p a f -> p (a f)"), 0.0), writes=[('qp', i) for i in range(4)])
        S.op('pool', lambda e: e.memset(ident[:], 0.0), writes=['ident'])
        S.op('pool', lambda e: e.affine_select(out=ident[:], in_=ident[:], pattern=[[-1, 128]], compare_op=ALU.not_equal, fill=1.0, base=0, channel_multiplier=1), reads=['ident'], writes=['ident'])

        X1f = X1[:].rearrange("p a f -> p (a f)")
        chk('const')
        S.barrier()
        for v_ in range(4):
            S.op('dve', lambda e: e.tensor_scalar_mul(out=Rg[:, v_, :], in0=rmat[:], scalar1=gvec[:, 32 + v_:33 + v_]), reads=['gvec'], writes=['Rg'])
        if skip_prep:
            S.dead = True
        hTf = hT[:].rearrange("p a f -> p (a f)")
        st_in = [X1f[:, i * 4096:(i + 1) * 4096] for i in range(2)]
        st_out = [hTf[:, i * 4096:(i + 1) * 4096] for i in range(2)]
        pj = [0]
        EW3 = ('act', 'dve', 'act', 'act', 'pool', 'act', 'act', 'dve', 'act', 'act', 'act', 'dve', 'act', 'pool', 'act', 'act')

        def scale_op(eng, o, i, sc):
            if eng == 'act':
                S_fn = lambda e: e.mul(out=o, in_=i, mul=sc)
            else:
                S_fn = lambda e: e.tensor_scalar_mul(out=o, in0=i, scalar1=sc)
            return S_fn

        def prep(loads, mode, gofs, stores, resident=None):
            i = pj[0] % 2
            pj[0] += 1
            for (dfn, src) in loads:
                S.dma('sp', 'pin%d' % i, lambda e, dfn=dfn, src=src: e.dma_start(out=dfn(st_in[i]), in_=src), writes=[('pin', i)])
            sin = st_in[i].rearrange("p (k c) -> p k c", c=256)
            for kc in range(16):
                sc = gvec[:, gofs + kc:gofs + kc + 1] if gofs is not None else gvec[:, 36:37]
                if resident is not None:
                    o, ii = resident(sin, kc)
                    wk = [resident.key]
                elif mode == 'A':
                    o = st_out[i].rearrange("p (n k c) -> p n k c", n=2, k=16)[:, :, kc, :]
                    ii = sin[:, kc, :].rearrange("p (n c) -> p n c", n=2)
                    wk = [('pout', i, kc)]
                elif mode == 'Aq':
                    o = st_out[i].rearrange("p (n k hf d) -> p n k hf d", n=2, k=16, hf=2)[:, :, kc, :, :]
                    ii = sin[:, kc, :].rearrange("p (hf r d) -> p r hf d", hf=2, r=2)
                    wk = [('pout', i, kc)]
                else:
                    o = st_out[i][:, kc * 256:(kc + 1) * 256]
                    ii = sin[:, kc, :]
                    wk = [('pout', i, kc)]
                eng = EW3[kc % 16]
                if o.shape[0] != 128:
                    sc = sc[0:o.shape[0]]
                if mode == 'Aq':
                    for n_ in range(2):
                        S.op(eng, scale_op(eng, o[:, n_], ii[:, n_], sc), reads=[('pin', i), 'gvec'], writes=wk)
                    continue
                S.op(eng, scale_op(eng, o, ii, sc), reads=[('pin', i), 'gvec'], writes=wk)
            prev = pend[:]
            del pend[:]
            for (dst, sfn) in stores:
                pend.append((i, dst, sfn))
            flush(prev)

        pend = []

        def flush(lst):
            for (i, dst, sfn) in lst:
                S.dma('sp', 'pout%d' % i, lambda e, dst=dst, sfn=sfn, i=i: e.dma_start(out=dst, in_=sfn(st_out[i])),
                      reads=[('pout', i, kc) for kc in range(16)], writes=['scratch'])

        def win_src(c0, n):
            return W['w_in'][:, c0:c0 + n].rearrange("(k p) c -> p k c", p=128)

        def st_cols(a, n):
            return lambda st: st.rearrange("p (k c) -> p k c", c=256)[:, :, a:a + n]

        def store_panels(p0):
            return [(WinP[p0:p0 + 2].rearrange("n p f -> p n f"), lambda st: st.rearrange("p (n f) -> p n f", n=2))]

        for m in range(2):
            for r2 in range(2):
                c_a = (8 * m + 2 * r2) * 64
                c_b = (8 * m + 4 + 2 * r2) * 64
                prep([(st_cols(0, 128), win_src(c_a, 128)), (st_cols(128, 128), win_src(c_b, 128))], 'Aq', 0,
                     store_panels(4 * m + 2 * r2))
        for pi, c0 in ((8, OFF_KC), (10, OFF_VC), (12, OFF_KS), (14, OFF_KW)):
            prep([(st_cols(0, 256), win_src(c0, 256))], 'A', 0, store_panels(pi))
        for i4 in range(4):
            prep([(st_cols(0, 256), win_src(OFF_POOL + i4 * 256, 256))], 'A', 0, store_panels(16 + 2 * i4))
        for i16 in range(16):
            prep([(st_cols(0, 256), win_src(OFF_MERGE + i16 * 256, 256))], 'A', 0, store_panels(24 + 2 * i16))
        for vi, c0 in ((0, OFF_VS), (1, OFF_VW)):
            prep([(st_cols(0, 256), win_src(c0, 256))], 'B', 0, [(WVS[vi], lambda st: st)])

        class Res:
            def __init__(self, fn, key):
                self.fn, self.key = fn, key

            def __call__(self, sin, kc):
                return self.fn(sin, kc)

        prep([(st_cols(0, 48), win_src(OFF_GATE, 48))], 'R', 0, [],
             resident=Res(lambda sin, kc: (WG[:, kc, :], sin[:, kc, 0:48]), 'WG'))
        def st_cols_k(a, n, k0, k1):
            return lambda st: st.rearrange("p (k c) -> p k c", c=256)[:, k0:k1, a:a + n]
        rl = []
        for q4 in range(4):
            rl.append((st_cols_k(0, 4, q4 * 4, q4 * 4 + 4), W['w_router_group'].rearrange("(k p) c -> p k c", p=128)[:, q4 * 4:q4 * 4 + 4, :]))
            rl.append((st_cols_k(4, 16, q4 * 4, q4 * 4 + 4), W['w_router_expert'].rearrange("(k p) c -> p k c", p=128)[:, q4 * 4:q4 * 4 + 4, :]))
        prep(rl, 'R', 16, [],
             resident=Res(lambda sin, kc: (Wr[:, kc, :], sin[:, kc, 0:20]), 'Wr'))
        for e_ in range(NE):
            for gi_, nm in enumerate(('w_expert_gate', 'w_expert_up')):
                prep([(st_cols(0, 256), W[nm][e_].rearrange("(k p) c -> p k c", p=128))], 'A', 16,
                     [(WguP[e_ * 4 + gi_ * 2:e_ * 4 + gi_ * 2 + 2].rearrange("n p f -> p n f"), lambda st: st.rearrange("p (n f) -> p n f", n=2))])
        for eg in range(4):
            for cc in range(4):
                src = W['w_expert_down'][eg * 4:eg * 4 + 4, :, cc * 512:(cc + 1) * 512].rearrange("e (h p) c -> p e h c", p=128)
                prep([(lambda st: st.rearrange("p (e h c) -> p e h c", e=4, h=2), src)], 'B', None, [(WdS[eg * 4 + cc], lambda st: st)])
        for c8 in range(8):
            prep([(st_cols(0, 256), W['w_out'][:, c8 * 256:(c8 + 1) * 256].rearrange("(k p) c -> p k c", p=128))], 'B', None,
                 [(WoS[c8], lambda st: st)])
        for kv, nm in enumerate(('cmp_w1_k', 'cmp_w1_v')):
            for lh in range(2):
                src = W[nm][lh * 1024:(lh + 1) * 1024, :].rearrange("(l d) h -> d l h", d=64)
                prep([(lambda st: st.rearrange("p (l h) -> p l h", h=256)[0:64], src),
                      (lambda st: st.rearrange("p (l h) -> p l h", h=256)[64:128], src)], 'B', None,
                     [(W1S[kv * 2 + lh], lambda st: st)])
        prep([(lambda st: st.rearrange("p (g j e) -> p g j e", g=4, j=2),
               W['w_pool'].rearrange("g (j p) e -> p g j e", p=128))], 'R', None, [],
             resident=Res(lambda sin, kc: (Wpool[:, kc // 2, (kc % 2) * 256:(kc % 2) * 256 + 256], sin[:, kc, :]), 'Wpool'))

        def res_small(sin, kc):
            flat = sin.rearrange("p k c -> p (k c)")
            if kc < 4:
                return Who[:, 2 * kc:2 * kc + 2, :], flat[:, kc * 256:(kc + 1) * 256].rearrange("p (a b) -> p a b", a=2)
            if kc == 4:
                return W2k[:, :, 0:64], flat[:, 1024:1280].rearrange("p (a b) -> p a b", a=2)[:, :, 0:64]
            if kc == 5:
                return W2k[:, :, 64:128], flat[:, 1024:1280].rearrange("p (a b) -> p a b", a=2)[:, :, 0:64]
            if kc == 6:
                return W2v[:, :, :], flat[:, 1280:1536].rearrange("p (a b) -> p a b", a=2)[:, :, 0:64]
            if kc == 7:
                return peT[:, :, :], flat[0:64, 1536:1664].rearrange("p (a b) -> p a b", a=2)[:, :, 0:32]
            return small[:, 32 + kc:33 + kc], flat[:, 4000 + kc:4001 + kc]

        ifl = lambda st: st
        who_src = W['w_head_out'].rearrange("(c two) d e -> (two d) c e", two=2)
        loads = [(lambda st: st[:, 0:1024].rearrange("p (c e) -> p c e", c=8), who_src)]
        for hc in range(2):
            loads.append((lambda st, hc=hc: st[:, 1024 + hc * 128:1024 + hc * 128 + 64], W['cmp_w2_k'][hc * 128:(hc + 1) * 128, :]))
            loads.append((lambda st, hc=hc: st[:, 1280 + hc * 128:1280 + hc * 128 + 64], W['cmp_w2_v'][hc * 128:(hc + 1) * 128, :]))
        i_sm = pj[0] % 2
        S.op('dve', lambda e: e.memset(st_in[i_sm], 0.0), writes=[('pin', i_sm)])
        for kv, nm in enumerate(('cmp_pos_emb_k', 'cmp_pos_emb_v')):
            for q4 in range(4):
                S.dma('sp', 'pin%d' % i_sm, lambda e, kv=kv, nm=nm: nsc(e, st_in[i_sm][0:64, 1536 + kv * 64 + q4 * 8:1536 + kv * 64 + q4 * 8 + 8], W[nm].rearrange("l d -> d l")[:, q4 * 8:q4 * 8 + 8]), writes=[('pin', i_sm)])
        prep(loads, 'R', None, [], resident=Res(res_small, 'smallres'))
        flush(pend)
        if skip_prep:
            S.dead = False
        chk('prep')
        S.barrier()

        base_pan = [(PAN[:, i], ('pan', i), 'pan%d' % i) for i in range(3)]
        pan_slots = list(base_pan)

        def set_pan_slots(extra):
            del pan_slots[:]
            pan_slots.extend(base_pan + extra)
            rr['pan'] = 0

        def load_panel(src):
            i = nxt('pan', len(pan_slots))
            ap_, key_, sname = pan_slots[i]
            S.dma('sp', sname, lambda e: e.dma_start(out=ap_.rearrange("p k c -> p (k c)"), in_=src), reads=['scratch'], writes=[key_])
            return ap_, key_

        base_wb = [(WB[:, i, :], ('wb', i), 'wb%d' % i) for i in range(2)]
        wb_slots = list(base_wb)

        def set_wb_slots(extra):
            del wb_slots[:]
            wb_slots.extend(base_wb + extra)
            rr['wb'] = 0

        def load_wb(src):
            i = nxt('wb', len(wb_slots))
            ap_, key_, sname = wb_slots[i]
            S.dma('sp', sname, lambda e: e.dma_start(out=ap_, in_=src), reads=['scratch'], writes=[key_])
            return ap_, key_

        def mmA(src, actT, akeys, n=512):
            pan_ap, pan_key = load_panel(src)
            pb, pk = psa()
            for kc in range(16):
                S.op('pe', lambda e, kc=kc: e.matmul(pb[:, 0:n], lhsT=pan_ap[:, kc, :], rhs=actT[:, kc, 0:n], start=(kc == 0), stop=(kc == 15)),
                     reads=[pan_key] + akeys, writes=[pk])
            return pb, pk

        def ew():
            i = nxt('ew', 2)
            return ('dve', 'pool')[i]

        def rope_tables(s, t0):
            S.dma('sp', 'posi', lambda e: e.dma_start(out=posi[:], in_=positions[s, t0:t0 + 512].partition_broadcast(128)), writes=['posi'])
            ang, kf, ki = TF[:, 0, :], TF[:, 1, :], posi[:]
            S.op('dve', lambda e: e.tensor_copy(out=ang, in_=posi[:]), reads=['posi'], writes=[('tf', 0)])
            S.op('dve', lambda e: e.tensor_scalar_mul(out=ang, in0=ang, scalar1=freq[:, 0:1]), reads=[('tf', 0), 'freq'], writes=[('tf', 0)])
            for ci, ph in ((0, 1.5707963267948966), (1, 0.0)):
                S.op('dve', lambda e, ph=ph: e.tensor_scalar(out=kf, in0=ang, scalar1=ph, scalar2=1.0 / TWO_PI, op0=ALU.add, op1=ALU.mult),
                     reads=[('tf', 0)], writes=[('tf', 1)])
                S.op('dve', lambda e: e.tensor_copy(out=ki, in_=kf), reads=[('tf', 1)], writes=['posi'])
                S.op('dve', lambda e: e.tensor_copy(out=kf, in_=ki), reads=['posi'], writes=[('tf', 1)])
                r = TF[:, 2, :]
                S.op('dve', lambda e: e.scalar_tensor_tensor(out=r, in0=kf, scalar=-6.28125, in1=ang, op0=ALU.mult, op1=ALU.add),
                     reads=[('tf', 0), ('tf', 1)], writes=[('tf', 2)])
                S.op('dve', lambda e: e.scalar_tensor_tensor(out=r, in0=kf, scalar=-(TWO_PI - 6.28125), in1=r, op0=ALU.mult, op1=ALU.add),
                     reads=[('tf', 1), ('tf', 2)], writes=[('tf', 2)])
                S.op('dve', lambda e, ph=ph: e.tensor_scalar(out=r, in0=r, scalar1=ph, scalar2=PI_SAFE, op0=ALU.add, op1=ALU.min),
                     reads=[('tf', 2)], writes=[('tf', 2)])
                S.op('dve', lambda e: e.tensor_scalar_max(out=r, in0=r, scalar1=-PI_SAFE), reads=[('tf', 2)], writes=[('tf', 2)])
                S.op('act', lambda e, ci=ci: e.activation(out=CSt[:, ci, :], in_=r, func=AF.Sin), reads=[('tf', 2)], writes=['CSt'])

        def normrope(pb, pk, gcol, Ct, St, cskeys, out_ap, okeys, n=512, view=None):
            vw = view if view is not None else (lambda a: a)
            v = gcol - 32
            sq, psb = TBb[:, 0, 0:n], TBb[:, 1, 0:n]
            rstd, t1, t2 = TF[:, 0, 0:n], TF[:, 1, 0:n], TF[:, 2, 0:n]
            S.op('act', lambda e: e.activation(out=sq, in_=pb[:, 0:n], func=AF.Square), reads=[pk], writes=[('tb', 0)])
            S.op('act', lambda e: e.copy(out=psb, in_=pb[:, 0:n]), reads=[pk], writes=[('tb', 1)])
            mb, mk = psa()
            S.op('pe', lambda e: e.matmul(mb[:, 0:n], lhsT=onesblk[:], rhs=sq, start=True, stop=True), reads=[('tb', 0), 'onesblk'], writes=[mk])
            rb, rk = psa()
            S.op('pe', lambda e: e.matmul(rb[:, 0:n], lhsT=Rg[:, v, :], rhs=psb, start=True, stop=True), reads=[('tb', 1), 'Rg'], writes=[rk])
            S.op('act', lambda e: e.activation(out=rstd, in_=mb[:, 0:n], func=AF.Sqrt, bias=small[:, 63:64], scale=1.0), reads=[mk, 'eps'], writes=[('tf', 0)])
            S.op('dve', lambda e: e.reciprocal(out=rstd, in_=rstd), reads=[('tf', 0)], writes=[('tf', 0)])
            S.op('dve', lambda e: e.scalar_tensor_tensor(out=vw(t1), in0=vw(pb[:, 0:n]), scalar=gvec[:, gcol:gcol + 1], in1=Ct, op0=ALU.mult, op1=ALU.mult),
                 reads=[pk, 'gvec'] + cskeys, writes=[('tf', 1)])
            S.op('dve', lambda e: e.tensor_tensor(out=vw(t2), in0=vw(rb[:, 0:n]), in1=St, op=ALU.mult), reads=[rk] + cskeys, writes=[('tf', 2)])
            S.op('pool', lambda e: e.tensor_tensor(out=t1, in0=t1, in1=t2, op=ALU.add), reads=[('tf', 1), ('tf', 2)], writes=[('tf', 1)])
            S.op('pool', lambda e: e.tensor_tensor(out=out_ap, in0=t1, in1=rstd, op=ALU.mult), reads=[('tf', 1), ('tf', 0)], writes=okeys)

        S.op('dve', lambda e: e.memset(small[:, 63:64], 1e-6), writes=['eps'])

        def make_hT():
            for qi in range(4):
                S.op('act', lambda e, qi=qi: e.activation(out=QO[:, 8:12].rearrange("p a f -> p (a f)"), in_=X1[:, qi, :], func=AF.Square, accum_out=ssq[:, qi:qi + 1]),
                     reads=[('X1', qi)], writes=[('QO', 1), ('ssq', qi)])
                chk('h1')
                S.op('dve', lambda e, qi=qi: e.tensor_scalar(out=ssq[:, 4 + qi:5 + qi], in0=ssq[:, qi:qi + 1], scalar1=1.0 / D, scalar2=1e-6, op0=ALU.mult, op1=ALU.add),
                     reads=[('ssq', qi)], writes=[('ssq2', qi)])
                S.op('dve', lambda e, qi=qi: e.memset(ssq[:, qi:qi + 1], 0.0), reads=[('ssq2', qi)], writes=[('ssq', qi)])
                S.op('act', lambda e, qi=qi: e.activation(out=ssq[:, 4 + qi:5 + qi], in_=ssq[:, 4 + qi:5 + qi], func=AF.Sqrt), reads=[('ssq2', qi)], writes=[('ssq2', qi)])
                S.op('dve', lambda e, qi=qi: e.reciprocal(out=ssq[:, 4 + qi:5 + qi], in_=ssq[:, 4 + qi:5 + qi]), reads=[('ssq2', qi)], writes=[('ssq2', qi)])
                S.op('dve', lambda e, qi=qi: e.tensor_scalar_mul(out=XNB, in0=X1[:, qi, :], scalar1=ssq[:, 4 + qi:5 + qi]),
                     reads=[('X1', qi), ('ssq2', qi)], writes=XK)
                chk('h3')
                for k4 in range(4):
                    ti = nxt('pst', 1)
                    for kk in range(4):
                        kc = k4 * 4 + kk
                        S.op('pe', lambda e, kc=kc, kk=kk, ti=ti: e.transpose(out=PST[:, ti, kk * 128:(kk + 1) * 128], in_=XNB[:, kc * 128:(kc + 1) * 128], identity=ident[:]),
                             reads=XK + ['ident'], writes=[('pst', ti)])
                    chk('h4')
                    eng = 'act'
                    o = hT[:, k4 * 4:k4 * 4 + 4, qi * 128:(qi + 1) * 128]
                    ii = PST[:, ti, :].rearrange("p (a b) -> p a b", a=4)
                    if eng == 'act':
                        S.op('act', lambda e, o=o, ii=ii: e.copy(out=o, in_=ii), reads=[('pst', ti)], writes=[('hT', qi)])
                    else:
                        S.op('dve', lambda e, o=o, ii=ii: e.tensor_copy(out=o, in_=ii), reads=[('pst', ti)], writes=[('hT', qi)])
                    chk('h5' if k4 == 0 else ('h6' if k4 == 1 else 'h7'))

        HK = [('hT', qi) for qi in range(4)]
        YK = [('yT', qi) for qi in range(4)]

        def load_x(s, t0):
            for qi in range(4):
                S.dma('sp', 'x%d' % qi, lambda e, qi=qi: e.dma_start(out=X1[:, qi, :], in_=x[s, t0 + qi * 128:t0 + (qi + 1) * 128, :]), writes=[('X1', qi)])

        def dump(name, ap_sb, shape, keys, dt=F32):
            if not dbg:
                return
            if name not in dbg_out:
                dbg_out[name] = nc.dram_tensor("dbg_" + name, shape, dt, kind="ExternalOutput").ap()
            S.dma('sp', 'dbg', lambda e: e.dma_start(out=dbg_out[name], in_=ap_sb), reads=keys, writes=['dbg_' + name])

        for s in range(nseq):
            S.op('pool', lambda e: e.memset(carry[:].rearrange("p a b -> p (a b)"), 0.0), writes=['carry'])
            for tb in range(NQB):
                t0 = tb * TB
                load_x(s, t0)
                chk('p1x')
                make_hT()
                chk('p1a')
                rope_tables(s, t0)
                chk('p1b')
                for ci in range(2):
                    m0 = 1 if tb == 0 else 0
                    S.op('pool', lambda e, ci=ci, m0=m0, tb=tb: e.tensor_copy(out=Cc[:, ci, 0, 32 * tb - 1 + m0:32 * tb + 31], in_=CSt[:, ci, 15 + 16 * m0:512:16]),
                         reads=['CSt'], writes=CCK)
                if dbg and s == 0 and tb == 0:
                    dump('hT', hT[:], [128, 16, 512], HK, BF16)
                    dump('CS', CSt[:], [128, 2, 512], ['CSt'])
                chk('p1c')
                rawv = yT[:].rearrange("p a f -> p (a f)").rearrange("p (c t) -> p c t", c=4)
                for pi in range(8, 12):
                    pb, pk = mmA(WinP[pi], hT, HK)
                    o = rawv[:, pi - 8, t0:t0 + 512]
                    S.op('act', lambda e, o=o, pb=pb: e.copy(out=o, in_=pb[:]), reads=[pk], writes=YK + ['raw'])
                chk('p1d')
                for br in range(2):
                    for j in range(2):
                        pb, pk = mmA(WinP[12 + br * 2 + j], hT, HK)
                        normrope(pb, pk, 34 + br, CSt[:, 0, :], CSt[:, 1, :], ['CSt'], KT[:, br, j, t0:t0 + 512], ['KT'])
                chk('p1e')
                for br in range(2):
                    wb_ap, wb_key = load_wb(WVS[br])
                    wv = wb_ap.rearrange("p (k c) -> p k c", c=256)
                    for qi in range(4):
                        pb, pk = psa()
                        for kc in range(16):
                            S.op('pe', lambda e, kc=kc, qi=qi, pb=pb, wv=wv: e.matmul(pb[:, 0:256], lhsT=hT[:, kc, qi * 128:(qi + 1) * 128], rhs=wv[:, kc, :], start=(kc == 0), stop=(kc == 15)),
                                 reads=[wb_key, ('hT', qi)], writes=[pk])
                        o = VA[:, br, tb * 4 + qi, :].rearrange("p (g c) -> p g c", c=65)[:, :, 0:64]
                        ii = pb[:, 0:256].rearrange("p (g c) -> p g c", c=64)
                        S.op('dve', lambda e, o=o, ii=ii: e.tensor_copy(out=o, in_=ii), reads=[pk], writes=['VA'])
            chk('pass1')
            for ci in range(2):
                for g in range(1, 4):
                    S.op('pool', lambda e, ci=ci, g=g: e.tensor_copy(out=Cc[:, ci, g, 0:127], in_=Cc[:, ci, 0, 0:127]), reads=['Cc'], writes=CCK)
            rawv = yT[:].rearrange("p a f -> p (a f)").rearrange("p (c t) -> p c t", c=4)
            for kv in range(2):
                wis = [load_wb(W1S[kv * 2 + lh]) for lh in range(2)]
                w1v = [wa.rearrange("p (l h) -> p l h", h=256) for (wa, _) in wis]
                wkeys = [wk_ for (_, wk_) in wis]
                pb, pk = psa()
                for hc in range(2):
                    for l in range(32):
                        S.op('pe', lambda e, hc=hc, l=l, pb=pb: e.matmul(pb[:, hc:hc + 1], lhsT=w1v[l // 16][0:64, l % 16, hc * 128:(hc + 1) * 128], rhs=peT[0:64, kv, l:l + 1], start=(l == 0), stop=(l == 31)),
                             reads=wkeys + ['smallres'], writes=[pk])
                S.op('dve', lambda e, pb=pb, kv=kv: e.tensor_copy(out=bh[:, kv * 2:kv * 2 + 2], in_=pb[:, 0:2]), reads=[pk], writes=['bh'])
                for g in range(4):
                    base = (g % 2) * 64
                    for hc in range(2):
                        pb, pk = psa()
                        for l in range(32):
                            S.op('pe', lambda e, hc=hc, l=l, pb=pb, base=base, g=g: e.matmul(
                                pb[:, 0:127], lhsT=w1v[l // 16][base:base + 64, l % 16, hc * 128:(hc + 1) * 128],
                                rhs=rawv[base:base + 64, kv * 2 + g // 2, l:l + 16 * 126 + 1:16], start=(l == 0), stop=(l == 31)),
                                reads=wkeys + ['raw'], writes=[pk])
                        S.op('act', lambda e, pb=pb, hc=hc, g=g, kv=kv: e.activation(out=hidT[:, kv, hc, g * 127:(g + 1) * 127], in_=pb[:, 0:127], func=AF.Silu, bias=bh[:, kv * 2 + hc:kv * 2 + hc + 1], scale=1.0),
                             reads=[pk, 'bh'], writes=[('QO', 0)])
            pb, pk = psa()
            for hc in range(2):
                S.op('pe', lambda e, hc=hc, pb=pb: e.matmul(pb[:, 0:508], lhsT=W2k[:, hc, :], rhs=hidT[:, 0, hc, 0:508], start=(hc == 0), stop=(hc == 1)),
                     reads=[('QO', 0), 'smallres'], writes=[pk])
            normrope(pb, pk, 33, Cc[:, 0, :, 0:127], Cc[:, 1, :, 0:127], ['Cc'], kcT[:, 0:508], ['kcT'], n=508,
                     view=lambda a_: a_.rearrange("p (g n) -> p g n", g=4))
            pb, pk = psa()
            for g in range(4):
                for hc in range(2):
                    S.op('pe', lambda e, hc=hc, g=g, pb=pb: e.matmul(pb[0:127, g * 64:(g + 1) * 64], lhsT=hidT[:, 1, hc, g * 127:(g + 1) * 127], rhs=W2v[:, hc, :], start=(hc == 0), stop=(hc == 1)),
                         reads=[('QO', 0), 'smallres'], writes=[pk])
            S.op('dve', lambda e, pb=pb: e.tensor_copy(out=vcA[0:127, :, 0:64], in_=pb[0:127, 0:256].rearrange("p (g c) -> p g c", g=4)), reads=[pk], writes=['vcA'])
            if dbg and s == 0:
                dump('KT', KT[:], [128, 2, 2, SEQ], ['KT'], BF16)
                dump('VA', VA[:], [128, 2, 16, 260], ['VA'], BF16)
                dump('kcT', kcT[:], [128, 512], ['kcT'], BF16)
                dump('vcA', vcA[:], [128, 4, 97], ['vcA'], BF16)
                dump('hidT', hidT, [128, 2, 2, 512], [('QO', 0)], BF16)

            chk('compress')
            qT = QO[:, 0:8]
            OT = QO[:, 8:16]
            for qb in range(nqb):
                t0 = qb * TB
                first = (s == 0 and qb == 0)
                load_x(s, t0)
                S.dma('pool', 'mk', lambda e, t0=t0: e.dma_start(out=maskc[:], in_=C['c_maskc'][:, t0:t0 + 512]), writes=['maskc'])
                S.dma('sp', 'bon', lambda e, qb=qb: e.dma_start(out=bonus[:], in_=C['c_bonus'][:, qb * 4:qb * 4 + 4, :]), writes=['bonus'])
                chk('blk')
                make_hT()
                chk('mkh')
                rope_tables(s, t0)
                for j in range(8):
                    pb, pk = mmA(WinP[j], hT, HK)
                    normrope(pb, pk, 32, CSt[:, 0, :], CSt[:, 1, :], ['CSt'], qT[:, j, :], [('QO', 0)])
                for qi in range(4):
                    pb, pk = psa()
                    for kc in range(16):
                        S.op('pe', lambda e, kc=kc, qi=qi, pb=pb: e.matmul(pb[:, 0:48], lhsT=hT[:, kc, qi * 128:(qi + 1) * 128], rhs=WG[:, kc, :], start=(kc == 0), stop=(kc == 15)),
                             reads=['WG', ('hT', qi)], writes=[pk])
                    S.op('act', lambda e, qi=qi, pb=pb: e.activation(out=gates[:, qi, :], in_=pb[:, 0:48], func=AF.Sigmoid), reads=[pk], writes=['gates'])
                if dbg and first:
                    dump('qT', QO[:, 0:8], [128, 8, 512], [('QO', 0)], BF16)
                    dump('gates', gates[:], [128, 4, 48], ['gates'])

                chk('qproj')
                def finalize(pso, pok, h, br, pi, hp, first_write, with_imp):
                    rs4 = small[:, 0:4]
                    fac = small[:, 4:8]
                    S.op('dve', lambda e: e.tensor_scalar_max(out=rs4, in0=pso[:, :, 64], scalar1=1e-30), reads=[pok], writes=['rs4'])
                    S.op('dve', lambda e: e.reciprocal(out=rs4, in_=rs4), reads=['rs4'], writes=['rs4'])
                    S.op('dve', lambda e: e.tensor_tensor(out=fac, in0=rs4, in1=gates[:, :, 3 * h + br], op=ALU.mult), reads=['rs4', 'gates'], writes=['fac'])
                    fb = fac.unsqueeze(2).to_broadcast([128, 4, 64])
                    od = Otot[:, pi, :, hp * 64:(hp + 1) * 64]
                    if first_write:
                        S.op('dve', lambda e: e.tensor_tensor(out=od, in0=pso[:, :, 0:64], in1=fb, op=ALU.mult), reads=[pok, 'fac'], writes=[('otot', pi), 'Cc'])
                    else:
                        S.op('dve', lambda e: e.tensor_tensor(out=tmpo, in0=pso[:, :, 0:64], in1=fb, op=ALU.mult), reads=[pok, 'fac'], writes=[('tf', 3)])
                        S.op('pool', lambda e: e.tensor_tensor(out=od, in0=od, in1=tmpo, op=ALU.add), reads=[('tf', 3), ('otot', pi)], writes=[('otot', pi)])
                    if with_imp is not None:
                        rb_ = rs4.unsqueeze(2).to_broadcast([128, 4, 32])
                        if with_imp == 0:
                            S.op('dve', lambda e: e.tensor_tensor(out=imp, in0=pso[:, :, 65:97], in1=rb_, op=ALU.mult), reads=[pok, 'rs4'], writes=[('tf', 2)])
                        else:
                            S.op('dve', lambda e: e.tensor_tensor(out=tmpi, in0=pso[:, :, 65:97], in1=rb_, op=ALU.mult), reads=[pok, 'rs4'], writes=[('tf', 3)])
                            S.op('pool', lambda e: e.tensor_tensor(out=imp, in0=imp, in1=tmpi, op=ALU.add), reads=[('tf', 3), ('tf', 2)], writes=[('tf', 2)])

                for g in range(G):
                    base = (g % 2) * 64
                    heads = [4 * g + r for r in range(4)]

                    def qsl(h):
                        m_, rem = h // 8, h % 8
                        return 4 * m_ + rem % 4
                    SCB = [(PSS[0], ('pss', 0)), (PSS[1], ('pss', 1)), (PSA[0], ('psa', 0)), (PSA[1], ('psa', 1)), (PSA[2], ('psa', 2))]
                    PTB = [(BFW[:, i, :], k_) for i, k_ in enumerate([('pt', 0), ('pt', 1), ('pt', 2), ('tb', 0), ('tb', 1)])]

                    def sc_next():
                        i = nxt('pss', 5)
                        return SCB[i]

                    def pt_next():
                        i = nxt('pt', 5)
                        return PTB[i]
                    cs = []
                    for r, h in enumerate(heads):
                        jq = qsl(h)
                        sb_, sk_ = sc_next()
                        S.op('pe', lambda e: e.matmul(sb_[0:127, :], lhsT=kcT[base:base + 64, g * 127:(g + 1) * 127], rhs=qT[base:base + 64, jq, :], start=True, stop=True),
                             reads=['kcT', ('QO', 0)], writes=[sk_])
                        cs.append((sb_, sk_))
                    for r, h in enumerate(heads):
                        sb_, sk_ = cs[r]
                        pb_, pk_ = pt_next()
                        S.op('act', lambda e: e.activation(out=pb_[0:127, :], in_=sb_[0:127, :], func=AF.Exp, scale=0.125), reads=[sk_], writes=[pk_])
                        S.op('pool', lambda e: e.tensor_tensor(out=pb_[0:127, :], in0=pb_[0:127, :], in1=maskc[0:127, :], op=ALU.mult),
                             reads=[pk_, 'maskc'], writes=[pk_])
                        oi = nxt('pso', 2)
                        for qi in range(4):
                            S.op('pe', lambda e: e.matmul(PSO[oi][:, qi, 0:97], lhsT=pb_[0:127, qi * 128:(qi + 1) * 128], rhs=vcA[0:127, g, :], start=True, stop=True),
                                 reads=[pk_, 'vcA'], writes=[('pso', oi)])
                        finalize(PSO[oi], ('pso', oi), h, 0, r // 2, r % 2, True, (r if qb >= 2 else None))
                    def emit_selection_dve(g=g):
                        S.op('dve', lambda e: e.tensor_tensor(out=scb, in0=imp, in1=bonus[:, :, :], op=ALU.add), reads=[('tf', 2), 'bonus'], writes=[('tf', 2)])
                        for qi in range(4):
                            S.op('dve', lambda e, qi=qi: e.max(out=top8[:, 0:8], in_=scb[:, qi, :]), reads=[('tf', 2)], writes=['top8'])
                            S.op('dve', lambda e, qi=qi: e.match_replace(out=sc2[:], in_to_replace=top8[:, 0:8], in_values=scb[:, qi, :], imm_value=-1e30), reads=[('tf', 2), 'top8'], writes=['sc2'])
                            S.op('dve', lambda e: e.max(out=top8[:, 8:16], in_=sc2[:]), reads=['sc2'], writes=['top8'])
                            S.op('dve', lambda e, qi=qi: e.tensor_scalar(out=sc2[:], in0=scb[:, qi, :], scalar1=top8[:, 15:16], scalar2=None, op0=ALU.is_ge), reads=[('tf', 2), 'top8'], writes=['sc2'])
                            S.op('dve', lambda e, qi=qi: e.tensor_scalar(out=selb[:, qi, :], in0=sc2[:], scalar1=-1.0, scalar2=30000.0, op0=ALU.add, op1=ALU.mult), reads=['sc2'], writes=['selb'])

                    def emit_selection_pe(g=g):
                        ti = nxt('pst', 1)
                        for qi in range(4):
                            S.op('pe', lambda e, qi=qi, ti=ti: e.transpose(out=PST[0:32, ti, qi * 128:(qi + 1) * 128], in_=selb[:, qi, :], identity=ident[:]),
                                 reads=['selb', 'ident'], writes=[('pst', ti)])
                        S.op('dve', lambda e, ti=ti: e.tensor_copy(out=selbT[0:32, :], in_=PST[0:32, ti, :]), reads=[('pst', ti)], writes=['selbT'])
                        if dbg and s == 0 and qb == 2 and g == 0:
                            dump('selb', selb[:], [128, 4, 32], ['selb'], BF16)
                            dump('imp', imp, [128, 4, 32], ['imp'])
                    if qb >= 2:
                        emit_selection_dve()
                    steps = []
                    qp_rr = [0]
                    cb_after = {}
                    order = [(0, 2), (1, 2), (0, 1), (1, 1), (2, 2), (3, 2), (2, 1), (3, 1)]
                    for ui, (r, br) in enumerate(order):
                        h = heads[r]
                        kts = list(range(0, 4 * qb + 4)) if br == 1 else list(range(max(4 * qb - 4, 0), 4 * qb + 4))
                        unit = {'h': h, 'r': r, 'br': br, 'oi': None, 'qp': None, 'newhead': br == 2}
                        for kt in kts:
                            steps.append({'u': unit, 'kt': kt, 'first': kt == kts[0], 'last': kt == kts[-1]})
                        if ui == 2 and qb >= 2:
                            steps[len(steps) - len(kts)]['pre'] = emit_selection_pe

                    hq = {}

                    def emit_score(st):
                        u, kt = st['u'], st['kt']
                        br, jq = u['br'], qsl(u['h'])
                        dloc = kt - 4 * qb
                        lo = max(dloc, 0)
                        hi = 3 if br == 1 else min(dloc + 4, 3)
                        c0 = lo * 128
                        n = (hi - lo + 1) * 128
                        sb_, sk_ = sc_next()
                        use_mask = (br == 1 and qb >= 2)
                        if st['first'] and u['newhead']:
                            qs = (g % 2) * 2 + (qp_rr[0] % 2)
                            qp_rr[0] += 1
                            hq[u['h']] = qs
                            S.op('dve', lambda e: e.tensor_copy(out=QP[base:base + 64, qs, :], in_=qT[base:base + 64, jq, :]), reads=[('QO', 0)], writes=[('qp', qs)])
                        qs = hq[u['h']]
                        S.op('pe', lambda e: e.matmul(
                            sb_[:, 0:n], lhsT=KT[:, br - 1, g // 2, kt * 128:(kt + 1) * 128], rhs=QP[:, qs, c0:c0 + n], start=True, stop=(not use_mask)),
                            reads=['KT', ('qp', qs)], writes=[sk_])
                        if use_mask:
                            S.op('pe', lambda e: e.matmul(sb_[:, 0:n], lhsT=emat[:, kt, :], rhs=selbT[:, c0:c0 + n], start=False, stop=True),
                                 reads=['emat', 'selbT'], writes=[sk_])
                        st['ctx'] = (dloc, lo, hi, c0, n, sb_, sk_)

                    def emit_rest(st):
                        u, kt = st['u'], st['kt']
                        br = u['br']
                        dloc, lo, hi, c0, n, sb_, sk_ = st['ctx']
                        if st['first']:
                            u['oi'] = nxt('pso', 2)
                            oi0 = u['oi']
                            S.op('pe', lambda e: e.matmul(PSO[oi0][:, :, :], lhsT=zeros128[:], rhs=emat[:, 0:4, :], start=True, stop=False),
                                 reads=['emat', 'zeros128'], writes=[('pso', oi0)])
                        oi = u['oi']
                        pb_, pk_ = pt_next()
                        S.op('act', lambda e: e.activation(out=pb_[:, 0:n], in_=sb_[:, 0:n], func=AF.Exp, scale=0.125), reads=[sk_], writes=[pk_])
                        if dloc >= 0:
                            S.op('pool', lambda e: e.tensor_tensor(out=pb_[:, 0:128], in0=pb_[:, 0:128], in1=tri[:, 0, :], op=ALU.mult),
                                 reads=[pk_, 'tri'], writes=[pk_])
                        if br == 2 and 0 <= dloc + 4 <= 3:
                            cf = (hi - lo) * 128
                            S.op('pool', lambda e: e.tensor_tensor(out=pb_[:, cf:cf + 128], in0=pb_[:, cf:cf + 128], in1=tri[:, 1, :], op=ALU.mult),
                                 reads=[pk_, 'tri'], writes=[pk_])
                        for qi in range(lo, hi + 1):
                            sp = bool(st['last'] and qi == hi)
                            S.op('pe', lambda e: e.matmul(
                                PSO[oi][:, qi, 0:65], lhsT=pb_[:, (qi - lo) * 128:(qi - lo + 1) * 128], rhs=VA[:, br - 1, kt, g * 65:(g + 1) * 65], start=False, stop=sp),
                                reads=[pk_, 'VA'], writes=[('pso', oi)])
                        if st['last']:
                            finalize(PSO[oi], ('pso', oi), u['h'], br, u['r'] // 2, u['r'] % 2, False, None)

                    LA = 3
                    n_sc = 0
                    for i_st in range(len(steps)):
                        while n_sc < min(len(steps), i_st + 1 + LA):
                            if 'pre' in steps[n_sc]:
                                steps[n_sc]['pre']()
                            emit_score(steps[n_sc])
                            n_sc += 1
                        emit_rest(steps[i_st])
                        if i_st in cb_after:
                            cb_after[i_st]()
                    for pi in range(2):
                        S.op('act', lambda e, pi=pi: e.copy(out=Obf[:, pi], in_=Otot[:, pi]), reads=[('otot', pi)], writes=[('obf', pi)])
                        ti = nxt('pst', 1)
                        for qi in range(4):
                            S.op('pe', lambda e, pi=pi, qi=qi, ti=ti: e.transpose(out=PST[:, ti, qi * 128:(qi + 1) * 128], in_=Obf[:, pi, qi, :], identity=ident[:]),
                                 reads=[('obf', pi), 'ident'], writes=[('pst', ti)])
                        S.op('act', lambda e, pi=pi, ti=ti, g=g: e.copy(out=OT[:, 2 * g + pi, :], in_=PST[:, ti, :]), reads=[('pst', ti)], writes=[('QO', 1)])
                if dbg and first:
                    dump('OT', QO[:, 8:16], [128, 8, 512], [('QO', 1)], BF16)

                chk('attn')
                QOf = QO[:].rearrange("p a f -> p (a f)")
                BFf = BFW[:].rearrange("p a f -> p (a f)")
                ex_y = [(QOf[:, i * 2048:(i + 1) * 2048].rearrange("p (k c) -> p k c", c=128), ('QOs', i), 'pxq%d' % i) for i in range(2)]
                ex_y.append((BFf[:, 0:2048].rearrange("p (k c) -> p k c", c=128), ('BFs', 0), 'pxb0'))
                S.alias_in([('QOs', 0), ('QOs', 1)], [('QO', 0)])
                S.alias_in([('BFs', 0)], XK + [('tb', 1)])
                set_pan_slots(ex_y)
                def pool_group(gi):
                    w = (2, 4, 8, 16)[gi]
                    for j in range(2):
                        pb, pk = mmA(WinP[16 + gi * 2 + j], hT, HK)
                        U = ubuf[:, 0, :]
                        S.op('pool', lambda e, gi=gi, j=j: e.tensor_copy(out=ubuf[:, 0, 0:16], in_=carry[:, gi * 2 + j, :]), reads=['carry'], writes=['ub0'])
                        S.op('act', lambda e, pb=pb: e.copy(out=ubuf[:, 0, 16:528], in_=pb[:]), reads=[pk], writes=['ub0'])
                        S.op('pool', lambda e, gi=gi, j=j: e.tensor_copy(out=carry[:, gi * 2 + j, :], in_=ubuf[:, 0, 512:528]), reads=['ub0'], writes=['carry'])
                        TFf = TF[:].rearrange("p a f -> p (a f)")
                        ubs = [ubuf[:, 0, :], TFf[:, 0:528], TFf[:, 528:1056]]
                        ubk = [['ub0'], [('tf', 0), ('tf', 1)], [('tf', 1), ('tf', 2)]]
                        cur = 0
                        for kstep in range(gi + 1):
                            sh = 1 << kstep
                            nx_ = 1 if cur != 1 else 2
                            S.op('pool', lambda e, cur=cur, nx_=nx_, sh=sh, ubs=ubs: e.tensor_tensor(out=ubs[nx_][:, sh:528], in0=ubs[cur][:, sh:528], in1=ubs[cur][:, 0:528 - sh], op=ALU.add),
                                 reads=ubk[cur], writes=ubk[nx_])
                            cur = nx_
                        S.op('dve', lambda e, cur=cur, j=j, w=w, ubs=ubs: e.scalar_tensor_tensor(out=PLT[:, gi % 2, j, :], in0=ubs[cur][:, 16:528], scalar=1.0 / w, in1=ubuf[:, 0, 16:528], op0=ALU.mult, op1=ALU.subtract),
                             reads=ubk[cur] + ['ub0'], writes=[('PLT', gi % 2)])
                        if qb == 0:
                            S.op('dve', lambda e, cur=cur, gi=gi, ubs=ubs: e.tensor_tensor(out=small[:, 8:24], in0=ubs[cur][:, 16:32], in1=icnt[:, gi, :], op=ALU.mult), reads=ubk[cur] + ['icnt'], writes=['t16'])
                            S.op('dve', lambda e, j=j: e.tensor_tensor(out=PLT[:, gi % 2, j, 0:16], in0=small[:, 8:24], in1=ubuf[:, 0, 16:32], op=ALU.subtract), reads=['t16', 'ub0', ('PLT', gi % 2)], writes=[('PLT', gi % 2)])
                pool_group(0)
                for c in range(16):
                    gi = c // 4
                    pa, pak = psa()
                    cb = (c % 2) * 64
                    S.op('pe', lambda e, pa=pa, c=c, cb=cb: e.matmul(pa[:], lhsT=Who[cb:cb + 64, c // 2, :], rhs=OT[cb:cb + 64, c // 2, :], start=True, stop=True),
                         reads=['smallres', ('QO', 1)], writes=[pak])
                    pm, pmk = mmA(WinP[24 + c], hT, HK)
                    S.op('act', lambda e, pm=pm: e.activation(out=TF[:, 0, :], in_=pm[:], func=AF.Sigmoid), reads=[pmk], writes=[('tf', 0)])
                    S.op('dve', lambda e, pa=pa: e.tensor_tensor(out=TF[:, 1, :], in0=pa[:], in1=TF[:, 0, :], op=ALU.mult), reads=[pak, ('tf', 0)], writes=[('tf', 1)])
                    pp, ppk = psa()
                    for j in range(2):
                        S.op('pe', lambda e, pp=pp, j=j, c=c, gi=gi: e.matmul(pp[:], lhsT=Wpool[:, gi * 2 + j, (c % 4) * 128:(c % 4 + 1) * 128], rhs=PLT[:, gi % 2, j, :], start=(j == 0), stop=(j == 1)),
                             reads=['Wpool', ('PLT', gi % 2)], writes=[ppk])
                    pm1, pm1k = mmA(WinP[40 + c], hT, HK)
                    S.op('act', lambda e, pm1=pm1: e.activation(out=TF[:, 2, :], in_=pm1[:], func=AF.Sigmoid), reads=[pm1k], writes=[('tf', 2)])
                    S.op('dve', lambda e, pp=pp, c=c: e.scalar_tensor_tensor(out=TF[:, 3, :], in0=pp[:], scalar=pscale[:, c:c + 1], in1=TF[:, 2, :], op0=ALU.mult, op1=ALU.mult),
                         reads=[ppk, ('tf', 2), 'pscale'], writes=[('tf', 3)])
                    S.op('pool', lambda e, c=c: e.tensor_tensor(out=yT[:, c, :], in0=TF[:, 1, :], in1=TF[:, 3, :], op=ALU.add), reads=[('tf', 1), ('tf', 3)], writes=YK + ['raw'])
                    if c % 4 == 0 and gi < 3:
                        pool_group(gi + 1)
                if dbg and first:
                    dump('yT', yT[:], [128, 16, 512], YK, BF16)

                chk('ymix')
                S.alias_out([('QOs', 0), ('QOs', 1)], [('QO', 0)])
                S.alias_out([('BFs', 0)], XK + [('tb', 1)])
                set_pan_slots([])
                S.alias_in([('QOw', 0)], [('QO', 0)])
                set_wb_slots([(QO[:, 0:8].rearrange("p a f -> p (a f)"), ('QOw', 0), 'wbx0')])
                for c8 in range(8):
                    wb_ap, wb_key = load_wb(WoS[c8])
                    wv = wb_ap.rearrange("p (k c) -> p k c", c=256)
                    for qi in range(4):
                        pb, pk = psa()
                        for kc in range(16):
                            S.op('pe', lambda e, kc=kc, qi=qi, pb=pb, wv=wv: e.matmul(pb[:, 0:256], lhsT=yT[:, kc, qi * 128:(qi + 1) * 128], rhs=wv[:, kc, :], start=(kc == 0), stop=(kc == 15)),
                                 reads=[wb_key, ('yT', qi)], writes=[pk])
                        xs = X1[:, qi, c8 * 256:(c8 + 1) * 256]
                        S.op('dve', lambda e, xs=xs, pb=pb: e.tensor_tensor(out=xs, in0=pb[:, 0:256], in1=xs, op=ALU.add), reads=[pk, ('X1', qi)], writes=[('X1', qi)])
                S.alias_out([('QOw', 0)], [('QO', 0)])
                set_wb_slots([])
                if dbg and first:
                    dump('x1', X1[:], [128, 4, 2048], [('X1', qi) for qi in range(4)])

                chk('wout')
                make_hT()
                chk('mkh2')
                for qi in range(4):
                    pb, pk = psa()
                    for kc in range(16):
                        S.op('pe', lambda e, kc=kc, qi=qi, pb=pb: e.matmul(pb[:, 0:20], lhsT=hT[:, kc, qi * 128:(qi + 1) * 128], rhs=Wr[:, kc, :], start=(kc == 0), stop=(kc == 15)),
                             reads=['Wr', ('hT', qi)], writes=[pk])
                    lg = rt[:, 0:20]
                    mx, nmx, se, pg, gm, pen = rt[:, 20:21], rt[:, 21:22], rt[:, 22:23], rt[:, 23:24], rt[:, 24:28], rt[:, 28:32]
                    lm = rt[:, 32:48]
                    t8 = rt[:, 48:56]
                    dv, e2, den, w1, w2 = rt[:, 56:57], rt[:, 57:58], rt[:, 58:59], rt[:, 59:60], rt[:, 60:61]
                    ex = rt[:, 64:68]
                    m1 = rt[:, 68:84]
                    RK = ['rt']
                    S.op('dve', lambda e, pb=pb: e.tensor_tensor(out=lg, in0=pb[:, 0:20], in1=brt[:], op=ALU.add), reads=[pk, 'brt'], writes=RK)
                    S.op('dve', lambda e: e.tensor_reduce(out=mx, in_=lg[:, 0:4], axis=AX.X, op=ALU.max), reads=RK, writes=RK)
                    S.op('dve', lambda e: e.tensor_scalar_mul(out=nmx, in0=mx, scalar1=-1.0), reads=RK, writes=RK)
                    S.op('dve', lambda e: e.memset(se, 0.0), reads=RK, writes=RK)
                    S.op('act', lambda e: e.activation(out=ex, in_=lg[:, 0:4], func=AF.Exp, bias=nmx, scale=1.0, accum_out=se), reads=RK, writes=RK)
                    S.op('dve', lambda e: e.reciprocal(out=pg, in_=se), reads=RK, writes=RK)
                    S.op('dve', lambda e: e.tensor_scalar(out=gm, in0=lg[:, 0:4], scalar1=mx, scalar2=None, op0=ALU.is_ge), reads=RK, writes=RK)
                    S.op('dve', lambda e: e.tensor_scalar(out=pen, in0=gm, scalar1=-1.0, scalar2=1e30, op0=ALU.add, op1=ALU.mult), reads=RK, writes=RK)
                    S.op('dve', lambda e: e.tensor_tensor(out=lm.rearrange("p (g j) -> p g j", g=4), in0=lg[:, 4:20].rearrange("p (g j) -> p g j", g=4),
                                                         in1=pen.unsqueeze(2).to_broadcast([128, 4, 4]), op=ALU.add), reads=RK, writes=RK)
                    S.op('dve', lambda e: e.max(out=t8, in_=lm), reads=RK, writes=RK)
                    S.op('dve', lambda e: e.tensor_tensor(out=dv, in0=t8[:, 1:2], in1=t8[:, 0:1], op=ALU.subtract), reads=RK, writes=RK)
                    S.op('act', lambda e: e.activation(out=e2, in_=dv, func=AF.Exp), reads=RK, writes=RK)
                    S.op('dve', lambda e: e.tensor_scalar_add(out=den, in0=e2, scalar1=1.0), reads=RK, writes=RK)
                    S.op('dve', lambda e: e.reciprocal(out=den, in_=den), reads=RK, writes=RK)
                    S.op('dve', lambda e: e.tensor_tensor(out=w1, in0=pg, in1=den, op=ALU.mult), reads=RK, writes=RK)
                    S.op('dve', lambda e: e.tensor_tensor(out=w2, in0=w1, in1=e2, op=ALU.mult), reads=RK, writes=RK)
                    S.op('dve', lambda e: e.tensor_scalar(out=m1, in0=lm, scalar1=t8[:, 0:1], scalar2=w1, op0=ALU.is_equal, op1=ALU.mult), reads=RK, writes=RK)
                    S.op('dve', lambda e: e.tensor_scalar(out=lm, in0=lm, scalar1=t8[:, 1:2], scalar2=w2, op0=ALU.is_equal, op1=ALU.mult), reads=RK, writes=RK)
                    S.op('dve', lambda e, qi=qi: e.tensor_tensor(out=comb[:, qi, :], in0=m1, in1=lm, op=ALU.add), reads=RK, writes=['comb'])
                S.op('dve', lambda e: e.tensor_copy(out=combb[:], in_=comb[:]), reads=['comb'], writes=['combb'])
                ti = nxt('pst', 1)
                for qi in range(4):
                    S.op('pe', lambda e, qi=qi, ti=ti: e.transpose(out=PST[0:16, ti, qi * 128:(qi + 1) * 128], in_=combb[:, qi, :], identity=ident[:]), reads=['combb', 'ident'], writes=[('pst', ti)])
                S.op('act', lambda e, ti=ti: e.copy(out=combT[:, :], in_=PST[0:16, ti, :]), reads=[('pst', ti)], writes=['combT'])
                if dbg and first:
                    dump('comb', comb[:], [128, 4, 16], ['comb'])
                    dump('h2T', hT[:], [128, 16, 512], HK, BF16)
                yTf = yT[:].rearrange("p a f -> p (a f)")
                ex_m = [(yTf[:, i * 2048:(i + 1) * 2048].rearrange("p (k c) -> p k c", c=128), ('yTs', i), 'pxy%d' % i) for i in range(4)]
                S.alias_in([('yTs', i) for i in range(4)], YK + ['raw'])
                set_pan_slots(ex_m)
                for eg in range(4):
                    par = eg % 2
                    hw = QO[:, par * 8:(par + 1) * 8]
                    for el in range(4):
                        e_ = eg * 4 + el
                        pc, pck = psa()
                        S.op('pe', lambda e, pc=pc, e_=e_: e.matmul(pc[:], lhsT=sele[0:16, e_, :], rhs=combT[:, :], start=True, stop=True), reads=['sele', 'combT'], writes=[pck])
                        S.op('act', lambda e, pc=pc: e.copy(out=TF[:, 2, :], in_=pc[:]), reads=[pck], writes=[('tf', 2)])
                        for hc in range(2):
                            pg_, pgk = mmA(WguP[e_ * 4 + hc], hT, HK)
                            S.op('act', lambda e, pg_=pg_: e.activation(out=TF[:, 0, :], in_=pg_[:], func=AF.Silu), reads=[pgk], writes=[('tf', 0)])
                            pu, puk = mmA(WguP[e_ * 4 + 2 + hc], hT, HK)
                            S.op('dve', lambda e, pu=pu: e.tensor_tensor(out=TF[:, 1, :], in0=pu[:], in1=TF[:, 0, :], op=ALU.mult), reads=[puk, ('tf', 0)], writes=[('tf', 1)])
                            S.op('pool', lambda e, hw=hw, el=el, hc=hc: e.tensor_tensor(out=hw[:, el * 2 + hc, :], in0=TF[:, 1, :], in1=TF[:, 2, :], op=ALU.mult),
                                 reads=[('tf', 1), ('tf', 2)], writes=[('QO', par)])
                    for cc in range(4):
                        wb_ap, wb_key = load_wb(WdS[eg * 4 + cc])
                        wv = wb_ap.rearrange("p (k c) -> p k c", c=512)
                        for qi in range(4):
                            pb, pk = psa()
                            for k8 in range(8):
                                S.op('pe', lambda e, k8=k8, qi=qi, pb=pb, wv=wv, hw=hw: e.matmul(pb[:], lhsT=hw[:, k8, qi * 128:(qi + 1) * 128], rhs=wv[:, k8, :], start=(k8 == 0), stop=(k8 == 7)),
                                     reads=[wb_key, ('QO', par)], writes=[pk])
                            xs = X1[:, qi, cc * 512:(cc + 1) * 512]
                            S.op('dve', lambda e, xs=xs, pb=pb: e.tensor_tensor(out=xs, in0=pb[:], in1=xs, op=ALU.add), reads=[pk, ('X1', qi)], writes=[('X1', qi)])
                S.alias_out([('yTs', i) for i in range(4)], YK + ['raw'])
                set_pan_slots([])
                chk('moe')
                for qi in range(4):
                    S.dma('sp', 'out%d' % qi, lambda e, qi=qi: e.dma_start(out=out[s, t0 + qi * 128:t0 + (qi + 1) * 128, :], in_=X1[:, qi, :]), reads=[('X1', qi)], writes=['out'])
        S.barrier()
        S.emit()
    return nc, dbg_out


_CACHE = {}


def _layout_inputs(inputs):
    consts = make_consts()
    wts = {}
    for k in WEIGHT_SHAPES:
        a = np.asarray(inputs[k])
        wts[k] = np.ascontiguousarray(a.reshape(a.shape[1:]), dtype=np.float32)
    xs = np.asarray(inputs['x'], dtype=np.float32)
    pos = np.asarray(inputs['positions'], dtype=np.int32)
    in_maps = []
    for c in range(NCORES):
        m = {'x': np.ascontiguousarray(xs[c * NSEQ:(c + 1) * NSEQ]), 'positions': np.ascontiguousarray(pos[c * NSEQ:(c + 1) * NSEQ])}
        m.update(wts)
        m.update(consts)
        in_maps.append(m)
    return in_maps


def kernel(**inputs):
    if 'nc' not in _CACHE:
        _CACHE['nc'] = build()[0]
    nc = _CACHE['nc']
    in_maps = _layout_inputs(inputs)
    res = run_bass_kernel_spmd(nc, in_maps, core_ids=list(range(NCORES)))
    outs = [np.asarray(r['out']) for r in res.results]
    return np.concatenate(outs, axis=0).astype(np.float32)
```

```python
import numpy as np
from contextlib import ExitStack
import concourse.bass as bass
import concourse.mybir as mybir
from concourse.bass_utils import run_bass_kernel_spmd

F32 = mybir.dt.float32
BF16 = mybir.dt.bfloat16
I32 = mybir.dt.int32
ALU = mybir.AluOpType
AF = mybir.ActivationFunctionType
AX = mybir.AxisListType

D = 2048
SEQ = 2048
NSEQ = 2
NCORES = 8
TB = 512
NQB = SEQ // TB
H = 16
G = 4
NCMP = 127
INW = 7728
OFF_Q, OFF_KC, OFF_VC, OFF_KS, OFF_VS, OFF_KW, OFF_VW, OFF_GATE, OFF_POOL, OFF_MERGE = (
    0, 1024, 1280, 1536, 1792, 2048, 2304, 2560, 2608, 3632)
NE = 16
TWO_PI = 6.283185307179586
PI_SAFE = 3.1415925


class _Rec:
    def __init__(self):
        self.call = None

    def __getattr__(self, name):
        def f(*a, **k):
            assert self.call is None
            self.call = (name, a, k)
            return self
        return f


def _replay(fn):
    rec = _Rec()
    fn(rec)
    name, a, k = rec.call
    return lambda e: getattr(e, name)(*a, **k)


class Sched:
    ENG = ('pe', 'act', 'dve', 'pool', 'sp')

    def __init__(self, nc, es):
        self.nc = nc
        self.es = es
        self.lists = {e: [] for e in self.ENG}
        self.sem = {e: es.enter_context(nc.semaphore('s_' + e)) for e in self.ENG}
        self.cnt = {e: 0 for e in self.ENG}
        self.seen = {e: {} for e in self.ENG}
        self.lastw = {}
        self.readers = {}
        self.dsem = {}

    def _deps(self, reads, writes):
        deps = []
        for k in reads:
            d = self.lastw.get(k)
            if d is not None:
                deps.append(d)
        for k in writes:
            d = self.lastw.get(k)
            if d is not None:
                deps.append(d)
            r = self.readers.get(k)
            if r:
                deps.extend(r.values())
        return deps

    def _emit_waits(self, eng, deps):
        need = {}
        seen = self.seen[eng]
        for (sname, sem, val) in deps:
            if eng == 'pe' and sname == 'Epe':
                continue
            if seen.get(sname, 0) < val:
                if sname not in need or need[sname][1] < val:
                    need[sname] = (sem, val)
        for sname, (sem, val) in need.items():
            seen[sname] = val
            self.lists[eng].append(lambda e, sem=sem, val=val: e.wait_ge(sem, val))

    def _reg(self, dep, reads, writes):
        for k in writes:
            self.lastw[k] = dep
            self.readers[k] = {}
        for k in reads:
            if k not in writes:
                r = self.readers.setdefault(k, {})
                o = r.get(dep[0])
                if o is None or o[2] < dep[2]:
                    r[dep[0]] = dep

    dead = False

    def op(self, eng, fn, reads=(), writes=()):
        if self.dead:
            return
        fn = _replay(fn)
        self._emit_waits(eng, self._deps(reads, writes))
        self.cnt[eng] += 1
        sem = self.sem[eng]
        self.lists[eng].append(lambda e, fn=fn, sem=sem: fn(e).then_inc(sem, 1))
        dep = ('E' + eng, sem, self.cnt[eng])
        self._reg(dep, reads, writes)

    def dma(self, eng, slot, fn, reads=(), writes=()):
        if self.dead:
            return
        fn = _replay(fn)
        self._emit_waits(eng, self._deps(reads, writes))
        if slot not in self.dsem:
            self.dsem[slot] = [self.es.enter_context(self.nc.semaphore('d_' + slot)), 0]
        ent = self.dsem[slot]
        ent[1] += 16
        sem = ent[0]
        self.lists[eng].append(lambda e, fn=fn, sem=sem: fn(e).then_inc(sem, 16))
        dep = ('D' + slot, sem, ent[1])
        self._reg(dep, reads, writes)

    def alias_in(self, new_keys, old_keys):
        acc = {}
        for k in old_keys:
            for d in [self.lastw.get(k)] + list(self.readers.get(k, {}).values()):
                if d is not None and (d[0] not in acc or acc[d[0]][2] < d[2]):
                    acc[d[0]] = d
        for k in new_keys:
            self.lastw.pop(k, None)
            self.readers[k] = dict(acc)

    def alias_out(self, new_keys, old_keys):
        acc = {}
        for k in new_keys:
            for d in [self.lastw.get(k)] + list(self.readers.get(k, {}).values()):
                if d is not None and (d[0] not in acc or acc[d[0]][2] < d[2]):
                    acc[d[0]] = d
        for k in old_keys:
            r = self.readers.setdefault(k, {})
            for n_, d in acc.items():
                if n_ not in r or r[n_][2] < d[2]:
                    r[n_] = d

    def barrier(self):
        deps = [('E' + e, self.sem[e], self.cnt[e]) for e in self.ENG if self.cnt[e] > 0]
        deps += [('D' + s, ent[0], ent[1]) for s, ent in self.dsem.items()]
        for e in self.ENG:
            self._emit_waits(e, deps)

    def emit(self):
        nc = self.nc
        L = self.lists
        with nc.Block() as block:
            @block.tensor
            def _(e):
                for f in L['pe']:
                    f(e)

            @block.scalar
            def _(e):
                for f in L['act']:
                    f(e)

            @block.vector
            def _(e):
                for f in L['dve']:
                    f(e)

            @block.gpsimd
            def _(e):
                for f in L['pool']:
                    f(e)

            @block.sync
            def _(e):
                for f in L['sp']:
                    f(e)


def make_consts():
    c = {}
    tri = np.zeros((2, 128, 128), np.float32)
    k = np.arange(128)[:, None]
    q = np.arange(128)[None, :]
    tri[0] = (k <= q)
    tri[1] = (k > q)
    c['c_tri'] = tri.transpose(1, 0, 2).copy()
    n = np.arange(NCMP)[:, None]
    t = np.arange(SEQ)[None, :]
    mk = np.zeros((128, SEQ), np.float32)
    mk[:NCMP] = (16 * n + 31 <= t)
    c['c_maskc'] = mk
    tt = np.arange(SEQ)
    tb = tt // 64
    j = np.arange(32)[None, :]
    dist = tb[:, None] - j
    valid = dist >= 0
    forced = (j == 0) | (valid & (dist < 2))
    bonus = np.where(valid, 1e4 * forced.astype(np.float32), -1e30).astype(np.float32)
    c['c_bonus'] = bonus.reshape(16, 128, 32).transpose(1, 0, 2).copy()
    em = np.zeros((32, 16, 128), np.float32)
    for kt in range(16):
        for kk in range(128):
            em[2 * kt + kk // 64, kt, kk] = 1.0
    emp = np.zeros((128, 16, 128), np.float32)
    emp[:32] = em
    c['c_emat'] = emp
    a0 = np.arange(NCMP)[:, None] * 16
    b0 = np.arange(32)[None, :] * 64
    ov = np.clip(np.minimum(a0 + 32, b0 + 64) - np.maximum(a0, b0), 0, None) / 32.0
    ovp = np.zeros((128, 33), np.float32)
    ovp[:NCMP, 0] = 1.0
    ovp[:NCMP, 1:] = ov
    c['c_ov'] = ovp
    inv_freq = (500000.0 ** (-np.arange(0, 16, 2, dtype=np.float32) / 16)).astype(np.float32)
    fr = np.zeros((128, 1), np.float32)
    for p in range(128):
        if p % 64 < 16:
            fr[p, 0] = inv_freq[p % 8]
    c['c_freq'] = fr
    rm = np.zeros((128, 128), np.float32)
    for b in (0, 64):
        for d in range(8):
            rm[b + d + 8, b + d] = -1.0
            rm[b + d, b + d + 8] = 1.0
    c['c_rm'] = rm
    ob = np.zeros((128, 128), np.float32)
    ob[:64, :64] = 1.0 / 64
    ob[64:, 64:] = 1.0 / 64
    c['c_onesblk'] = ob
    se = np.zeros((128, 16, 128), np.float32)
    for e in range(16):
        se[e, e, :] = 1.0
    c['c_sele'] = se
    ic = np.zeros((128, 4, 16), np.float32)
    for gi, w in enumerate((2, 4, 8, 16)):
        for xx in range(16):
            ic[:, gi, xx] = 1.0 / min(xx + 1, w)
    c['c_icnt'] = ic
    return c


CONST_SHAPES = {
    'c_tri': [128, 2, 128], 'c_maskc': [128, SEQ], 'c_bonus': [128, 16, 32], 'c_emat': [128, 16, 128],
    'c_ov': [128, 33], 'c_freq': [128, 1], 'c_rm': [128, 128], 'c_onesblk': [128, 128],
    'c_sele': [128, 16, 128], 'c_icnt': [128, 4, 16],
}

WEIGHT_SHAPES = {
    'attn_norm_g': [D], 'w_in': [D, INW], 'q_norm_g': [64], 'k_norm_cmp_g': [64], 'k_norm_slc_g': [64],
    'k_norm_swa_g': [64], 'cmp_pos_emb_k': [32, 64], 'cmp_w1_k': [2048, 256], 'cmp_w2_k': [256, 64],
    'cmp_pos_emb_v': [32, 64], 'cmp_w1_v': [2048, 256], 'cmp_w2_v': [256, 64],
    'w_head_out': [16, 64, 128], 'w_pool': [4, 256, 512], 'pool_scale': [D], 'w_out': [D, D],
    'ffn_norm_g': [D], 'w_router_group': [D, 4], 'b_router_group': [4], 'w_router_expert': [D, 16],
    'b_router_expert': [16], 'w_expert_gate': [16, D, 256], 'w_expert_up': [16, D, 256],
    'w_expert_down': [16, 256, D],
}


MARKS = []


class _Stop(Exception):
    pass


def build(dbg=False, nseq=NSEQ, nqb=NQB, stop_after=None, skip_prep=False):
    nc = bass.Bass("TRN2", target_bir_lowering=False)
    x = nc.dram_tensor("x", [NSEQ, SEQ, D], F32, kind="ExternalInput").ap()
    positions = nc.dram_tensor("positions", [NSEQ, SEQ], I32, kind="ExternalInput").ap()
    W = {k: nc.dram_tensor(k, s, F32, kind="ExternalInput").ap() for k, s in WEIGHT_SHAPES.items()}
    C = {k: nc.dram_tensor(k, s, F32, kind="ExternalInput").ap() for k, s in CONST_SHAPES.items()}
    out = nc.dram_tensor("out", [NSEQ, SEQ, D], F32, kind="ExternalOutput").ap()
    WinP = nc.dram_tensor("WinP", [56, 128, 2048], BF16).ap()
    WVS = nc.dram_tensor("WVS", [2, 128, 4096], BF16).ap()
    WguP = nc.dram_tensor("WguP", [64, 128, 2048], BF16).ap()
    WdS = nc.dram_tensor("WdS", [16, 128, 4096], BF16).ap()
    WoS = nc.dram_tensor("WoS", [8, 128, 4096], BF16).ap()
    W1S = nc.dram_tensor("W1S", [4, 128, 4096], BF16).ap()
    dbg_out = {}

    with ExitStack() as es:
        S = Sched(nc, es)

        def sb(name, shape, dt):
            return es.enter_context(nc.sbuf_tensor(name, shape, dt))

        def ps(name, shape, dt):
            return es.enter_context(nc.psum_tensor(name, shape, dt))

        X1 = sb("X1", [128, 4, 2048], F32)
        hT = sb("hT", [128, 16, 512], BF16)
        QO = sb("QO", [128, 16, 512], BF16)
        yT = sb("yT", [128, 16, 512], BF16)
        KT = sb("KT", [128, 2, 2, SEQ], BF16)
        VA = sb("VA", [128, 2, 16, 4 * 65], BF16)
        kcT = sb("kcT", [128, 512], BF16)
        vcA = sb("vcA", [128, 4, 97], BF16)
        CSt = sb("CSt", [128, 2, 512], F32)
        posi = sb("posi", [128, 512], I32)
        TF = sb("TF", [128, 4, 512], F32)
        BFW = sb("BFW", [128, 5, 512], BF16)
        PT = BFW[:, 0:3]
        TBb = BFW[:, 3:5]
        XNB = BFW[:, 0:4].rearrange("p a f -> p (a f)")
        XK = [('pt', 0), ('pt', 1), ('pt', 2), ('tb', 0)]
        Otot = sb("Otot", [128, 2, 4, 128], F32)
        Obf = sb("Obf", [128, 2, 4, 128], BF16)
        Cc = Otot
        hidT = QO[:, 0:4].rearrange("p (a b) f -> p a b f", a=2)
        CCK = ['Cc', ('otot', 0), ('otot', 1)]
        gates = sb("gates", [128, 4, 48], F32)
        sc2 = sb("sc2", [128, 32], F32)
        top8 = sb("top8", [128, 16], F32)
        selb = sb("selb", [128, 4, 32], BF16)
        selbT = sb("selbT", [128, 512], BF16)
        QP = sb("QP", [128, 4, 512], BF16)
        zeros128 = sb("zeros128", [128, 128], BF16)
        Rg = sb("Rg", [128, 4, 128], BF16)
        small = sb("small", [128, 64], F32)
        ubuf = sb("ubuf", [128, 1, 528], F32)
        carry = sb("carry", [128, 8, 16], F32)
        PLT = sb("PLT", [128, 2, 2, 512], BF16)
        rt = sb("rt", [128, 96], F32)
        comb = sb("comb", [128, 4, 16], F32)
        combb = sb("combb", [128, 4, 16], BF16)
        combT = sb("combT", [16, 512], BF16)
        PAN = sb("PAN", [128, 3, 16, 128], BF16)
        WB = sb("WB", [128, 2, 4096], BF16)
        WG = sb("WG", [128, 16, 48], BF16)
        Wpool = sb("Wpool", [128, 8, 512], BF16)
        Who = sb("Who", [128, 8, 128], BF16)
        W2k = sb("W2k", [128, 2, 128], BF16)
        W2v = sb("W2v", [128, 2, 64], BF16)
        Wr = sb("Wr", [128, 16, 20], BF16)
        peT = sb("peT", [64, 2, 32], BF16)
        bh = sb("bh", [128, 4], F32)
        gvec = sb("gvec", [128, 40], F32)
        pscale = sb("pscale", [128, 16], F32)
        brt = sb("brt", [128, 20], F32)
        ident = sb("ident", [128, 128], BF16)
        tri = sb("tri", [128, 2, 128], BF16)
        maskc = sb("maskc", [128, 512], BF16)
        bonus = sb("bonus", [128, 4, 32], F32)
        emat = sb("emat", [128, 16, 128], BF16)
        freq = sb("freq", [128, 1], F32)
        rmat = sb("rmat", [128, 128], BF16)
        onesblk = sb("onesblk", [128, 128], BF16)
        sele = sb("sele", [128, 16, 128], BF16)
        icnt = sb("icnt", [128, 4, 16], F32)
        ssq = sb("ssq", [128, 8], F32)

        tmpo = TF[:, 3, 0:256].rearrange("p (a b) -> p a b", a=4)
        tmpi = TF[:, 3, 256:384].rearrange("p (a b) -> p a b", a=4)
        imp = TF[:, 2, 0:128].rearrange("p (a b) -> p a b", a=4)
        scb = TF[:, 2, 128:256].rearrange("p (a b) -> p a b", a=4)
        PSA = [ps("psa%d" % i, [128, 512], F32) for i in range(3)]
        PSS = [ps("pss%d" % i, [128, 512], F32) for i in range(2)]
        PSO = [ps("pso%d" % i, [128, 4, 128], F32) for i in range(2)]
        PST = ps("pst", [128, 1, 512], BF16)

        rr = {'psa': 0, 'pss': 0, 'pso': 0, 'pst': 0, 'pan': 0, 'wb': 0, 'pt': 0, 'ew': 0}

        def nxt(name, n):
            i = rr[name]
            rr[name] = (i + 1) % n
            return i

        def psa():
            i = nxt('psa', 3)
            return PSA[i], ('psa', i)

        def chk(stage):
            if stage[0] != 'h':
                MARKS.append((stage, S.cnt['pe'], S.cnt['act']))
            if stop_after == stage:
                S.dead = True

        chk('start')
        ukey = [0]

        def uk():
            ukey[0] += 1
            return ('cst', ukey[0])

        def ld(dst, src, key, eng='sp', slot='cst'):
            S.dma(eng, slot, lambda e: e.dma_start(out=dst, in_=src), writes=[uk()])

        ld(tri[:], C['c_tri'], 'tri', 'pool', 'cstp')
        ld(emat[:], C['c_emat'], 'emat', 'pool', 'cstp')
        ld(rmat[:], C['c_rm'], 'rmat', 'pool', 'cstp')
        ld(onesblk[:], C['c_onesblk'], 'onesblk', 'pool', 'cstp')
        ld(freq[:], C['c_freq'], 'freq')
        ld(sele[:], C['c_sele'], 'sele', 'pool', 'cstp')
        ld(icnt[:], C['c_icnt'], 'icnt')
        chk('c0')
        for g in range(4):
            ld(vcA[:, g, 64:97], C['c_ov'], 'vcA', 'pool', 'cstp')
        chk('c1')
        nsc = lambda e, o, i: e.dma_start(out=o, in_=i, allow_slow_non_contiguous=True)
        for q4 in range(4):
            cs_ = slice(q4 * 4, q4 * 4 + 4)
            S.dma('sp', 'cst', lambda e: nsc(e, gvec[:, q4 * 4:q4 * 4 + 4], W['attn_norm_g'].rearrange("(c p) -> p c", p=128)[:, cs_]), writes=[uk()])
            S.dma('sp', 'cst', lambda e: nsc(e, gvec[:, 16 + q4 * 4:16 + q4 * 4 + 4], W['ffn_norm_g'].rearrange("(c p) -> p c", p=128)[:, cs_]), writes=[uk()])
            S.dma('sp', 'cst', lambda e: nsc(e, pscale[:, cs_], W['pool_scale'].rearrange("(c p) -> p c", p=128)[:, cs_]), writes=[uk()])
        for i, nm in enumerate(('q_norm_g', 'k_norm_cmp_g', 'k_norm_slc_g', 'k_norm_swa_g')):
            for hb in (0, 64):
                S.dma('sp', 'cst', lambda e, i=i, nm=nm, hb=hb: nsc(e, gvec[hb:hb + 64, 32 + i:33 + i], W[nm].rearrange("(p o) -> p o", o=1)), writes=[uk()])
        chk('c2')
        S.dma('sp', 'cst', lambda e: e.dma_start(out=brt[:, 0:4], in_=W['b_router_group'].partition_broadcast(128)), writes=[uk()])
        S.dma('sp', 'cst', lambda e: e.dma_start(out=brt[:, 4:20], in_=W['b_router_expert'].partition_broadcast(128)), writes=[uk()])
        chk('c3')
        S.op('dve', lambda e: e.memset(gvec[:, 36:37], 1.0), writes=['gvec'])
        S.op('dve', lambda e: e.memset(VA[:].rearrange("p a b c -> p (a b c)"), 1.0), writes=['VA'])
        S.op('dve', lambda e: e.memset(ssq[:], 0.0), writes=['ssq'])
        S.op('dve', lambda e: e.memset(selbT[:], 0.0), writes=['selbT'])
        S.op('dve', lambda e: e.memset(zeros128[:], 0.0), writes=['zeros128'])
        S.op('pool', lambda e: e.memset(QP[:].rearrange("p a f -> p (a f)"), 0.0), writes=[('qp', i) for i in range(4)])
        S.op('pool', lambda e: e.memset(ident[:], 0.0), writes=['ident'])
        S.op('pool', lambda e: e.affine_select(out=ident[:], in_=ident[:], pattern=[[-1, 128]], compare_op=ALU.not_equal, fill=1.0, base=0, channel_multiplier=1), reads=['ident'], writes=['ident'])

        X1f = X1[:].rearrange("p a f -> p (a f)")
        chk('const')
        S.barrier()
        for v_ in range(4):
            S.op('dve', lambda e: e.tensor_scalar_mul(out=Rg[:, v_, :], in0=rmat[:], scalar1=gvec[:, 32 + v_:33 + v_]), reads=['gvec'], writes=['Rg'])
        if skip_prep:
            S.dead = True
        hTf = hT[:].rearrange("p a f -> p (a f)")
        st_in = [X1f[:, i * 4096:(i + 1) * 4096] for i in range(2)]
        st_out = [hTf[:, i * 4096:(i + 1) * 4096] for i in range(2)]
        pj = [0]
        EW3 = ('act', 'dve', 'act', 'act', 'pool', 'act', 'act', 'dve', 'act', 'act', 'act', 'dve', 'act', 'pool', 'act', 'act')

        def scale_op(eng, o, i, sc):
            if eng == 'act':
                S_fn = lambda e: e.mul(out=o, in_=i, mul=sc)
            else:
                S_fn = lambda e: e.tensor_scalar_mul(out=o, in0=i, scalar1=sc)
            return S_fn

        def prep(loads, mode, gofs, stores, resident=None):
            i = pj[0] % 2
            pj[0] += 1
            for (dfn, src) in loads:
                S.dma('sp', 'pin%d' % i, lambda e, dfn=dfn, src=src: e.dma_start(out=dfn(st_in[i]), in_=src), writes=[('pin', i)])
            sin = st_in[i].rearrange("p (k c) -> p k c", c=256)
            for kc in range(16):
                sc = gvec[:, gofs + kc:gofs + kc + 1] if gofs is not None else gvec[:, 36:37]
                if resident is not None:
                    o, ii = resident(sin, kc)
                    wk = [resident.key]
                elif mode == 'A':
                    o = st_out[i].rearrange("p (n k c) -> p n k c", n=2, k=16)[:, :, kc, :]
                    ii = sin[:, kc, :].rearrange("p (n c) -> p n c", n=2)
                    wk = [('pout', i, kc)]
                elif mode == 'Aq':
                    o = st_out[i].rearrange("p (n k hf d) -> p n k hf d", n=2, k=16, hf=2)[:, :, kc, :, :]
                    ii = sin[:, kc, :].rearrange("p (hf r d) -> p r hf d", hf=2, r=2)
                    wk = [('pout', i, kc)]
                else:
                    o = st_out[i][:, kc * 256:(kc + 1) * 256]
                    ii = sin[:, kc, :]
                    wk = [('pout', i, kc)]
                eng = EW3[kc % 16]
                if o.shape[0] != 128:
                    sc = sc[0:o.shape[0]]
                if mode == 'Aq':
                    for n_ in range(2):
                        S.op(eng, scale_op(eng, o[:, n_], ii[:, n_], sc), reads=[('pin', i), 'gvec'], writes=wk)
                    continue
                S.op(eng, scale_op(eng, o, ii, sc), reads=[('pin', i), 'gvec'], writes=wk)
            prev = pend[:]
            del pend[:]
            for (dst, sfn) in stores:
                pend.append((i, dst, sfn))
            flush(prev)

        pend = []

        def flush(lst):
            for (i, dst, sfn) in lst:
                S.dma('sp', 'pout%d' % i, lambda e, dst=dst, sfn=sfn, i=i: e.dma_start(out=dst, in_=sfn(st_out[i])),
                      reads=[('pout', i, kc) for kc in range(16)], writes=['scratch'])

        def win_src(c0, n):
            return W['w_in'][:, c0:c0 + n].rearrange("(k p) c -> p k c", p=128)

        def st_cols(a, n):
            return lambda st: st.rearrange("p (k c) -> p k c", c=256)[:, :, a:a + n]

        def store_panels(p0):
            return [(WinP[p0:p0 + 2].rearrange("n p f -> p n f"), lambda st: st.rearrange("p (n f) -> p n f", n=2))]

        for m in range(2):
            for r2 in range(2):
                c_a = (8 * m + 2 * r2) * 64
                c_b = (8 * m + 4 + 2 * r2) * 64
                prep([(st_cols(0, 128), win_src(c_a, 128)), (st_cols(128, 128), win_src(c_b, 128))], 'Aq', 0,
                     store_panels(4 * m + 2 * r2))
        for pi, c0 in ((8, OFF_KC), (10, OFF_VC), (12, OFF_KS), (14, OFF_KW)):
            prep([(st_cols(0, 256), win_src(c0, 256))], 'A', 0, store_panels(pi))
        for i4 in range(4):
            prep([(st_cols(0, 256), win_src(OFF_POOL + i4 * 256, 256))], 'A', 0, store_panels(16 + 2 * i4))
        for i16 in range(16):
            prep([(st_cols(0, 256), win_src(OFF_MERGE + i16 * 256, 256))], 'A', 0, store_panels(24 + 2 * i16))
        for vi, c0 in ((0, OFF_VS), (1, OFF_VW)):
            prep([(st_cols(0, 256), win_src(c0, 256))], 'B', 0, [(WVS[vi], lambda st: st)])

        class Res:
            def __init__(self, fn, key):
                self.fn, self.key = fn, key

            def __call__(self, sin, kc):
                return self.fn(sin, kc)

        prep([(st_cols(0, 48), win_src(OFF_GATE, 48))], 'R', 0, [],
             resident=Res(lambda sin, kc: (WG[:, kc, :], sin[:, kc, 0:48]), 'WG'))
        def st_cols_k(a, n, k0, k1):
            return lambda st: st.rearrange("p (k c) -> p k c", c=256)[:, k0:k1, a:a + n]
        rl = []
        for q4 in range(4):
            rl.append((st_cols_k(0, 4, q4 * 4, q4 * 4 + 4), W['w_router_group'].rearrange("(k p) c -> p k c", p=128)[:, q4 * 4:q4 * 4 + 4, :]))
            rl.append((st_cols_k(4, 16, q4 * 4, q4 * 4 + 4), W['w_router_expert'].rearrange("(k p) c -> p k c", p=128)[:, q4 * 4:q4 * 4 + 4, :]))
        prep(rl, 'R', 16, [],
             resident=Res(lambda sin, kc: (Wr[:, kc, :], sin[:, kc, 0:20]), 'Wr'))
        for e_ in range(NE):
            for gi_, nm in enumerate(('w_expert_gate', 'w_expert_up')):
                prep([(st_cols(0, 256), W[nm][e_].rearrange("(k p) c -> p k c", p=128))], 'A', 16,
                     [(WguP[e_ * 4 + gi_ * 2:e_ * 4 + gi_ * 2 + 2].rearrange("n p f -> p n f"), lambda st: st.rearrange("p (n f) -> p n f", n=2))])
        for eg in range(4):
            for cc in range(4):
                src = W['w_expert_down'][eg * 4:eg * 4 + 4, :, cc * 512:(cc + 1) * 512].rearrange("e (h p) c -> p e h c", p=128)
                prep([(lambda st: st.rearrange("p (e h c) -> p e h c", e=4, h=2), src)], 'B', None, [(WdS[eg * 4 + cc], lambda st: st)])
        for c8 in range(8):
            prep([(st_cols(0, 256), W['w_out'][:, c8 * 256:(c8 + 1) * 256].rearrange("(k p) c -> p k c", p=128))], 'B', None,
                 [(WoS[c8], lambda st: st)])
        for kv, nm in enumerate(('cmp_w1_k', 'cmp_w1_v')):
            for lh in range(2):
                src = W[nm][lh * 1024:(lh + 1) * 1024, :].rearrange("(l d) h -> d l h", d=64)
                prep([(lambda st: st.rearrange("p (l h) -> p l h", h=256)[0:64], src),
                      (lambda st: st.rearrange("p (l h) -> p l h", h=256)[64:128], src)], 'B', None,
                     [(W1S[kv * 2 + lh], lambda st: st)])
        prep([(lambda st: st.rearrange("p (g j e) -> p g j e", g=4, j=2),
               W['w_pool'].rearrange("g (j p) e -> p g j e", p=128))], 'R', None, [],
             resident=Res(lambda sin, kc: (Wpool[:, kc // 2, (kc % 2) * 256:(kc % 2) * 256 + 256], sin[:, kc, :]), 'Wpool'))

        def res_small(sin, kc):
            flat = sin.rearrange("p k c -> p (k c)")
            if kc < 4:
                return Who[:, 2 * kc:2 * kc + 2, :], flat[:, kc * 256:(kc + 1) * 256].rearrange("p (a b) -> p a b", a=2)
            if kc == 4:
                return W2k[:, :, 0:64], flat[:, 1024:1280].rearrange("p (a b) -> p a b", a=2)[:, :, 0:64]
            if kc == 5:
                return W2k[:, :, 64:128], flat[:, 1024:1280].rearrange("p (a b) -> p a b", a=2)[:, :, 0:64]
            if kc == 6:
                return W2v[:, :, :], flat[:, 1280:1536].rearrange("p (a b) -> p a b", a=2)[:, :, 0:64]
            if kc == 7:
                return peT[:, :, :], flat[0:64, 1536:1664].rearrange("p (a b) -> p a b", a=2)[:, :, 0:32]
            return small[:, 32 + kc:33 + kc], flat[:, 4000 + kc:4001 + kc]

        ifl = lambda st: st
        who_src = W['w_head_out'].rearrange("(c two) d e -> (two d) c e", two=2)
        loads = [(lambda st: st[:, 0:1024].rearrange("p (c e) -> p c e", c=8), who_src)]
        for hc in range(2):
            loads.append((lambda st, hc=hc: st[:, 1024 + hc * 128:1024 + hc * 128 + 64], W['cmp_w2_k'][hc * 128:(hc + 1) * 128, :]))
            loads.append((lambda st, hc=hc: st[:, 1280 + hc * 128:1280 + hc * 128 + 64], W['cmp_w2_v'][hc * 128:(hc + 1) * 128, :]))
        i_sm = pj[0] % 2
        S.op('dve', lambda e: e.memset(st_in[i_sm], 0.0), writes=[('pin', i_sm)])
        for kv, nm in enumerate(('cmp_pos_emb_k', 'cmp_pos_emb_v')):
            for q4 in range(4):
                S.dma('sp', 'pin%d' % i_sm, lambda e, kv=kv, nm=nm: nsc(e, st_in[i_sm][0:64, 1536 + kv * 64 + q4 * 8:1536 + kv * 64 + q4 * 8 + 8], W[nm].rearrange("l d -> d l")[:, q4 * 8:q4 * 8 + 8]), writes=[('pin', i_sm)])
        prep(loads, 'R', None, [], resident=Res(res_small, 'smallres'))
        flush(pend)
        if skip_prep:
            S.dead = False
        chk('prep')
        S.barrier()

        base_pan = [(PAN[:, i], ('pan', i), 'pan%d' % i) for i in range(3)]
        pan_slots = list(base_pan)

        def set_pan_slots(extra):
            del pan_slots[:]
            pan_slots.extend(base_pan + extra)
            rr['pan'] = 0

        def load_panel(src):
            i = nxt('pan', len(pan_slots))
            ap_, key_, sname = pan_slots[i]
            S.dma('sp', sname, lambda e: e.dma_start(out=ap_.rearrange("p k c -> p (k c)"), in_=src), reads=['scratch'], writes=[key_])
            return ap_, key_

        base_wb = [(WB[:, i, :], ('wb', i), 'wb%d' % i) for i in range(2)]
        wb_slots = list(base_wb)

        def set_wb_slots(extra):
            del wb_slots[:]
            wb_slots.extend(base_wb + extra)
            rr['wb'] = 0

        def load_wb(src):
            i = nxt('wb', len(wb_slots))
            ap_, key_, sname = wb_slots[i]
            S.dma('sp', sname, lambda e: e.dma_start(out=ap_, in_=src), reads=['scratch'], writes=[key_])
            return ap_, key_

        def mmA(src, actT, akeys, n=512):
            pan_ap, pan_key = load_panel(src)
            pb, pk = psa()
            for kc in range(16):
                S.op('pe', lambda e, kc=kc: e.matmul(pb[:, 0:n], lhsT=pan_ap[:, kc, :], rhs=actT[:, kc, 0:n], start=(kc == 0), stop=(kc == 15)),
                     reads=[pan_key] + akeys, writes=[pk])
            return pb, pk

        def ew():
            i = nxt('ew', 2)
            return ('dve', 'pool')[i]

        def rope_tables(s, t0):
            S.dma('sp', 'posi', lambda e: e.dma_start(out=posi[:], in_=positions[s, t0:t0 + 512].partition_broadcast(128)), writes=['posi'])
            ang, kf, ki = TF[:, 0, :], TF[:, 1, :], posi[:]
            S.op('dve', lambda e: e.tensor_copy(out=ang, in_=posi[:]), reads=['posi'], writes=[('tf', 0)])
            S.op('dve', lambda e: e.tensor_scalar_mul(out=ang, in0=ang, scalar1=freq[:, 0:1]), reads=[('tf', 0), 'freq'], writes=[('tf', 0)])
            for ci, ph in ((0, 1.5707963267948966), (1, 0.0)):
                S.op('dve', lambda e, ph=ph: e.tensor_scalar(out=kf, in0=ang, scalar1=ph, scalar2=1.0 / TWO_PI, op0=ALU.add, op1=ALU.mult),
                     reads=[('tf', 0)], writes=[('tf', 1)])
                S.op('dve', lambda e: e.tensor_copy(out=ki, in_=kf), reads=[('tf', 1)], writes=['posi'])
                S.op('dve', lambda e: e.tensor_copy(out=kf, in_=ki), reads=['posi'], writes=[('tf', 1)])
                r = TF[:, 2, :]
                S.op('dve', lambda e: e.scalar_tensor_tensor(out=r, in0=kf, scalar=-6.28125, in1=ang, op0=ALU.mult, op1=ALU.add),
                     reads=[('tf', 0), ('tf', 1)], writes=[('tf', 2)])
                S.op('dve', lambda e: e.scalar_tensor_tensor(out=r, in0=kf, scalar=-(TWO_PI - 6.28125), in1=r, op0=ALU.mult, op1=ALU.add),
                     reads=[('tf', 1), ('tf', 2)], writes=[('tf', 2)])
                S.op('dve', lambda e, ph=ph: e.tensor_scalar(out=r, in0=r, scalar1=ph, scalar2=PI_SAFE, op0=ALU.add, op1=ALU.min),
                     reads=[('tf', 2)], writes=[('tf', 2)])
                S.op('dve', lambda e: e.tensor_scalar_max(out=r, in0=r, scalar1=-PI_SAFE), reads=[('tf', 2)], writes=[('tf', 2)])
                S.op('act', lambda e, ci=ci: e.activation(out=CSt[:, ci, :], in_=r, func=AF.Sin), reads=[('tf', 2)], writes=['CSt'])

        def normrope(pb, pk, gcol, Ct, St, cskeys, out_ap, okeys, n=512, view=None):
            vw = view if view is not None else (lambda a: a)
            v = gcol - 32
            sq, psb = TBb[:, 0, 0:n], TBb[:, 1, 0:n]
            rstd, t1, t2 = TF[:, 0, 0:n], TF[:, 1, 0:n], TF[:, 2, 0:n]
            S.op('act', lambda e: e.activation(out=sq, in_=pb[:, 0:n], func=AF.Square), reads=[pk], writes=[('tb', 0)])
            S.op('act', lambda e: e.copy(out=psb, in_=pb[:, 0:n]), reads=[pk], writes=[('tb', 1)])
            mb, mk = psa()
            S.op('pe', lambda e: e.matmul(mb[:, 0:n], lhsT=onesblk[:], rhs=sq, start=True, stop=True), reads=[('tb', 0), 'onesblk'], writes=[mk])
            rb, rk = psa()
            S.op('pe', lambda e: e.matmul(rb[:, 0:n], lhsT=Rg[:, v, :], rhs=psb, start=True, stop=True), reads=[('tb', 1), 'Rg'], writes=[rk])
            S.op('act', lambda e: e.activation(out=rstd, in_=mb[:, 0:n], func=AF.Sqrt, bias=small[:, 63:64], scale=1.0), reads=[mk, 'eps'], writes=[('tf', 0)])
            S.op('dve', lambda e: e.reciprocal(out=rstd, in_=rstd), reads=[('tf', 0)], writes=[('tf', 0)])
            S.op('dve', lambda e: e.scalar_tensor_tensor(out=vw(t1), in0=vw(pb[:, 0:n]), scalar=gvec[:, gcol:gcol + 1], in1=Ct, op0=ALU.mult, op1=ALU.mult),
                 reads=[pk, 'gvec'] + cskeys, writes=[('tf', 1)])
            S.op('dve', lambda e: e.tensor_tensor(out=vw(t2), in0=vw(rb[:, 0:n]), in1=St, op=ALU.mult), reads=[rk] + cskeys, writes=[('tf', 2)])
            S.op('pool', lambda e: e.tensor_tensor(out=t1, in0=t1, in1=t2, op=ALU.add), reads=[('tf', 1), ('tf', 2)], writes=[('tf', 1)])
            S.op('pool', lambda e: e.tensor_tensor(out=out_ap, in0=t1, in1=rstd, op=ALU.mult), reads=[('tf', 1), ('tf', 0)], writes=okeys)

        S.op('dve', lambda e: e.memset(small[:, 63:64], 1e-6), writes=['eps'])

        def make_hT():
            for qi in range(4):
                S.op('act', lambda e, qi=qi: e.activation(out=QO[:, 8:12].rearrange("p a f -> p (a f)"), in_=X1[:, qi, :], func=AF.Square, accum_out=ssq[:, qi:qi + 1]),
                     reads=[('X1', qi)], writes=[('QO', 1), ('ssq', qi)])
                chk('h1')
                S.op('dve', lambda e, qi=qi: e.tensor_scalar(out=ssq[:, 4 + qi:5 + qi], in0=ssq[:, qi:qi + 1], scalar1=1.0 / D, scalar2=1e-6, op0=ALU.mult, op1=ALU.add),
                     reads=[('ssq', qi)], writes=[('ssq2', qi)])
                S.op('dve', lambda e, qi=qi: e.memset(ssq[:, qi:qi + 1], 0.0), reads=[('ssq2', qi)], writes=[('ssq', qi)])
                S.op('act', lambda e, qi=qi: e.activation(out=ssq[:, 4 + qi:5 + qi], in_=ssq[:, 4 + qi:5 + qi], func=AF.Sqrt), reads=[('ssq2', qi)], writes=[('ssq2', qi)])
                S.op('dve', lambda e, qi=qi: e.reciprocal(out=ssq[:, 4 + qi:5 + qi], in_=ssq[:, 4 + qi:5 + qi]), reads=[('ssq2', qi)], writes=[('ssq2', qi)])
                S.op('dve', lambda e, qi=qi: e.tensor_scalar_mul(out=XNB, in0=X1[:, qi, :], scalar1=ssq[:, 4 + qi:5 + qi]),
                     reads=[('X1', qi), ('ssq2', qi)], writes=XK)
                chk('h3')
                for k4 in range(4):
                    ti = nxt('pst', 1)
                    for kk in range(4):
                        kc = k4 * 4 + kk
                        S.op('pe', lambda e, kc=kc, kk=kk, ti=ti: e.transpose(out=PST[:, ti, kk * 128:(kk + 1) * 128], in_=XNB[:, kc * 128:(kc + 1) * 128], identity=ident[:]),
                             reads=XK + ['ident'], writes=[('pst', ti)])
                    chk('h4')
                    eng = 'act'
                    o = hT[:, k4 * 4:k4 * 4 + 4, qi * 128:(qi + 1) * 128]
                    ii = PST[:, ti, :].rearrange("p (a b) -> p a b", a=4)
                    if eng == 'act':
                        S.op('act', lambda e, o=o, ii=ii: e.copy(out=o, in_=ii), reads=[('pst', ti)], writes=[('hT', qi)])
                    else:
                        S.op('dve', lambda e, o=o, ii=ii: e.tensor_copy(out=o, in_=ii), reads=[('pst', ti)], writes=[('hT', qi)])
                    chk('h5' if k4 == 0 else ('h6' if k4 == 1 else 'h7'))

        HK = [('hT', qi) for qi in range(4)]
        YK = [('yT', qi) for qi in range(4)]

        def load_x(s, t0):
            for qi in range(4):
                S.dma('sp', 'x%d' % qi, lambda e, qi=qi: e.dma_start(out=X1[:, qi, :], in_=x[s, t0 + qi * 128:t0 + (qi + 1) * 128, :]), writes=[('X1', qi)])

        def dump(name, ap_sb, shape, keys, dt=F32):
            if not dbg:
                return
            if name not in dbg_out:
                dbg_out[name] = nc.dram_tensor("dbg_" + name, shape, dt, kind="ExternalOutput").ap()
            S.dma('sp', 'dbg', lambda e: e.dma_start(out=dbg_out[name], in_=ap_sb), reads=keys, writes=['dbg_' + name])

        for s in range(nseq):
            S.op('pool', lambda e: e.memset(carry[:].rearrange("p a b -> p (a b)"), 0.0), writes=['carry'])
            for tb in range(NQB):
                t0 = tb * TB
                load_x(s, t0)
                chk('p1x')
                make_hT()
                chk('p1a')
                rope_tables(s, t0)
                chk('p1b')
                for ci in range(2):
                    m0 = 1 if tb == 0 else 0
                    S.op('pool', lambda e, ci=ci, m0=m0, tb=tb: e.tensor_copy(out=Cc[:, ci, 0, 32 * tb - 1 + m0:32 * tb + 31], in_=CSt[:, ci, 15 + 16 * m0:512:16]),
                         reads=['CSt'], writes=CCK)
                if dbg and s == 0 and tb == 0:
                    dump('hT', hT[:], [128, 16, 512], HK, BF16)
                    dump('CS', CSt[:], [128, 2, 512], ['CSt'])
                chk('p1c')
                rawv = yT[:].rearrange("p a f -> p (a f)").rearrange("p (c t) -> p c t", c=4)
                for pi in range(8, 12):
                    pb, pk = mmA(WinP[pi], hT, HK)
                    o = rawv[:, pi - 8, t0:t0 + 512]
                    S.op('act', lambda e, o=o, pb=pb: e.copy(out=o, in_=pb[:]), reads=[pk], writes=YK + ['raw'])
                chk('p1d')
                for br in range(2):
                    for j in range(2):
                        pb, pk = mmA(WinP[12 + br * 2 + j], hT, HK)
                        normrope(pb, pk, 34 + br, CSt[:, 0, :], CSt[:, 1, :], ['CSt'], KT[:, br, j, t0:t0 + 512], ['KT'])
                chk('p1e')
                for br in range(2):
                    wb_ap, wb_key = load_wb(WVS[br])
                    wv = wb_ap.rearrange("p (k c) -> p k c", c=256)
                    for qi in range(4):
                        pb, pk = psa()
                        for kc in range(16):
                            S.op('pe', lambda e, kc=kc, qi=qi, pb=pb, wv=wv: e.matmul(pb[:, 0:256], lhsT=hT[:, kc, qi * 128:(qi + 1) * 128], rhs=wv[:, kc, :], start=(kc == 0), stop=(kc == 15)),
                                 reads=[wb_key, ('hT', qi)], writes=[pk])
                        o = VA[:, br, tb * 4 + qi, :].rearrange("p (g c) -> p g c", c=65)[:, :, 0:64]
                        ii = pb[:, 0:256].rearrange("p (g c) -> p g c", c=64)
                        S.op('dve', lambda e, o=o, ii=ii: e.tensor_copy(out=o, in_=ii), reads=[pk], writes=['VA'])
            chk('pass1')
            for ci in range(2):
                for g in range(1, 4):
                    S.op('pool', lambda e, ci=ci, g=g: e.tensor_copy(out=Cc[:, ci, g, 0:127], in_=Cc[:, ci, 0, 0:127]), reads=['Cc'], writes=CCK)
            rawv = yT[:].rearrange("p a f -> p (a f)").rearrange("p (c t) -> p c t", c=4)
            for kv in range(2):
                wis = [load_wb(W1S[kv * 2 + lh]) for lh in range(2)]
                w1v = [wa.rearrange("p (l h) -> p l h", h=256) for (wa, _) in wis]
                wkeys = [wk_ for (_, wk_) in wis]
                pb, pk = psa()
                for hc in range(2):
                    for l in range(32):
                        S.op('pe', lambda e, hc=hc, l=l, pb=pb: e.matmul(pb[:, hc:hc + 1], lhsT=w1v[l // 16][0:64, l % 16, hc * 128:(hc + 1) * 128], rhs=peT[0:64, kv, l:l + 1], start=(l == 0), stop=(l == 31)),
                             reads=wkeys + ['smallres'], writes=[pk])
                S.op('dve', lambda e, pb=pb, kv=kv: e.tensor_copy(out=bh[:, kv * 2:kv * 2 + 2], in_=pb[:, 0:2]), reads=[pk], writes=['bh'])
                for g in range(4):
                    base = (g % 2) * 64
                    for hc in range(2):
                        pb, pk = psa()
                        for l in range(32):
                            S.op('pe', lambda e, hc=hc, l=l, pb=pb, base=base, g=g: e.matmul(
                                pb[:, 0:127], lhsT=w1v[l // 16][base:base + 64, l % 16, hc * 128:(hc + 1) * 128],
                                rhs=rawv[base:base + 64, kv * 2 + g // 2, l:l + 16 * 126 + 1:16], start=(l == 0), stop=(l == 31)),
                                reads=wkeys + ['raw'], writes=[pk])
                        S.op('act', lambda e, pb=pb, hc=hc, g=g, kv=kv: e.activation(out=hidT[:, kv, hc, g * 127:(g + 1) * 127], in_=pb[:, 0:127], func=AF.Silu, bias=bh[:, kv * 2 + hc:kv * 2 + hc + 1], scale=1.0),
                             reads=[pk, 'bh'], writes=[('QO', 0)])
            pb, pk = psa()
            for hc in range(2):
                S.op('pe', lambda e, hc=hc, pb=pb: e.matmul(pb[:, 0:508], lhsT=W2k[:, hc, :], rhs=hidT[:, 0, hc, 0:508], start=(hc == 0), stop=(hc == 1)),
                     reads=[('QO', 0), 'smallres'], writes=[pk])
            normrope(pb, pk, 33, Cc[:, 0, :, 0:127], Cc[:, 1, :, 0:127], ['Cc'], kcT[:, 0:508], ['kcT'], n=508,
                     view=lambda a_: a_.rearrange("p (g n) -> p g n", g=4))
            pb, pk = psa()
            for g in range(4):
                for hc in range(2):
                    S.op('pe', lambda e, hc=hc, g=g, pb=pb: e.matmul(pb[0:127, g * 64:(g + 1) * 64], lhsT=hidT[:, 1, hc, g * 127:(g + 1) * 127], rhs=W2v[:, hc, :], start=(hc == 0), stop=(hc == 1)),
                         reads=[('QO', 0), 'smallres'], writes=[pk])
            S.op('dve', lambda e, pb=pb: e.tensor_copy(out=vcA[0:127, :, 0:64], in_=pb[0:127, 0:256].rearrange("p (g c) -> p g c", g=4)), reads=[pk], writes=['vcA'])
            if dbg and s == 0:
                dump('KT', KT[:], [128, 2, 2, SEQ], ['KT'], BF16)
                dump('VA', VA[:], [128, 2, 16, 260], ['VA'], BF16)
                dump('kcT', kcT[:], [128, 512], ['kcT'], BF16)
                dump('vcA', vcA[:], [128, 4, 97], ['vcA'], BF16)
                dump('hidT', hidT, [128, 2, 2, 512], [('QO', 0)], BF16)

            chk('compress')
            qT = QO[:, 0:8]
            OT = QO[:, 8:16]
            for qb in range(nqb):
                t0 = qb * TB
                first = (s == 0 and qb == 0)
                load_x(s, t0)
                S.dma('pool', 'mk', lambda e, t0=t0: e.dma_start(out=maskc[:], in_=C['c_maskc'][:, t0:t0 + 512]), writes=['maskc'])
                S.dma('sp', 'bon', lambda e, qb=qb: e.dma_start(out=bonus[:], in_=C['c_bonus'][:, qb * 4:qb * 4 + 4, :]), writes=['bonus'])
                chk('blk')
                make_hT()
                chk('mkh')
                rope_tables(s, t0)
                for j in range(8):
                    pb, pk = mmA(WinP[j], hT, HK)
                    normrope(pb, pk, 32, CSt[:, 0, :], CSt[:, 1, :], ['CSt'], qT[:, j, :], [('QO', 0)])
                for qi in range(4):
                    pb, pk = psa()
                    for kc in range(16):
                        S.op('pe', lambda e, kc=kc, qi=qi, pb=pb: e.matmul(pb[:, 0:48], lhsT=hT[:, kc, qi * 128:(qi + 1) * 128], rhs=WG[:, kc, :], start=(kc == 0), stop=(kc == 15)),
                             reads=['WG', ('hT', qi)], writes=[pk])
                    S.op('act', lambda e, qi=qi, pb=pb: e.activation(out=gates[:, qi, :], in_=pb[:, 0:48], func=AF.Sigmoid), reads=[pk], writes=['gates'])
                if dbg and first:
                    dump('qT', QO[:, 0:8], [128, 8, 512], [('QO', 0)], BF16)
                    dump('gates', gates[:], [128, 4, 48], ['gates'])

                chk('qproj')
                def finalize(pso, pok, h, br, pi, hp, first_write, with_imp):
                    rs4 = small[:, 0:4]
                    fac = small[:, 4:8]
                    S.op('dve', lambda e: e.tensor_scalar_max(out=rs4, in0=pso[:, :, 64], scalar1=1e-30), reads=[pok], writes=['rs4'])
                    S.op('dve', lambda e: e.reciprocal(out=rs4, in_=rs4), reads=['rs4'], writes=['rs4'])
                    S.op('dve', lambda e: e.tensor_tensor(out=fac, in0=rs4, in1=gates[:, :, 3 * h + br], op=ALU.mult), reads=['rs4', 'gates'], writes=['fac'])
                    fb = fac.unsqueeze(2).to_broadcast([128, 4, 64])
                    od = Otot[:, pi, :, hp * 64:(hp + 1) * 64]
                    if first_write:
                        S.op('dve', lambda e: e.tensor_tensor(out=od, in0=pso[:, :, 0:64], in1=fb, op=ALU.mult), reads=[pok, 'fac'], writes=[('otot', pi), 'Cc'])
                    else:
                        S.op('dve', lambda e: e.tensor_tensor(out=tmpo, in0=pso[:, :, 0:64], in1=fb, op=ALU.mult), reads=[pok, 'fac'], writes=[('tf', 3)])
                        S.op('pool', lambda e: e.tensor_tensor(out=od, in0=od, in1=tmpo, op=ALU.add), reads=[('tf', 3), ('otot', pi)], writes=[('otot', pi)])
                    if with_imp is not None:
                        rb_ = rs4.unsqueeze(2).to_broadcast([128, 4, 32])
                        if with_imp == 0:
                            S.op('dve', lambda e: e.tensor_tensor(out=imp, in0=pso[:, :, 65:97], in1=rb_, op=ALU.mult), reads=[pok, 'rs4'], writes=[('tf', 2)])
                        else:
                            S.op('dve', lambda e: e.tensor_tensor(out=tmpi, in0=pso[:, :, 65:97], in1=rb_, op=ALU.mult), reads=[pok, 'rs4'], writes=[('tf', 3)])
                            S.op('pool', lambda e: e.tensor_tensor(out=imp, in0=imp, in1=tmpi, op=ALU.add), reads=[('tf', 3), ('tf', 2)], writes=[('tf', 2)])

                for g in range(G):
                    base = (g % 2) * 64
                    heads = [4 * g + r for r in range(4)]

                    def qsl(h):
                        m_, rem = h // 8, h % 8
                        return 4 * m_ + rem % 4
                    SCB = [(PSS[0], ('pss', 0)), (PSS[1], ('pss', 1)), (PSA[0], ('psa', 0)), (PSA[1], ('psa', 1)), (PSA[2], ('psa', 2))]
                    PTB = [(BFW[:, i, :], k_) for i, k_ in enumerate([('pt', 0), ('pt', 1), ('pt', 2), ('tb', 0), ('tb', 1)])]

                    def sc_next():
                        i = nxt('pss', 5)
                        return SCB[i]

                    def pt_next():
                        i = nxt('pt', 5)
                        return PTB[i]
                    cs = []
                    for r, h in enumerate(heads):
                        jq = qsl(h)
                        sb_, sk_ = sc_next()
                        S.op('pe', lambda e: e.matmul(sb_[0:127, :], lhsT=kcT[base:base + 64, g * 127:(g + 1) * 127], rhs=qT[base:base + 64, jq, :], start=True, stop=True),
                             reads=['kcT', ('QO', 0)], writes=[sk_])
                        cs.append((sb_, sk_))
                    for r, h in enumerate(heads):
                        sb_, sk_ = cs[r]
                        pb_, pk_ = pt_next()
                        S.op('act', lambda e: e.activation(out=pb_[0:127, :], in_=sb_[0:127, :], func=AF.Exp, scale=0.125), reads=[sk_], writes=[pk_])
                        S.op('pool', lambda e: e.tensor_tensor(out=pb_[0:127, :], in0=pb_[0:127, :], in1=maskc[0:127, :], op=ALU.mult),
                             reads=[pk_, 'maskc'], writes=[pk_])
                        oi = nxt('pso', 2)
                        for qi in range(4):
                            S.op('pe', lambda e: e.matmul(PSO[oi][:, qi, 0:97], lhsT=pb_[0:127, qi * 128:(qi + 1) * 128], rhs=vcA[0:127, g, :], start=True, stop=True),
                                 reads=[pk_, 'vcA'], writes=[('pso', oi)])
                        finalize(PSO[oi], ('pso', oi), h, 0, r // 2, r % 2, True, (r if qb >= 2 else None))
                    def emit_selection_dve(g=g):
                        S.op('dve', lambda e: e.tensor_tensor(out=scb, in0=imp, in1=bonus[:, :, :], op=ALU.add), reads=[('tf', 2), 'bonus'], writes=[('tf', 2)])
                        for qi in range(4):
                            S.op('dve', lambda e, qi=qi: e.max(out=top8[:, 0:8], in_=scb[:, qi, :]), reads=[('tf', 2)], writes=['top8'])
                            S.op('dve', lambda e, qi=qi: e.match_replace(out=sc2[:], in_to_replace=top8[:, 0:8], in_values=scb[:, qi, :], imm_value=-1e30), reads=[('tf', 2), 'top8'], writes=['sc2'])
                            S.op('dve', lambda e: e.max(out=top8[:, 8:16], in_=sc2[:]), reads=['sc2'], writes=['top8'])
                            S.op('dve', lambda e, qi=qi: e.tensor_scalar(out=sc2[:], in0=scb[:, qi, :], scalar1=top8[:, 15:16], scalar2=None, op0=ALU.is_ge), reads=[('tf', 2), 'top8'], writes=['sc2'])
                            S.op('dve', lambda e, qi=qi: e.tensor_scalar(out=selb[:, qi, :], in0=sc2[:], scalar1=-1.0, scalar2=30000.0, op0=ALU.add, op1=ALU.mult), reads=['sc2'], writes=['selb'])

                    def emit_selection_pe(g=g):
                        ti = nxt('pst', 1)
                        for qi in range(4):
                            S.op('pe', lambda e, qi=qi, ti=ti: e.transpose(out=PST[0:32, ti, qi * 128:(qi + 1) * 128], in_=selb[:, qi, :], identity=ident[:]),
                                 reads=['selb', 'ident'], writes=[('pst', ti)])
                        S.op('dve', lambda e, ti=ti: e.tensor_copy(out=selbT[0:32, :], in_=PST[0:32, ti, :]), reads=[('pst', ti)], writes=['selbT'])
                        if dbg and s == 0 and qb == 2 and g == 0:
                            dump('selb', selb[:], [128, 4, 32], ['selb'], BF16)
                            dump('imp', imp, [128, 4, 32], ['imp'])
                    if qb >= 2:
                        emit_selection_dve()
                    steps = []
                    qp_rr = [0]
                    cb_after = {}
                    order = [(0, 2), (1, 2), (0, 1), (1, 1), (2, 2), (3, 2), (2, 1), (3, 1)]
                    for ui, (r, br) in enumerate(order):
                        h = heads[r]
                        kts = list(range(0, 4 * qb + 4)) if br == 1 else list(range(max(4 * qb - 4, 0), 4 * qb + 4))
                        unit = {'h': h, 'r': r, 'br': br, 'oi': None, 'qp': None, 'newhead': br == 2}
                        for kt in kts:
                            steps.append({'u': unit, 'kt': kt, 'first': kt == kts[0], 'last': kt == kts[-1]})
                        if ui == 2 and qb >= 2:
                            steps[len(steps) - len(kts)]['pre'] = emit_selection_pe

                    hq = {}

                    def emit_score(st):
                        u, kt = st['u'], st['kt']
                        br, jq = u['br'], qsl(u['h'])
                        dloc = kt - 4 * qb
                        lo = max(dloc, 0)
                        hi = 3 if br == 1 else min(dloc + 4, 3)
                        c0 = lo * 128
                        n = (hi - lo + 1) * 128
                        sb_, sk_ = sc_next()
                        use_mask = (br == 1 and qb >= 2)
                        if st['first'] and u['newhead']:
                            qs = (g % 2) * 2 + (qp_rr[0] % 2)
                            qp_rr[0] += 1
                            hq[u['h']] = qs
                            S.op('dve', lambda e: e.tensor_copy(out=QP[base:base + 64, qs, :], in_=qT[base:base + 64, jq, :]), reads=[('QO', 0)], writes=[('qp', qs)])
                        qs = hq[u['h']]
                        S.op('pe', lambda e: e.matmul(
                            sb_[:, 0:n], lhsT=KT[:, br - 1, g // 2, kt * 128:(kt + 1) * 128], rhs=QP[:, qs, c0:c0 + n], start=True, stop=(not use_mask)),
                            reads=['KT', ('qp', qs)], writes=[sk_])
                        if use_mask:
                            S.op('pe', lambda e: e.matmul(sb_[:, 0:n], lhsT=emat[:, kt, :], rhs=selbT[:, c0:c0 + n], start=False, stop=True),
                                 reads=['emat', 'selbT'], writes=[sk_])
                        st['ctx'] = (dloc, lo, hi, c0, n, sb_, sk_)

                    def emit_rest(st):
                        u, kt = st['u'], st['kt']
                        br = u['br']
                        dloc, lo, hi, c0, n, sb_, sk_ = st['ctx']
                        if st['first']:
                            u['oi'] = nxt('pso', 2)
                            oi0 = u['oi']
                            S.op('pe', lambda e: e.matmul(PSO[oi0][:, :, :], lhsT=zeros128[:], rhs=emat[:, 0:4, :], start=True, stop=False),
                                 reads=['emat', 'zeros128'], writes=[('pso', oi0)])
                        oi = u['oi']
                        pb_, pk_ = pt_next()
                        S.op('act', lambda e: e.activation(out=pb_[:, 0:n], in_=sb_[:, 0:n], func=AF.Exp, scale=0.125), reads=[sk_], writes=[pk_])
                        if dloc >= 0:
                            S.op('pool', lambda e: e.tensor_tensor(out=pb_[:, 0:128], in0=pb_[:, 0:128], in1=tri[:, 0, :], op=ALU.mult),
                                 reads=[pk_, 'tri'], writes=[pk_])
                        if br == 2 and 0 <= dloc + 4 <= 3:
                            cf = (hi - lo) * 128
                            S.op('pool', lambda e: e.tensor_tensor(out=pb_[:, cf:cf + 128], in0=pb_[:, cf:cf + 128], in1=tri[:, 1, :], op=ALU.mult),
                                 reads=[pk_, 'tri'], writes=[pk_])
                        for qi in range(lo, hi + 1):
                            sp = bool(st['last'] and qi == hi)
                            S.op('pe', lambda e: e.matmul(
                                PSO[oi][:, qi, 0:65], lhsT=pb_[:, (qi - lo) * 128:(qi - lo + 1) * 128], rhs=VA[:, br - 1, kt, g * 65:(g + 1) * 65], start=False, stop=sp),
                                reads=[pk_, 'VA'], writes=[('pso', oi)])
                        if st['last']:
                            finalize(PSO[oi], ('pso', oi), u['h'], br, u['r'] // 2, u['r'] % 2, False, None)

                    LA = 4
                    n_sc = 0
                    for i_st in range(len(steps)):
                        while n_sc < min(len(steps), i_st + 1 + LA):
                            if 'pre' in steps[n_sc]:
                                steps[n_sc]['pre']()
                            emit_score(steps[n_sc])
                            n_sc += 1
                        emit_rest(steps[i_st])
                        if i_st in cb_after:
                            cb_after[i_st]()
                    for pi in range(2):
                        S.op('act', lambda e, pi=pi: e.copy(out=Obf[:, pi], in_=Otot[:, pi]), reads=[('otot', pi)], writes=[('obf', pi)])
                        ti = nxt('pst', 1)
                        for qi in range(4):
                            S.op('pe', lambda e, pi=pi, qi=qi, ti=ti: e.transpose(out=PST[:, ti, qi * 128:(qi + 1) * 128], in_=Obf[:, pi, qi, :], identity=ident[:]),
                                 reads=[('obf', pi), 'ident'], writes=[('pst', ti)])
                        S.op('act', lambda e, pi=pi, ti=ti, g=g: e.copy(out=OT[:, 2 * g + pi, :], in_=PST[:, ti, :]), reads=[('pst', ti)], writes=[('QO', 1)])
                if dbg and first:
                    dump('OT', QO[:, 8:16], [128, 8, 512], [('QO', 1)], BF16)

                chk('attn')
                QOf = QO[:].rearrange("p a f -> p (a f)")
                BFf = BFW[:].rearrange("p a f -> p (a f)")
                ex_y = [(QOf[:, i * 2048:(i + 1) * 2048].rearrange("p (k c) -> p k c", c=128), ('QOs', i), 'pxq%d' % i) for i in range(2)]
                ex_y.append((BFf[:, 0:2048].rearrange("p (k c) -> p k c", c=128), ('BFs', 0), 'pxb0'))
                S.alias_in([('QOs', 0), ('QOs', 1)], [('QO', 0)])
                S.alias_in([('BFs', 0)], XK + [('tb', 1)])
                set_pan_slots(ex_y)
                def pool_group(gi):
                    w = (2, 4, 8, 16)[gi]
                    for j in range(2):
                        pb, pk = mmA(WinP[16 + gi * 2 + j], hT, HK)
                        U = ubuf[:, 0, :]
                        S.op('pool', lambda e, gi=gi, j=j: e.tensor_copy(out=ubuf[:, 0, 0:16], in_=carry[:, gi * 2 + j, :]), reads=['carry'], writes=['ub0'])
                        S.op('act', lambda e, pb=pb: e.copy(out=ubuf[:, 0, 16:528], in_=pb[:]), reads=[pk], writes=['ub0'])
                        S.op('pool', lambda e, gi=gi, j=j: e.tensor_copy(out=carry[:, gi * 2 + j, :], in_=ubuf[:, 0, 512:528]), reads=['ub0'], writes=['carry'])
                        TFf = TF[:].rearrange("p a f -> p (a f)")
                        ubs = [ubuf[:, 0, :], TFf[:, 0:528], TFf[:, 528:1056]]
                        ubk = [['ub0'], [('tf', 0), ('tf', 1)], [('tf', 1), ('tf', 2)]]
                        cur = 0
                        for kstep in range(gi + 1):
                            sh = 1 << kstep
                            nx_ = 1 if cur != 1 else 2
                            S.op('pool', lambda e, cur=cur, nx_=nx_, sh=sh, ubs=ubs: e.tensor_tensor(out=ubs[nx_][:, sh:528], in0=ubs[cur][:, sh:528], in1=ubs[cur][:, 0:528 - sh], op=ALU.add),
                                 reads=ubk[cur], writes=ubk[nx_])
                            cur = nx_
                        S.op('dve', lambda e, cur=cur, j=j, w=w, ubs=ubs: e.scalar_tensor_tensor(out=PLT[:, gi % 2, j, :], in0=ubs[cur][:, 16:528], scalar=1.0 / w, in1=ubuf[:, 0, 16:528], op0=ALU.mult, op1=ALU.subtract),
                             reads=ubk[cur] + ['ub0'], writes=[('PLT', gi % 2)])
                        if qb == 0:
                            S.op('dve', lambda e, cur=cur, gi=gi, ubs=ubs: e.tensor_tensor(out=small[:, 8:24], in0=ubs[cur][:, 16:32], in1=icnt[:, gi, :], op=ALU.mult), reads=ubk[cur] + ['icnt'], writes=['t16'])
                            S.op('dve', lambda e, j=j: e.tensor_tensor(out=PLT[:, gi % 2, j, 0:16], in0=small[:, 8:24], in1=ubuf[:, 0, 16:32], op=ALU.subtract), reads=['t16', 'ub0', ('PLT', gi % 2)], writes=[('PLT', gi % 2)])
                pool_group(0)
                for c in range(16):
                    gi = c // 4
                    pa, pak = psa()
                    cb = (c % 2) * 64
                    S.op('pe', lambda e, pa=pa, c=c, cb=cb: e.matmul(pa[:], lhsT=Who[cb:cb + 64, c // 2, :], rhs=OT[cb:cb + 64, c // 2, :], start=True, stop=True),
                         reads=['smallres', ('QO', 1)], writes=[pak])
                    pm, pmk = mmA(WinP[24 + c], hT, HK)
                    S.op('act', lambda e, pm=pm: e.activation(out=TF[:, 0, :], in_=pm[:], func=AF.Sigmoid), reads=[pmk], writes=[('tf', 0)])
                    S.op('dve', lambda e, pa=pa: e.tensor_tensor(out=TF[:, 1, :], in0=pa[:], in1=TF[:, 0, :], op=ALU.mult), reads=[pak, ('tf', 0)], writes=[('tf', 1)])
                    pp, ppk = psa()
                    for j in range(2):
                        S.op('pe', lambda e, pp=pp, j=j, c=c, gi=gi: e.matmul(pp[:], lhsT=Wpool[:, gi * 2 + j, (c % 4) * 128:(c % 4 + 1) * 128], rhs=PLT[:, gi % 2, j, :], start=(j == 0), stop=(j == 1)),
                             reads=['Wpool', ('PLT', gi % 2)], writes=[ppk])
                    pm1, pm1k = mmA(WinP[40 + c], hT, HK)
                    S.op('act', lambda e, pm1=pm1: e.activation(out=TF[:, 2, :], in_=pm1[:], func=AF.Sigmoid), reads=[pm1k], writes=[('tf', 2)])
                    S.op('dve', lambda e, pp=pp, c=c: e.scalar_tensor_tensor(out=TF[:, 3, :], in0=pp[:], scalar=pscale[:, c:c + 1], in1=TF[:, 2, :], op0=ALU.mult, op1=ALU.mult),
                         reads=[ppk, ('tf', 2), 'pscale'], writes=[('tf', 3)])
                    S.op('pool', lambda e, c=c: e.tensor_tensor(out=yT[:, c, :], in0=TF[:, 1, :], in1=TF[:, 3, :], op=ALU.add), reads=[('tf', 1), ('tf', 3)], writes=YK + ['raw'])
                    if c % 4 == 0 and gi < 3:
                        pool_group(gi + 1)
                if dbg and first:
                    dump('yT', yT[:], [128, 16, 512], YK, BF16)

                chk('ymix')
                S.alias_out([('QOs', 0), ('QOs', 1)], [('QO', 0)])
                S.alias_out([('BFs', 0)], XK + [('tb', 1)])
                set_pan_slots([])
                S.alias_in([('QOw', 0)], [('QO', 0)])
                set_wb_slots([(QO[:, 0:8].rearrange("p a f -> p (a f)"), ('QOw', 0), 'wbx0')])
                for c8 in range(8):
                    wb_ap, wb_key = load_wb(WoS[c8])
                    wv = wb_ap.rearrange("p (k c) -> p k c", c=256)
                    for qi in range(4):
                        pb, pk = psa()
                        for kc in range(16):
                            S.op('pe', lambda e, kc=kc, qi=qi, pb=pb, wv=wv: e.matmul(pb[:, 0:256], lhsT=yT[:, kc, qi * 128:(qi + 1) * 128], rhs=wv[:, kc, :], start=(kc == 0), stop=(kc == 15)),
                                 reads=[wb_key, ('yT', qi)], writes=[pk])
                        xs = X1[:, qi, c8 * 256:(c8 + 1) * 256]
                        S.op('dve', lambda e, xs=xs, pb=pb: e.tensor_tensor(out=xs, in0=pb[:, 0:256], in1=xs, op=ALU.add), reads=[pk, ('X1', qi)], writes=[('X1', qi)])
                S.alias_out([('QOw', 0)], [('QO', 0)])
                set_wb_slots([])
                if dbg and first:
                    dump('x1', X1[:], [128, 4, 2048], [('X1', qi) for qi in range(4)])

                chk('wout')
                make_hT()
                chk('mkh2')
                pb, pk = psa()
                for qi in range(4):
                    for kc in range(16):
                        S.op('pe', lambda e, kc=kc, qi=qi: e.matmul(pb[:, qi * 20:(qi + 1) * 20], lhsT=hT[:, kc, qi * 128:(qi + 1) * 128], rhs=Wr[:, kc, :], start=(kc == 0), stop=(kc == 15)),
                             reads=['Wr', ('hT', qi)], writes=[pk])
                R = TF[:, 3, :]
                RK = [('tf', 3)]
                v3 = lambda ap_, a_: ap_.rearrange("p (a b) -> p a b", a=a_)
                lgg, le = R[:, 0:16], R[:, 16:80]
                mx, se, pg = R[:, 80:84], R[:, 84:88], R[:, 88:92]
                ex, gm, pen = R[:, 96:112], R[:, 112:128], R[:, 128:144]
                lm = R[:, 144:208]
                t8 = R[:, 208:240]
                dv, e2, den, w1, w2 = R[:, 240:244], R[:, 244:248], R[:, 248:252], R[:, 252:256], R[:, 256:260]
                m1, m2 = R[:, 272:336], R[:, 336:400]
                pb3 = v3(pb[:, 0:80], 4)
                bc = lambda ap_, n_: ap_.unsqueeze(2).to_broadcast([128, ap_.shape[1], n_])
                S.op('dve', lambda e: e.tensor_tensor(out=v3(lgg, 4), in0=pb3[:, :, 0:4], in1=brt[:, 0:4].unsqueeze(1).to_broadcast([128, 4, 4]), op=ALU.add), reads=[pk, 'brt'], writes=RK)
                S.op('dve', lambda e: e.tensor_tensor(out=v3(le, 4), in0=pb3[:, :, 4:20], in1=brt[:, 4:20].unsqueeze(1).to_broadcast([128, 4, 16]), op=ALU.add), reads=[pk, 'brt'], writes=RK)
                S.op('dve', lambda e: e.tensor_reduce(out=mx, in_=v3(lgg, 4), axis=AX.X, op=ALU.max), reads=RK, writes=RK)
                S.op('dve', lambda e: e.tensor_tensor(out=v3(ex, 4), in0=v3(lgg, 4), in1=bc(mx, 4), op=ALU.subtract), reads=RK, writes=RK)
                S.op('act', lambda e: e.activation(out=ex, in_=ex, func=AF.Exp), reads=RK, writes=RK)
                S.op('dve', lambda e: e.tensor_reduce(out=se, in_=v3(ex, 4), axis=AX.X, op=ALU.add), reads=RK, writes=RK)
                S.op('dve', lambda e: e.reciprocal(out=pg, in_=se), reads=RK, writes=RK)
                S.op('dve', lambda e: e.tensor_tensor(out=v3(gm, 4), in0=v3(lgg, 4), in1=bc(mx, 4), op=ALU.is_ge), reads=RK, writes=RK)
                S.op('dve', lambda e: e.tensor_scalar(out=pen, in0=gm, scalar1=-1.0, scalar2=1e30, op0=ALU.add, op1=ALU.mult), reads=RK, writes=RK)
                S.op('dve', lambda e: e.tensor_tensor(out=v3(lm, 16), in0=v3(le, 16), in1=bc(pen, 4), op=ALU.add), reads=RK, writes=RK)
                for qi in range(4):
                    S.op('dve', lambda e, qi=qi: e.max(out=t8[:, qi * 8:(qi + 1) * 8], in_=lm[:, qi * 16:(qi + 1) * 16]), reads=RK, writes=RK)
                t83 = v3(t8, 4)
                S.op('dve', lambda e: e.tensor_tensor(out=dv, in0=t83[:, :, 1], in1=t83[:, :, 0], op=ALU.subtract), reads=RK, writes=RK)
                S.op('act', lambda e: e.activation(out=e2, in_=dv, func=AF.Exp), reads=RK, writes=RK)
                S.op('dve', lambda e: e.tensor_scalar_add(out=den, in0=e2, scalar1=1.0), reads=RK, writes=RK)
                S.op('dve', lambda e: e.reciprocal(out=den, in_=den), reads=RK, writes=RK)
                S.op('dve', lambda e: e.tensor_tensor(out=w1, in0=pg, in1=den, op=ALU.mult), reads=RK, writes=RK)
                S.op('dve', lambda e: e.tensor_tensor(out=w2, in0=w1, in1=e2, op=ALU.mult), reads=RK, writes=RK)
                S.op('dve', lambda e: e.tensor_tensor(out=v3(m1, 4), in0=v3(lm, 4), in1=bc(t83[:, :, 0], 16), op=ALU.is_equal), reads=RK, writes=RK)
                S.op('dve', lambda e: e.tensor_tensor(out=v3(m1, 4), in0=v3(m1, 4), in1=bc(w1, 16), op=ALU.mult), reads=RK, writes=RK)
                S.op('dve', lambda e: e.tensor_tensor(out=v3(m2, 4), in0=v3(lm, 4), in1=bc(t83[:, :, 1], 16), op=ALU.is_equal), reads=RK, writes=RK)
                S.op('dve', lambda e: e.tensor_tensor(out=v3(m2, 4), in0=v3(m2, 4), in1=bc(w2, 16), op=ALU.mult), reads=RK, writes=RK)
                S.op('dve', lambda e: e.tensor_tensor(out=comb[:].rearrange("p a b -> p (a b)"), in0=m1, in1=m2, op=ALU.add), reads=RK, writes=['comb'])
                S.op('dve', lambda e: e.tensor_copy(out=combb[:], in_=comb[:]), reads=['comb'], writes=['combb'])
                ti = nxt('pst', 1)
                for qi in range(4):
                    S.op('pe', lambda e, qi=qi, ti=ti: e.transpose(out=PST[0:16, ti, qi * 128:(qi + 1) * 128], in_=combb[:, qi, :], identity=ident[:]), reads=['combb', 'ident'], writes=[('pst', ti)])
                S.op('act', lambda e, ti=ti: e.copy(out=combT[:, :], in_=PST[0:16, ti, :]), reads=[('pst', ti)], writes=['combT'])
                if dbg and first:
                    dump('comb', comb[:], [128, 4, 16], ['comb'])
                    dump('h2T', hT[:], [128, 16, 512], HK, BF16)
                yTf = yT[:].rearrange("p a f -> p (a f)")
                ex_m = [(yTf[:, i * 2048:(i + 1) * 2048].rearrange("p (k c) -> p k c", c=128), ('yTs', i), 'pxy%d' % i) for i in range(4)]
                S.alias_in([('yTs', i) for i in range(4)], YK + ['raw'])
                set_pan_slots(ex_m)
                for eg in range(4):
                    par = eg % 2
                    hw = QO[:, par * 8:(par + 1) * 8]
                    for el in range(4):
                        e_ = eg * 4 + el
                        pc, pck = psa()
                        S.op('pe', lambda e, pc=pc, e_=e_: e.matmul(pc[:], lhsT=sele[0:16, e_, :], rhs=combT[:, :], start=True, stop=True), reads=['sele', 'combT'], writes=[pck])
                        S.op('act', lambda e, pc=pc: e.copy(out=TF[:, 2, :], in_=pc[:]), reads=[pck], writes=[('tf', 2)])
                        for hc in range(2):
                            pg_, pgk = mmA(WguP[e_ * 4 + hc], hT, HK)
                            S.op('act', lambda e, pg_=pg_: e.activation(out=TF[:, 0, :], in_=pg_[:], func=AF.Silu), reads=[pgk], writes=[('tf', 0)])
                            pu, puk = mmA(WguP[e_ * 4 + 2 + hc], hT, HK)
                            S.op('dve', lambda e, pu=pu: e.tensor_tensor(out=TF[:, 1, :], in0=pu[:], in1=TF[:, 0, :], op=ALU.mult), reads=[puk, ('tf', 0)], writes=[('tf', 1)])
                            S.op('pool', lambda e, hw=hw, el=el, hc=hc: e.tensor_tensor(out=hw[:, el * 2 + hc, :], in0=TF[:, 1, :], in1=TF[:, 2, :], op=ALU.mult),
                                 reads=[('tf', 1), ('tf', 2)], writes=[('QO', par)])
                    for cc in range(4):
                        wb_ap, wb_key = load_wb(WdS[eg * 4 + cc])
                        wv = wb_ap.rearrange("p (k c) -> p k c", c=512)
                        for qi in range(4):
                            pb, pk = psa()
                            for k8 in range(8):
                                S.op('pe', lambda e, k8=k8, qi=qi, pb=pb, wv=wv, hw=hw: e.matmul(pb[:], lhsT=hw[:, k8, qi * 128:(qi + 1) * 128], rhs=wv[:, k8, :], start=(k8 == 0), stop=(k8 == 7)),
                                     reads=[wb_key, ('QO', par)], writes=[pk])
                            xs = X1[:, qi, cc * 512:(cc + 1) * 512]
                            S.op('dve', lambda e, xs=xs, pb=pb: e.tensor_tensor(out=xs, in0=pb[:], in1=xs, op=ALU.add), reads=[pk, ('X1', qi)], writes=[('X1', qi)])
                S.alias_out([('yTs', i) for i in range(4)], YK + ['raw'])
                set_pan_slots([])
                chk('moe')
                for qi in range(4):
                    S.dma('sp', 'out%d' % qi, lambda e, qi=qi: e.dma_start(out=out[s, t0 + qi * 128:t0 + (qi + 1) * 128, :], in_=X1[:, qi, :]), reads=[('X1', qi)], writes=['out'])
        S.barrier()
        S.emit()
    return nc, dbg_out


_CACHE = {}


def _layout_inputs(inputs):
    consts = make_consts()
    wts = {}
    for k in WEIGHT_SHAPES:
        a = np.asarray(inputs[k])
        wts[k] = np.ascontiguousarray(a.reshape(a.shape[1:]), dtype=np.float32)
    xs = np.asarray(inputs['x'], dtype=np.float32)
    pos = np.asarray(inputs['positions'], dtype=np.int32)
    in_maps = []
    for c in range(NCORES):
        m = {'x': np.ascontiguousarray(xs[c * NSEQ:(c + 1) * NSEQ]), 'positions': np.ascontiguousarray(pos[c * NSEQ:(c + 1) * NSEQ])}
        m.update(wts)
        m.update(consts)
        in_maps.append(m)
    return in_maps


def kernel(**inputs):
    if 'nc' not in _CACHE:
        _CACHE['nc'] = build()[0]
    nc = _CACHE['nc']
    in_maps = _layout_inputs(inputs)
    res = run_bass_kernel_spmd(nc, in_maps, core_ids=list(range(NCORES)))
    outs = [np.asarray(r['out']) for r in res.results]
    return np.concatenate(outs, axis=0).astype(np.float32)
```

```python
import numpy as np
from contextlib import ExitStack
import concourse.bass as bass
import concourse.mybir as mybir
from concourse.bass_utils import run_bass_kernel_spmd

F32 = mybir.dt.float32
BF16 = mybir.dt.bfloat16
I32 = mybir.dt.int32
ALU = mybir.AluOpType
AF = mybir.ActivationFunctionType
AX = mybir.AxisListType

D = 2048
SEQ = 2048
NSEQ = 2
NCORES = 8
TB = 512
NQB = SEQ // TB
H = 16
G = 4
NCMP = 127
INW = 7728
OFF_Q, OFF_KC, OFF_VC, OFF_KS, OFF_VS, OFF_KW, OFF_VW, OFF_GATE, OFF_POOL, OFF_MERGE = (
    0, 1024, 1280, 1536, 1792, 2048, 2304, 2560, 2608, 3632)
NE = 16
TWO_PI = 6.283185307179586
PI_SAFE = 3.1415925


class _Rec:
    def __init__(self):
        self.call = None

    def __getattr__(self, name):
        def f(*a, **k):
            assert self.call is None
            self.call = (name, a, k)
            return self
        return f


def _replay(fn):
    rec = _Rec()
    fn(rec)
    name, a, k = rec.call
    return lambda e: getattr(e, name)(*a, **k)


class Sched:
    ENG = ('pe', 'act', 'dve', 'pool', 'sp')

    def __init__(self, nc, es):
        self.nc = nc
        self.es = es
        self.lists = {e: [] for e in self.ENG}
        self.sem = {e: es.enter_context(nc.semaphore('s_' + e)) for e in self.ENG}
        self.cnt = {e: 0 for e in self.ENG}
        self.seen = {e: {} for e in self.ENG}
        self.lastw = {}
        self.readers = {}
        self.dsem = {}

    def _deps(self, reads, writes):
        deps = []
        for k in reads:
            d = self.lastw.get(k)
            if d is not None:
                deps.append(d)
        for k in writes:
            d = self.lastw.get(k)
            if d is not None:
                deps.append(d)
            r = self.readers.get(k)
            if r:
                deps.extend(r.values())
        return deps

    def _emit_waits(self, eng, deps):
        need = {}
        seen = self.seen[eng]
        for (sname, sem, val) in deps:
            if eng == 'pe' and sname == 'Epe':
                continue
            if seen.get(sname, 0) < val:
                if sname not in need or need[sname][1] < val:
                    need[sname] = (sem, val)
        for sname, (sem, val) in need.items():
            seen[sname] = val
            self.lists[eng].append(lambda e, sem=sem, val=val: e.wait_ge(sem, val))

    def _reg(self, dep, reads, writes):
        for k in writes:
            self.lastw[k] = dep
            self.readers[k] = {}
        for k in reads:
            if k not in writes:
                r = self.readers.setdefault(k, {})
                o = r.get(dep[0])
                if o is None or o[2] < dep[2]:
                    r[dep[0]] = dep

    dead = False

    def op(self, eng, fn, reads=(), writes=()):
        if self.dead:
            return
        fn = _replay(fn)
        self._emit_waits(eng, self._deps(reads, writes))
        self.cnt[eng] += 1
        sem = self.sem[eng]
        self.lists[eng].append(lambda e, fn=fn, sem=sem: fn(e).then_inc(sem, 1))
        dep = ('E' + eng, sem, self.cnt[eng])
        self._reg(dep, reads, writes)

    def dma(self, eng, slot, fn, reads=(), writes=()):
        if self.dead:
            return
        fn = _replay(fn)
        self._emit_waits(eng, self._deps(reads, writes))
        if slot not in self.dsem:
            self.dsem[slot] = [self.es.enter_context(self.nc.semaphore('d_' + slot)), 0]
        ent = self.dsem[slot]
        ent[1] += 16
        sem = ent[0]
        self.lists[eng].append(lambda e, fn=fn, sem=sem: fn(e).then_inc(sem, 16))
        dep = ('D' + slot, sem, ent[1])
        self._reg(dep, reads, writes)

    def alias_in(self, new_keys, old_keys):
        acc = {}
        for k in old_keys:
            for d in [self.lastw.get(k)] + list(self.readers.get(k, {}).values()):
                if d is not None and (d[0] not in acc or acc[d[0]][2] < d[2]):
                    acc[d[0]] = d
        for k in new_keys:
            self.lastw.pop(k, None)
            self.readers[k] = dict(acc)

    def alias_out(self, new_keys, old_keys):
        acc = {}
        for k in new_keys:
            for d in [self.lastw.get(k)] + list(self.readers.get(k, {}).values()):
                if d is not None and (d[0] not in acc or acc[d[0]][2] < d[2]):
                    acc[d[0]] = d
        for k in old_keys:
            r = self.readers.setdefault(k, {})
            for n_, d in acc.items():
                if n_ not in r or r[n_][2] < d[2]:
                    r[n_] = d

    def barrier(self):
        deps = [('E' + e, self.sem[e], self.cnt[e]) for e in self.ENG if self.cnt[e] > 0]
        deps += [('D' + s, ent[0], ent[1]) for s, ent in self.dsem.items()]
        for e in self.ENG:
            self._emit_waits(e, deps)

    def emit(self):
        nc = self.nc
        L = self.lists
        with nc.Block() as block:
            @block.tensor
            def _(e):
                for f in L['pe']:
                    f(e)

            @block.scalar
            def _(e):
                for f in L['act']:
                    f(e)

            @block.vector
            def _(e):
                for f in L['dve']:
                    f(e)

            @block.gpsimd
            def _(e):
                for f in L['pool']:
                    f(e)

            @block.sync
            def _(e):
                for f in L['sp']:
                    f(e)


def make_consts():
    c = {}
    tri = np.zeros((2, 128, 128), np.float32)
    k = np.arange(128)[:, None]
    q = np.arange(128)[None, :]
    tri[0] = (k <= q)
    tri[1] = (k > q)
    c['c_tri'] = tri.transpose(1, 0, 2).copy()
    n = np.arange(NCMP)[:, None]
    t = np.arange(SEQ)[None, :]
    mk = np.zeros((128, SEQ), np.float32)
    mk[:NCMP] = (16 * n + 31 <= t)
    c['c_maskc'] = mk
    tt = np.arange(SEQ)
    tb = tt // 64
    j = np.arange(32)[None, :]
    dist = tb[:, None] - j
    valid = dist >= 0
    forced = (j == 0) | (valid & (dist < 2))
    bonus = np.where(valid, 1e4 * forced.astype(np.float32), -1e30).astype(np.float32)
    c['c_bonus'] = bonus.reshape(16, 128, 32).transpose(1, 0, 2).copy()
    em = np.zeros((32, 16, 128), np.float32)
    for kt in range(16):
        for kk in range(128):
            em[2 * kt + kk // 64, kt, kk] = 1.0
    emp = np.zeros((128, 16, 128), np.float32)
    emp[:32] = em
    c['c_emat'] = emp
    a0 = np.arange(NCMP)[:, None] * 16
    b0 = np.arange(32)[None, :] * 64
    ov = np.clip(np.minimum(a0 + 32, b0 + 64) - np.maximum(a0, b0), 0, None) / 32.0
    ovp = np.zeros((128, 33), np.float32)
    ovp[:NCMP, 0] = 1.0
    ovp[:NCMP, 1:] = ov
    c['c_ov'] = ovp
    inv_freq = (500000.0 ** (-np.arange(0, 16, 2, dtype=np.float32) / 16)).astype(np.float32)
    fr = np.zeros((128, 1), np.float32)
    for p in range(128):
        if p % 64 < 16:
            fr[p, 0] = inv_freq[p % 8]
    c['c_freq'] = fr
    rm = np.zeros((128, 128), np.float32)
    for b in (0, 64):
        for d in range(8):
            rm[b + d + 8, b + d] = -1.0
            rm[b + d, b + d + 8] = 1.0
    c['c_rm'] = rm
    ob = np.zeros((128, 128), np.float32)
    ob[:64, :64] = 1.0 / 64
    ob[64:, 64:] = 1.0 / 64
    c['c_onesblk'] = ob
    se = np.zeros((128, 16, 128), np.float32)
    for e in range(16):
        se[e, e, :] = 1.0
    c['c_sele'] = se
    ic = np.zeros((128, 4, 16), np.float32)
    for gi, w in enumerate((2, 4, 8, 16)):
        for xx in range(16):
            ic[:, gi, xx] = 1.0 / min(xx + 1, w)
    c['c_icnt'] = ic
    return c


CONST_SHAPES = {
    'c_tri': [128, 2, 128], 'c_maskc': [128, SEQ], 'c_bonus': [128, 16, 32], 'c_emat': [128, 16, 128],
    'c_ov': [128, 33], 'c_freq': [128, 1], 'c_rm': [128, 128], 'c_onesblk': [128, 128],
    'c_sele': [128, 16, 128], 'c_icnt': [128, 4, 16],
}

WEIGHT_SHAPES = {
    'attn_norm_g': [D], 'w_in': [D, INW], 'q_norm_g': [64], 'k_norm_cmp_g': [64], 'k_norm_slc_g': [64],
    'k_norm_swa_g': [64], 'cmp_pos_emb_k': [32, 64], 'cmp_w1_k': [2048, 256], 'cmp_w2_k': [256, 64],
    'cmp_pos_emb_v': [32, 64], 'cmp_w1_v': [2048, 256], 'cmp_w2_v': [256, 64],
    'w_head_out': [16, 64, 128], 'w_pool': [4, 256, 512], 'pool_scale': [D], 'w_out': [D, D],
    'ffn_norm_g': [D], 'w_router_group': [D, 4], 'b_router_group': [4], 'w_router_expert': [D, 16],
    'b_router_expert': [16], 'w_expert_gate': [16, D, 256], 'w_expert_up': [16, D, 256],
    'w_expert_down': [16, 256, D],
}


MARKS = []


class _Stop(Exception):
    pass


def build(dbg=False, nseq=NSEQ, nqb=NQB, stop_after=None, skip_prep=False):
    nc = bass.Bass("TRN2", target_bir_lowering=False)
    x = nc.dram_tensor("x", [NSEQ, SEQ, D], F32, kind="ExternalInput").ap()
    positions = nc.dram_tensor("positions", [NSEQ, SEQ], I32, kind="ExternalInput").ap()
    W = {k: nc.dram_tensor(k, s, F32, kind="ExternalInput").ap() for k, s in WEIGHT_SHAPES.items()}
    C = {k: nc.dram_tensor(k, s, F32, kind="ExternalInput").ap() for k, s in CONST_SHAPES.items()}
    out = nc.dram_tensor("out", [NSEQ, SEQ, D], F32, kind="ExternalOutput").ap()
    WinP = nc.dram_tensor("WinP", [56, 128, 2048], BF16).ap()
    WVS = nc.dram_tensor("WVS", [2, 128, 4096], BF16).ap()
    WguP = nc.dram_tensor("WguP", [64, 128, 2048], BF16).ap()
    WdS = nc.dram_tensor("WdS", [16, 128, 4096], BF16).ap()
    WoS = nc.dram_tensor("WoS", [8, 128, 4096], BF16).ap()
    W1S = nc.dram_tensor("W1S", [4, 128, 4096], BF16).ap()
    dbg_out = {}

    with ExitStack() as es:
        S = Sched(nc, es)

        def sb(name, shape, dt):
            return es.enter_context(nc.sbuf_tensor(name, shape, dt))

        def ps(name, shape, dt):
            return es.enter_context(nc.psum_tensor(name, shape, dt))

        X1 = sb("X1", [128, 4, 2048], F32)
        hT = sb("hT", [128, 16, 512], BF16)
        QO = sb("QO", [128, 16, 512], BF16)
        yT = sb("yT", [128, 16, 512], BF16)
        KT = sb("KT", [128, 2, 2, SEQ], BF16)
        VA = sb("VA", [128, 2, 16, 4 * 65], BF16)
        kcT = sb("kcT", [128, 512], BF16)
        vcA = sb("vcA", [128, 4, 97], BF16)
        CSt = sb("CSt", [128, 2, 512], F32)
        posi = sb("posi", [128, 512], I32)
        TF = sb("TF", [128, 4, 512], F32)
        BFW = sb("BFW", [128, 5, 512], BF16)
        PT = BFW[:, 0:3]
        TBb = BFW[:, 3:5]
        XNB = BFW[:, 0:4].rearrange("p a f -> p (a f)")
        XK = [('pt', 0), ('pt', 1), ('pt', 2), ('tb', 0)]
        Otot = sb("Otot", [128, 2, 4, 128], F32)
        Obf = sb("Obf", [128, 2, 4, 128], BF16)
        Cc = Otot
        hidT = QO[:, 0:4].rearrange("p (a b) f -> p a b f", a=2)
        CCK = ['Cc', ('otot', 0), ('otot', 1)]
        gates = sb("gates", [128, 4, 48], F32)
        sc2 = sb("sc2", [128, 32], F32)
        top8 = sb("top8", [128, 16], F32)
        selb = sb("selb", [128, 4, 32], BF16)
        selbT = sb("selbT", [128, 512], BF16)
        QP = sb("QP", [128, 4, 512], BF16)
        zeros128 = sb("zeros128", [128, 128], BF16)
        Rg = sb("Rg", [128, 4, 128], BF16)
        small = sb("small", [128, 64], F32)
        ubuf = sb("ubuf", [128, 1, 528], F32)
        carry = sb("carry", [128, 8, 16], F32)
        PLT = sb("PLT", [128, 2, 2, 512], BF16)
        rt = sb("rt", [128, 96], F32)
        comb = sb("comb", [128, 4, 16], F32)
        combb = sb("combb", [128, 4, 16], BF16)
        combT = sb("combT", [16, 512], BF16)
        PAN = sb("PAN", [128, 3, 16, 128], BF16)
        WB = sb("WB", [128, 2, 4096], BF16)
        WG = sb("WG", [128, 16, 48], BF16)
        Wpool = sb("Wpool", [128, 8, 512], BF16)
        Who = sb("Who", [128, 8, 128], BF16)
        W2k = sb("W2k", [128, 2, 128], BF16)
        W2v = sb("W2v", [128, 2, 64], BF16)
        Wr = sb("Wr", [128, 16, 20], BF16)
        peT = sb("peT", [64, 2, 32], BF16)
        bh = sb("bh", [128, 4], F32)
        gvec = sb("gvec", [128, 40], F32)
        pscale = sb("pscale", [128, 16], F32)
        brt = sb("brt", [128, 20], F32)
        ident = sb("ident", [128, 128], BF16)
        tri = sb("tri", [128, 2, 128], BF16)
        maskc = sb("maskc", [128, 512], BF16)
        bonus = sb("bonus", [128, 4, 32], F32)
        emat = sb("emat", [128, 16, 128], BF16)
        freq = sb("freq", [128, 1], F32)
        rmat = sb("rmat", [128, 128], BF16)
        onesblk = sb("onesblk", [128, 128], BF16)
        sele = sb("sele", [128, 16, 128], BF16)
        icnt = sb("icnt", [128, 4, 16], F32)
        ssq = sb("ssq", [128, 8], F32)

        tmpo = TF[:, 3, 0:256].rearrange("p (a b) -> p a b", a=4)
        tmpi = TF[:, 3, 256:384].rearrange("p (a b) -> p a b", a=4)
        imp = TF[:, 2, 0:128].rearrange("p (a b) -> p a b", a=4)
        scb = TF[:, 2, 128:256].rearrange("p (a b) -> p a b", a=4)
        PSA = [ps("psa%d" % i, [128, 512], F32) for i in range(3)]
        PSS = [ps("pss%d" % i, [128, 512], F32) for i in range(2)]
        PSO = [ps("pso%d" % i, [128, 4, 128], F32) for i in range(2)]
        PST = ps("pst", [128, 1, 512], BF16)

        rr = {'psa': 0, 'pss': 0, 'pso': 0, 'pst': 0, 'pan': 0, 'wb': 0, 'pt': 0, 'ew': 0}

        def nxt(name, n):
            i = rr[name]
            rr[name] = (i + 1) % n
            return i

        def psa():
            i = nxt('psa', 3)
            return PSA[i], ('psa', i)

        def chk(stage):
            if stage[0] != 'h':
                MARKS.append((stage, S.cnt['pe'], S.cnt['act']))
            if stop_after == stage:
                S.dead = True

        chk('start')
        ukey = [0]

        def uk():
            ukey[0] += 1
            return ('cst', ukey[0])

        def ld(dst, src, key, eng='sp', slot='cst'):
            S.dma(eng, slot, lambda e: e.dma_start(out=dst, in_=src), writes=[uk()])

        ld(tri[:], C['c_tri'], 'tri', 'pool', 'cstp')
        ld(emat[:], C['c_emat'], 'emat', 'pool', 'cstp')
        ld(rmat[:], C['c_rm'], 'rmat', 'pool', 'cstp')
        ld(onesblk[:], C['c_onesblk'], 'onesblk', 'pool', 'cstp')
        ld(freq[:], C['c_freq'], 'freq')
        ld(sele[:], C['c_sele'], 'sele', 'pool', 'cstp')
        ld(icnt[:], C['c_icnt'], 'icnt')
        chk('c0')
        for g in range(4):
            ld(vcA[:, g, 64:97], C['c_ov'], 'vcA', 'pool', 'cstp')
        chk('c1')
        nsc = lambda e, o, i: e.dma_start(out=o, in_=i, allow_slow_non_contiguous=True)
        for q4 in range(4):
            cs_ = slice(q4 * 4, q4 * 4 + 4)
            S.dma('sp', 'cst', lambda e: nsc(e, gvec[:, q4 * 4:q4 * 4 + 4], W['attn_norm_g'].rearrange("(c p) -> p c", p=128)[:, cs_]), writes=[uk()])
            S.dma('sp', 'cst', lambda e: nsc(e, gvec[:, 16 + q4 * 4:16 + q4 * 4 + 4], W['ffn_norm_g'].rearrange("(c p) -> p c", p=128)[:, cs_]), writes=[uk()])
            S.dma('sp', 'cst', lambda e: nsc(e, pscale[:, cs_], W['pool_scale'].rearrange("(c p) -> p c", p=128)[:, cs_]), writes=[uk()])
        for i, nm in enumerate(('q_norm_g', 'k_norm_cmp_g', 'k_norm_slc_g', 'k_norm_swa_g')):
            for hb in (0, 64):
                S.dma('sp', 'cst', lambda e, i=i, nm=nm, hb=hb: nsc(e, gvec[hb:hb + 64, 32 + i:33 + i], W[nm].rearrange("(p o) -> p o", o=1)), writes=[uk()])
        chk('c2')
        S.dma('sp', 'cst', lambda e: e.dma_start(out=brt[:, 0:4], in_=W['b_router_group'].partition_broadcast(128)), writes=[uk()])
        S.dma('sp', 'cst', lambda e: e.dma_start(out=brt[:, 4:20], in_=W['b_router_expert'].partition_broadcast(128)), writes=[uk()])
        chk('c3')
        S.op('dve', lambda e: e.memset(gvec[:, 36:37], 1.0), writes=['gvec'])
        S.op('dve', lambda e: e.memset(VA[:].rearrange("p a b c -> p (a b c)"), 1.0), writes=['VA'])
        S.op('dve', lambda e: e.memset(ssq[:], 0.0), writes=['ssq'])
        S.op('dve', lambda e: e.memset(selbT[:], 0.0), writes=['selbT'])
        S.op('dve', lambda e: e.memset(zeros128[:], 0.0), writes=['zeros128'])
        S.op('pool', lambda e: e.memset(QP[:].rearrange("p a f -> p (a f)"), 0.0), writes=[('qp', i) for i in range(4)])
        S.op('pool', lambda e: e.memset(ident[:], 0.0), writes=['ident'])
        S.op('pool', lambda e: e.affine_select(out=ident[:], in_=ident[:], pattern=[[-1, 128]], compare_op=ALU.not_equal, fill=1.0, base=0, channel_multiplier=1), reads=['ident'], writes=['ident'])

        X1f = X1[:].rearrange("p a f -> p (a f)")
        chk('const')
        S.barrier()
        for v_ in range(4):
            S.op('dve', lambda e: e.tensor_scalar_mul(out=Rg[:, v_, :], in0=rmat[:], scalar1=gvec[:, 32 + v_:33 + v_]), reads=['gvec'], writes=['Rg'])
        if skip_prep:
            S.dead = True
        hTf = hT[:].rearrange("p a f -> p (a f)")
        st_in = [X1f[:, i * 4096:(i + 1) * 4096] for i in range(2)]
        st_out = [hTf[:, i * 4096:(i + 1) * 4096] for i in range(2)]
        pj = [0]
        EW3 = ('act', 'dve', 'act', 'act', 'pool', 'act', 'act', 'dve', 'act', 'act', 'act', 'dve', 'act', 'pool', 'act', 'act')

        def scale_op(eng, o, i, sc):
            if eng == 'act':
                S_fn = lambda e: e.mul(out=o, in_=i, mul=sc)
            else:
                S_fn = lambda e: e.tensor_scalar_mul(out=o, in0=i, scalar1=sc)
            return S_fn

        def prep(loads, mode, gofs, stores, resident=None):
            i = pj[0] % 2
            pj[0] += 1
            for (dfn, src) in loads:
                S.dma('sp', 'pin%d' % i, lambda e, dfn=dfn, src=src: e.dma_start(out=dfn(st_in[i]), in_=src), writes=[('pin', i)])
            sin = st_in[i].rearrange("p (k c) -> p k c", c=256)
            for kc in range(16):
                sc = gvec[:, gofs + kc:gofs + kc + 1] if gofs is not None else gvec[:, 36:37]
                if resident is not None:
                    o, ii = resident(sin, kc)
                    wk = [resident.key]
                elif mode == 'A':
                    o = st_out[i].rearrange("p (n k c) -> p n k c", n=2, k=16)[:, :, kc, :]
                    ii = sin[:, kc, :].rearrange("p (n c) -> p n c", n=2)
                    wk = [('pout', i, kc)]
                elif mode == 'Aq':
                    o = st_out[i].rearrange("p (n k hf d) -> p n k hf d", n=2, k=16, hf=2)[:, :, kc, :, :]
                    ii = sin[:, kc, :].rearrange("p (hf r d) -> p r hf d", hf=2, r=2)
                    wk = [('pout', i, kc)]
                else:
                    o = st_out[i][:, kc * 256:(kc + 1) * 256]
                    ii = sin[:, kc, :]
                    wk = [('pout', i, kc)]
                eng = EW3[kc % 16]
                if o.shape[0] != 128:
                    sc = sc[0:o.shape[0]]
                if mode == 'Aq':
                    for n_ in range(2):
                        S.op(eng, scale_op(eng, o[:, n_], ii[:, n_], sc), reads=[('pin', i), 'gvec'], writes=wk)
                    continue
                S.op(eng, scale_op(eng, o, ii, sc), reads=[('pin', i), 'gvec'], writes=wk)
            prev = pend[:]
            del pend[:]
            for (dst, sfn) in stores:
                pend.append((i, dst, sfn))
            flush(prev)

        pend = []

        def flush(lst):
            for (i, dst, sfn) in lst:
                S.dma('sp', 'pout%d' % i, lambda e, dst=dst, sfn=sfn, i=i: e.dma_start(out=dst, in_=sfn(st_out[i])),
                      reads=[('pout', i, kc) for kc in range(16)], writes=['scratch'])

        def win_src(c0, n):
            return W['w_in'][:, c0:c0 + n].rearrange("(k p) c -> p k c", p=128)

        def st_cols(a, n):
            return lambda st: st.rearrange("p (k c) -> p k c", c=256)[:, :, a:a + n]

        def store_panels(p0):
            return [(WinP[p0:p0 + 2].rearrange("n p f -> p n f"), lambda st: st.rearrange("p (n f) -> p n f", n=2))]

        for m in range(2):
            for r2 in range(2):
                c_a = (8 * m + 2 * r2) * 64
                c_b = (8 * m + 4 + 2 * r2) * 64
                prep([(st_cols(0, 128), win_src(c_a, 128)), (st_cols(128, 128), win_src(c_b, 128))], 'Aq', 0,
                     store_panels(4 * m + 2 * r2))
        for pi, c0 in ((8, OFF_KC), (10, OFF_VC), (12, OFF_KS), (14, OFF_KW)):
            prep([(st_cols(0, 256), win_src(c0, 256))], 'A', 0, store_panels(pi))
        for i4 in range(4):
            prep([(st_cols(0, 256), win_src(OFF_POOL + i4 * 256, 256))], 'A', 0, store_panels(16 + 2 * i4))
        for i16 in range(16):
            prep([(st_cols(0, 256), win_src(OFF_MERGE + i16 * 256, 256))], 'A', 0, store_panels(24 + 2 * i16))
        for vi, c0 in ((0, OFF_VS), (1, OFF_VW)):
            prep([(st_cols(0, 256), win_src(c0, 256))], 'B', 0, [(WVS[vi], lambda st: st)])

        class Res:
            def __init__(self, fn, key):
                self.fn, self.key = fn, key

            def __call__(self, sin, kc):
                return self.fn(sin, kc)

        prep([(st_cols(0, 48), win_src(OFF_GATE, 48))], 'R', 0, [],
             resident=Res(lambda sin, kc: (WG[:, kc, :], sin[:, kc, 0:48]), 'WG'))
        def st_cols_k(a, n, k0, k1):
            return lambda st: st.rearrange("p (k c) -> p k c", c=256)[:, k0:k1, a:a + n]
        rl = []
        for q4 in range(4):
            rl.append((st_cols_k(0, 4, q4 * 4, q4 * 4 + 4), W['w_router_group'].rearrange("(k p) c -> p k c", p=128)[:, q4 * 4:q4 * 4 + 4, :]))
            rl.append((st_cols_k(4, 16, q4 * 4, q4 * 4 + 4), W['w_router_expert'].rearrange("(k p) c -> p k c", p=128)[:, q4 * 4:q4 * 4 + 4, :]))
        prep(rl, 'R', 16, [],
             resident=Res(lambda sin, kc: (Wr[:, kc, :], sin[:, kc, 0:20]), 'Wr'))
        for e_ in range(NE):
            for gi_, nm in enumerate(('w_expert_gate', 'w_expert_up')):
                prep([(st_cols(0, 256), W[nm][e_].rearrange("(k p) c -> p k c", p=128))], 'A', 16,
                     [(WguP[e_ * 4 + gi_ * 2:e_ * 4 + gi_ * 2 + 2].rearrange("n p f -> p n f"), lambda st: st.rearrange("p (n f) -> p n f", n=2))])
        for eg in range(4):
            for cc in range(4):
                src = W['w_expert_down'][eg * 4:eg * 4 + 4, :, cc * 512:(cc + 1) * 512].rearrange("e (h p) c -> p e h c", p=128)
                prep([(lambda st: st.rearrange("p (e h c) -> p e h c", e=4, h=2), src)], 'B', None, [(WdS[eg * 4 + cc], lambda st: st)])
        for c8 in range(8):
            prep([(st_cols(0, 256), W['w_out'][:, c8 * 256:(c8 + 1) * 256].rearrange("(k p) c -> p k c", p=128))], 'B', None,
                 [(WoS[c8], lambda st: st)])
        for kv, nm in enumerate(('cmp_w1_k', 'cmp_w1_v')):
            for lh in range(2):
                src = W[nm][lh * 1024:(lh + 1) * 1024, :].rearrange("(l d) h -> d l h", d=64)
                prep([(lambda st: st.rearrange("p (l h) -> p l h", h=256)[0:64], src),
                      (lambda st: st.rearrange("p (l h) -> p l h", h=256)[64:128], src)], 'B', None,
                     [(W1S[kv * 2 + lh], lambda st: st)])
        prep([(lambda st: st.rearrange("p (g j e) -> p g j e", g=4, j=2),
               W['w_pool'].rearrange("g (j p) e -> p g j e", p=128))], 'R', None, [],
             resident=Res(lambda sin, kc: (Wpool[:, kc // 2, (kc % 2) * 256:(kc % 2) * 256 + 256], sin[:, kc, :]), 'Wpool'))

        def res_small(sin, kc):
            flat = sin.rearrange("p k c -> p (k c)")
            if kc < 4:
                return Who[:, 2 * kc:2 * kc + 2, :], flat[:, kc * 256:(kc + 1) * 256].rearrange("p (a b) -> p a b", a=2)
            if kc == 4:
                return W2k[:, :, 0:64], flat[:, 1024:1280].rearrange("p (a b) -> p a b", a=2)[:, :, 0:64]
            if kc == 5:
                return W2k[:, :, 64:128], flat[:, 1024:1280].rearrange("p (a b) -> p a b", a=2)[:, :, 0:64]
            if kc == 6:
                return W2v[:, :, :], flat[:, 1280:1536].rearrange("p (a b) -> p a b", a=2)[:, :, 0:64]
            if kc == 7:
                return peT[:, :, :], flat[0:64, 1536:1664].rearrange("p (a b) -> p a b", a=2)[:, :, 0:32]
            return small[:, 32 + kc:33 + kc], flat[:, 4000 + kc:4001 + kc]

        ifl = lambda st: st
        who_src = W['w_head_out'].rearrange("(c two) d e -> (two d) c e", two=2)
        loads = [(lambda st: st[:, 0:1024].rearrange("p (c e) -> p c e", c=8), who_src)]
        for hc in range(2):
            loads.append((lambda st, hc=hc: st[:, 1024 + hc * 128:1024 + hc * 128 + 64], W['cmp_w2_k'][hc * 128:(hc + 1) * 128, :]))
            loads.append((lambda st, hc=hc: st[:, 1280 + hc * 128:1280 + hc * 128 + 64], W['cmp_w2_v'][hc * 128:(hc + 1) * 128, :]))
        i_sm = pj[0] % 2
        S.op('dve', lambda e: e.memset(st_in[i_sm], 0.0), writes=[('pin', i_sm)])
        for kv, nm in enumerate(('cmp_pos_emb_k', 'cmp_pos_emb_v')):
            for q4 in range(4):
                S.dma('sp', 'pin%d' % i_sm, lambda e, kv=kv, nm=nm: nsc(e, st_in[i_sm][0:64, 1536 + kv * 64 + q4 * 8:1536 + kv * 64 + q4 * 8 + 8], W[nm].rearrange("l d -> d l")[:, q4 * 8:q4 * 8 + 8]), writes=[('pin', i_sm)])
        prep(loads, 'R', None, [], resident=Res(res_small, 'smallres'))
        flush(pend)
        if skip_prep:
            S.dead = False
        chk('prep')
        S.barrier()

        base_pan = [(PAN[:, i], ('pan', i), 'pan%d' % i) for i in range(3)]
        pan_slots = list(base_pan)

        def set_pan_slots(extra):
            del pan_slots[:]
            pan_slots.extend(base_pan + extra)
            rr['pan'] = 0

        def load_panel(src):
            i = nxt('pan', len(pan_slots))
            ap_, key_, sname = pan_slots[i]
            S.dma('sp', sname, lambda e: e.dma_start(out=ap_.rearrange("p k c -> p (k c)"), in_=src), reads=['scratch'], writes=[key_])
            return ap_, key_

        base_wb = [(WB[:, i, :], ('wb', i), 'wb%d' % i) for i in range(2)]
        wb_slots = list(base_wb)

        def set_wb_slots(extra):
            del wb_slots[:]
            wb_slots.extend(base_wb + extra)
            rr['wb'] = 0

        def load_wb(src):
            i = nxt('wb', len(wb_slots))
            ap_, key_, sname = wb_slots[i]
            S.dma('sp', sname, lambda e: e.dma_start(out=ap_, in_=src), reads=['scratch'], writes=[key_])
            return ap_, key_

        def mmA(src, actT, akeys, n=512):
            pan_ap, pan_key = load_panel(src)
            pb, pk = psa()
            for kc in range(16):
                S.op('pe', lambda e, kc=kc: e.matmul(pb[:, 0:n], lhsT=pan_ap[:, kc, :], rhs=actT[:, kc, 0:n], start=(kc == 0), stop=(kc == 15)),
                     reads=[pan_key] + akeys, writes=[pk])
            return pb, pk

        def ew():
            i = nxt('ew', 2)
            return ('dve', 'pool')[i]

        def rope_tables(s, t0):
            S.dma('sp', 'posi', lambda e: e.dma_start(out=posi[:], in_=positions[s, t0:t0 + 512].partition_broadcast(128)), writes=['posi'])
            ang, kf, ki = TF[:, 0, :], TF[:, 1, :], posi[:]
            S.op('dve', lambda e: e.tensor_copy(out=ang, in_=posi[:]), reads=['posi'], writes=[('tf', 0)])
            S.op('dve', lambda e: e.tensor_scalar_mul(out=ang, in0=ang, scalar1=freq[:, 0:1]), reads=[('tf', 0), 'freq'], writes=[('tf', 0)])
            for ci, ph in ((0, 1.5707963267948966), (1, 0.0)):
                S.op('dve', lambda e, ph=ph: e.tensor_scalar(out=kf, in0=ang, scalar1=ph, scalar2=1.0 / TWO_PI, op0=ALU.add, op1=ALU.mult),
                     reads=[('tf', 0)], writes=[('tf', 1)])
                S.op('dve', lambda e: e.tensor_copy(out=ki, in_=kf), reads=[('tf', 1)], writes=['posi'])
                S.op('dve', lambda e: e.tensor_copy(out=kf, in_=ki), reads=['posi'], writes=[('tf', 1)])
                r = TF[:, 2, :]
                S.op('dve', lambda e: e.scalar_tensor_tensor(out=r, in0=kf, scalar=-6.28125, in1=ang, op0=ALU.mult, op1=ALU.add),
                     reads=[('tf', 0), ('tf', 1)], writes=[('tf', 2)])
                S.op('dve', lambda e: e.scalar_tensor_tensor(out=r, in0=kf, scalar=-(TWO_PI - 6.28125), in1=r, op0=ALU.mult, op1=ALU.add),
                     reads=[('tf', 1), ('tf', 2)], writes=[('tf', 2)])
                S.op('dve', lambda e, ph=ph: e.tensor_scalar(out=r, in0=r, scalar1=ph, scalar2=PI_SAFE, op0=ALU.add, op1=ALU.min),
                     reads=[('tf', 2)], writes=[('tf', 2)])
                S.op('dve', lambda e: e.tensor_scalar_max(out=r, in0=r, scalar1=-PI_SAFE), reads=[('tf', 2)], writes=[('tf', 2)])
                S.op('act', lambda e, ci=ci: e.activation(out=CSt[:, ci, :], in_=r, func=AF.Sin), reads=[('tf', 2)], writes=['CSt'])

        def normrope(pb, pk, gcol, Ct, St, cskeys, out_ap, okeys, n=512, view=None):
            vw = view if view is not None else (lambda a: a)
            v = gcol - 32
            sq, psb = TBb[:, 0, 0:n], TBb[:, 1, 0:n]
            rstd, t1, t2 = TF[:, 0, 0:n], TF[:, 1, 0:n], TF[:, 2, 0:n]
            S.op('act', lambda e: e.activation(out=sq, in_=pb[:, 0:n], func=AF.Square), reads=[pk], writes=[('tb', 0)])
            S.op('act', lambda e: e.copy(out=psb, in_=pb[:, 0:n]), reads=[pk], writes=[('tb', 1)])
            mb, mk = psa()
            S.op('pe', lambda e: e.matmul(mb[:, 0:n], lhsT=onesblk[:], rhs=sq, start=True, stop=True), reads=[('tb', 0), 'onesblk'], writes=[mk])
            rb, rk = psa()
            S.op('pe', lambda e: e.matmul(rb[:, 0:n], lhsT=Rg[:, v, :], rhs=psb, start=True, stop=True), reads=[('tb', 1), 'Rg'], writes=[rk])
            S.op('act', lambda e: e.activation(out=rstd, in_=mb[:, 0:n], func=AF.Sqrt, bias=small[:, 63:64], scale=1.0), reads=[mk, 'eps'], writes=[('tf', 0)])
            S.op('dve', lambda e: e.reciprocal(out=rstd, in_=rstd), reads=[('tf', 0)], writes=[('tf', 0)])
            S.op('dve', lambda e: e.scalar_tensor_tensor(out=vw(t1), in0=vw(pb[:, 0:n]), scalar=gvec[:, gcol:gcol + 1], in1=Ct, op0=ALU.mult, op1=ALU.mult),
                 reads=[pk, 'gvec'] + cskeys, writes=[('tf', 1)])
            S.op('dve', lambda e: e.tensor_tensor(out=vw(t2), in0=vw(rb[:, 0:n]), in1=St, op=ALU.mult), reads=[rk] + cskeys, writes=[('tf', 2)])
            S.op('pool', lambda e: e.tensor_tensor(out=t1, in0=t1, in1=t2, op=ALU.add), reads=[('tf', 1), ('tf', 2)], writes=[('tf', 1)])
            S.op('pool', lambda e: e.tensor_tensor(out=out_ap, in0=t1, in1=rstd, op=ALU.mult), reads=[('tf', 1), ('tf', 0)], writes=okeys)

        S.op('dve', lambda e: e.memset(small[:, 63:64], 1e-6), writes=['eps'])

        def make_hT():
            for qi in range(4):
                S.op('act', lambda e, qi=qi: e.activation(out=QO[:, 8:12].rearrange("p a f -> p (a f)"), in_=X1[:, qi, :], func=AF.Square, accum_out=ssq[:, qi:qi + 1]),
                     reads=[('X1', qi)], writes=[('QO', 1), ('ssq', qi)])
                chk('h1')
                S.op('dve', lambda e, qi=qi: e.tensor_scalar(out=ssq[:, 4 + qi:5 + qi], in0=ssq[:, qi:qi + 1], scalar1=1.0 / D, scalar2=1e-6, op0=ALU.mult, op1=ALU.add),
                     reads=[('ssq', qi)], writes=[('ssq2', qi)])
                S.op('dve', lambda e, qi=qi: e.memset(ssq[:, qi:qi + 1], 0.0), reads=[('ssq2', qi)], writes=[('ssq', qi)])
                S.op('act', lambda e, qi=qi: e.activation(out=ssq[:, 4 + qi:5 + qi], in_=ssq[:, 4 + qi:5 + qi], func=AF.Sqrt), reads=[('ssq2', qi)], writes=[('ssq2', qi)])
                S.op('dve', lambda e, qi=qi: e.reciprocal(out=ssq[:, 4 + qi:5 + qi], in_=ssq[:, 4 + qi:5 + qi]), reads=[('ssq2', qi)], writes=[('ssq2', qi)])
                S.op('dve', lambda e, qi=qi: e.tensor_scalar_mul(out=XNB, in0=X1[:, qi, :], scalar1=ssq[:, 4 + qi:5 + qi]),
                     reads=[('X1', qi), ('ssq2', qi)], writes=XK)
                chk('h3')
                for k4 in range(4):
                    ti = nxt('pst', 1)
                    for kk in range(4):
                        kc = k4 * 4 + kk
                        S.op('pe', lambda e, kc=kc, kk=kk, ti=ti: e.transpose(out=PST[:, ti, kk * 128:(kk + 1) * 128], in_=XNB[:, kc * 128:(kc + 1) * 128], identity=ident[:]),
                             reads=XK + ['ident'], writes=[('pst', ti)])
                    chk('h4')
                    eng = 'act'
                    o = hT[:, k4 * 4:k4 * 4 + 4, qi * 128:(qi + 1) * 128]
                    ii = PST[:, ti, :].rearrange("p (a b) -> p a b", a=4)
                    if eng == 'act':
                        S.op('act', lambda e, o=o, ii=ii: e.copy(out=o, in_=ii), reads=[('pst', ti)], writes=[('hT', qi)])
                    else:
                        S.op('dve', lambda e, o=o, ii=ii: e.tensor_copy(out=o, in_=ii), reads=[('pst', ti)], writes=[('hT', qi)])
                    chk('h5' if k4 == 0 else ('h6' if k4 == 1 else 'h7'))

        HK = [('hT', qi) for qi in range(4)]
        YK = [('yT', qi) for qi in range(4)]

        def load_x(s, t0):
            for qi in range(4):
                S.dma('sp', 'x%d' % qi, lambda e, qi=qi: e.dma_start(out=X1[:, qi, :], in_=x[s, t0 + qi * 128:t0 + (qi + 1) * 128, :]), writes=[('X1', qi)])

        def dump(name, ap_sb, shape, keys, dt=F32):
            if not dbg:
                return
            if name not in dbg_out:
                dbg_out[name] = nc.dram_tensor("dbg_" + name, shape, dt, kind="ExternalOutput").ap()
            S.dma('sp', 'dbg', lambda e: e.dma_start(out=dbg_out[name], in_=ap_sb), reads=keys, writes=['dbg_' + name])

        for s in range(nseq):
            S.op('pool', lambda e: e.memset(carry[:].rearrange("p a b -> p (a b)"), 0.0), writes=['carry'])
            for tb in range(NQB):
                t0 = tb * TB
                load_x(s, t0)
                chk('p1x')
                make_hT()
                chk('p1a')
                rope_tables(s, t0)
                chk('p1b')
                for ci in range(2):
                    m0 = 1 if tb == 0 else 0
                    S.op('pool', lambda e, ci=ci, m0=m0, tb=tb: e.tensor_copy(out=Cc[:, ci, 0, 32 * tb - 1 + m0:32 * tb + 31], in_=CSt[:, ci, 15 + 16 * m0:512:16]),
                         reads=['CSt'], writes=CCK)
                if dbg and s == 0 and tb == 0:
                    dump('hT', hT[:], [128, 16, 512], HK, BF16)
                    dump('CS', CSt[:], [128, 2, 512], ['CSt'])
                chk('p1c')
                rawv = yT[:].rearrange("p a f -> p (a f)").rearrange("p (c t) -> p c t", c=4)
                for pi in range(8, 12):
                    pb, pk = mmA(WinP[pi], hT, HK)
                    o = rawv[:, pi - 8, t0:t0 + 512]
                    S.op('act', lambda e, o=o, pb=pb: e.copy(out=o, in_=pb[:]), reads=[pk], writes=YK + ['raw'])
                chk('p1d')
                for br in range(2):
                    for j in range(2):
                        pb, pk = mmA(WinP[12 + br * 2 + j], hT, HK)
                        normrope(pb, pk, 34 + br, CSt[:, 0, :], CSt[:, 1, :], ['CSt'], KT[:, br, j, t0:t0 + 512], ['KT'])
                chk('p1e')
                for br in range(2):
                    wb_ap, wb_key = load_wb(WVS[br])
                    wv = wb_ap.rearrange("p (k c) -> p k c", c=256)
                    for qi in range(4):
                        pb, pk = psa()
                        for kc in range(16):
                            S.op('pe', lambda e, kc=kc, qi=qi, pb=pb, wv=wv: e.matmul(pb[:, 0:256], lhsT=hT[:, kc, qi * 128:(qi + 1) * 128], rhs=wv[:, kc, :], start=(kc == 0), stop=(kc == 15)),
                                 reads=[wb_key, ('hT', qi)], writes=[pk])
                        o = VA[:, br, tb * 4 + qi, :].rearrange("p (g c) -> p g c", c=65)[:, :, 0:64]
                        ii = pb[:, 0:256].rearrange("p (g c) -> p g c", c=64)
                        S.op('dve', lambda e, o=o, ii=ii: e.tensor_copy(out=o, in_=ii), reads=[pk], writes=['VA'])
            chk('pass1')
            for ci in range(2):
                for g in range(1, 4):
                    S.op('pool', lambda e, ci=ci, g=g: e.tensor_copy(out=Cc[:, ci, g, 0:127], in_=Cc[:, ci, 0, 0:127]), reads=['Cc'], writes=CCK)
            rawv = yT[:].rearrange("p a f -> p (a f)").rearrange("p (c t) -> p c t", c=4)
            for kv in range(2):
                wis = [load_wb(W1S[kv * 2 + lh]) for lh in range(2)]
                w1v = [wa.rearrange("p (l h) -> p l h", h=256) for (wa, _) in wis]
                wkeys = [wk_ for (_, wk_) in wis]
                pb, pk = psa()
                for hc in range(2):
                    for l in range(32):
                        S.op('pe', lambda e, hc=hc, l=l, pb=pb: e.matmul(pb[:, hc:hc + 1], lhsT=w1v[l // 16][0:64, l % 16, hc * 128:(hc + 1) * 128], rhs=peT[0:64, kv, l:l + 1], start=(l == 0), stop=(l == 31)),
                             reads=wkeys + ['smallres'], writes=[pk])
                S.op('dve', lambda e, pb=pb, kv=kv: e.tensor_copy(out=bh[:, kv * 2:kv * 2 + 2], in_=pb[:, 0:2]), reads=[pk], writes=['bh'])
                for g in range(4):
                    base = (g % 2) * 64
                    for hc in range(2):
                        pb, pk = psa()
                        for l in range(32):
                            S.op('pe', lambda e, hc=hc, l=l, pb=pb, base=base, g=g: e.matmul(
                                pb[:, 0:127], lhsT=w1v[l // 16][base:base + 64, l % 16, hc * 128:(hc + 1) * 128],
                                rhs=rawv[base:base + 64, kv * 2 + g // 2, l:l + 16 * 126 + 1:16], start=(l == 0), stop=(l == 31)),
                                reads=wkeys + ['raw'], writes=[pk])
                        S.op('act', lambda e, pb=pb, hc=hc, g=g, kv=kv: e.activation(out=hidT[:, kv, hc, g * 127:(g + 1) * 127], in_=pb[:, 0:127], func=AF.Silu, bias=bh[:, kv * 2 + hc:kv * 2 + hc + 1], scale=1.0),
                             reads=[pk, 'bh'], writes=[('QO', 0)])
            pb, pk = psa()
            for hc in range(2):
                S.op('pe', lambda e, hc=hc, pb=pb: e.matmul(pb[:, 0:508], lhsT=W2k[:, hc, :], rhs=hidT[:, 0, hc, 0:508], start=(hc == 0), stop=(hc == 1)),
                     reads=[('QO', 0), 'smallres'], writes=[pk])
            normrope(pb, pk, 33, Cc[:, 0, :, 0:127], Cc[:, 1, :, 0:127], ['Cc'], kcT[:, 0:508], ['kcT'], n=508,
                     view=lambda a_: a_.rearrange("p (g n) -> p g n", g=4))
            pb, pk = psa()
            for g in range(4):
                for hc in range(2):
                    S.op('pe', lambda e, hc=hc, g=g, pb=pb: e.matmul(pb[0:127, g * 64:(g + 1) * 64], lhsT=hidT[:, 1, hc, g * 127:(g + 1) * 127], rhs=W2v[:, hc, :], start=(hc == 0), stop=(hc == 1)),
                         reads=[('QO', 0), 'smallres'], writes=[pk])
            S.op('dve', lambda e, pb=pb: e.tensor_copy(out=vcA[0:127, :, 0:64], in_=pb[0:127, 0:256].rearrange("p (g c) -> p g c", g=4)), reads=[pk], writes=['vcA'])
            if dbg and s == 0:
                dump('KT', KT[:], [128, 2, 2, SEQ], ['KT'], BF16)
                dump('VA', VA[:], [128, 2, 16, 260], ['VA'], BF16)
                dump('kcT', kcT[:], [128, 512], ['kcT'], BF16)
                dump('vcA', vcA[:], [128, 4, 97], ['vcA'], BF16)
                dump('hidT', hidT, [128, 2, 2, 512], [('QO', 0)], BF16)

            chk('compress')
            qT = QO[:, 0:8]
            OT = QO[:, 8:16]
            for qb in range(nqb):
                t0 = qb * TB
                first = (s == 0 and qb == 0)
                load_x(s, t0)
                S.dma('pool', 'mk', lambda e, t0=t0: e.dma_start(out=maskc[:], in_=C['c_maskc'][:, t0:t0 + 512]), writes=['maskc'])
                S.dma('sp', 'bon', lambda e, qb=qb: e.dma_start(out=bonus[:], in_=C['c_bonus'][:, qb * 4:qb * 4 + 4, :]), writes=['bonus'])
                chk('blk')
                make_hT()
                chk('mkh')
                rope_tables(s, t0)
                for j in range(8):
                    pb, pk = mmA(WinP[j], hT, HK)
                    normrope(pb, pk, 32, CSt[:, 0, :], CSt[:, 1, :], ['CSt'], qT[:, j, :], [('QO', 0)])
                for qi in range(4):
                    pb, pk = psa()
                    for kc in range(16):
                        S.op('pe', lambda e, kc=kc, qi=qi, pb=pb: e.matmul(pb[:, 0:48], lhsT=hT[:, kc, qi * 128:(qi + 1) * 128], rhs=WG[:, kc, :], start=(kc == 0), stop=(kc == 15)),
                             reads=['WG', ('hT', qi)], writes=[pk])
                    S.op('act', lambda e, qi=qi, pb=pb: e.activation(out=gates[:, qi, :], in_=pb[:, 0:48], func=AF.Sigmoid), reads=[pk], writes=['gates'])
                if dbg and first:
                    dump('qT', QO[:, 0:8], [128, 8, 512], [('QO', 0)], BF16)
                    dump('gates', gates[:], [128, 4, 48], ['gates'])

                chk('qproj')
                def finalize(pso, pok, h, br, pi, hp, first_write, with_imp):
                    rs4 = small[:, 0:4]
                    fac = small[:, 4:8]
                    S.op('dve', lambda e: e.tensor_scalar_max(out=rs4, in0=pso[:, :, 64], scalar1=1e-30), reads=[pok], writes=['rs4'])
                    S.op('dve', lambda e: e.reciprocal(out=rs4, in_=rs4), reads=['rs4'], writes=['rs4'])
                    S.op('dve', lambda e: e.tensor_tensor(out=fac, in0=rs4, in1=gates[:, :, 3 * h + br], op=ALU.mult), reads=['rs4', 'gates'], writes=['fac'])
                    fb = fac.unsqueeze(2).to_broadcast([128, 4, 64])
                    od = Otot[:, pi, :, hp * 64:(hp + 1) * 64]
                    if first_write:
                        S.op('dve', lambda e: e.tensor_tensor(out=od, in0=pso[:, :, 0:64], in1=fb, op=ALU.mult), reads=[pok, 'fac'], writes=[('otot', pi), 'Cc'])
                    else:
                        S.op('dve', lambda e: e.tensor_tensor(out=tmpo, in0=pso[:, :, 0:64], in1=fb, op=ALU.mult), reads=[pok, 'fac'], writes=[('tf', 3)])
                        S.op('pool', lambda e: e.tensor_tensor(out=od, in0=od, in1=tmpo, op=ALU.add), reads=[('tf', 3), ('otot', pi)], writes=[('otot', pi)])
                    if with_imp is not None:
                        rb_ = rs4.unsqueeze(2).to_broadcast([128, 4, 32])
                        if with_imp == 0:
                            S.op('dve', lambda e: e.tensor_tensor(out=imp, in0=pso[:, :, 65:97], in1=rb_, op=ALU.mult), reads=[pok, 'rs4'], writes=[('tf', 2)])
                        else:
                            S.op('dve', lambda e: e.tensor_tensor(out=tmpi, in0=pso[:, :, 65:97], in1=rb_, op=ALU.mult), reads=[pok, 'rs4'], writes=[('tf', 3)])
                            S.op('pool', lambda e: e.tensor_tensor(out=imp, in0=imp, in1=tmpi, op=ALU.add), reads=[('tf', 3), ('tf', 2)], writes=[('tf', 2)])

                for g in range(G):
                    base = (g % 2) * 64
                    heads = [4 * g + r for r in range(4)]

                    def qsl(h):
                        m_, rem = h // 8, h % 8
                        return 4 * m_ + rem % 4
                    SCB = [(PSS[0], ('pss', 0)), (PSS[1], ('pss', 1)), (PSA[0], ('psa', 0)), (PSA[1], ('psa', 1)), (PSA[2], ('psa', 2))]
                    PTB = [(BFW[:, i, :], k_) for i, k_ in enumerate([('pt', 0), ('pt', 1), ('pt', 2), ('tb', 0), ('tb', 1)])]

                    def sc_next():
                        i = nxt('pss', 5)
                        return SCB[i]

                    def pt_next():
                        i = nxt('pt', 5)
                        return PTB[i]
                    cs = []
                    for r, h in enumerate(heads):
                        jq = qsl(h)
                        sb_, sk_ = sc_next()
                        S.op('pe', lambda e: e.matmul(sb_[0:127, :], lhsT=kcT[base:base + 64, g * 127:(g + 1) * 127], rhs=qT[base:base + 64, jq, :], start=True, stop=True),
                             reads=['kcT', ('QO', 0)], writes=[sk_])
                        cs.append((sb_, sk_))
                    for r, h in enumerate(heads):
                        sb_, sk_ = cs[r]
                        pb_, pk_ = pt_next()
                        S.op('act', lambda e: e.activation(out=pb_[0:127, :], in_=sb_[0:127, :], func=AF.Exp, scale=0.125), reads=[sk_], writes=[pk_])
                        S.op('pool', lambda e: e.tensor_tensor(out=pb_[0:127, :], in0=pb_[0:127, :], in1=maskc[0:127, :], op=ALU.mult),
                             reads=[pk_, 'maskc'], writes=[pk_])
                        oi = nxt('pso', 2)
                        for qi in range(4):
                            S.op('pe', lambda e: e.matmul(PSO[oi][:, qi, 0:97], lhsT=pb_[0:127, qi * 128:(qi + 1) * 128], rhs=vcA[0:127, g, :], start=True, stop=True),
                                 reads=[pk_, 'vcA'], writes=[('pso', oi)])
                        finalize(PSO[oi], ('pso', oi), h, 0, r // 2, r % 2, True, (r if qb >= 2 else None))
                    def emit_selection_dve(g=g):
                        S.op('dve', lambda e: e.tensor_tensor(out=scb, in0=imp, in1=bonus[:, :, :], op=ALU.add), reads=[('tf', 2), 'bonus'], writes=[('tf', 2)])
                        for qi in range(4):
                            S.op('dve', lambda e, qi=qi: e.max(out=top8[:, 0:8], in_=scb[:, qi, :]), reads=[('tf', 2)], writes=['top8'])
                            S.op('dve', lambda e, qi=qi: e.match_replace(out=sc2[:], in_to_replace=top8[:, 0:8], in_values=scb[:, qi, :], imm_value=-1e30), reads=[('tf', 2), 'top8'], writes=['sc2'])
                            S.op('dve', lambda e: e.max(out=top8[:, 8:16], in_=sc2[:]), reads=['sc2'], writes=['top8'])
                            S.op('dve', lambda e, qi=qi: e.tensor_scalar(out=sc2[:], in0=scb[:, qi, :], scalar1=top8[:, 15:16], scalar2=None, op0=ALU.is_ge), reads=[('tf', 2), 'top8'], writes=['sc2'])
                            S.op('dve', lambda e, qi=qi: e.tensor_scalar(out=selb[:, qi, :], in0=sc2[:], scalar1=-1.0, scalar2=30000.0, op0=ALU.add, op1=ALU.mult), reads=['sc2'], writes=['selb'])

                    def emit_selection_pe(g=g):
                        ti = nxt('pst', 1)
                        for qi in range(4):
                            S.op('pe', lambda e, qi=qi, ti=ti: e.transpose(out=PST[0:32, ti, qi * 128:(qi + 1) * 128], in_=selb[:, qi, :], identity=ident[:]),
                                 reads=['selb', 'ident'], writes=[('pst', ti)])
                        S.op('dve', lambda e, ti=ti: e.tensor_copy(out=selbT[0:32, :], in_=PST[0:32, ti, :]), reads=[('pst', ti)], writes=['selbT'])
                        if dbg and s == 0 and qb == 2 and g == 0:
                            dump('selb', selb[:], [128, 4, 32], ['selb'], BF16)
                            dump('imp', imp, [128, 4, 32], ['imp'])
                    if qb >= 2:
                        emit_selection_dve()
                    steps = []
                    qp_rr = [0]
                    cb_after = {}
                    order = [(0, 2), (1, 2), (0, 1), (1, 1), (2, 2), (3, 2), (2, 1), (3, 1)]
                    for ui, (r, br) in enumerate(order):
                        h = heads[r]
                        kts = list(range(0, 4 * qb + 4)) if br == 1 else list(range(max(4 * qb - 4, 0), 4 * qb + 4))
                        unit = {'h': h, 'r': r, 'br': br, 'oi': None, 'qp': None, 'newhead': br == 2}
                        for kt in kts:
                            steps.append({'u': unit, 'kt': kt, 'first': kt == kts[0], 'last': kt == kts[-1]})
                        if ui == 2 and qb >= 2:
                            steps[len(steps) - len(kts)]['pre'] = emit_selection_pe

                    hq = {}

                    def emit_score(st):
                        u, kt = st['u'], st['kt']
                        br, jq = u['br'], qsl(u['h'])
                        dloc = kt - 4 * qb
                        lo = max(dloc, 0)
                        hi = 3 if br == 1 else min(dloc + 4, 3)
                        c0 = lo * 128
                        n = (hi - lo + 1) * 128
                        sb_, sk_ = sc_next()
                        use_mask = (br == 1 and qb >= 2)
                        if st['first'] and u['newhead']:
                            qs = (g % 2) * 2 + (qp_rr[0] % 2)
                            qp_rr[0] += 1
                            hq[u['h']] = qs
                            S.op('dve', lambda e: e.tensor_copy(out=QP[base:base + 64, qs, :], in_=qT[base:base + 64, jq, :]), reads=[('QO', 0)], writes=[('qp', qs)])
                        qs = hq[u['h']]
                        S.op('pe', lambda e: e.matmul(
                            sb_[:, 0:n], lhsT=KT[:, br - 1, g // 2, kt * 128:(kt + 1) * 128], rhs=QP[:, qs, c0:c0 + n], start=True, stop=(not use_mask)),
                            reads=['KT', ('qp', qs)], writes=[sk_])
                        if use_mask:
                            S.op('pe', lambda e: e.matmul(sb_[:, 0:n], lhsT=emat[:, kt, :], rhs=selbT[:, c0:c0 + n], start=False, stop=True),
                                 reads=['emat', 'selbT'], writes=[sk_])
                        st['ctx'] = (dloc, lo, hi, c0, n, sb_, sk_)

                    def emit_rest(st):
                        u, kt = st['u'], st['kt']
                        br = u['br']
                        dloc, lo, hi, c0, n, sb_, sk_ = st['ctx']
                        if st['first']:
                            u['oi'] = nxt('pso', 2)
                            oi0 = u['oi']
                            S.op('pe', lambda e: e.matmul(PSO[oi0][:, :, :], lhsT=zeros128[:], rhs=emat[:, 0:4, :], start=True, stop=False),
                                 reads=['emat', 'zeros128'], writes=[('pso', oi0)])
                        oi = u['oi']
                        pb_, pk_ = pt_next()
                        S.op('act', lambda e: e.activation(out=pb_[:, 0:n], in_=sb_[:, 0:n], func=AF.Exp, scale=0.125), reads=[sk_], writes=[pk_])
                        if dloc >= 0:
                            S.op('pool', lambda e: e.tensor_tensor(out=pb_[:, 0:128], in0=pb_[:, 0:128], in1=tri[:, 0, :], op=ALU.mult),
                                 reads=[pk_, 'tri'], writes=[pk_])
                        if br == 2 and 0 <= dloc + 4 <= 3:
                            cf = (hi - lo) * 128
                            S.op('pool', lambda e: e.tensor_tensor(out=pb_[:, cf:cf + 128], in0=pb_[:, cf:cf + 128], in1=tri[:, 1, :], op=ALU.mult),
                                 reads=[pk_, 'tri'], writes=[pk_])
                        for qi in range(lo, hi + 1):
                            sp = bool(st['last'] and qi == hi)
                            S.op('pe', lambda e: e.matmul(
                                PSO[oi][:, qi, 0:65], lhsT=pb_[:, (qi - lo) * 128:(qi - lo + 1) * 128], rhs=VA[:, br - 1, kt, g * 65:(g + 1) * 65], start=False, stop=sp),
                                reads=[pk_, 'VA'], writes=[('pso', oi)])
                        if st['last']:
                            finalize(PSO[oi], ('pso', oi), u['h'], br, u['r'] // 2, u['r'] % 2, False, None)

                    LA = 3
                    n_sc = 0
                    for i_st in range(len(steps)):
                        while n_sc < min(len(steps), i_st + 1 + LA):
                            if 'pre' in steps[n_sc]:
                                steps[n_sc]['pre']()
                            emit_score(steps[n_sc])
                            n_sc += 1
                        emit_rest(steps[i_st])
                        if i_st in cb_after:
                            cb_after[i_st]()
                    for pi in range(2):
                        S.op('act', lambda e, pi=pi: e.copy(out=Obf[:, pi], in_=Otot[:, pi]), reads=[('otot', pi)], writes=[('obf', pi)])
                        ti = nxt('pst', 1)
                        for qi in range(4):
                            S.op('pe', lambda e, pi=pi, qi=qi, ti=ti: e.transpose(out=PST[:, ti, qi * 128:(qi + 1) * 128], in_=Obf[:, pi, qi, :], identity=ident[:]),
                                 reads=[('obf', pi), 'ident'], writes=[('pst', ti)])
                        S.op('act', lambda e, pi=pi, ti=ti, g=g: e.copy(out=OT[:, 2 * g + pi, :], in_=PST[:, ti, :]), reads=[('pst', ti)], writes=[('QO', 1)])
                if dbg and first:
                    dump('OT', QO[:, 8:16], [128, 8, 512], [('QO', 1)], BF16)

                chk('attn')
                QOf = QO[:].rearrange("p a f -> p (a f)")
                BFf = BFW[:].rearrange("p a f -> p (a f)")
                ex_y = [(QOf[:, i * 2048:(i + 1) * 2048].rearrange("p (k c) -> p k c", c=128), ('QOs', i), 'pxq%d' % i) for i in range(2)]
                ex_y.append((BFf[:, 0:2048].rearrange("p (k c) -> p k c", c=128), ('BFs', 0), 'pxb0'))
                S.alias_in([('QOs', 0), ('QOs', 1)], [('QO', 0)])
                S.alias_in([('BFs', 0)], XK + [('tb', 1)])
                set_pan_slots(ex_y)
                def pool_group(gi):
                    w = (2, 4, 8, 16)[gi]
                    for j in range(2):
                        pb, pk = mmA(WinP[16 + gi * 2 + j], hT, HK)
                        U = ubuf[:, 0, :]
                        S.op('pool', lambda e, gi=gi, j=j: e.tensor_copy(out=ubuf[:, 0, 0:16], in_=carry[:, gi * 2 + j, :]), reads=['carry'], writes=['ub0'])
                        S.op('act', lambda e, pb=pb: e.copy(out=ubuf[:, 0, 16:528], in_=pb[:]), reads=[pk], writes=['ub0'])
                        S.op('pool', lambda e, gi=gi, j=j: e.tensor_copy(out=carry[:, gi * 2 + j, :], in_=ubuf[:, 0, 512:528]), reads=['ub0'], writes=['carry'])
                        TFf = TF[:].rearrange("p a f -> p (a f)")
                        ubs = [ubuf[:, 0, :], TFf[:, 0:528], TFf[:, 528:1056]]
                        ubk = [['ub0'], [('tf', 0), ('tf', 1)], [('tf', 1), ('tf', 2)]]
                        cur = 0
                        for kstep in range(gi + 1):
                            sh = 1 << kstep
                            nx_ = 1 if cur != 1 else 2
                            S.op('pool', lambda e, cur=cur, nx_=nx_, sh=sh, ubs=ubs: e.tensor_tensor(out=ubs[nx_][:, sh:528], in0=ubs[cur][:, sh:528], in1=ubs[cur][:, 0:528 - sh], op=ALU.add),
                                 reads=ubk[cur], writes=ubk[nx_])
                            cur = nx_
                        S.op('dve', lambda e, cur=cur, j=j, w=w, ubs=ubs: e.scalar_tensor_tensor(out=PLT[:, gi % 2, j, :], in0=ubs[cur][:, 16:528], scalar=1.0 / w, in1=ubuf[:, 0, 16:528], op0=ALU.mult, op1=ALU.subtract),
                             reads=ubk[cur] + ['ub0'], writes=[('PLT', gi % 2)])
                        if qb == 0:
                            S.op('dve', lambda e, cur=cur, gi=gi, ubs=ubs: e.tensor_tensor(out=small[:, 8:24], in0=ubs[cur][:, 16:32], in1=icnt[:, gi, :], op=ALU.mult), reads=ubk[cur] + ['icnt'], writes=['t16'])
                            S.op('dve', lambda e, j=j: e.tensor_tensor(out=PLT[:, gi % 2, j, 0:16], in0=small[:, 8:24], in1=ubuf[:, 0, 16:32], op=ALU.subtract), reads=['t16', 'ub0', ('PLT', gi % 2)], writes=[('PLT', gi % 2)])
                pool_group(0)
                for c in range(16):
                    gi = c // 4
                    pa, pak = psa()
                    cb = (c % 2) * 64
                    S.op('pe', lambda e, pa=pa, c=c, cb=cb: e.matmul(pa[:], lhsT=Who[cb:cb + 64, c // 2, :], rhs=OT[cb:cb + 64, c // 2, :], start=True, stop=True),
                         reads=['smallres', ('QO', 1)], writes=[pak])
                    pm, pmk = mmA(WinP[24 + c], hT, HK)
                    S.op('act', lambda e, pm=pm: e.activation(out=TF[:, 0, :], in_=pm[:], func=AF.Sigmoid), reads=[pmk], writes=[('tf', 0)])
                    S.op('dve', lambda e, pa=pa: e.tensor_tensor(out=TF[:, 1, :], in0=pa[:], in1=TF[:, 0, :], op=ALU.mult), reads=[pak, ('tf', 0)], writes=[('tf', 1)])
                    pp, ppk = psa()
                    for j in range(2):
                        S.op('pe', lambda e, pp=pp, j=j, c=c, gi=gi: e.matmul(pp[:], lhsT=Wpool[:, gi * 2 + j, (c % 4) * 128:(c % 4 + 1) * 128], rhs=PLT[:, gi % 2, j, :], start=(j == 0), stop=(j == 1)),
                             reads=['Wpool', ('PLT', gi % 2)], writes=[ppk])
                    pm1, pm1k = mmA(WinP[40 + c], hT, HK)
                    S.op('act', lambda e, pm1=pm1: e.activation(out=TF[:, 2, :], in_=pm1[:], func=AF.Sigmoid), reads=[pm1k], writes=[('tf', 2)])
                    S.op('dve', lambda e, pp=pp, c=c: e.scalar_tensor_tensor(out=TF[:, 3, :], in0=pp[:], scalar=pscale[:, c:c + 1], in1=TF[:, 2, :], op0=ALU.mult, op1=ALU.mult),
                         reads=[ppk, ('tf', 2), 'pscale'], writes=[('tf', 3)])
                    S.op('pool', lambda e, c=c: e.tensor_tensor(out=yT[:, c, :], in0=TF[:, 1, :], in1=TF[:, 3, :], op=ALU.add), reads=[('tf', 1), ('tf', 3)], writes=YK + ['raw'])
                    if c % 4 == 0 and gi < 3:
                        pool_group(gi + 1)
                if dbg and first:
                    dump('yT', yT[:], [128, 16, 512], YK, BF16)

                chk('ymix')
                S.alias_out([('QOs', 0), ('QOs', 1)], [('QO', 0)])
                S.alias_out([('BFs', 0)], XK + [('tb', 1)])
                set_pan_slots([])
                S.alias_in([('QOw', 0)], [('QO', 0)])
                set_wb_slots([(QO[:, 0:8].rearrange("p a f -> p (a f)"), ('QOw', 0), 'wbx0')])
                for c8 in range(8):
                    wb_ap, wb_key = load_wb(WoS[c8])
                    wv = wb_ap.rearrange("p (k c) -> p k c", c=256)
                    for qi in range(4):
                        pb, pk = psa()
                        for kc in range(16):
                            S.op('pe', lambda e, kc=kc, qi=qi, pb=pb, wv=wv: e.matmul(pb[:, 0:256], lhsT=yT[:, kc, qi * 128:(qi + 1) * 128], rhs=wv[:, kc, :], start=(kc == 0), stop=(kc == 15)),
                                 reads=[wb_key, ('yT', qi)], writes=[pk])
                        xs = X1[:, qi, c8 * 256:(c8 + 1) * 256]
                        S.op('dve', lambda e, xs=xs, pb=pb: e.tensor_tensor(out=xs, in0=pb[:, 0:256], in1=xs, op=ALU.add), reads=[pk, ('X1', qi)], writes=[('X1', qi)])
                S.alias_out([('QOw', 0)], [('QO', 0)])
                set_wb_slots([])
                if dbg and first:
                    dump('x1', X1[:], [128, 4, 2048], [('X1', qi) for qi in range(4)])

                chk('wout')
                make_hT()
                chk('mkh2')
                pb, pk = psa()
                for qi in range(4):
                    for kc in range(16):
                        S.op('pe', lambda e, kc=kc, qi=qi: e.matmul(pb[:, qi * 20:(qi + 1) * 20], lhsT=hT[:, kc, qi * 128:(qi + 1) * 128], rhs=Wr[:, kc, :], start=(kc == 0), stop=(kc == 15)),
                             reads=['Wr', ('hT', qi)], writes=[pk])
                R = TF[:, 3, :]
                RK = [('tf', 3)]
                v3 = lambda ap_, a_: ap_.rearrange("p (a b) -> p a b", a=a_)
                lgg, le = R[:, 0:16], R[:, 16:80]
                mx, se, pg = R[:, 80:84], R[:, 84:88], R[:, 88:92]
                ex, gm, pen = R[:, 96:112], R[:, 112:128], R[:, 128:144]
                lm = R[:, 144:208]
                t8 = R[:, 208:240]
                dv, e2, den, w1, w2 = R[:, 240:244], R[:, 244:248], R[:, 248:252], R[:, 252:256], R[:, 256:260]
                m1, m2 = R[:, 272:336], R[:, 336:400]
                pb3 = v3(pb[:, 0:80], 4)
                bc = lambda ap_, n_: ap_.unsqueeze(2).to_broadcast([128, ap_.shape[1], n_])
                S.op('dve', lambda e: e.tensor_tensor(out=v3(lgg, 4), in0=pb3[:, :, 0:4], in1=brt[:, 0:4].unsqueeze(1).to_broadcast([128, 4, 4]), op=ALU.add), reads=[pk, 'brt'], writes=RK)
                S.op('dve', lambda e: e.tensor_tensor(out=v3(le, 4), in0=pb3[:, :, 4:20], in1=brt[:, 4:20].unsqueeze(1).to_broadcast([128, 4, 16]), op=ALU.add), reads=[pk, 'brt'], writes=RK)
                S.op('dve', lambda e: e.tensor_reduce(out=mx, in_=v3(lgg, 4), axis=AX.X, op=ALU.max), reads=RK, writes=RK)
                S.op('dve', lambda e: e.tensor_tensor(out=v3(ex, 4), in0=v3(lgg, 4), in1=bc(mx, 4), op=ALU.subtract), reads=RK, writes=RK)
                S.op('act', lambda e: e.activation(out=ex, in_=ex, func=AF.Exp), reads=RK, writes=RK)
                S.op('dve', lambda e: e.tensor_reduce(out=se, in_=v3(ex, 4), axis=AX.X, op=ALU.add), reads=RK, writes=RK)
                S.op('dve', lambda e: e.reciprocal(out=pg, in_=se), reads=RK, writes=RK)
                S.op('dve', lambda e: e.tensor_tensor(out=v3(gm, 4), in0=v3(lgg, 4), in1=bc(mx, 4), op=ALU.is_ge), reads=RK, writes=RK)
                S.op('dve', lambda e: e.tensor_scalar(out=pen, in0=gm, scalar1=-1.0, scalar2=1e30, op0=ALU.add, op1=ALU.mult), reads=RK, writes=RK)
                S.op('dve', lambda e: e.tensor_tensor(out=v3(lm, 16), in0=v3(le, 16), in1=bc(pen, 4), op=ALU.add), reads=RK, writes=RK)
                for qi in range(4):
                    S.op('dve', lambda e, qi=qi: e.max(out=t8[:, qi * 8:(qi + 1) * 8], in_=lm[:, qi * 16:(qi + 1) * 16]), reads=RK, writes=RK)
                t83 = v3(t8, 4)
                S.op('dve', lambda e: e.tensor_tensor(out=dv, in0=t83[:, :, 1], in1=t83[:, :, 0], op=ALU.subtract), reads=RK, writes=RK)
                S.op('act', lambda e: e.activation(out=e2, in_=dv, func=AF.Exp), reads=RK, writes=RK)
                S.op('dve', lambda e: e.tensor_scalar_add(out=den, in0=e2, scalar1=1.0), reads=RK, writes=RK)
                S.op('dve', lambda e: e.reciprocal(out=den, in_=den), reads=RK, writes=RK)
                S.op('dve', lambda e: e.tensor_tensor(out=w1, in0=pg, in1=den, op=ALU.mult), reads=RK, writes=RK)
                S.op('dve', lambda e: e.tensor_tensor(out=w2, in0=w1, in1=e2, op=ALU.mult), reads=RK, writes=RK)
                S.op('dve', lambda e: e.tensor_tensor(out=v3(m1, 4), in0=v3(lm, 4), in1=bc(t83[:, :, 0], 16), op=ALU.is_equal), reads=RK, writes=RK)
                S.op('dve', lambda e: e.tensor_tensor(out=v3(m1, 4), in0=v3(m1, 4), in1=bc(w1, 16), op=ALU.mult), reads=RK, writes=RK)
                S.op('dve', lambda e: e.tensor_tensor(out=v3(m2, 4), in0=v3(lm, 4), in1=bc(t83[:, :, 1], 16), op=ALU.is_equal), reads=RK, writes=RK)
                S.op('dve', lambda e: e.tensor_tensor(out=v3(m2, 4), in0=v3(m2, 4), in1=bc(w2, 16), op=ALU.mult), reads=RK, writes=RK)
                S.op('dve', lambda e: e.tensor_tensor(out=comb[:].rearrange("p a b -> p (a b)"), in0=m1, in1=m2, op=ALU.add), reads=RK, writes=['comb'])
                S.op('dve', lambda e: e.tensor_copy(out=combb[:], in_=comb[:]), reads=['comb'], writes=['combb'])
                ti = nxt('pst', 1)
                for qi in range(4):
                    S.op('pe', lambda e, qi=qi, ti=ti: e.transpose(out=PST[0:16, ti, qi * 128:(qi + 1) * 128], in_=combb[:, qi, :], identity=ident[:]), reads=['combb', 'ident'], writes=[('pst', ti)])
                S.op('act', lambda e, ti=ti: e.copy(out=combT[:, :], in_=PST[0:16, ti, :]), reads=[('pst', ti)], writes=['combT'])
                if dbg and first:
                    dump('comb', comb[:], [128, 4, 16], ['comb'])
                    dump('h2T', hT[:], [128, 16, 512], HK, BF16)
                yTf = yT[:].rearrange("p a f -> p (a f)")
                ex_m = [(yTf[:, i * 2048:(i + 1) * 2048].rearrange("p (k c) -> p k c", c=128), ('yTs', i), 'pxy%d' % i) for i in range(4)]
                S.alias_in([('yTs', i) for i in range(4)], YK + ['raw'])
                set_pan_slots(ex_m)
                for eg in range(4):
                    par = eg % 2
                    hw = QO[:, par * 8:(par + 1) * 8]
                    for el in range(4):
                        e_ = eg * 4 + el
                        pc, pck = psa()
                        S.op('pe', lambda e, pc=pc, e_=e_: e.matmul(pc[:], lhsT=sele[0:16, e_, :], rhs=combT[:, :], start=True, stop=True), reads=['sele', 'combT'], writes=[pck])
                        S.op('act', lambda e, pc=pc: e.copy(out=TF[:, 2, :], in_=pc[:]), reads=[pck], writes=[('tf', 2)])
                        for hc in range(2):
                            pg_, pgk = mmA(WguP[e_ * 4 + hc], hT, HK)
                            S.op('act', lambda e, pg_=pg_: e.activation(out=TF[:, 0, :], in_=pg_[:], func=AF.Silu), reads=[pgk], writes=[('tf', 0)])
                            pu, puk = mmA(WguP[e_ * 4 + 2 + hc], hT, HK)
                            S.op('dve', lambda e, pu=pu: e.tensor_tensor(out=TF[:, 1, :], in0=pu[:], in1=TF[:, 0, :], op=ALU.mult), reads=[puk, ('tf', 0)], writes=[('tf', 1)])
                            S.op('pool', lambda e, hw=hw, el=el, hc=hc: e.tensor_tensor(out=hw[:, el * 2 + hc, :], in0=TF[:, 1, :], in1=TF[:, 2, :], op=ALU.mult),
                                 reads=[('tf', 1), ('tf', 2)], writes=[('QO', par)])
                    for cc in range(4):
                        wb_ap, wb_key = load_wb(WdS[eg * 4 + cc])
                        wv = wb_ap.rearrange("p (k c) -> p k c", c=512)
                        for qi in range(4):
                            pb, pk = psa()
                            for k8 in range(8):
                                S.op('pe', lambda e, k8=k8, qi=qi, pb=pb, wv=wv, hw=hw: e.matmul(pb[:], lhsT=hw[:, k8, qi * 128:(qi + 1) * 128], rhs=wv[:, k8, :], start=(k8 == 0), stop=(k8 == 7)),
                                     reads=[wb_key, ('QO', par)], writes=[pk])
                            xs = X1[:, qi, cc * 512:(cc + 1) * 512]
                            S.op('dve', lambda e, xs=xs, pb=pb: e.tensor_tensor(out=xs, in0=pb[:], in1=xs, op=ALU.add), reads=[pk, ('X1', qi)], writes=[('X1', qi)])
                S.alias_out([('yTs', i) for i in range(4)], YK + ['raw'])
                set_pan_slots([])
                chk('moe')
                for qi in range(4):
                    S.dma('sp', 'out%d' % qi, lambda e, qi=qi: e.dma_start(out=out[s, t0 + qi * 128:t0 + (qi + 1) * 128, :], in_=X1[:, qi, :]), reads=[('X1', qi)], writes=['out'])
        S.barrier()
        S.emit()
    return nc, dbg_out


_CACHE = {}


def _layout_inputs(inputs):
    consts = make_consts()
    wts = {}
    for k in WEIGHT_SHAPES:
        a = np.asarray(inputs[k])
        wts[k] = np.ascontiguousarray(a.reshape(a.shape[1:]), dtype=np.float32)
    xs = np.asarray(inputs['x'], dtype=np.float32)
    pos = np.asarray(inputs['positions'], dtype=np.int32)
    in_maps = []
    for c in range(NCORES):
        m = {'x': np.ascontiguousarray(xs[c * NSEQ:(c + 1) * NSEQ]), 'positions': np.ascontiguousarray(pos[c * NSEQ:(c + 1) * NSEQ])}
        m.update(wts)
        m.update(consts)
        in_maps.append(m)
    return in_maps


def kernel(**inputs):
    if 'nc' not in _CACHE:
        _CACHE['nc'] = build()[0]
    nc = _CACHE['nc']
    in_maps = _layout_inputs(inputs)
    res = run_bass_kernel_spmd(nc, in_maps, core_ids=list(range(NCORES)))
    outs = [np.asarray(r['out']) for r in res.results]
    return np.concatenate(outs, axis=0).astype(np.float32)
```

```python
import numpy as np
from contextlib import ExitStack
import concourse.bass as bass
import concourse.mybir as mybir
from concourse.bass_utils import run_bass_kernel_spmd

F32 = mybir.dt.float32
BF16 = mybir.dt.bfloat16
I32 = mybir.dt.int32
ALU = mybir.AluOpType
AF = mybir.ActivationFunctionType
AX = mybir.AxisListType

D = 2048
SEQ = 2048
NSEQ = 2
NCORES = 8
TB = 512
NQB = SEQ // TB
H = 16
G = 4
NCMP = 127
INW = 7728
OFF_Q, OFF_KC, OFF_VC, OFF_KS, OFF_VS, OFF_KW, OFF_VW, OFF_GATE, OFF_POOL, OFF_MERGE = (
    0, 1024, 1280, 1536, 1792, 2048, 2304, 2560, 2608, 3632)
NE = 16
TWO_PI = 6.283185307179586
PI_SAFE = 3.1415925


class _Rec:
    def __init__(self):
        self.call = None

    def __getattr__(self, name):
        def f(*a, **k):
            assert self.call is None
            self.call = (name, a, k)
            return self
        return f


def _replay(fn):
    rec = _Rec()
    fn(rec)
    name, a, k = rec.call
    return lambda e: getattr(e, name)(*a, **k)


class Sched:
    ENG = ('pe', 'act', 'dve', 'pool', 'sp')

    def __init__(self, nc, es):
        self.nc = nc
        self.es = es
        self.lists = {e: [] for e in self.ENG}
        self.sem = {e: es.enter_context(nc.semaphore('s_' + e)) for e in self.ENG}
        self.cnt = {e: 0 for e in self.ENG}
        self.seen = {e: {} for e in self.ENG}
        self.lastw = {}
        self.readers = {}
        self.dsem = {}

    def _deps(self, reads, writes):
        deps = []
        for k in reads:
            d = self.lastw.get(k)
            if d is not None:
                deps.append(d)
        for k in writes:
            d = self.lastw.get(k)
            if d is not None:
                deps.append(d)
            r = self.readers.get(k)
            if r:
                deps.extend(r.values())
        return deps

    def _emit_waits(self, eng, deps):
        need = {}
        seen = self.seen[eng]
        for (sname, sem, val) in deps:
            if eng == 'pe' and sname == 'Epe':
                continue
            if seen.get(sname, 0) < val:
                if sname not in need or need[sname][1] < val:
                    need[sname] = (sem, val)
        for sname, (sem, val) in need.items():
            seen[sname] = val
            self.lists[eng].append(lambda e, sem=sem, val=val: e.wait_ge(sem, val))

    def _reg(self, dep, reads, writes):
        for k in writes:
            self.lastw[k] = dep
            self.readers[k] = {}
        for k in reads:
            if k not in writes:
                r = self.readers.setdefault(k, {})
                o = r.get(dep[0])
                if o is None or o[2] < dep[2]:
                    r[dep[0]] = dep

    dead = False

    def op(self, eng, fn, reads=(), writes=()):
        if self.dead:
            return
        fn = _replay(fn)
        self._emit_waits(eng, self._deps(reads, writes))
        self.cnt[eng] += 1
        sem = self.sem[eng]
        self.lists[eng].append(lambda e, fn=fn, sem=sem: fn(e).then_inc(sem, 1))
        dep = ('E' + eng, sem, self.cnt[eng])
        self._reg(dep, reads, writes)

    def dma(self, eng, slot, fn, reads=(), writes=()):
        if self.dead:
            return
        fn = _replay(fn)
        self._emit_waits(eng, self._deps(reads, writes))
        if slot not in self.dsem:
            self.dsem[slot] = [self.es.enter_context(self.nc.semaphore('d_' + slot)), 0]
        ent = self.dsem[slot]
        ent[1] += 16
        sem = ent[0]
        self.lists[eng].append(lambda e, fn=fn, sem=sem: fn(e).then_inc(sem, 16))
        dep = ('D' + slot, sem, ent[1])
        self._reg(dep, reads, writes)

    def alias_in(self, new_keys, old_keys):
        acc = {}
        for k in old_keys:
            for d in [self.lastw.get(k)] + list(self.readers.get(k, {}).values()):
                if d is not None and (d[0] not in acc or acc[d[0]][2] < d[2]):
                    acc[d[0]] = d
        for k in new_keys:
            self.lastw.pop(k, None)
            self.readers[k] = dict(acc)

    def alias_out(self, new_keys, old_keys):
        acc = {}
        for k in new_keys:
            for d in [self.lastw.get(k)] + list(self.readers.get(k, {}).values()):
                if d is not None and (d[0] not in acc or acc[d[0]][2] < d[2]):
                    acc[d[0]] = d
        for k in old_keys:
            r = self.readers.setdefault(k, {})
            for n_, d in acc.items():
                if n_ not in r or r[n_][2] < d[2]:
                    r[n_] = d

    def barrier(self):
        deps = [('E' + e, self.sem[e], self.cnt[e]) for e in self.ENG if self.cnt[e] > 0]
        deps += [('D' + s, ent[0], ent[1]) for s, ent in self.dsem.items()]
        for e in self.ENG:
            self._emit_waits(e, deps)

    def emit(self):
        nc = self.nc
        L = self.lists
        with nc.Block() as block:
            @block.tensor
            def _(e):
                for f in L['pe']:
                    f(e)

            @block.scalar
            def _(e):
                for f in L['act']:
                    f(e)

            @block.vector
            def _(e):
                for f in L['dve']:
                    f(e)

            @block.gpsimd
            def _(e):
                for f in L['pool']:
                    f(e)

            @block.sync
            def _(e):
                for f in L['sp']:
                    f(e)


def make_consts():
    c = {}
    tri = np.zeros((2, 128, 128), np.float32)
    k = np.arange(128)[:, None]
    q = np.arange(128)[None, :]
    tri[0] = (k <= q)
    tri[1] = (k > q)
    c['c_tri'] = tri.transpose(1, 0, 2).copy()
    n = np.arange(NCMP)[:, None]
    t = np.arange(SEQ)[None, :]
    mk = np.zeros((128, SEQ), np.float32)
    mk[:NCMP] = (16 * n + 31 <= t)
    c['c_maskc'] = mk
    tt = np.arange(SEQ)
    tb = tt // 64
    j = np.arange(32)[None, :]
    dist = tb[:, None] - j
    valid = dist >= 0
    forced = (j == 0) | (valid & (dist < 2))
    bonus = np.where(valid, 1e4 * forced.astype(np.float32), -1e30).astype(np.float32)
    c['c_bonus'] = bonus.reshape(16, 128, 32).transpose(1, 0, 2).copy()
    em = np.zeros((32, 16, 128), np.float32)
    for kt in range(16):
        for kk in range(128):
            em[2 * kt + kk // 64, kt, kk] = 1.0
    emp = np.zeros((128, 16, 128), np.float32)
    emp[:32] = em
    c['c_emat'] = emp
    a0 = np.arange(NCMP)[:, None] * 16
    b0 = np.arange(32)[None, :] * 64
    ov = np.clip(np.minimum(a0 + 32, b0 + 64) - np.maximum(a0, b0), 0, None) / 32.0
    ovp = np.zeros((128, 33), np.float32)
    ovp[:NCMP, 0] = 1.0
    ovp[:NCMP, 1:] = ov
    c['c_ov'] = ovp
    inv_freq = (500000.0 ** (-np.arange(0, 16, 2, dtype=np.float32) / 16)).astype(np.float32)
    fr = np.zeros((128, 1), np.float32)
    for p in range(128):
        if p % 64 < 16:
            fr[p, 0] = inv_freq[p % 8]
    c['c_freq'] = fr
    rm = np.zeros((128, 128), np.float32)
    for b in (0, 64):
        for d in range(8):
            rm[b + d + 8, b + d] = -1.0
            rm[b + d, b + d + 8] = 1.0
    c['c_rm'] = rm
    ob = np.zeros((128, 128), np.float32)
    ob[:64, :64] = 1.0 / 64
    ob[64:, 64:] = 1.0 / 64
    c['c_onesblk'] = ob
    se = np.zeros((128, 16, 128), np.float32)
    for e in range(16):
        se[e, e, :] = 1.0
    c['c_sele'] = se
    ic = np.zeros((128, 4, 16), np.float32)
    for gi, w in enumerate((2, 4, 8, 16)):
        for xx in range(16):
            ic[:, gi, xx] = 1.0 / min(xx + 1, w)
    c['c_icnt'] = ic
    return c


CONST_SHAPES = {
    'c_tri': [128, 2, 128], 'c_maskc': [128, SEQ], 'c_bonus': [128, 16, 32], 'c_emat': [128, 16, 128],
    'c_ov': [128, 33], 'c_freq': [128, 1], 'c_rm': [128, 128], 'c_onesblk': [128, 128],
    'c_sele': [128, 16, 128], 'c_icnt': [128, 4, 16],
}

WEIGHT_SHAPES = {
    'attn_norm_g': [D], 'w_in': [D, INW], 'q_norm_g': [64], 'k_norm_cmp_g': [64], 'k_norm_slc_g': [64],
    'k_norm_swa_g': [64], 'cmp_pos_emb_k': [32, 64], 'cmp_w1_k': [2048, 256], 'cmp_w2_k': [256, 64],
    'cmp_pos_emb_v': [32, 64], 'cmp_w1_v': [2048, 256], 'cmp_w2_v': [256, 64],
    'w_head_out': [16, 64, 128], 'w_pool': [4, 256, 512], 'pool_scale': [D], 'w_out': [D, D],
    'ffn_norm_g': [D], 'w_router_group': [D, 4], 'b_router_group': [4], 'w_router_expert': [D, 16],
    'b_router_expert': [16], 'w_expert_gate': [16, D, 256], 'w_expert_up': [16, D, 256],
    'w_expert_down': [16, 256, D],
}


MARKS = []


class _Stop(Exception):
    pass


def build(dbg=False, nseq=NSEQ, nqb=NQB, stop_after=None, skip_prep=False):
    nc = bass.Bass("TRN2", target_bir_lowering=False)
    x = nc.dram_tensor("x", [NSEQ, SEQ, D], F32, kind="ExternalInput").ap()
    positions = nc.dram_tensor("positions", [NSEQ, SEQ], I32, kind="ExternalInput").ap()
    W = {k: nc.dram_tensor(k, s, F32, kind="ExternalInput").ap() for k, s in WEIGHT_SHAPES.items()}
    C = {k: nc.dram_tensor(k, s, F32, kind="ExternalInput").ap() for k, s in CONST_SHAPES.items()}
    out = nc.dram_tensor("out", [NSEQ, SEQ, D], F32, kind="ExternalOutput").ap()
    WinP = nc.dram_tensor("WinP", [56, 128, 2048], BF16).ap()
    WVS = nc.dram_tensor("WVS", [2, 128, 4096], BF16).ap()
    WguP = nc.dram_tensor("WguP", [64, 128, 2048], BF16).ap()
    WdS = nc.dram_tensor("WdS", [16, 128, 4096], BF16).ap()
    WoS = nc.dram_tensor("WoS", [8, 128, 4096], BF16).ap()
    W1S = nc.dram_tensor("W1S", [4, 128, 4096], BF16).ap()
    dbg_out = {}

    with ExitStack() as es:
        S = Sched(nc, es)

        def sb(name, shape, dt):
            return es.enter_context(nc.sbuf_tensor(name, shape, dt))

        def ps(name, shape, dt):
            return es.enter_context(nc.psum_tensor(name, shape, dt))

        X1 = sb("X1", [128, 4, 2048], F32)
        hT = sb("hT", [128, 16, 512], BF16)
        QO = sb("QO", [128, 16, 512], BF16)
        yT = sb("yT", [128, 16, 512], BF16)
        KT = sb("KT", [128, 2, 2, SEQ], BF16)
        VA = sb("VA", [128, 2, 16, 4 * 65], BF16)
        kcT = sb("kcT", [128, 512], BF16)
        vcA = sb("vcA", [128, 4, 97], BF16)
        CSt = sb("CSt", [128, 2, 512], F32)
        posi = sb("posi", [128, 512], I32)
        TF = sb("TF", [128, 4, 512], F32)
        BFW = sb("BFW", [128, 5, 512], BF16)
        PT = BFW[:, 0:3]
        TBb = BFW[:, 3:5]
        XNB = BFW[:, 0:4].rearrange("p a f -> p (a f)")
        XK = [('pt', 0), ('pt', 1), ('pt', 2), ('tb', 0)]
        Otot = sb("Otot", [128, 2, 4, 128], F32)
        Obf = sb("Obf", [128, 2, 4, 128], BF16)
        Cc = Otot
        hidT = QO[:, 0:4].rearrange("p (a b) f -> p a b f", a=2)
        CCK = ['Cc', ('otot', 0), ('otot', 1)]
        gates = sb("gates", [128, 4, 48], F32)
        sc2 = sb("sc2", [128, 32], F32)
        top8 = sb("top8", [128, 16], F32)
        selb = sb("selb", [128, 4, 32], BF16)
        selbT = sb("selbT", [128, 512], BF16)
        QP = sb("QP", [128, 4, 512], BF16)
        zeros128 = sb("zeros128", [128, 128], BF16)
        Rg = sb("Rg", [128, 4, 128], BF16)
        small = sb("small", [128, 64], F32)
        ubuf = sb("ubuf", [128, 1, 528], F32)
        carry = sb("carry", [128, 8, 16], F32)
        PLT = sb("PLT", [128, 2, 2, 512], BF16)
        rt = sb("rt", [128, 96], F32)
        comb = sb("comb", [128, 4, 16], F32)
        combb = sb("combb", [128, 4, 16], BF16)
        combT = sb("combT", [16, 512], BF16)
        PAN = sb("PAN", [128, 3, 16, 128], BF16)
        WB = sb("WB", [128, 2, 4096], BF16)
        WG = sb("WG", [128, 16, 48], BF16)
        Wpool = sb("Wpool", [128, 8, 512], BF16)
        Who = sb("Who", [128, 8, 128], BF16)
        W2k = sb("W2k", [128, 2, 128], BF16)
        W2v = sb("W2v", [128, 2, 64], BF16)
        Wr = sb("Wr", [128, 16, 20], BF16)
        peT = sb("peT", [64, 2, 32], BF16)
        bh = sb("bh", [128, 4], F32)
        gvec = sb("gvec", [128, 40], F32)
        pscale = sb("pscale", [128, 16], F32)
        brt = sb("brt", [128, 20], F32)
        ident = sb("ident", [128, 128], BF16)
        tri = sb("tri", [128, 2, 128], BF16)
        maskc = sb("maskc", [128, 512], BF16)
        bonus = sb("bonus", [128, 4, 32], F32)
        emat = sb("emat", [128, 16, 128], BF16)
        freq = sb("freq", [128, 1], F32)
        rmat = sb("rmat", [128, 128], BF16)
        onesblk = sb("onesblk", [128, 128], BF16)
        sele = sb("sele", [128, 16, 128], BF16)
        icnt = sb("icnt", [128, 4, 16], F32)
        ssq = sb("ssq", [128, 8], F32)

        tmpo = TF[:, 3, 0:256].rearrange("p (a b) -> p a b", a=4)
        tmpi = TF[:, 3, 256:384].rearrange("p (a b) -> p a b", a=4)
        imp = TF[:, 2, 0:128].rearrange("p (a b) -> p a b", a=4)
        scb = TF[:, 2, 128:256].rearrange("p (a b) -> p a b", a=4)
        PSA = [ps("psa%d" % i, [128, 512], F32) for i in range(3)]
        PSS = [ps("pss%d" % i, [128, 512], F32) for i in range(2)]
        PSO = [ps("pso%d" % i, [128, 4, 128], F32) for i in range(2)]
        PST = ps("pst", [128, 1, 512], BF16)

        rr = {'psa': 0, 'pss': 0, 'pso': 0, 'pst': 0, 'pan': 0, 'wb': 0, 'pt': 0, 'ew': 0}

        def nxt(name, n):
            i = rr[name]
            rr[name] = (i + 1) % n
            return i

        def psa():
            i = nxt('psa', 3)
            return PSA[i], ('psa', i)

        def chk(stage):
            if stage[0] != 'h':
                MARKS.append((stage, S.cnt['pe'], S.cnt['act']))
            if stop_after == stage:
                S.dead = True

        chk('start')
        ukey = [0]

        def uk():
            ukey[0] += 1
            return ('cst', ukey[0])

        def ld(dst, src, key, eng='sp', slot='cst'):
            S.dma(eng, slot, lambda e: e.dma_start(out=dst, in_=src), writes=[uk()])

        ld(tri[:], C['c_tri'], 'tri', 'pool', 'cstp')
        ld(emat[:], C['c_emat'], 'emat', 'pool', 'cstp')
        ld(rmat[:], C['c_rm'], 'rmat', 'pool', 'cstp')
        ld(onesblk[:], C['c_onesblk'], 'onesblk', 'pool', 'cstp')
        ld(freq[:], C['c_freq'], 'freq')
        ld(sele[:], C['c_sele'], 'sele', 'pool', 'cstp')
        ld(icnt[:], C['c_icnt'], 'icnt')
        chk('c0')
        for g in range(4):
            ld(vcA[:, g, 64:97], C['c_ov'], 'vcA', 'pool', 'cstp')
        chk('c1')
        nsc = lambda e, o, i: e.dma_start(out=o, in_=i, allow_slow_non_contiguous=True)
        for q4 in range(4):
            cs_ = slice(q4 * 4, q4 * 4 + 4)
            S.dma('sp', 'cst', lambda e: nsc(e, gvec[:, q4 * 4:q4 * 4 + 4], W['attn_norm_g'].rearrange("(c p) -> p c", p=128)[:, cs_]), writes=[uk()])
            S.dma('sp', 'cst', lambda e: nsc(e, gvec[:, 16 + q4 * 4:16 + q4 * 4 + 4], W['ffn_norm_g'].rearrange("(c p) -> p c", p=128)[:, cs_]), writes=[uk()])
            S.dma('sp', 'cst', lambda e: nsc(e, pscale[:, cs_], W['pool_scale'].rearrange("(c p) -> p c", p=128)[:, cs_]), writes=[uk()])
        for i, nm in enumerate(('q_norm_g', 'k_norm_cmp_g', 'k_norm_slc_g', 'k_norm_swa_g')):
            for hb in (0, 64):
                S.dma('sp', 'cst', lambda e, i=i, nm=nm, hb=hb: nsc(e, gvec[hb:hb + 64, 32 + i:33 + i], W[nm].rearrange("(p o) -> p o", o=1)), writes=[uk()])
        chk('c2')
        S.dma('sp', 'cst', lambda e: e.dma_start(out=brt[:, 0:4], in_=W['b_router_group'].partition_broadcast(128)), writes=[uk()])
        S.dma('sp', 'cst', lambda e: e.dma_start(out=brt[:, 4:20], in_=W['b_router_expert'].partition_broadcast(128)), writes=[uk()])
        chk('c3')
        S.op('dve', lambda e: e.memset(gvec[:, 36:37], 1.0), writes=['gvec'])
        S.op('dve', lambda e: e.memset(VA[:].rearrange("p a b c -> p (a b c)"), 1.0), writes=['VA'])
        S.op('dve', lambda e: e.memset(ssq[:], 0.0), writes=['ssq'])
        S.op('dve', lambda e: e.memset(selbT[:], 0.0), writes=['selbT'])
        S.op('dve', lambda e: e.memset(zeros128[:], 0.0), writes=['zeros128'])
        S.op('pool', lambda e: e.memset(QP[:].rearrange("p a f -> p (a f)"), 0.0), writes=[('qp', i) for i in range(4)])
        S.op('pool', lambda e: e.memset(ident[:], 0.0), writes=['ident'])
        S.op('pool', lambda e: e.affine_select(out=ident[:], in_=ident[:], pattern=[[-1, 128]], compare_op=ALU.not_equal, fill=1.0, base=0, channel_multiplier=1), reads=['ident'], writes=['ident'])

        X1f = X1[:].rearrange("p a f -> p (a f)")
        chk('const')
        S.barrier()
        for v_ in range(4):
            S.op('dve', lambda e: e.tensor_scalar_mul(out=Rg[:, v_, :], in0=rmat[:], scalar1=gvec[:, 32 + v_:33 + v_]), reads=['gvec'], writes=['Rg'])
        if skip_prep:
            S.dead = True
        hTf = hT[:].rearrange("p a f -> p (a f)")
        st_in = [X1f[:, i * 4096:(i + 1) * 4096] for i in range(2)]
        st_out = [hTf[:, i * 4096:(i + 1) * 4096] for i in range(2)]
        pj = [0]
        EW3 = ('act', 'dve', 'act', 'act', 'pool', 'act', 'act', 'dve', 'act', 'act', 'act', 'dve', 'act', 'pool', 'act', 'act')

        def scale_op(eng, o, i, sc):
            if eng == 'act':
                S_fn = lambda e: e.mul(out=o, in_=i, mul=sc)
            else:
                S_fn = lambda e: e.tensor_scalar_mul(out=o, in0=i, scalar1=sc)
            return S_fn

        def prep(loads, mode, gofs, stores, resident=None):
            i = pj[0] % 2
            pj[0] += 1
            for (dfn, src) in loads:
                S.dma('sp', 'pin%d' % i, lambda e, dfn=dfn, src=src: e.dma_start(out=dfn(st_in[i]), in_=src), writes=[('pin', i)])
            sin = st_in[i].rearrange("p (k c) -> p k c", c=256)
            for kc in range(16):
                sc = gvec[:, gofs + kc:gofs + kc + 1] if gofs is not None else gvec[:, 36:37]
                if resident is not None:
                    o, ii = resident(sin, kc)
                    wk = [resident.key]
                elif mode == 'A':
                    o = st_out[i].rearrange("p (n k c) -> p n k c", n=2, k=16)[:, :, kc, :]
                    ii = sin[:, kc, :].rearrange("p (n c) -> p n c", n=2)
                    wk = [('pout', i, kc)]
                elif mode == 'Aq':
                    o = st_out[i].rearrange("p (n k hf d) -> p n k hf d", n=2, k=16, hf=2)[:, :, kc, :, :]
                    ii = sin[:, kc, :].rearrange("p (hf r d) -> p r hf d", hf=2, r=2)
                    wk = [('pout', i, kc)]
                else:
                    o = st_out[i][:, kc * 256:(kc + 1) * 256]
                    ii = sin[:, kc, :]
                    wk = [('pout', i, kc)]
                eng = EW3[kc % 16]
                if o.shape[0] != 128:
                    sc = sc[0:o.shape[0]]
                if mode == 'Aq':
                    for n_ in range(2):
                        S.op(eng, scale_op(eng, o[:, n_], ii[:, n_], sc), reads=[('pin', i), 'gvec'], writes=wk)
                    continue
                S.op(eng, scale_op(eng, o, ii, sc), reads=[('pin', i), 'gvec'], writes=wk)
            prev = pend[:]
            del pend[:]
            for (dst, sfn) in stores:
                pend.append((i, dst, sfn))
            flush(prev)

        pend = []

        def flush(lst):
            for (i, dst, sfn) in lst:
                S.dma('sp', 'pout%d' % i, lambda e, dst=dst, sfn=sfn, i=i: e.dma_start(out=dst, in_=sfn(st_out[i])),
                      reads=[('pout', i, kc) for kc in range(16)], writes=['scratch'])

        def win_src(c0, n):
            return W['w_in'][:, c0:c0 + n].rearrange("(k p) c -> p k c", p=128)

        def st_cols(a, n):
            return lambda st: st.rearrange("p (k c) -> p k c", c=256)[:, :, a:a + n]

        def store_panels(p0):
            return [(WinP[p0:p0 + 2].rearrange("n p f -> p n f"), lambda st: st.rearrange("p (n f) -> p n f", n=2))]

        for m in range(2):
            for r2 in range(2):
                c_a = (8 * m + 2 * r2) * 64
                c_b = (8 * m + 4 + 2 * r2) * 64
                prep([(st_cols(0, 128), win_src(c_a, 128)), (st_cols(128, 128), win_src(c_b, 128))], 'Aq', 0,
                     store_panels(4 * m + 2 * r2))
        for pi, c0 in ((8, OFF_KC), (10, OFF_VC), (12, OFF_KS), (14, OFF_KW)):
            prep([(st_cols(0, 256), win_src(c0, 256))], 'A', 0, store_panels(pi))
        for i4 in range(4):
            prep([(st_cols(0, 256), win_src(OFF_POOL + i4 * 256, 256))], 'A', 0, store_panels(16 + 2 * i4))
        for i16 in range(16):
            prep([(st_cols(0, 256), win_src(OFF_MERGE + i16 * 256, 256))], 'A', 0, store_panels(24 + 2 * i16))
        for vi, c0 in ((0, OFF_VS), (1, OFF_VW)):
            prep([(st_cols(0, 256), win_src(c0, 256))], 'B', 0, [(WVS[vi], lambda st: st)])

        class Res:
            def __init__(self, fn, key):
                self.fn, self.key = fn, key

            def __call__(self, sin, kc):
                return self.fn(sin, kc)

        prep([(st_cols(0, 48), win_src(OFF_GATE, 48))], 'R', 0, [],
             resident=Res(lambda sin, kc: (WG[:, kc, :], sin[:, kc, 0:48]), 'WG'))
        def st_cols_k(a, n, k0, k1):
            return lambda st: st.rearrange("p (k c) -> p k c", c=256)[:, k0:k1, a:a + n]
        rl = []
        for q4 in range(4):
            rl.append((st_cols_k(0, 4, q4 * 4, q4 * 4 + 4), W['w_router_group'].rearrange("(k p) c -> p k c", p=128)[:, q4 * 4:q4 * 4 + 4, :]))
            rl.append((st_cols_k(4, 16, q4 * 4, q4 * 4 + 4), W['w_router_expert'].rearrange("(k p) c -> p k c", p=128)[:, q4 * 4:q4 * 4 + 4, :]))
        prep(rl, 'R', 16, [],
             resident=Res(lambda sin, kc: (Wr[:, kc, :], sin[:, kc, 0:20]), 'Wr'))
        for e_ in range(NE):
            for gi_, nm in enumerate(('w_expert_gate', 'w_expert_up')):
                prep([(st_cols(0, 256), W[nm][e_].rearrange("(k p) c -> p k c", p=128))], 'A', 16,
                     [(WguP[e_ * 4 + gi_ * 2:e_ * 4 + gi_ * 2 + 2].rearrange("n p f -> p n f"), lambda st: st.rearrange("p (n f) -> p n f", n=2))])
        for eg in range(4):
            for cc in range(4):
                src = W['w_expert_down'][eg * 4:eg * 4 + 4, :, cc * 512:(cc + 1) * 512].rearrange("e (h p) c -> p e h c", p=128)
                prep([(lambda st: st.rearrange("p (e h c) -> p e h c", e=4, h=2), src)], 'B', None, [(WdS[eg * 4 + cc], lambda st: st)])
        for c8 in range(8):
            prep([(st_cols(0, 256), W['w_out'][:, c8 * 256:(c8 + 1) * 256].rearrange("(k p) c -> p k c", p=128))], 'B', None,
                 [(WoS[c8], lambda st: st)])
        for kv, nm in enumerate(('cmp_w1_k', 'cmp_w1_v')):
            for lh in range(2):
                src = W[nm][lh * 1024:(lh + 1) * 1024, :].rearrange("(l d) h -> d l h", d=64)
                prep([(lambda st: st.rearrange("p (l h) -> p l h", h=256)[0:64], src),
                      (lambda st: st.rearrange("p (l h) -> p l h", h=256)[64:128], src)], 'B', None,
                     [(W1S[kv * 2 + lh], lambda st: st)])
        prep([(lambda st: st.rearrange("p (g j e) -> p g j e", g=4, j=2),
               W['w_pool'].rearrange("g (j p) e -> p g j e", p=128))], 'R', None, [],
             resident=Res(lambda sin, kc: (Wpool[:, kc // 2, (kc % 2) * 256:(kc % 2) * 256 + 256], sin[:, kc, :]), 'Wpool'))

        def res_small(sin, kc):
            flat = sin.rearrange("p k c -> p (k c)")
            if kc < 4:
                return Who[:, 2 * kc:2 * kc + 2, :], flat[:, kc * 256:(kc + 1) * 256].rearrange("p (a b) -> p a b", a=2)
            if kc == 4:
                return W2k[:, :, 0:64], flat[:, 1024:1280].rearrange("p (a b) -> p a b", a=2)[:, :, 0:64]
            if kc == 5:
                return W2k[:, :, 64:128], flat[:, 1024:1280].rearrange("p (a b) -> p a b", a=2)[:, :, 0:64]
            if kc == 6:
                return W2v[:, :, :], flat[:, 1280:1536].rearrange("p (a b) -> p a b", a=2)[:, :, 0:64]
            if kc == 7:
                return peT[:, :, :], flat[0:64, 1536:1664].rearrange("p (a b) -> p a b", a=2)[:, :, 0:32]
            return small[:, 32 + kc:33 + kc], flat[:, 4000 + kc:4001 + kc]

        ifl = lambda st: st
        who_src = W['w_head_out'].rearrange("(c two) d e -> (two d) c e", two=2)
        loads = [(lambda st: st[:, 0:1024].rearrange("p (c e) -> p c e", c=8), who_src)]
        for hc in range(2):
            loads.append((lambda st, hc=hc: st[:, 1024 + hc * 128:1024 + hc * 128 + 64], W['cmp_w2_k'][hc * 128:(hc + 1) * 128, :]))
            loads.append((lambda st, hc=hc: st[:, 1280 + hc * 128:1280 + hc * 128 + 64], W['cmp_w2_v'][hc * 128:(hc + 1) * 128, :]))
        i_sm = pj[0] % 2
        S.op('dve', lambda e: e.memset(st_in[i_sm], 0.0), writes=[('pin', i_sm)])
        for kv, nm in enumerate(('cmp_pos_emb_k', 'cmp_pos_emb_v')):
            for q4 in range(4):
                S.dma('sp', 'pin%d' % i_sm, lambda e, kv=kv, nm=nm: nsc(e, st_in[i_sm][0:64, 1536 + kv * 64 + q4 * 8:1536 + kv * 64 + q4 * 8 + 8], W[nm].rearrange("l d -> d l")[:, q4 * 8:q4 * 8 + 8]), writes=[('pin', i_sm)])
        prep(loads, 'R', None, [], resident=Res(res_small, 'smallres'))
        flush(pend)
        if skip_prep:
            S.dead = False
        chk('prep')
        S.barrier()

        base_pan = [(PAN[:, i], ('pan', i), 'pan%d' % i) for i in range(3)]
        pan_slots = list(base_pan)

        def set_pan_slots(extra):
            del pan_slots[:]
            pan_slots.extend(base_pan + extra)
            rr['pan'] = 0

        def load_panel(src):
            i = nxt('pan', len(pan_slots))
            ap_, key_, sname = pan_slots[i]
            S.dma('sp', sname, lambda e: e.dma_start(out=ap_.rearrange("p k c -> p (k c)"), in_=src), reads=['scratch'], writes=[key_])
            return ap_, key_

        base_wb = [(WB[:, i, :], ('wb', i), 'wb%d' % i) for i in range(2)]
        wb_slots = list(base_wb)

        def set_wb_slots(extra):
            del wb_slots[:]
            wb_slots.extend(base_wb + extra)
            rr['wb'] = 0

        def load_wb(src):
            i = nxt('wb', len(wb_slots))
            ap_, key_, sname = wb_slots[i]
            S.dma('sp', sname, lambda e: e.dma_start(out=ap_, in_=src), reads=['scratch'], writes=[key_])
            return ap_, key_

        def mmA(src, actT, akeys, n=512):
            pan_ap, pan_key = load_panel(src)
            pb, pk = psa()
            for kc in range(16):
                S.op('pe', lambda e, kc=kc: e.matmul(pb[:, 0:n], lhsT=pan_ap[:, kc, :], rhs=actT[:, kc, 0:n], start=(kc == 0), stop=(kc == 15)),
                     reads=[pan_key] + akeys, writes=[pk])
            return pb, pk

        def ew():
            i = nxt('ew', 2)
            return ('dve', 'pool')[i]

        def rope_tables(s, t0):
            S.dma('sp', 'posi', lambda e: e.dma_start(out=posi[:], in_=positions[s, t0:t0 + 512].partition_broadcast(128)), writes=['posi'])
            ang, kf, ki = TF[:, 0, :], TF[:, 1, :], posi[:]
            S.op('dve', lambda e: e.tensor_copy(out=ang, in_=posi[:]), reads=['posi'], writes=[('tf', 0)])
            S.op('dve', lambda e: e.tensor_scalar_mul(out=ang, in0=ang, scalar1=freq[:, 0:1]), reads=[('tf', 0), 'freq'], writes=[('tf', 0)])
            for ci, ph in ((0, 1.5707963267948966), (1, 0.0)):
                S.op('dve', lambda e, ph=ph: e.tensor_scalar(out=kf, in0=ang, scalar1=ph, scalar2=1.0 / TWO_PI, op0=ALU.add, op1=ALU.mult),
                     reads=[('tf', 0)], writes=[('tf', 1)])
                S.op('dve', lambda e: e.tensor_copy(out=ki, in_=kf), reads=[('tf', 1)], writes=['posi'])
                S.op('dve', lambda e: e.tensor_copy(out=kf, in_=ki), reads=['posi'], writes=[('tf', 1)])
                r = TF[:, 2, :]
                S.op('dve', lambda e: e.scalar_tensor_tensor(out=r, in0=kf, scalar=-6.28125, in1=ang, op0=ALU.mult, op1=ALU.add),
                     reads=[('tf', 0), ('tf', 1)], writes=[('tf', 2)])
                S.op('dve', lambda e: e.scalar_tensor_tensor(out=r, in0=kf, scalar=-(TWO_PI - 6.28125), in1=r, op0=ALU.mult, op1=ALU.add),
                     reads=[('tf', 1), ('tf', 2)], writes=[('tf', 2)])
                S.op('dve', lambda e, ph=ph: e.tensor_scalar(out=r, in0=r, scalar1=ph, scalar2=PI_SAFE, op0=ALU.add, op1=ALU.min),
                     reads=[('tf', 2)], writes=[('tf', 2)])
                S.op('dve', lambda e: e.tensor_scalar_max(out=r, in0=r, scalar1=-PI_SAFE), reads=[('tf', 2)], writes=[('tf', 2)])
                S.op('act', lambda e, ci=ci: e.activation(out=CSt[:, ci, :], in_=r, func=AF.Sin), reads=[('tf', 2)], writes=['CSt'])

        def normrope(pb, pk, gcol, Ct, St, cskeys, out_ap, okeys, n=512, view=None):
            vw = view if view is not None else (lambda a: a)
            v = gcol - 32
            sq, psb = TBb[:, 0, 0:n], TBb[:, 1, 0:n]
            rstd, t1, t2 = TF[:, 0, 0:n], TF[:, 1, 0:n], TF[:, 2, 0:n]
            S.op('act', lambda e: e.activation(out=sq, in_=pb[:, 0:n], func=AF.Square), reads=[pk], writes=[('tb', 0)])
            S.op('act', lambda e: e.copy(out=psb, in_=pb[:, 0:n]), reads=[pk], writes=[('tb', 1)])
            mb, mk = psa()
            S.op('pe', lambda e: e.matmul(mb[:, 0:n], lhsT=onesblk[:], rhs=sq, start=True, stop=True), reads=[('tb', 0), 'onesblk'], writes=[mk])
            rb, rk = psa()
            S.op('pe', lambda e: e.matmul(rb[:, 0:n], lhsT=Rg[:, v, :], rhs=psb, start=True, stop=True), reads=[('tb', 1), 'Rg'], writes=[rk])
            S.op('act', lambda e: e.activation(out=rstd, in_=mb[:, 0:n], func=AF.Sqrt, bias=small[:, 63:64], scale=1.0), reads=[mk, 'eps'], writes=[('tf', 0)])
            S.op('dve', lambda e: e.reciprocal(out=rstd, in_=rstd), reads=[('tf', 0)], writes=[('tf', 0)])
            S.op('dve', lambda e: e.scalar_tensor_tensor(out=vw(t1), in0=vw(pb[:, 0:n]), scalar=gvec[:, gcol:gcol + 1], in1=Ct, op0=ALU.mult, op1=ALU.mult),
                 reads=[pk, 'gvec'] + cskeys, writes=[('tf', 1)])
            S.op('dve', lambda e: e.tensor_tensor(out=vw(t2), in0=vw(rb[:, 0:n]), in1=St, op=ALU.mult), reads=[rk] + cskeys, writes=[('tf', 2)])
            S.op('pool', lambda e: e.tensor_tensor(out=t1, in0=t1, in1=t2, op=ALU.add), reads=[('tf', 1), ('tf', 2)], writes=[('tf', 1)])
            S.op('pool', lambda e: e.tensor_tensor(out=out_ap, in0=t1, in1=rstd, op=ALU.mult), reads=[('tf', 1), ('tf', 0)], writes=okeys)

        S.op('dve', lambda e: e.memset(small[:, 63:64], 1e-6), writes=['eps'])

        def make_hT():
            for qi in range(4):
                S.op('act', lambda e, qi=qi: e.activation(out=QO[:, 8:12].rearrange("p a f -> p (a f)"), in_=X1[:, qi, :], func=AF.Square, accum_out=ssq[:, qi:qi + 1]),
                     reads=[('X1', qi)], writes=[('QO', 1), ('ssq', qi)])
                chk('h1')
                S.op('dve', lambda e, qi=qi: e.tensor_scalar(out=ssq[:, 4 + qi:5 + qi], in0=ssq[:, qi:qi + 1], scalar1=1.0 / D, scalar2=1e-6, op0=ALU.mult, op1=ALU.add),
                     reads=[('ssq', qi)], writes=[('ssq2', qi)])
                S.op('dve', lambda e, qi=qi: e.memset(ssq[:, qi:qi + 1], 0.0), reads=[('ssq2', qi)], writes=[('ssq', qi)])
                S.op('act', lambda e, qi=qi: e.activation(out=ssq[:, 4 + qi:5 + qi], in_=ssq[:, 4 + qi:5 + qi], func=AF.Sqrt), reads=[('ssq2', qi)], writes=[('ssq2', qi)])
                S.op('dve', lambda e, qi=qi: e.reciprocal(out=ssq[:, 4 + qi:5 + qi], in_=ssq[:, 4 + qi:5 + qi]), reads=[('ssq2', qi)], writes=[('ssq2', qi)])
                S.op('dve', lambda e, qi=qi: e.tensor_scalar_mul(out=XNB, in0=X1[:, qi, :], scalar1=ssq[:, 4 + qi:5 + qi]),
                     reads=[('X1', qi), ('ssq2', qi)], writes=XK)
                chk('h3')
                for k4 in range(4):
                    ti = nxt('pst', 1)
                    for kk in range(4):
                        kc = k4 * 4 + kk
                        S.op('pe', lambda e, kc=kc, kk=kk, ti=ti: e.transpose(out=PST[:, ti, kk * 128:(kk + 1) * 128], in_=XNB[:, kc * 128:(kc + 1) * 128], identity=ident[:]),
                             reads=XK + ['ident'], writes=[('pst', ti)])
                    chk('h4')
                    eng = ('act', 'dve')[k4 % 2]
                    o = hT[:, k4 * 4:k4 * 4 + 4, qi * 128:(qi + 1) * 128]
                    ii = PST[:, ti, :].rearrange("p (a b) -> p a b", a=4)
                    if eng == 'act':
                        S.op('act', lambda e, o=o, ii=ii: e.copy(out=o, in_=ii), reads=[('pst', ti)], writes=[('hT', qi)])
                    else:
                        S.op('dve', lambda e, o=o, ii=ii: e.tensor_copy(out=o, in_=ii), reads=[('pst', ti)], writes=[('hT', qi)])
                    chk('h5' if k4 == 0 else ('h6' if k4 == 1 else 'h7'))

        HK = [('hT', qi) for qi in range(4)]
        YK = [('yT', qi) for qi in range(4)]

        def load_x(s, t0):
            for qi in range(4):
                S.dma('sp', 'x%d' % qi, lambda e, qi=qi: e.dma_start(out=X1[:, qi, :], in_=x[s, t0 + qi * 128:t0 + (qi + 1) * 128, :]), writes=[('X1', qi)])

        def dump(name, ap_sb, shape, keys, dt=F32):
            if not dbg:
                return
            if name not in dbg_out:
                dbg_out[name] = nc.dram_tensor("dbg_" + name, shape, dt, kind="ExternalOutput").ap()
            S.dma('sp', 'dbg', lambda e: e.dma_start(out=dbg_out[name], in_=ap_sb), reads=keys, writes=['dbg_' + name])

        for s in range(nseq):
            S.op('pool', lambda e: e.memset(carry[:].rearrange("p a b -> p (a b)"), 0.0), writes=['carry'])
            for tb in range(NQB):
                t0 = tb * TB
                load_x(s, t0)
                chk('p1x')
                make_hT()
                chk('p1a')
                rope_tables(s, t0)
                chk('p1b')
                for ci in range(2):
                    m0 = 1 if tb == 0 else 0
                    S.op('pool', lambda e, ci=ci, m0=m0, tb=tb: e.tensor_copy(out=Cc[:, ci, 0, 32 * tb - 1 + m0:32 * tb + 31], in_=CSt[:, ci, 15 + 16 * m0:512:16]),
                         reads=['CSt'], writes=CCK)
                if dbg and s == 0 and tb == 0:
                    dump('hT', hT[:], [128, 16, 512], HK, BF16)
                    dump('CS', CSt[:], [128, 2, 512], ['CSt'])
                chk('p1c')
                rawv = yT[:].rearrange("p a f -> p (a f)").rearrange("p (c t) -> p c t", c=4)
                for pi in range(8, 12):
                    pb, pk = mmA(WinP[pi], hT, HK)
                    o = rawv[:, pi - 8, t0:t0 + 512]
                    S.op('act', lambda e, o=o, pb=pb: e.copy(out=o, in_=pb[:]), reads=[pk], writes=YK + ['raw'])
                chk('p1d')
                for br in range(2):
                    for j in range(2):
                        pb, pk = mmA(WinP[12 + br * 2 + j], hT, HK)
                        normrope(pb, pk, 34 + br, CSt[:, 0, :], CSt[:, 1, :], ['CSt'], KT[:, br, j, t0:t0 + 512], ['KT'])
                chk('p1e')
                for br in range(2):
                    wb_ap, wb_key = load_wb(WVS[br])
                    wv = wb_ap.rearrange("p (k c) -> p k c", c=256)
                    for qi in range(4):
                        pb, pk = psa()
                        for kc in range(16):
                            S.op('pe', lambda e, kc=kc, qi=qi, pb=pb, wv=wv: e.matmul(pb[:, 0:256], lhsT=hT[:, kc, qi * 128:(qi + 1) * 128], rhs=wv[:, kc, :], start=(kc == 0), stop=(kc == 15)),
                                 reads=[wb_key, ('hT', qi)], writes=[pk])
                        o = VA[:, br, tb * 4 + qi, :].rearrange("p (g c) -> p g c", c=65)[:, :, 0:64]
                        ii = pb[:, 0:256].rearrange("p (g c) -> p g c", c=64)
                        S.op('dve', lambda e, o=o, ii=ii: e.tensor_copy(out=o, in_=ii), reads=[pk], writes=['VA'])
            chk('pass1')
            for ci in range(2):
                for g in range(1, 4):
                    S.op('pool', lambda e, ci=ci, g=g: e.tensor_copy(out=Cc[:, ci, g, 0:127], in_=Cc[:, ci, 0, 0:127]), reads=['Cc'], writes=CCK)
            rawv = yT[:].rearrange("p a f -> p (a f)").rearrange("p (c t) -> p c t", c=4)
            for kv in range(2):
                wis = [load_wb(W1S[kv * 2 + lh]) for lh in range(2)]
                w1v = [wa.rearrange("p (l h) -> p l h", h=256) for (wa, _) in wis]
                wkeys = [wk_ for (_, wk_) in wis]
                pb, pk = psa()
                for hc in range(2):
                    for l in range(32):
                        S.op('pe', lambda e, hc=hc, l=l, pb=pb: e.matmul(pb[:, hc:hc + 1], lhsT=w1v[l // 16][0:64, l % 16, hc * 128:(hc + 1) * 128], rhs=peT[0:64, kv, l:l + 1], start=(l == 0), stop=(l == 31)),
                             reads=wkeys + ['smallres'], writes=[pk])
                S.op('dve', lambda e, pb=pb, kv=kv: e.tensor_copy(out=bh[:, kv * 2:kv * 2 + 2], in_=pb[:, 0:2]), reads=[pk], writes=['bh'])
                for g in range(4):
                    base = (g % 2) * 64
                    for hc in range(2):
                        pb, pk = psa()
                        for l in range(32):
                            S.op('pe', lambda e, hc=hc, l=l, pb=pb, base=base, g=g: e.matmul(
                                pb[:, 0:127], lhsT=w1v[l // 16][base:base + 64, l % 16, hc * 128:(hc + 1) * 128],
                                rhs=rawv[base:base + 64, kv * 2 + g // 2, l:l + 16 * 126 + 1:16], start=(l == 0), stop=(l == 31)),
                                reads=wkeys + ['raw'], writes=[pk])
                        S.op('act', lambda e, pb=pb, hc=hc, g=g, kv=kv: e.activation(out=hidT[:, kv, hc, g * 127:(g + 1) * 127], in_=pb[:, 0:127], func=AF.Silu, bias=bh[:, kv * 2 + hc:kv * 2 + hc + 1], scale=1.0),
                             reads=[pk, 'bh'], writes=[('QO', 0)])
            pb, pk = psa()
            for hc in range(2):
                S.op('pe', lambda e, hc=hc, pb=pb: e.matmul(pb[:, 0:508], lhsT=W2k[:, hc, :], rhs=hidT[:, 0, hc, 0:508], start=(hc == 0), stop=(hc == 1)),
                     reads=[('QO', 0), 'smallres'], writes=[pk])
            normrope(pb, pk, 33, Cc[:, 0, :, 0:127], Cc[:, 1, :, 0:127], ['Cc'], kcT[:, 0:508], ['kcT'], n=508,
                     view=lambda a_: a_.rearrange("p (g n) -> p g n", g=4))
            pb, pk = psa()
            for g in range(4):
                for hc in range(2):
                    S.op('pe', lambda e, hc=hc, g=g, pb=pb: e.matmul(pb[0:127, g * 64:(g + 1) * 64], lhsT=hidT[:, 1, hc, g * 127:(g + 1) * 127], rhs=W2v[:, hc, :], start=(hc == 0), stop=(hc == 1)),
                         reads=[('QO', 0), 'smallres'], writes=[pk])
            S.op('dve', lambda e, pb=pb: e.tensor_copy(out=vcA[0:127, :, 0:64], in_=pb[0:127, 0:256].rearrange("p (g c) -> p g c", g=4)), reads=[pk], writes=['vcA'])
            if dbg and s == 0:
                dump('KT', KT[:], [128, 2, 2, SEQ], ['KT'], BF16)
                dump('VA', VA[:], [128, 2, 16, 260], ['VA'], BF16)
                dump('kcT', kcT[:], [128, 512], ['kcT'], BF16)
                dump('vcA', vcA[:], [128, 4, 97], ['vcA'], BF16)
                dump('hidT', hidT, [128, 2, 2, 512], [('QO', 0)], BF16)

            chk('compress')
            qT = QO[:, 0:8]
            OT = QO[:, 8:16]
            for qb in range(nqb):
                t0 = qb * TB
                first = (s == 0 and qb == 0)
                load_x(s, t0)
                S.dma('pool', 'mk', lambda e, t0=t0: e.dma_start(out=maskc[:], in_=C['c_maskc'][:, t0:t0 + 512]), writes=['maskc'])
                S.dma('sp', 'bon', lambda e, qb=qb: e.dma_start(out=bonus[:], in_=C['c_bonus'][:, qb * 4:qb * 4 + 4, :]), writes=['bonus'])
                chk('blk')
                make_hT()
                chk('mkh')
                rope_tables(s, t0)
                for j in range(8):
                    pb, pk = mmA(WinP[j], hT, HK)
                    normrope(pb, pk, 32, CSt[:, 0, :], CSt[:, 1, :], ['CSt'], qT[:, j, :], [('QO', 0)])
                for qi in range(4):
                    pb, pk = psa()
                    for kc in range(16):
                        S.op('pe', lambda e, kc=kc, qi=qi, pb=pb: e.matmul(pb[:, 0:48], lhsT=hT[:, kc, qi * 128:(qi + 1) * 128], rhs=WG[:, kc, :], start=(kc == 0), stop=(kc == 15)),
                             reads=['WG', ('hT', qi)], writes=[pk])
                    S.op('act', lambda e, qi=qi, pb=pb: e.activation(out=gates[:, qi, :], in_=pb[:, 0:48], func=AF.Sigmoid), reads=[pk], writes=['gates'])
                if dbg and first:
                    dump('qT', QO[:, 0:8], [128, 8, 512], [('QO', 0)], BF16)
                    dump('gates', gates[:], [128, 4, 48], ['gates'])

                chk('qproj')
                def finalize(pso, pok, h, br, pi, hp, first_write, with_imp):
                    rs4 = small[:, 0:4]
                    fac = small[:, 4:8]
                    S.op('dve', lambda e: e.tensor_scalar_max(out=rs4, in0=pso[:, :, 64], scalar1=1e-30), reads=[pok], writes=['rs4'])
                    S.op('dve', lambda e: e.reciprocal(out=rs4, in_=rs4), reads=['rs4'], writes=['rs4'])
                    S.op('dve', lambda e: e.tensor_tensor(out=fac, in0=rs4, in1=gates[:, :, 3 * h + br], op=ALU.mult), reads=['rs4', 'gates'], writes=['fac'])
                    fb = fac.unsqueeze(2).to_broadcast([128, 4, 64])
                    od = Otot[:, pi, :, hp * 64:(hp + 1) * 64]
                    if first_write:
                        S.op('dve', lambda e: e.tensor_tensor(out=od, in0=pso[:, :, 0:64], in1=fb, op=ALU.mult), reads=[pok, 'fac'], writes=[('otot', pi), 'Cc'])
                    else:
                        S.op('dve', lambda e: e.tensor_tensor(out=tmpo, in0=pso[:, :, 0:64], in1=fb, op=ALU.mult), reads=[pok, 'fac'], writes=[('tf', 3)])
                        S.op('pool', lambda e: e.tensor_tensor(out=od, in0=od, in1=tmpo, op=ALU.add), reads=[('tf', 3), ('otot', pi)], writes=[('otot', pi)])
                    if with_imp is not None:
                        rb_ = rs4.unsqueeze(2).to_broadcast([128, 4, 32])
                        if with_imp == 0:
                            S.op('dve', lambda e: e.tensor_tensor(out=imp, in0=pso[:, :, 65:97], in1=rb_, op=ALU.mult), reads=[pok, 'rs4'], writes=[('tf', 2)])
                        else:
                            S.op('dve', lambda e: e.tensor_tensor(out=tmpi, in0=pso[:, :, 65:97], in1=rb_, op=ALU.mult), reads=[pok, 'rs4'], writes=[('tf', 3)])
                            S.op('pool', lambda e: e.tensor_tensor(out=imp, in0=imp, in1=tmpi, op=ALU.add), reads=[('tf', 3), ('tf', 2)], writes=[('tf', 2)])

                for g in range(G):
                    base = (g % 2) * 64
                    heads = [4 * g + r for r in range(4)]

                    def qsl(h):
                        m_, rem = h // 8, h % 8
                        return 4 * m_ + rem % 4
                    SCB = [(PSS[0], ('pss', 0)), (PSS[1], ('pss', 1)), (PSA[0], ('psa', 0)), (PSA[1], ('psa', 1)), (PSA[2], ('psa', 2))]
                    PTB = [(BFW[:, i, :], k_) for i, k_ in enumerate([('pt', 0), ('pt', 1), ('pt', 2), ('tb', 0), ('tb', 1)])]

                    def sc_next():
                        i = nxt('pss', 5)
                        return SCB[i]

                    def pt_next():
                        i = nxt('pt', 5)
                        return PTB[i]
                    cs = []
                    for r, h in enumerate(heads):
                        jq = qsl(h)
                        sb_, sk_ = sc_next()
                        S.op('pe', lambda e: e.matmul(sb_[0:127, :], lhsT=kcT[base:base + 64, g * 127:(g + 1) * 127], rhs=qT[base:base + 64, jq, :], start=True, stop=True),
                             reads=['kcT', ('QO', 0)], writes=[sk_])
                        cs.append((sb_, sk_))
                    for r, h in enumerate(heads):
                        sb_, sk_ = cs[r]
                        pb_, pk_ = pt_next()
                        S.op('act', lambda e: e.activation(out=pb_[0:127, :], in_=sb_[0:127, :], func=AF.Exp, scale=0.125), reads=[sk_], writes=[pk_])
                        S.op('pool', lambda e: e.tensor_tensor(out=pb_[0:127, :], in0=pb_[0:127, :], in1=maskc[0:127, :], op=ALU.mult),
                             reads=[pk_, 'maskc'], writes=[pk_])
                        oi = nxt('pso', 2)
                        for qi in range(4):
                            S.op('pe', lambda e: e.matmul(PSO[oi][:, qi, 0:97], lhsT=pb_[0:127, qi * 128:(qi + 1) * 128], rhs=vcA[0:127, g, :], start=True, stop=True),
                                 reads=[pk_, 'vcA'], writes=[('pso', oi)])
                        finalize(PSO[oi], ('pso', oi), h, 0, r // 2, r % 2, True, (r if qb >= 2 else None))
                    def emit_selection_dve(g=g):
                        S.op('dve', lambda e: e.tensor_tensor(out=scb, in0=imp, in1=bonus[:, :, :], op=ALU.add), reads=[('tf', 2), 'bonus'], writes=[('tf', 2)])
                        for qi in range(4):
                            S.op('dve', lambda e, qi=qi: e.max(out=top8[:, 0:8], in_=scb[:, qi, :]), reads=[('tf', 2)], writes=['top8'])
                            S.op('dve', lambda e, qi=qi: e.match_replace(out=sc2[:], in_to_replace=top8[:, 0:8], in_values=scb[:, qi, :], imm_value=-1e30), reads=[('tf', 2), 'top8'], writes=['sc2'])
                            S.op('dve', lambda e: e.max(out=top8[:, 8:16], in_=sc2[:]), reads=['sc2'], writes=['top8'])
                            S.op('dve', lambda e, qi=qi: e.tensor_scalar(out=sc2[:], in0=scb[:, qi, :], scalar1=top8[:, 15:16], scalar2=None, op0=ALU.is_ge), reads=[('tf', 2), 'top8'], writes=['sc2'])
                            S.op('dve', lambda e, qi=qi: e.tensor_scalar(out=selb[:, qi, :], in0=sc2[:], scalar1=-1.0, scalar2=30000.0, op0=ALU.add, op1=ALU.mult), reads=['sc2'], writes=['selb'])

                    def emit_selection_pe(g=g):
                        ti = nxt('pst', 1)
                        for qi in range(4):
                            S.op('pe', lambda e, qi=qi, ti=ti: e.transpose(out=PST[0:32, ti, qi * 128:(qi + 1) * 128], in_=selb[:, qi, :], identity=ident[:]),
                                 reads=['selb', 'ident'], writes=[('pst', ti)])
                        S.op('dve', lambda e, ti=ti: e.tensor_copy(out=selbT[0:32, :], in_=PST[0:32, ti, :]), reads=[('pst', ti)], writes=['selbT'])
                        if dbg and s == 0 and qb == 2 and g == 0:
                            dump('selb', selb[:], [128, 4, 32], ['selb'], BF16)
                            dump('imp', imp, [128, 4, 32], ['imp'])
                    if qb >= 2:
                        emit_selection_dve()
                    steps = []
                    qp_rr = [0]
                    cb_after = {}
                    order = [(0, 2), (1, 2), (0, 1), (1, 1), (2, 2), (3, 2), (2, 1), (3, 1)]
                    for ui, (r, br) in enumerate(order):
                        h = heads[r]
                        kts = list(range(0, 4 * qb + 4)) if br == 1 else list(range(max(4 * qb - 4, 0), 4 * qb + 4))
                        unit = {'h': h, 'r': r, 'br': br, 'oi': None, 'qp': None, 'newhead': br == 2}
                        for kt in kts:
                            steps.append({'u': unit, 'kt': kt, 'first': kt == kts[0], 'last': kt == kts[-1]})
                        if ui == 2 and qb >= 2:
                            steps[len(steps) - len(kts)]['pre'] = emit_selection_pe

                    hq = {}

                    def emit_score(st):
                        u, kt = st['u'], st['kt']
                        br, jq = u['br'], qsl(u['h'])
                        dloc = kt - 4 * qb
                        lo = max(dloc, 0)
                        hi = 3 if br == 1 else min(dloc + 4, 3)
                        c0 = lo * 128
                        n = (hi - lo + 1) * 128
                        sb_, sk_ = sc_next()
                        use_mask = (br == 1 and qb >= 2)
                        if st['first'] and u['newhead']:
                            qs = (g % 2) * 2 + (qp_rr[0] % 2)
                            qp_rr[0] += 1
                            hq[u['h']] = qs
                            S.op('dve', lambda e: e.tensor_copy(out=QP[base:base + 64, qs, :], in_=qT[base:base + 64, jq, :]), reads=[('QO', 0)], writes=[('qp', qs)])
                        qs = hq[u['h']]
                        S.op('pe', lambda e: e.matmul(
                            sb_[:, 0:n], lhsT=KT[:, br - 1, g // 2, kt * 128:(kt + 1) * 128], rhs=QP[:, qs, c0:c0 + n], start=True, stop=(not use_mask)),
                            reads=['KT', ('qp', qs)], writes=[sk_])
                        if use_mask:
                            S.op('pe', lambda e: e.matmul(sb_[:, 0:n], lhsT=emat[:, kt, :], rhs=selbT[:, c0:c0 + n], start=False, stop=True),
                                 reads=['emat', 'selbT'], writes=[sk_])
                        st['ctx'] = (dloc, lo, hi, c0, n, sb_, sk_)

                    def emit_rest(st):
                        u, kt = st['u'], st['kt']
                        br = u['br']
                        dloc, lo, hi, c0, n, sb_, sk_ = st['ctx']
                        if st['first']:
                            u['oi'] = nxt('pso', 2)
                            oi0 = u['oi']
                            S.op('pe', lambda e: e.matmul(PSO[oi0][:, :, :], lhsT=zeros128[:], rhs=emat[:, 0:4, :], start=True, stop=False),
                                 reads=['emat', 'zeros128'], writes=[('pso', oi0)])
                        oi = u['oi']
                        pb_, pk_ = pt_next()
                        S.op('act', lambda e: e.activation(out=pb_[:, 0:n], in_=sb_[:, 0:n], func=AF.Exp, scale=0.125), reads=[sk_], writes=[pk_])
                        if dloc >= 0:
                            S.op('pool', lambda e: e.tensor_tensor(out=pb_[:, 0:128], in0=pb_[:, 0:128], in1=tri[:, 0, :], op=ALU.mult),
                                 reads=[pk_, 'tri'], writes=[pk_])
                        if br == 2 and 0 <= dloc + 4 <= 3:
                            cf = (hi - lo) * 128
                            S.op('pool', lambda e: e.tensor_tensor(out=pb_[:, cf:cf + 128], in0=pb_[:, cf:cf + 128], in1=tri[:, 1, :], op=ALU.mult),
                                 reads=[pk_, 'tri'], writes=[pk_])
                        for qi in range(lo, hi + 1):
                            sp = bool(st['last'] and qi == hi)
                            S.op('pe', lambda e: e.matmul(
                                PSO[oi][:, qi, 0:65], lhsT=pb_[:, (qi - lo) * 128:(qi - lo + 1) * 128], rhs=VA[:, br - 1, kt, g * 65:(g + 1) * 65], start=False, stop=sp),
                                reads=[pk_, 'VA'], writes=[('pso', oi)])
                        if st['last']:
                            finalize(PSO[oi], ('pso', oi), u['h'], br, u['r'] // 2, u['r'] % 2, False, None)

                    LA = 3
                    n_sc = 0
                    for i_st in range(len(steps)):
                        while n_sc < min(len(steps), i_st + 1 + LA):
                            if 'pre' in steps[n_sc]:
                                steps[n_sc]['pre']()
                            emit_score(steps[n_sc])
                            n_sc += 1
                        emit_rest(steps[i_st])
                        if i_st in cb_after:
                            cb_after[i_st]()
                    for pi in range(2):
                        S.op('act', lambda e, pi=pi: e.copy(out=Obf[:, pi], in_=Otot[:, pi]), reads=[('otot', pi)], writes=[('obf', pi)])
                        ti = nxt('pst', 1)
                        for qi in range(4):
                            S.op('pe', lambda e, pi=pi, qi=qi, ti=ti: e.transpose(out=PST[:, ti, qi * 128:(qi + 1) * 128], in_=Obf[:, pi, qi, :], identity=ident[:]),
                                 reads=[('obf', pi), 'ident'], writes=[('pst', ti)])
                        S.op('act', lambda e, pi=pi, ti=ti, g=g: e.copy(out=OT[:, 2 * g + pi, :], in_=PST[:, ti, :]), reads=[('pst', ti)], writes=[('QO', 1)])
                if dbg and first:
                    dump('OT', QO[:, 8:16], [128, 8, 512], [('QO', 1)], BF16)

                chk('attn')
                QOf = QO[:].rearrange("p a f -> p (a f)")
                BFf = BFW[:].rearrange("p a f -> p (a f)")
                ex_y = [(QOf[:, i * 2048:(i + 1) * 2048].rearrange("p (k c) -> p k c", c=128), ('QOs', i), 'pxq%d' % i) for i in range(2)]
                ex_y.append((BFf[:, 0:2048].rearrange("p (k c) -> p k c", c=128), ('BFs', 0), 'pxb0'))
                S.alias_in([('QOs', 0), ('QOs', 1)], [('QO', 0)])
                S.alias_in([('BFs', 0)], XK + [('tb', 1)])
                set_pan_slots(ex_y)
                def pool_group(gi):
                    w = (2, 4, 8, 16)[gi]
                    for j in range(2):
                        pb, pk = mmA(WinP[16 + gi * 2 + j], hT, HK)
                        U = ubuf[:, 0, :]
                        S.op('pool', lambda e, gi=gi, j=j: e.tensor_copy(out=ubuf[:, 0, 0:16], in_=carry[:, gi * 2 + j, :]), reads=['carry'], writes=['ub0'])
                        S.op('act', lambda e, pb=pb: e.copy(out=ubuf[:, 0, 16:528], in_=pb[:]), reads=[pk], writes=['ub0'])
                        S.op('pool', lambda e, gi=gi, j=j: e.tensor_copy(out=carry[:, gi * 2 + j, :], in_=ubuf[:, 0, 512:528]), reads=['ub0'], writes=['carry'])
                        TFf = TF[:].rearrange("p a f -> p (a f)")
                        ubs = [ubuf[:, 0, :], TFf[:, 0:528], TFf[:, 528:1056]]
                        ubk = [['ub0'], [('tf', 0), ('tf', 1)], [('tf', 1), ('tf', 2)]]
                        cur = 0
                        for kstep in range(gi + 1):
                            sh = 1 << kstep
                            nx_ = 1 if cur != 1 else 2
                            S.op('pool', lambda e, cur=cur, nx_=nx_, sh=sh, ubs=ubs: e.tensor_tensor(out=ubs[nx_][:, sh:528], in0=ubs[cur][:, sh:528], in1=ubs[cur][:, 0:528 - sh], op=ALU.add),
                                 reads=ubk[cur], writes=ubk[nx_])
                            cur = nx_
                        S.op('dve', lambda e, cur=cur, j=j, w=w, ubs=ubs: e.scalar_tensor_tensor(out=PLT[:, gi % 2, j, :], in0=ubs[cur][:, 16:528], scalar=1.0 / w, in1=ubuf[:, 0, 16:528], op0=ALU.mult, op1=ALU.subtract),
                             reads=ubk[cur] + ['ub0'], writes=[('PLT', gi % 2)])
                        if qb == 0:
                            S.op('dve', lambda e, cur=cur, gi=gi, ubs=ubs: e.tensor_tensor(out=small[:, 8:24], in0=ubs[cur][:, 16:32], in1=icnt[:, gi, :], op=ALU.mult), reads=ubk[cur] + ['icnt'], writes=['t16'])
                            S.op('dve', lambda e, j=j: e.tensor_tensor(out=PLT[:, gi % 2, j, 0:16], in0=small[:, 8:24], in1=ubuf[:, 0, 16:32], op=ALU.subtract), reads=['t16', 'ub0', ('PLT', gi % 2)], writes=[('PLT', gi % 2)])
                pool_group(0)
                for c in range(16):
                    gi = c // 4
                    pa, pak = psa()
                    cb = (c % 2) * 64
                    S.op('pe', lambda e, pa=pa, c=c, cb=cb: e.matmul(pa[:], lhsT=Who[cb:cb + 64, c // 2, :], rhs=OT[cb:cb + 64, c // 2, :], start=True, stop=True),
                         reads=['smallres', ('QO', 1)], writes=[pak])
                    pm, pmk = mmA(WinP[24 + c], hT, HK)
                    S.op('act', lambda e, pm=pm: e.activation(out=TF[:, 0, :], in_=pm[:], func=AF.Sigmoid), reads=[pmk], writes=[('tf', 0)])
                    S.op('dve', lambda e, pa=pa: e.tensor_tensor(out=TF[:, 1, :], in0=pa[:], in1=TF[:, 0, :], op=ALU.mult), reads=[pak, ('tf', 0)], writes=[('tf', 1)])
                    pp, ppk = psa()
                    for j in range(2):
                        S.op('pe', lambda e, pp=pp, j=j, c=c, gi=gi: e.matmul(pp[:], lhsT=Wpool[:, gi * 2 + j, (c % 4) * 128:(c % 4 + 1) * 128], rhs=PLT[:, gi % 2, j, :], start=(j == 0), stop=(j == 1)),
                             reads=['Wpool', ('PLT', gi % 2)], writes=[ppk])
                    pm1, pm1k = mmA(WinP[40 + c], hT, HK)
                    S.op('act', lambda e, pm1=pm1: e.activation(out=TF[:, 2, :], in_=pm1[:], func=AF.Sigmoid), reads=[pm1k], writes=[('tf', 2)])
                    S.op('dve', lambda e, pp=pp, c=c: e.scalar_tensor_tensor(out=TF[:, 3, :], in0=pp[:], scalar=pscale[:, c:c + 1], in1=TF[:, 2, :], op0=ALU.mult, op1=ALU.mult),
                         reads=[ppk, ('tf', 2), 'pscale'], writes=[('tf', 3)])
                    S.op('pool', lambda e, c=c: e.tensor_tensor(out=yT[:, c, :], in0=TF[:, 1, :], in1=TF[:, 3, :], op=ALU.add), reads=[('tf', 1), ('tf', 3)], writes=YK + ['raw'])
                    if c % 4 == 0 and gi < 3:
                        pool_group(gi + 1)
                if dbg and first:
                    dump('yT', yT[:], [128, 16, 512], YK, BF16)

                chk('ymix')
                S.alias_out([('QOs', 0), ('QOs', 1)], [('QO', 0)])
                S.alias_out([('BFs', 0)], XK + [('tb', 1)])
                set_pan_slots([])
                S.alias_in([('QOw', 0)], [('QO', 0)])
                set_wb_slots([(QO[:, 0:8].rearrange("p a f -> p (a f)"), ('QOw', 0), 'wbx0')])
                for c8 in range(8):
                    wb_ap, wb_key = load_wb(WoS[c8])
                    wv = wb_ap.rearrange("p (k c) -> p k c", c=256)
                    for qi in range(4):
                        pb, pk = psa()
                        for kc in range(16):
                            S.op('pe', lambda e, kc=kc, qi=qi, pb=pb, wv=wv: e.matmul(pb[:, 0:256], lhsT=yT[:, kc, qi * 128:(qi + 1) * 128], rhs=wv[:, kc, :], start=(kc == 0), stop=(kc == 15)),
                                 reads=[wb_key, ('yT', qi)], writes=[pk])
                        xs = X1[:, qi, c8 * 256:(c8 + 1) * 256]
                        S.op('dve', lambda e, xs=xs, pb=pb: e.tensor_tensor(out=xs, in0=pb[:, 0:256], in1=xs, op=ALU.add), reads=[pk, ('X1', qi)], writes=[('X1', qi)])
                S.alias_out([('QOw', 0)], [('QO', 0)])
                set_wb_slots([])
                if dbg and first:
                    dump('x1', X1[:], [128, 4, 2048], [('X1', qi) for qi in range(4)])

                chk('wout')
                make_hT()
                chk('mkh2')
                pb, pk = psa()
                for qi in range(4):
                    for kc in range(16):
                        S.op('pe', lambda e, kc=kc, qi=qi: e.matmul(pb[:, qi * 20:(qi + 1) * 20], lhsT=hT[:, kc, qi * 128:(qi + 1) * 128], rhs=Wr[:, kc, :], start=(kc == 0), stop=(kc == 15)),
                             reads=['Wr', ('hT', qi)], writes=[pk])
                R = TF[:, 3, :]
                RK = [('tf', 3)]
                v3 = lambda ap_, a_: ap_.rearrange("p (a b) -> p a b", a=a_)
                lgg, le = R[:, 0:16], R[:, 16:80]
                mx, se, pg = R[:, 80:84], R[:, 84:88], R[:, 88:92]
                ex, gm, pen = R[:, 96:112], R[:, 112:128], R[:, 128:144]
                lm = R[:, 144:208]
                t8 = R[:, 208:240]
                dv, e2, den, w1, w2 = R[:, 240:244], R[:, 244:248], R[:, 248:252], R[:, 252:256], R[:, 256:260]
                m1, m2 = R[:, 272:336], R[:, 336:400]
                pb3 = v3(pb[:, 0:80], 4)
                bc = lambda ap_, n_: ap_.unsqueeze(2).to_broadcast([128, ap_.shape[1], n_])
                S.op('dve', lambda e: e.tensor_tensor(out=v3(lgg, 4), in0=pb3[:, :, 0:4], in1=brt[:, 0:4].unsqueeze(1).to_broadcast([128, 4, 4]), op=ALU.add), reads=[pk, 'brt'], writes=RK)
                S.op('dve', lambda e: e.tensor_tensor(out=v3(le, 4), in0=pb3[:, :, 4:20], in1=brt[:, 4:20].unsqueeze(1).to_broadcast([128, 4, 16]), op=ALU.add), reads=[pk, 'brt'], writes=RK)
                S.op('dve', lambda e: e.tensor_reduce(out=mx, in_=v3(lgg, 4), axis=AX.X, op=ALU.max), reads=RK, writes=RK)
                S.op('dve', lambda e: e.tensor_tensor(out=v3(ex, 4), in0=v3(lgg, 4), in1=bc(mx, 4), op=ALU.subtract), reads=RK, writes=RK)
                S.op('act', lambda e: e.activation(out=ex, in_=ex, func=AF.Exp), reads=RK, writes=RK)
                S.op('dve', lambda e: e.tensor_reduce(out=se, in_=v3(ex, 4), axis=AX.X, op=ALU.add), reads=RK, writes=RK)
                S.op('dve', lambda e: e.reciprocal(out=pg, in_=se), reads=RK, writes=RK)
                S.op('dve', lambda e: e.tensor_tensor(out=v3(gm, 4), in0=v3(lgg, 4), in1=bc(mx, 4), op=ALU.is_ge), reads=RK, writes=RK)
                S.op('dve', lambda e: e.tensor_scalar(out=pen, in0=gm, scalar1=-1.0, scalar2=1e30, op0=ALU.add, op1=ALU.mult), reads=RK, writes=RK)
                S.op('dve', lambda e: e.tensor_tensor(out=v3(lm, 16), in0=v3(le, 16), in1=bc(pen, 4), op=ALU.add), reads=RK, writes=RK)
                for qi in range(4):
                    S.op('dve', lambda e, qi=qi: e.max(out=t8[:, qi * 8:(qi + 1) * 8], in_=lm[:, qi * 16:(qi + 1) * 16]), reads=RK, writes=RK)
                t83 = v3(t8, 4)
                S.op('dve', lambda e: e.tensor_tensor(out=dv, in0=t83[:, :, 1], in1=t83[:, :, 0], op=ALU.subtract), reads=RK, writes=RK)
                S.op('act', lambda e: e.activation(out=e2, in_=dv, func=AF.Exp), reads=RK, writes=RK)
                S.op('dve', lambda e: e.tensor_scalar_add(out=den, in0=e2, scalar1=1.0), reads=RK, writes=RK)
                S.op('dve', lambda e: e.reciprocal(out=den, in_=den), reads=RK, writes=RK)
                S.op('dve', lambda e: e.tensor_tensor(out=w1, in0=pg, in1=den, op=ALU.mult), reads=RK, writes=RK)
                S.op('dve', lambda e: e.tensor_tensor(out=w2, in0=w1, in1=e2, op=ALU.mult), reads=RK, writes=RK)
                S.op('dve', lambda e: e.tensor_tensor(out=v3(m1, 4), in0=v3(lm, 4), in1=bc(t83[:, :, 0], 16), op=ALU.is_equal), reads=RK, writes=RK)
                S.op('dve', lambda e: e.tensor_tensor(out=v3(m1, 4), in0=v3(m1, 4), in1=bc(w1, 16), op=ALU.mult), reads=RK, writes=RK)
                S.op('dve', lambda e: e.tensor_tensor(out=v3(m2, 4), in0=v3(lm, 4), in1=bc(t83[:, :, 1], 16), op=ALU.is_equal), reads=RK, writes=RK)
                S.op('dve', lambda e: e.tensor_tensor(out=v3(m2, 4), in0=v3(m2, 4), in1=bc(w2, 16), op=ALU.mult), reads=RK, writes=RK)
                S.op('dve', lambda e: e.tensor_tensor(out=comb[:].rearrange("p a b -> p (a b)"), in0=m1, in1=m2, op=ALU.add), reads=RK, writes=['comb'])
                S.op('dve', lambda e: e.tensor_copy(out=combb[:], in_=comb[:]), reads=['comb'], writes=['combb'])
                ti = nxt('pst', 1)
                for qi in range(4):
                    S.op('pe', lambda e, qi=qi, ti=ti: e.transpose(out=PST[0:16, ti, qi * 128:(qi + 1) * 128], in_=combb[:, qi, :], identity=ident[:]), reads=['combb', 'ident'], writes=[('pst', ti)])
                S.op('act', lambda e, ti=ti: e.copy(out=combT[:, :], in_=PST[0:16, ti, :]), reads=[('pst', ti)], writes=['combT'])
                if dbg and first:
                    dump('comb', comb[:], [128, 4, 16], ['comb'])
                    dump('h2T', hT[:], [128, 16, 512], HK, BF16)
                yTf = yT[:].rearrange("p a f -> p (a f)")
                ex_m = [(yTf[:, i * 2048:(i + 1) * 2048].rearrange("p (k c) -> p k c", c=128), ('yTs', i), 'pxy%d' % i) for i in range(4)]
                S.alias_in([('yTs', i) for i in range(4)], YK + ['raw'])
                set_pan_slots(ex_m)
                for eg in range(4):
                    par = eg % 2
                    hw = QO[:, par * 8:(par + 1) * 8]
                    for el in range(4):
                        e_ = eg * 4 + el
                        pc, pck = psa()
                        S.op('pe', lambda e, pc=pc, e_=e_: e.matmul(pc[:], lhsT=sele[0:16, e_, :], rhs=combT[:, :], start=True, stop=True), reads=['sele', 'combT'], writes=[pck])
                        S.op('act', lambda e, pc=pc: e.copy(out=TF[:, 2, :], in_=pc[:]), reads=[pck], writes=[('tf', 2)])
                        for hc in range(2):
                            pg_, pgk = mmA(WguP[e_ * 4 + hc], hT, HK)
                            S.op('act', lambda e, pg_=pg_: e.activation(out=TF[:, 0, :], in_=pg_[:], func=AF.Silu), reads=[pgk], writes=[('tf', 0)])
                            pu, puk = mmA(WguP[e_ * 4 + 2 + hc], hT, HK)
                            S.op('dve', lambda e, pu=pu: e.tensor_tensor(out=TF[:, 1, :], in0=pu[:], in1=TF[:, 0, :], op=ALU.mult), reads=[puk, ('tf', 0)], writes=[('tf', 1)])
                            S.op('pool', lambda e, hw=hw, el=el, hc=hc: e.tensor_tensor(out=hw[:, el * 2 + hc, :], in0=TF[:, 1, :], in1=TF[:, 2, :], op=ALU.mult),
                                 reads=[('tf', 1), ('tf', 2)], writes=[('QO', par)])
                    for cc in range(4):
                        wb_ap, wb_key = load_wb(WdS[eg * 4 + cc])
                        wv = wb_ap.rearrange("p (k c) -> p k c", c=512)
                        for qi in range(4):
                            pb, pk = psa()
                            for k8 in range(8):
                                S.op('pe', lambda e, k8=k8, qi=qi, pb=pb, wv=wv, hw=hw: e.matmul(pb[:], lhsT=hw[:, k8, qi * 128:(qi + 1) * 128], rhs=wv[:, k8, :], start=(k8 == 0), stop=(k8 == 7)),
                                     reads=[wb_key, ('QO', par)], writes=[pk])
                            xs = X1[:, qi, cc * 512:(cc + 1) * 512]
                            S.op('dve', lambda e, xs=xs, pb=pb: e.tensor_tensor(out=xs, in0=pb[:], in1=xs, op=ALU.add), reads=[pk, ('X1', qi)], writes=[('X1', qi)])
                S.alias_out([('yTs', i) for i in range(4)], YK + ['raw'])
                set_pan_slots([])
                chk('moe')
                for qi in range(4):
                    S.dma('sp', 'out%d' % qi, lambda e, qi=qi: e.dma_start(out=out[s, t0 + qi * 128:t0 + (qi + 1) * 128, :], in_=X1[:, qi, :]), reads=[('X1', qi)], writes=['out'])
        S.barrier()
        S.emit()
    return nc, dbg_out


_CACHE = {}


def _layout_inputs(inputs):
    consts = make_consts()
    wts = {}
    for k in WEIGHT_SHAPES:
        a = np.asarray(inputs[k])
        wts[k] = np.ascontiguousarray(a.reshape(a.shape[1:]), dtype=np.float32)
    xs = np.asarray(inputs['x'], dtype=np.float32)
    pos = np.asarray(inputs['positions'], dtype=np.int32)
    in_maps = []
    for c in range(NCORES):
        m = {'x': np.ascontiguousarray(xs[c * NSEQ:(c + 1) * NSEQ]), 'positions': np.ascontiguousarray(pos[c * NSEQ:(c + 1) * NSEQ])}
        m.update(wts)
        m.update(consts)
        in_maps.append(m)
    return in_maps


def kernel(**inputs):
    if 'nc' not in _CACHE:
        _CACHE['nc'] = build()[0]
    nc = _CACHE['nc']
    in_maps = _layout_inputs(inputs)
    res = run_bass_kernel_spmd(nc, in_maps, core_ids=list(range(NCORES)))
    outs = [np.asarray(r['out']) for r in res.results]
    return np.concatenate(outs, axis=0).astype(np.float32)
```
